# Optimizing a Trainium2 kernel written in Bass

```python
import jax, jax.numpy as jnp
from jax import lax
import numpy as np

D_MODEL = 1024
BATCH = 8
SEQ = 2048
DEPTH = 1

PLE_DIM = 256
A_WIDTH = 512
A_GROUPS = 8
A_CHUNK = 128
B_HEAD_DIM = 64
B_HEADS = 8
B_CONFIGS = ((128, 1), (512, 4), (2048, 16))
B_GROUPS = len(B_CONFIGS)
B_WIDTH = B_HEADS * B_HEAD_DIM
N_EXPERT_GROUPS = 4
EXPERTS_PER_GROUP = 8
EXPERT_TOPK = 2
D_EXPERT = 256
DEEPNORM_ALPHA = (2 * DEPTH) ** 0.25
DEEPNORM_BETA = (8 * DEPTH) ** -0.25
LN_EPS = 1e-5
IN_A = 2 * A_WIDTH
IN_B = 3 * B_GROUPS * B_WIDTH
IN_G = 2 * D_MODEL
IN_TOTAL = IN_A + IN_B + IN_G

kernel_name = 'hybrid_gmlp_dilated_attn_hmoe_block'


def layer_norm(x, g, b):
    xf = x.astype(jnp.float32)
    mu = jnp.mean(xf, axis=-1, keepdims=True)
    var = jnp.mean(jnp.square(xf - mu), axis=-1, keepdims=True)
    return ((xf - mu) * lax.rsqrt(var + LN_EPS) * g + b).astype(x.dtype)


def chunked_spatial_gating(z, ln_g, ln_b, w_s, b_s):
    bsz, s, _ = z.shape
    u, v = jnp.split(z, 2, axis=-1)
    v = layer_norm(v, ln_g, ln_b)
    v = v.reshape(bsz, s // A_CHUNK, A_CHUNK, A_GROUPS, A_WIDTH // A_GROUPS)
    causal = jnp.tril(jnp.ones((A_CHUNK, A_CHUNK), dtype=bool))
    w = jnp.where(causal, w_s, jnp.zeros_like(w_s))
    mixed = jnp.einsum('gts,bcsgd->bctgd', w, v) + b_s.T[None, None, :, :, None]
    return u * mixed.reshape(bsz, s, A_WIDTH)


def dilated_causal_attention(q, k, v, window, dilation):
    bsz, s, h, dh = q.shape
    span = window // dilation
    n_sub = s // dilation
    nb = -(-n_sub // span)
    n_pad = nb * span

    def to_residue(t):
        t = t.reshape(bsz, n_sub, dilation, h, dh)
        return jnp.pad(t, ((0, 0), (0, n_pad - n_sub), (0, 0), (0, 0), (0, 0)))

    def windows(t):
        t = jnp.pad(to_residue(t), ((0, 0), (span, 0), (0, 0), (0, 0), (0, 0)))
        t = t.reshape(bsz, nb + 1, span, dilation, h, dh)
        return jnp.concatenate([t[:, :-1], t[:, 1:]], axis=2)

    qb = to_residue(q).reshape(bsz, nb, span, dilation, h, dh)
    kw, vw = windows(k), windows(v)
    scores = jnp.einsum('bnqrhd,bnkrhd->bnrhqk', qb, kw).astype(jnp.float32) * (dh ** -0.5)
    qi = jnp.arange(span)[:, None]
    ki = jnp.arange(2 * span)[None, :]
    dist = span + qi - ki
    kpos = (jnp.arange(nb)[:, None, None] - 1) * span + ki[None]
    valid = (dist >= 0) & (dist <= span) & (kpos >= 0)
    scores = jnp.where(valid[None, :, None, None], scores, -jnp.inf)
    lse = jax.nn.logsumexp(scores, axis=-1)
    probs = jnp.exp(scores - lse[..., None]).astype(v.dtype)
    out = jnp.einsum('bnrhqk,bnkrhd->bnqrhd', probs, vw)
    out = out.reshape(bsz, n_pad, dilation, h, dh)[:, :n_sub].reshape(bsz, s, h, dh)
    lse = lse.transpose(0, 1, 4, 2, 3).reshape(bsz, n_pad, dilation, h)[:, :n_sub].reshape(bsz, s, h)
    return out, lse


def token_mixer(x, w_in, a_ln_g, a_ln_b, a_ws, a_bs, w_a_proj, w_b_proj, w_o):
    bsz, s, _ = x.shape
    proj = x @ w_in
    za, zb, zg = jnp.split(proj, [IN_A, IN_A + IN_B], axis=-1)
    y_a = chunked_spatial_gating(jax.nn.gelu(za), a_ln_g, a_ln_b, a_ws, a_bs) @ w_a_proj
    qkv = zb.reshape(bsz, s, 3, B_GROUPS, B_HEADS, B_HEAD_DIM)
    outs, lses = [], []
    for gi, (window, dilation) in enumerate(B_CONFIGS):
        o, l = dilated_causal_attention(qkv[:, :, 0, gi], qkv[:, :, 1, gi], qkv[:, :, 2, gi], window, dilation)
        outs.append(o)
        lses.append(l)
    weights = jax.nn.softmax(jnp.stack(lses), axis=0)
    o_b = jnp.einsum('gbsh,gbshd->bshd', weights.astype(outs[0].dtype), jnp.stack(outs))
    y_b = o_b.reshape(bsz, s, B_WIDTH) @ w_b_proj
    gate_a, gate_b = jnp.split(jax.nn.sigmoid(zg), 2, axis=-1)
    return (gate_a * y_a + gate_b * y_b) @ w_o


def hierarchical_moe(x, w_group_router, b_group_router, w_expert_router, b_expert_router, w_gate, w_up, w_down):
    glogits = (x @ w_group_router).astype(jnp.float32) + b_group_router
    gprob = jax.nn.softmax(glogits, axis=-1)
    gmask = jax.nn.one_hot(jnp.argmax(glogits, axis=-1), N_EXPERT_GROUPS, dtype=jnp.float32)
    elogits = jnp.einsum('nd,dge->nge', x, w_expert_router).astype(jnp.float32) + b_expert_router
    top_v, top_i = lax.top_k(elogits, EXPERT_TOPK)
    top_w = jax.nn.softmax(top_v, axis=-1)
    ew = jnp.sum(jax.nn.one_hot(top_i, EXPERTS_PER_GROUP, dtype=jnp.float32) * top_w[..., None], axis=-2)
    combine = (gprob * gmask)[..., None] * ew
    y = jnp.zeros_like(x)
    for g in range(N_EXPERT_GROUPS):
        hid = jax.nn.silu(jnp.einsum('nd,edf->nef', x, w_gate[g])) * jnp.einsum('nd,edf->nef', x, w_up[g])
        hid = hid * combine[:, g, :, None].astype(x.dtype)
        y = y + jnp.einsum('nef,efd->nd', hid, w_down[g])
    return y


def setup_inputs(seed: int = 0) -> dict:
    key = jax.random.key(seed)
    ks = jax.random.split(key, 24)
    f32 = jnp.float32

    def nrm(k, shape, scale):
        return jax.random.normal(k, shape, f32) * scale

    L = DEPTH
    G, E = N_EXPERT_GROUPS, EXPERTS_PER_GROUP
    return {
        'x': nrm(ks[0], (BATCH, SEQ, D_MODEL), 1.0),
        'p': nrm(ks[1], (DEPTH, BATCH, SEQ, PLE_DIM), 1.0),
        'w_in': nrm(ks[2], (L, D_MODEL, IN_TOTAL), D_MODEL ** -0.5),
        'a_ln_g': 1.0 + nrm(ks[3], (L, A_WIDTH), 0.02),
        'a_ln_b': nrm(ks[4], (L, A_WIDTH), 0.02),
        'a_ws': nrm(ks[5], (L, A_GROUPS, A_CHUNK, A_CHUNK), A_CHUNK ** -0.5),
        'a_bs': 1.0 + nrm(ks[6], (L, A_GROUPS, A_CHUNK), 0.02),
        'w_a_proj': nrm(ks[7], (L, A_WIDTH, D_MODEL), A_WIDTH ** -0.5),
        'w_b_proj': nrm(ks[8], (L, B_WIDTH, D_MODEL), B_WIDTH ** -0.5),
        'w_o': nrm(ks[9], (L, D_MODEL, D_MODEL), D_MODEL ** -0.5 * DEEPNORM_BETA),
        'ln1_g': 1.0 + nrm(ks[10], (L, D_MODEL), 0.02),
        'ln1_b': nrm(ks[11], (L, D_MODEL), 0.02),
        'w_group_router': nrm(ks[12], (L, D_MODEL, G), D_MODEL ** -0.5),
        'b_group_router': nrm(ks[13], (L, G), 0.01),
        'w_expert_router': nrm(ks[14], (L, D_MODEL, G, E), D_MODEL ** -0.5),
        'b_expert_router': nrm(ks[15], (L, G, E), 0.01),
        'w_gate': nrm(ks[16], (L, G, E, D_MODEL, D_EXPERT), D_MODEL ** -0.5),
        'w_up': nrm(ks[17], (L, G, E, D_MODEL, D_EXPERT), D_MODEL ** -0.5),
        'w_down': nrm(ks[18], (L, G, E, D_EXPERT, D_MODEL), D_EXPERT ** -0.5 * DEEPNORM_BETA),
        'w_ple': nrm(ks[19], (L, PLE_DIM, D_MODEL), PLE_DIM ** -0.5),
        'w_ple_gate': nrm(ks[20], (L, D_MODEL, D_MODEL), D_MODEL ** -0.5),
        'ln2_g': 1.0 + nrm(ks[21], (L, D_MODEL), 0.02),
        'ln2_b': nrm(ks[22], (L, D_MODEL), 0.02),
    }


def reference(x, p, w_in, a_ln_g, a_ln_b, a_ws, a_bs, w_a_proj, w_b_proj, w_o, ln1_g, ln1_b,
              w_group_router, b_group_router, w_expert_router, b_expert_router, w_gate, w_up, w_down,
              w_ple, w_ple_gate, ln2_g, ln2_b):
    bsz, s, d = x.shape
    for i in range(DEPTH):
        mix = token_mixer(x, w_in[i], a_ln_g[i], a_ln_b[i], a_ws[i], a_bs[i], w_a_proj[i], w_b_proj[i], w_o[i])
        x = layer_norm(DEEPNORM_ALPHA * x + mix, ln1_g[i], ln1_b[i])
        ffn = hierarchical_moe(x.reshape(bsz * s, d), w_group_router[i], b_group_router[i], w_expert_router[i],
                               b_expert_router[i], w_gate[i], w_up[i], w_down[i]).reshape(bsz, s, d)
        ple = (p[i] @ w_ple[i]) * jax.nn.sigmoid(x @ w_ple_gate[i])
        x = layer_norm(DEEPNORM_ALPHA * x + ffn + ple, ln2_g[i], ln2_b[i])
    return x
```

```python
import contextlib
import numpy as np
import ml_dtypes
import concourse.bass as bass
import concourse.mybir as mybir
from concourse.bass_utils import run_bass_kernel_spmd
from concourse.alu_op_type import AluOpType as ALU

F32 = mybir.dt.float32
BF16 = mybir.dt.bfloat16
U32 = mybir.dt.uint32
I32 = mybir.dt.int32
AF = mybir.ActivationFunctionType
AX = mybir.AxisListType

S_TOK = 2048
D = 1024
NT = 16
KC = 8
ALPHA = 2.0 ** 0.25
LN_EPS = 1e-5
GELU_C = 0.7978845608028654
NTILE_E = 63
ENGS = ("pe", "act", "dve", "pool", "sp")
DEBUG = []
HEAD_BARRIER = False
STOP_AFTER = None


class Op:
    __slots__ = ("eng", "fn", "deps", "idx", "is_dma", "sem", "val", "signal")

    def __init__(self, eng, fn, is_dma):
        self.eng = eng
        self.fn = fn
        self.deps = []
        self.is_dma = is_dma
        self.sem = None
        self.val = None
        self.signal = False


class Sched:
    def __init__(self, nc):
        self.nc = nc
        self.ops = []
        self.last_w = {}
        self.readers = {}
        self.dma_slots = {}
        self.final_dma = []
        self.bar_deps = []
        self.bar_need = set()
        self.last_eng = {}
        self.last_slot = {}

    def barrier(self):
        self.bar_deps = list(self.last_eng.values()) + list(self.last_slot.values())
        self.bar_need = set(ENGS)

    def _add(self, op, reads, writes):
        op.idx = len(self.ops)
        deps = set()
        for r in reads:
            w = self.last_w.get(r)
            if w is not None:
                deps.add(w)
        for r in writes:
            w = self.last_w.get(r)
            if w is not None:
                deps.add(w)
            for rd in self.readers.get(r, ()):
                deps.add(rd)
        if op.eng in self.bar_need:
            self.bar_need.discard(op.eng)
            deps.update(self.bar_deps)
        deps.discard(op.idx)
        for d in sorted(deps):
            dop = self.ops[d]
            if dop.eng == op.eng and not dop.is_dma and op.eng in ("pe", "sp"):
                continue
            op.deps.append(d)
            dop.signal = True
        for r in reads:
            self.readers.setdefault(r, []).append(op.idx)
        for r in writes:
            self.last_w[r] = op.idx
            self.readers[r] = []
        self.ops.append(op)
        if not op.is_dma:
            self.last_eng[op.eng] = op.idx
        return op

    def op(self, eng, fn, reads=(), writes=()):
        return self._add(Op(eng, fn, False), tuple(reads), tuple(writes))

    def dma(self, eng, fn, slot, reads=(), writes=(), final=False):
        op = Op(eng, fn, True)
        self.dma_slots.setdefault(slot, []).append(op)
        op.signal = True
        self._add(op, tuple(reads), tuple(writes))
        self.last_slot[slot] = op.idx
        if final:
            self.final_dma.append(op)
        return op

    def emit(self):
        nc = self.nc
        with contextlib.ExitStack() as es:
            esem = {e: es.enter_context(nc.semaphore("s_" + e)) for e in ENGS}
            ssem = {s: es.enter_context(nc.semaphore("d%d" % i)) for i, s in enumerate(self.dma_slots)}
            cnt = {e: 0 for e in ENGS}
            for op in self.ops:
                if op.is_dma:
                    continue
                if op.signal:
                    cnt[op.eng] += 1
                    op.sem = esem[op.eng]
                    op.val = cnt[op.eng]
            for s, ops in self.dma_slots.items():
                c = 0
                for op in ops:
                    c += 16
                    op.sem = ssem[s]
                    op.val = c
            block = es.enter_context(nc.Block())
            per_eng = {e: [o for o in self.ops if o.eng == e] for e in ENGS}

            def run(engname, eng):
                waited = {}
                for op in per_eng[engname]:
                    need = {}
                    for d in op.deps:
                        dop = self.ops[d]
                        k = id(dop.sem)
                        if waited.get(k, 0) >= dop.val:
                            continue
                        if k not in need or need[k][1] < dop.val:
                            need[k] = (dop.sem, dop.val)
                    for k, (sem, val) in need.items():
                        eng.wait_ge(sem, val)
                        waited[k] = val
                    ins = op.fn(eng)
                    if op.is_dma:
                        ins.then_inc(op.sem, 16)
                    elif op.signal:
                        ins.then_inc(op.sem, 1)
                if engname == "sp":
                    fin = {}
                    for op in self.final_dma:
                        k = id(op.sem)
                        if k not in fin or fin[k][1] < op.val:
                            fin[k] = (op.sem, op.val)
                    for sem, val in fin.values():
                        eng.wait_ge(sem, val)

            @block.tensor
            def _(e):
                run("pe", e)

            @block.scalar
            def _(e):
                run("act", e)

            @block.vector
            def _(e):
                run("dve", e)

            @block.gpsimd
            def _(e):
                run("pool", e)

            @block.sync
            def _(e):
                run("sp", e)


def _win_perm():
    perm = []
    blocks = {}

    def add(name, cols):
        blocks[name] = (len(perm), len(cols))
        perm.extend(cols)

    add("u", list(range(0, 512)))
    add("v", list(range(512, 1024)))

    def zb(s, g, h):
        base = 1024 + ((s * 3 + g) * 8 + h) * 64
        return list(range(base, base + 64))

    for ps_ in range(2):
        for g in range(3):
            cols = []
            for h in range(4 * ps_, 4 * ps_ + 4):
                cols += zb(2, g, h)
            add(("vv", ps_, g), cols)
        for hp in range(2 * ps_, 2 * ps_ + 2):
            for s, nm in ((0, "q"), (1, "k")):
                cols = []
                for g in range(3):
                    cols += zb(s, g, 2 * hp) + zb(s, g, 2 * hp + 1)
                add((nm, hp), cols)
    ga = 5632
    gb = 5632 + 1024
    add(("g", 0), list(range(ga, ga + 512)))
    add(("g", 1), list(range(gb, gb + 512)))
    add(("g", 2), list(range(ga + 512, ga + 1024)))
    add(("g", 3), list(range(gb + 512, gb + 1024)))
    assert len(perm) == 7680 and sorted(perm) == list(range(7680))
    return np.array(perm), blocks


WIN_PERM, WIN_BLOCKS = _win_perm()


def build_nc(debug=()):
    nc = bass.Bass("TRN2", target_bir_lowering=False)

    def din(name, shape, dt=F32):
        return nc.dram_tensor(name, list(shape), dt, kind="ExternalInput").ap()

    x_d = din("x", [S_TOK, D])
    p_d = din("p", [S_TOK, 256])
    win_d = din("w_in", [D, 7680])
    alng_d = din("a_ln_g", [1, 512])
    alnb_d = din("a_ln_b", [1, 512])
    awsT_d = din("a_wsT", [128, 8, 128])
    abs_d = din("a_bs", [1, 1024])
    wa_d = din("w_a", [512, D])
    wb_d = din("w_b", [512, D])
    wo_d = din("w_o", [D, D])
    ln1g_d = din("ln1_g", [1, D])
    ln1b_d = din("ln1_b", [1, D])
    wr_d = din("w_r", [D, 36])
    br_d = din("b_r", [1, 36])
    wexp_d = din("wexp", [4096, 6144])
    wple_d = din("w_ple", [256, D])
    wpg_d = din("w_pg", [D, D])
    ln2g_d = din("ln2_g", [1, D])
    ln2b_d = din("ln2_b", [1, D])
    identf_d = din("c_identf", [128, 128])
    identb_d = din("c_identb", [128, 128], BF16)
    mask4_d = din("c_mask4", [128, 512], BF16)
    mown4_d = din("c_mown4", [128, 512], BF16)
    lstr_d = din("c_lstrict", [128, 128], BF16)
    onesb_d = din("c_onesb", [128, 128], BF16)
    kidx_d = din("c_kidx", [128, NTILE_E])
    pidx_d = din("c_pidx", [128, 1])

    out_d = nc.dram_tensor("out", [S_TOK, D], F32, kind="ExternalOutput").ap()
    xs_d = nc.dram_tensor("xs_scr", [NTILE_E * 128, D], BF16, kind="Internal").ap()
    ys_d = nc.dram_tensor("ys_scr", [NTILE_E * 128, D], F32, kind="Internal").ap()
    base_d = nc.dram_tensor("base_scr", [S_TOK, D], F32, kind="Internal").ap()
    dbg = {}

    def dbg_out(name, shape, dt=F32):
        dbg[name] = nc.dram_tensor("dbg_" + name, list(shape), dt, kind="ExternalOutput").ap()
        return dbg[name]

    S = Sched(nc)
    uid = [0]

    def slot(prefix="o"):
        uid[0] += 1
        return "%s%d" % (prefix, uid[0])

    with contextlib.ExitStack() as es0:
        def sbt(es, name, shape, dt):
            return es.enter_context(nc.sbuf_tensor(name, list(shape), dt))

        ps = es0.enter_context(nc.psum_tensor("ps", [128, 8, 512], F32))

        def B(b):
            return ("B", b)

        identf = sbt(es0, "identf", [128, 128], F32)
        identb = sbt(es0, "identb", [128, 128], BF16)
        mask4 = sbt(es0, "mask4", [128, 512], BF16)
        mown4 = sbt(es0, "mown4", [128, 512], BF16)
        lstr = sbt(es0, "lstr", [128, 128], BF16)
        onesb = sbt(es0, "onesb", [128, 128], BF16)
        kidx = sbt(es0, "kidx", [128, NTILE_E], F32)
        pidx = sbt(es0, "pidx", [128, 1], F32)
        for t, d_, nm in ((identf, identf_d, "identf"), (identb, identb_d, "identb"), (mask4, mask4_d, "mask4"),
                          (mown4, mown4_d, "mown4"), (lstr, lstr_d, "lstr"), (onesb, onesb_d, "onesb"),
                          (kidx, kidx_d, "kidx"), (pidx, pidx_d, "pidx")):
            S.dma("sp", (lambda t=t, d_=d_: (lambda e: e.dma_start(out=t[:], in_=d_)))(), slot("c"), writes=[nm])

        slotu = sbt(es0, "slotu", [128, NT, 2], U32)
        cw = sbt(es0, "cw", [128, NT, 2], F32)
        idxw = sbt(es0, "idxw", [128, NTILE_E], U32)
        rowst = sbt(es0, "rowst", [128, NTILE_E], I32)
        mix_es = contextlib.ExitStack()
        mixinT = sbt(mix_es, "mixinT", [128, KC, S_TOK], BF16)

        with contextlib.ExitStack() as esz:
            zt = sbt(esz, "zt", [128, 8064], BF16)
            S.op("pool", lambda e: e.memset(zt[:], 0.0), writes=["zt"])
            xs_flat = xs_d.rearrange("(p r) c -> p (r c)", p=128)
            for q in range(8):
                S.dma("sp", (lambda q=q: (lambda e: e.dma_start(out=xs_flat[:, q * 8064:(q + 1) * 8064], in_=zt[:])))(),
                      "xsz", reads=["zt"], writes=[("xsz", q)])
            S.barrier()

        mx_es = contextlib.ExitStack()
        xT = sbt(mx_es, "xT", [128, KC, S_TOK], BF16)
        obT = sbt(mx_es, "obT", [128, 4, S_TOK], BF16)
        wring = []
        win_v = win_d.rearrange("(kc p) c -> p kc c", p=128)
        wr_state = {"n": 0}
        WB = {}

        def load_wblk(name):
            c0, ncol = WIN_BLOCKS[name]
            bi = wr_state["n"] % len(wring)
            wr_state["n"] += 1
            buf_ = wring[bi]
            S.dma("pool", lambda e: e.dma_start(out=buf_[:, :, 0:ncol], in_=win_v[:, :, c0:c0 + ncol]),
                  "wr%s%d" % (buf_.name, bi), writes=[("wr", bi)])
            WB[bi] = buf_
            return bi

        reg_cache = {}

        def preg(e, val):
            if val not in reg_cache:
                reg_cache[val] = e.to_reg(val)
            return reg_cache[val]

        bank_rr = {"n": 0}

        def nb_(pool):
            bank_rr["n"] += 1
            return pool[bank_rr["n"] % len(pool)]

        evac_rr = {"n": 0}

        def evac_copy(out_ap, in_ap, reads, writes):
            evac_rr["n"] += 1
            if evac_rr["n"] % 2:
                S.op("act", lambda e: e.activation(out=out_ap, in_=in_ap, func=AF.Copy), reads=reads, writes=writes)
            else:
                S.op("dve", lambda e: e.tensor_copy(out_ap, in_ap), reads=reads, writes=writes)

        def rsqrt_batch(es, tag, x_ap, n):
            tf = sbt(es, "rs_tf" + tag, [128, n], F32)
            yi = sbt(es, "rs_yi" + tag, [128, n], I32)
            a_ = sbt(es, "rs_a" + tag, [128, n], F32)
            kx, ky, ka, kt = ("rsx", tag), ("rsy", tag), ("rsa", tag), ("rst", tag)
            S.op("dve", lambda e: e.tensor_copy(tf[:], x_ap.bitcast(I32)), reads=[kx], writes=[kt])
            S.op("dve", lambda e: e.tensor_scalar(out=tf[:], in0=tf[:], scalar1=-0.5, scalar2=1597463007.0,
                                                  op0=ALU.mult, op1=ALU.add), reads=[kt], writes=[kt])
            S.op("dve", lambda e: e.tensor_copy(yi[:], tf[:]), reads=[kt], writes=[ky])
            y = yi[:].bitcast(F32)
            for _ in range(2):
                S.op("dve", lambda e: e.tensor_tensor(out=a_[:], in0=y, in1=y, op=ALU.mult), reads=[ky], writes=[ka])
                S.op("dve", lambda e: e.tensor_tensor(out=a_[:], in0=a_[:], in1=x_ap, op=ALU.mult), reads=[ka, kx], writes=[ka])
                S.op("dve", lambda e: e.tensor_scalar(out=a_[:], in0=a_[:], scalar1=-0.5, scalar2=1.5,
                                                      op0=ALU.mult, op1=ALU.add), reads=[ka], writes=[ka])
                S.op("dve", lambda e: e.tensor_tensor(out=y, in0=y, in1=a_[:], op=ALU.mult), reads=[ka, ky], writes=[ky])
            return y, ky, kx


        with contextlib.ExitStack() as es1:
            wring[:] = [sbt(es1, "wringA%d" % i, [128, KC, 512], BF16) for i in range(3)]
            order = []
            for ps_ in range(2):
                order += [("vv", ps_, 0), ("vv", ps_, 1), ("vv", ps_, 2)]
                for hp in range(2 * ps_, 2 * ps_ + 2):
                    order += [("q", hp), ("k", hp)]
            loaded = {}
            nxt = [0]

            def ensure(upto):
                while nxt[0] < len(order) and nxt[0] <= upto:
                    loaded[order[nxt[0]]] = load_wblk(order[nxt[0]])
                    nxt[0] += 1

            ensure(1)
            with contextlib.ExitStack() as esx:
                xst = [sbt(esx, "xst%d" % i, [128, D], BF16) for i in range(2)]
                for i in range(NT):
                    xs_ = xst[i % 2]
                    S.dma("pool", (lambda i=i, xs_=xs_: (lambda e: e.dma_start(out=xs_[:], in_=x_d[i * 128:(i + 1) * 128, :])))(),
                          "xst%d" % (i % 2), writes=[("xst", i % 2)])
                    bk = nb_([0, 1, 2, 3])
                    psb = ps[:, bk, :].bitcast(BF16)
                    for kc in range(KC):
                        S.op("pe", (lambda psb=psb, kc=kc, xs_=xs_: (lambda e: e.transpose(
                            out=psb[:, kc * 128:(kc + 1) * 128], in_=xs_[:, kc * 128:(kc + 1) * 128], identity=identb[:])))(),
                            reads=[("xst", i % 2), "identb"], writes=[B(bk)])
                    evac_copy(xT[:, :, i * 128:(i + 1) * 128], psb.rearrange("p (j t) -> p j t", j=KC),
                              reads=[], writes=[B(bk), ("xT", i)])
                S.barrier()
            if "xT" in debug:
                o = dbg_out("xT", [128, KC, S_TOK], BF16)
                S.dma("sp", lambda e, o=o: e.dma_start(out=o, in_=xT[:]), slot(), reads=[("xT", i) for i in range(NT)], final=True)

            XT_ALL = [("xT", i) for i in range(NT)]
            Vaug = [sbt(es1, "vaug%d" % g, [128, 16, 4, 128], BF16) for g in range(3)]
            qk = sbt(es1, "qk", [128, 6, S_TOK], BF16)
            PTb = [sbt(es1, "ptb%d" % i, [128, 512], BF16) for i in range(2)]
            PT2 = sbt(es1, "pt2", [128, 16, 128], BF16)
            rd = [sbt(es1, "rd%d" % i, [64, 512], F32) for i in range(2)]
            for g in range(3):
                S.op("pool", (lambda g=g: (lambda e: e.memset(Vaug[g][:, :, :, 64:128], 1.0)))(), writes=[("vones", g)])

            def v_tile_tokens(g, t):
                if g == 0:
                    return slice(t * 128, (t + 1) * 128), [("xT", t)]
                if g == 1:
                    r4, nb = t // 4, t % 4
                    return slice(512 * nb + r4, 512 * (nb + 1), 4), [("xT", 4 * nb + j) for j in range(4)]
                return slice(t, S_TOK, 16), XT_ALL

            PROJ = [4, 5, 6, 7]
            blk_i = [0]
            pt_rr = [0]
            acc_rr = [0]
            st_rr = [0]

            for ps_ in range(2):
                for g in range(3):
                    ensure(blk_i[0] + 2)
                    wb_i = loaded[("vv", ps_, g)]
                    blk_i[0] += 1
                    for t0 in range(0, 16, 2):
                        bk = nb_(PROJ)
                        rk = []
                        for tt in range(2):
                            sl, keys = v_tile_tokens(g, t0 + tt)
                            rk += keys
                            for kc in range(KC):
                                S.op("pe", (lambda bk=bk, tt=tt, kc=kc, sl=sl, wt=WB[wb_i]: (lambda e: e.matmul(
                                    ps[:, bk, tt * 256:(tt + 1) * 256], lhsT=xT[:, kc, sl], rhs=wt[:, kc, 0:256],
                                    start=(kc == 0), stop=(kc == KC - 1))))(),
                                    reads=keys + [("wr", wb_i)], writes=[B(bk)])
                        evac_copy(Vaug[g][:, t0:t0 + 2, :, 0:64],
                                  ps[:, bk, :].rearrange("p (t h d) -> p t h d", t=2, h=4),
                                  reads=[], writes=[B(bk), ("V", g, t0), ("V", g, t0 + 1)])
                for hp in range(2 * ps_, 2 * ps_ + 2):
                    for si, nm in ((0, "q"), (1, "k")):
                        ensure(blk_i[0] + 2)
                        wb_i = loaded[(nm, hp)]
                        blk_i[0] += 1
                        for g in range(3):
                            sl_ = si * 3 + g
                            for sp in range(4):
                                bk = nb_(PROJ)
                                for kc in range(KC):
                                    S.op("pe", (lambda bk=bk, kc=kc, g=g, sp=sp, wt=WB[wb_i]: (lambda e: e.matmul(
                                        ps[:, bk, :], lhsT=wt[:, kc, g * 128:(g + 1) * 128],
                                        rhs=xT[:, kc, sp * 512:(sp + 1) * 512], start=(kc == 0), stop=(kc == KC - 1))))(),
                                        reads=[("xT", 4 * sp + j) for j in range(4)] + [("wr", wb_i)], writes=[B(bk)])
                                if g == 0:
                                    o_ap = qk[:, sl_, sp * 512:(sp + 1) * 512]
                                    i_ap = ps[:, bk, :]
                                elif g == 1:
                                    o_ap = qk[:, sl_, :].rearrange("p (r n i) -> p r n i", r=4, n=4)[:, :, sp, :]
                                    i_ap = ps[:, bk, :].rearrange("p (i r) -> p r i", r=4)
                                else:
                                    o_ap = qk[:, sl_, :].rearrange("p (r a) -> p r a", r=16)[:, :, sp * 32:(sp + 1) * 32]
                                    i_ap = ps[:, bk, :].rearrange("p (a r) -> p r a", r=16)
                                evac_copy(o_ap, i_ap, reads=[], writes=[B(bk), ("qk", sl_, sp)])
                    for h in (2 * hp, 2 * hp + 1):
                        b0 = (h % 2) * 64
                        hl = h % 4
                        QK_ALL = lambda s_: [("qk", s_, sp) for sp in range(4)]
                        for grp in range(4):
                            bk = nb_([2, 3])
                            for jj in range(4):
                                r = grp * 4 + jj
                                S.op("pe", (lambda bk=bk, jj=jj, r=r, b0=b0: (lambda e: e.matmul(
                                    ps[:, bk, jj * 128:(jj + 1) * 128], lhsT=qk[b0:b0 + 64, 5, r * 128:(r + 1) * 128],
                                    rhs=qk[b0:b0 + 64, 2, r * 128:(r + 1) * 128], start=True, stop=True)))(),
                                    reads=QK_ALL(5) + QK_ALL(2), writes=[B(bk)])
                            pview = PT2[:, grp * 4:(grp + 1) * 4, :]
                            S.op("act", (lambda bk=bk, pview=pview: (lambda e: e.activation(
                                out=pview, in_=ps[:, bk, :].rearrange("p (a b) -> p a b", a=4), func=AF.Exp, scale=0.125)))(),
                                reads=[], writes=[B(bk), ("pt2", grp)])
                            S.op("pool", (lambda pview=pview: (lambda e: e.tensor_tensor(
                                out=pview, in0=pview, in1=mown4[:].rearrange("p (a b) -> p a b", a=4), op=ALU.mult)))(),
                                reads=["mown4"], writes=[("pt2", grp)])
                        for s in range(4):
                            acc_rr[0] += 1
                            ab = acc_rr[0] % 2
                            first = [True]
                            blocks_ = []
                            for j in range(4 * s, 4 * s + 4):
                                q_ap = qk[b0:b0 + 64, 0, j * 128:(j + 1) * 128]
                                o_ap = ps[:, ab, (j - 4 * s) * 128:(j - 4 * s + 1) * 128]
                                prev = None
                                if j > 0:
                                    prev = (qk[b0:b0 + 64, 3, (j - 1) * 128:j * 128], Vaug[0][:, j - 1, hl, :],
                                            [("qk", 3, (j - 1) // 4), ("V", 0, j - 1), ("vones", 0)])
                                own = (qk[b0:b0 + 64, 3, j * 128:(j + 1) * 128], Vaug[0][:, j, hl, :],
                                       [("qk", 3, j // 4), ("V", 0, j), ("vones", 0)])
                                blocks_.append((q_ap, [("qk", 0, s)], o_ap, prev, own))
                            for r4 in range(4):
                                q_ap = qk[b0:b0 + 64, 1, r4 * 512 + s * 128:r4 * 512 + (s + 1) * 128]
                                o_ap = ps[:, ab, r4:512:4]
                                prev = None
                                if s > 0:
                                    prev = (qk[b0:b0 + 64, 4, r4 * 512 + (s - 1) * 128:r4 * 512 + s * 128],
                                            Vaug[1][:, r4 * 4 + s - 1, hl, :],
                                            [("qk", 4, s - 1), ("V", 1, r4 * 4 + s - 1), ("vones", 1)])
                                own = (qk[b0:b0 + 64, 4, r4 * 512 + s * 128:r4 * 512 + (s + 1) * 128],
                                       Vaug[1][:, r4 * 4 + s, hl, :],
                                       [("qk", 4, s), ("V", 1, r4 * 4 + s), ("vones", 1)])
                                blocks_.append((q_ap, [("qk", 1, s)], o_ap, prev, own))
                            for bp in range(0, 8, 2):
                                st_rr[0] += 1
                                sb_ = 2 + st_rr[0] % 2
                                pt_rr[0] += 1
                                ptb = PTb[pt_rr[0] % 2]
                                ptk = ("ptb", pt_rr[0] % 2)
                                used = []
                                pv = []
                                for bi_, blk in enumerate(blocks_[bp:bp + 2]):
                                    q_ap, qkeys, o_ap, prev, own = blk
                                    for kind, it in ((0, prev), (1, own)):
                                        if it is None:
                                            continue
                                        sl_i = bi_ * 2 + kind
                                        used.append(sl_i)
                                        k_ap, v_ap, rkeys = it
                                        S.op("pe", (lambda sb_=sb_, sl_i=sl_i, k_ap=k_ap, q_ap=q_ap: (lambda e: e.matmul(
                                            ps[:, sb_, sl_i * 128:(sl_i + 1) * 128], lhsT=k_ap, rhs=q_ap, start=True, stop=True)))(),
                                            reads=qkeys + [rkeys[0]], writes=[B(sb_)])
                                        pv.append((sl_i, v_ap, o_ap, rkeys[1:]))
                                if used == [0, 1, 2, 3]:
                                    sel = lambda ap: ap
                                elif used == [1, 2, 3]:
                                    sel = lambda ap: ap[:, 128:512]
                                else:
                                    assert used == [1, 3], used
                                    sel = lambda ap: ap.rearrange("p (a b) -> p a b", a=4)[:, 1:4:2, :]
                                S.op("act", (lambda sb_=sb_, ptb=ptb, sel=sel: (lambda e: e.activation(
                                    out=sel(ptb[:]), in_=sel(ps[:, sb_, :]), func=AF.Exp, scale=0.125)))(),
                                    reads=[], writes=[B(sb_), ptk])
                                S.op("pool", (lambda ptb=ptb, sel=sel: (lambda e: e.tensor_tensor(
                                    out=sel(ptb[:]), in0=sel(ptb[:]), in1=sel(mask4[:]), op=ALU.mult)))(),
                                    reads=["mask4"], writes=[ptk])
                                for sl_i, v_ap, o_ap, rkeys in pv:
                                    st_flag = first[0]
                                    first[0] = False
                                    S.op("pe", (lambda sl_i=sl_i, v_ap=v_ap, o_ap=o_ap, ptb=ptb, st_flag=st_flag: (lambda e: e.matmul(
                                        o_ap, lhsT=v_ap, rhs=ptb[:, sl_i * 128:(sl_i + 1) * 128], start=st_flag, stop=False)))(),
                                        reads=[ptk] + rkeys, writes=[B(ab)])
                            for r in range(16):
                                S.op("pe", (lambda r=r, s=s, ab=ab, hl=hl: (lambda e: e.matmul(
                                    ps[:, ab, r:512:16], lhsT=Vaug[2][:, r, hl, :], rhs=PT2[:, r, 32 * s:32 * (s + 1)],
                                    start=False, stop=(r == 15))))(),
                                    reads=[("pt2", r // 4), ("V", 2, r), ("vones", 2)], writes=[B(ab)])
                            rdt = rd[ab]
                            S.op("dve", (lambda ab=ab, rdt=rdt: (lambda e: e.reciprocal(out=rdt[:], in_=ps[64:128, ab, :])))(),
                                 reads=[], writes=[B(ab), ("rd", ab)])
                            S.op("dve", (lambda ab=ab, rdt=rdt, b0=b0, hp=hp, s=s: (lambda e: e.tensor_tensor(
                                out=obT[b0:b0 + 64, hp, s * 512:(s + 1) * 512], in0=ps[0:64, ab, :], in1=rdt[:], op=ALU.mult)))(),
                                reads=[("rd", ab)], writes=[B(ab), ("obT", hp, s, h % 2)])
                        if HEAD_BARRIER:
                            S.barrier()
            S.barrier()
        OBT_ALL = [("obT", hp, s, hh) for hp in range(4) for s in range(4) for hh in range(2)]
        if "obT" in debug:
            o = dbg_out("obT", [128, 4, S_TOK], BF16)
            S.dma("sp", lambda e, o=o: e.dma_start(out=o, in_=obT[:]), slot(), reads=OBT_ALL, final=True)

        XT_ALL = [("xT", i) for i in range(NT)]
        yaT_es = contextlib.ExitStack()
        yaT = sbt(yaT_es, "yaT", [128, 4, S_TOK], BF16)
        wring[:] = [sbt(yaT_es, "wringB%d" % i, [128, KC, 512], BF16) for i in range(4)]
        wr_state["n"] = 0
        with contextlib.ExitStack() as es2:
            vg = sbt(es2, "vg", [128, NT, 512], F32)
            lng = sbt(es2, "lng", [128, 512], F32)
            lnb = sbt(es2, "lnb", [128, 512], F32)
            wsf = sbt(es2, "wsf", [128, 8, 128], F32)
            WmT = sbt(es2, "wmT", [128, 8, 128], BF16)
            bsf = sbt(es2, "bsf", [2, 1024], F32)
            bsh = sbt(es2, "bsh", [2, 1024], BF16)
            bshf = sbt(es2, "bshf", [2, 1024], F32)
            bsl = sbt(es2, "bsl", [2, 1024], BF16)
            sqb = [sbt(es2, "sqb%d" % i, [128, 512], F32) for i in range(3)]
            tnb = [sbt(es2, "tnb%d" % i, [128, 512], F32) for i in range(3)]
            st6 = sbt(es2, "st6", [128, NT, 6], F32)
            mv = sbt(es2, "mv", [128, NT, 2], F32)
            xe = sbt(es2, "xeA", [128, NT], F32)
            lnt = [sbt(es2, "lnt%d" % i, [128, 512], F32) for i in range(2)]
            vln = [sbt(es2, "vln%d" % i, [128, 512], BF16) for i in range(2)]
            bu = load_wblk("u")
            bv = load_wblk("v")
            S.dma("sp", lambda e: e.dma_start(out=lng[:], in_=alng_d[0].partition_broadcast(128)), slot("c"), writes=["lng"])
            S.dma("sp", lambda e: e.dma_start(out=lnb[:], in_=alnb_d[0].partition_broadcast(128)), slot("c"), writes=["lnb"])
            S.dma("sp", lambda e: e.dma_start(out=wsf[:], in_=awsT_d), slot("c"), writes=["wsf"])
            S.dma("sp", lambda e: e.dma_start(out=bsf[0:1, :], in_=abs_d), slot("c"), writes=["bsf0"])
            S.dma("sp", lambda e: e.dma_start(out=bsf[1:2, :], in_=abs_d), slot("c"), writes=["bsf1"])
            S.op("dve", lambda e: e.tensor_tensor(out=WmT[:], in0=wsf[:],
                                                  in1=mown4[:].rearrange("p (a b) -> p a b", a=4)[:, 0:1, :].to_broadcast([128, 8, 128]),
                                                  op=ALU.mult), reads=["wsf", "mown4"], writes=["WmT"])
            S.op("dve", lambda e: e.tensor_copy(bsh[:], bsf[:]), reads=["bsf0", "bsf1"], writes=["bsh"])
            S.op("dve", lambda e: e.tensor_copy(bshf[:], bsh[:]), reads=["bsh"], writes=["bshf"])
            S.op("dve", lambda e: e.tensor_tensor(out=bshf[:], in0=bsf[:], in1=bshf[:], op=ALU.subtract), reads=["bshf"], writes=["bshf"])
            S.op("dve", lambda e: e.tensor_copy(bsl[:], bshf[:]), reads=["bshf"], writes=["bsl"])
            S.dma("sp", lambda e: e.dma_start(out=bsh[1:2, :], in_=bsl[1:2, :]), slot("c"), reads=["bsl", "bsh"], writes=["bsh"])

            grr = [0]

            def gelu2(bk, out_ap, wkeys):
                grr[0] += 1
                sq = sqb[grr[0] % 3]
                tn = tnb[grr[0] % 3]
                ks, kt = ("sqb", grr[0] % 3), ("tnb", grr[0] % 3)
                S.op("act", lambda e: e.activation(out=sq[:], in_=ps[:, bk, :], func=AF.Square), reads=[], writes=[B(bk), ks])
                S.op("pool", lambda e: e.tensor_scalar(out=sq[:], in0=sq[:], scalar1=0.044715, scalar2=1.0,
                                                       op0=ALU.mult, op1=ALU.add), reads=[], writes=[ks])
                S.op("dve", lambda e: e.tensor_tensor(out=sq[:], in0=sq[:], in1=ps[:, bk, :], op=ALU.mult),
                     reads=[], writes=[B(bk), ks])
                S.op("act", lambda e: e.activation(out=tn[:], in_=sq[:], func=AF.Tanh, scale=GELU_C), reads=[ks], writes=[kt])
                S.op("dve", lambda e: e.scalar_tensor_tensor(out=out_ap, in0=tn[:], scalar=1.0, in1=ps[:, bk, :],
                                                             op0=ALU.add, op1=ALU.mult), reads=[kt], writes=[B(bk)] + wkeys)

            ALLB = [0, 1, 2, 3, 4, 5, 6, 7]
            for fc in range(4):
                for sp in range(4):
                    bk = nb_(ALLB)
                    for kc in range(KC):
                        S.op("pe", (lambda bk=bk, kc=kc, fc=fc, sp=sp, wt=WB[bu]: (lambda e: e.matmul(
                            ps[:, bk, :], lhsT=wt[:, kc, fc * 128:(fc + 1) * 128], rhs=xT[:, kc, sp * 512:(sp + 1) * 512],
                            start=(kc == 0), stop=(kc == KC - 1))))(),
                            reads=[("xT", 4 * sp + j) for j in range(4)] + [("wr", bu)], writes=[B(bk)])
                    gelu2(bk, yaT[:, fc, sp * 512:(sp + 1) * 512], [("yaT", fc, 4 * sp + j) for j in range(4)])
            for i in range(NT):
                bk = nb_(ALLB)
                for kc in range(KC):
                    S.op("pe", (lambda bk=bk, kc=kc, i=i, wt=WB[bv]: (lambda e: e.matmul(
                        ps[:, bk, :], lhsT=xT[:, kc, i * 128:(i + 1) * 128], rhs=wt[:, kc, 0:512],
                        start=(kc == 0), stop=(kc == KC - 1))))(),
                        reads=[("xT", i), ("wr", bv)], writes=[B(bk)])
                gelu2(bk, vg[:, i, :], [("vg", i)])
                S.op("dve", (lambda i=i: (lambda e: e.bn_stats(out=st6[:, i, :], in_=vg[:, i, :])))(), reads=[("vg", i)], writes=[("st6", i)])
                S.op("dve", (lambda i=i: (lambda e: e.bn_aggr(out=mv[:, i, :], in_=st6[:, i, :])))(), reads=[("st6", i)], writes=[("mvA", i)])
            S.op("dve", lambda e: e.tensor_scalar(out=xe[:], in0=mv[:, :, 1], scalar1=4.0 * LN_EPS, scalar2=None, op0=ALU.add),
                 reads=[("mvA", i) for i in range(NT)], writes=[("rsx", "A")])
            rstd, krs, _ = rsqrt_batch(es2, "A", xe[:], NT)
            for i in range(NT):
                lt = lnt[i % 2]
                vl = vln[i % 2]
                S.op("dve", (lambda i=i, lt=lt: (lambda e: e.tensor_scalar(out=lt[:], in0=vg[:, i, :], scalar1=mv[:, i, 0:1],
                                                                           scalar2=rstd[:, i:i + 1], op0=ALU.subtract, op1=ALU.mult)))(),
                     reads=[("vg", i), ("mvA", i), krs], writes=[("lnt", i % 2)])
                S.op("pool", (lambda lt=lt: (lambda e: e.tensor_tensor(out=lt[:], in0=lt[:], in1=lng[:], op=ALU.mult)))(),
                     reads=["lng"], writes=[("lnt", i % 2)])
                S.op("pool", (lambda lt=lt, vl=vl: (lambda e: e.tensor_tensor(out=vl[:], in0=lt[:], in1=lnb[:], op=ALU.add)))(),
                     reads=["lnb", ("lnt", i % 2)], writes=[("vln", i % 2)])
                bk = nb_(ALLB)
                for cc in range(4):
                    for gg in range(2):
                        g = 2 * cc + gg
                        o_ap = ps[gg * 64:(gg + 1) * 64, bk, cc * 128:(cc + 1) * 128]
                        S.op("pe", (lambda o_ap=o_ap, g=g, vl=vl: (lambda e: e.matmul(
                            o_ap, lhsT=vl[:, g * 64:(g + 1) * 64], rhs=WmT[:, g, :], start=True, stop=False)))(),
                            reads=[("vln", i % 2), "WmT"], writes=[B(bk)])
                        S.op("pe", (lambda o_ap=o_ap, g=g: (lambda e: e.matmul(
                            o_ap, lhsT=onesb[0:2, 0:64], rhs=bsh[0:2, g * 128:(g + 1) * 128], start=False, stop=True)))(),
                            reads=["bsh", "onesb"], writes=[B(bk)])
                ya_v = yaT[:, :, i * 128:(i + 1) * 128]
                S.op("dve", (lambda bk=bk, ya_v=ya_v: (lambda e: e.scalar_tensor_tensor(
                    out=ya_v, in0=ps[:, bk, :].rearrange("p (c t) -> p c t", c=4), scalar=0.5, in1=ya_v, op0=ALU.mult, op1=ALU.mult)))(),
                    reads=[], writes=[B(bk)] + [("yaT", fc, i) for fc in range(4)])
            S.barrier()
        YAT_ALL = [("yaT", fc, i) for fc in range(4) for i in range(NT)]
        if "yaT" in debug:
            o = dbg_out("yaT", [128, 4, S_TOK], BF16)
            S.dma("sp", lambda e, o=o: e.dma_start(out=o, in_=yaT[:]), slot(), reads=YAT_ALL, final=True)

        with contextlib.ExitStack() as es3:
            wa = sbt(es3, "wa", [128, 4, D], BF16)
            wb = sbt(es3, "wb", [128, 4, D], BF16)
            ta = [sbt(es3, "ta%d" % i, [128, 512], BF16) for i in range(2)]
            tb = [sbt(es3, "tb%d" % i, [128, 512], BF16) for i in range(2)]
            m1 = [sbt(es3, "m1_%d" % i, [128, 512], F32) for i in range(2)]
            m2 = [sbt(es3, "m2_%d" % i, [128, 512], F32) for i in range(2)]
            S.dma("pool", lambda e: e.dma_start(out=wa[:], in_=wa_d.rearrange("(c p) d -> p c d", p=128)), slot("c"), writes=["wa"])
            S.dma("pool", lambda e: e.dma_start(out=wb[:], in_=wb_d.rearrange("(c p) d -> p c d", p=128)), slot("c"), writes=["wb"])
            gblk = {}
            gblk[0] = load_wblk(("g", 0))
            gblk[1] = load_wblk(("g", 1))
            gblk[2] = load_wblk(("g", 2))
            gblk[3] = load_wblk(("g", 3))
            it_ = 0
            for dc in range(KC):
                ga_i = gblk[2 * (dc // 4)]
                gb_i = gblk[2 * (dc // 4) + 1]
                for sp in range(4):
                    it_ += 1
                    par = it_ % 2
                    bA, bB, bC, bD = [4 * par + j for j in range(4)]
                    tok = slice(sp * 512, (sp + 1) * 512)
                    xk = [("xT", 4 * sp + j) for j in range(4)]
                    for cc in range(4):
                        S.op("pe", (lambda cc=cc, bA=bA, dc=dc, tok=tok: (lambda e: e.matmul(
                            ps[:, bA, :], lhsT=wa[:, cc, dc * 128:(dc + 1) * 128], rhs=yaT[:, cc, tok], start=(cc == 0), stop=(cc == 3))))(),
                            reads=["wa"] + [("yaT", cc, 4 * sp + j) for j in range(4)], writes=[B(bA)])
                    for cc in range(4):
                        S.op("pe", (lambda cc=cc, bB=bB, dc=dc, tok=tok: (lambda e: e.matmul(
                            ps[:, bB, :], lhsT=wb[:, cc, dc * 128:(dc + 1) * 128], rhs=obT[:, cc, tok], start=(cc == 0), stop=(cc == 3))))(),
                            reads=["wb"] + [("obT", cc, sp, 0), ("obT", cc, sp, 1)], writes=[B(bB)])
                    for (bX, gi) in ((bC, ga_i), (bD, gb_i)):
                        for kc in range(KC):
                            S.op("pe", (lambda kc=kc, bX=bX, wt=WB[gi], dc=dc, tok=tok: (lambda e: e.matmul(
                                ps[:, bX, :], lhsT=wt[:, kc, (dc % 4) * 128:(dc % 4 + 1) * 128], rhs=xT[:, kc, tok],
                                start=(kc == 0), stop=(kc == KC - 1))))(),
                                reads=xk + [("wr", gi)], writes=[B(bX)])
                    S.op("act", (lambda bC=bC, par=par: (lambda e: e.activation(out=ta[par][:], in_=ps[:, bC, :], func=AF.Tanh, scale=0.5)))(),
                         reads=[], writes=[B(bC), ("ta", par)])
                    S.op("act", (lambda bD=bD, par=par: (lambda e: e.activation(out=tb[par][:], in_=ps[:, bD, :], func=AF.Tanh, scale=0.5)))(),
                         reads=[], writes=[B(bD), ("tb", par)])
                    S.op("dve", (lambda bA=bA, par=par: (lambda e: e.scalar_tensor_tensor(
                        out=m1[par][:], in0=ta[par][:], scalar=1.0, in1=ps[:, bA, :], op0=ALU.add, op1=ALU.mult)))(),
                        reads=[("ta", par)], writes=[B(bA), ("m1", par)])
                    S.op("dve", (lambda bB=bB, par=par: (lambda e: e.scalar_tensor_tensor(
                        out=m2[par][:], in0=tb[par][:], scalar=1.0, in1=ps[:, bB, :], op0=ALU.add, op1=ALU.mult)))(),
                        reads=[("tb", par)], writes=[B(bB), ("m2", par)])
                    S.op("pool", (lambda par=par, dc=dc, tok=tok: (lambda e: e.tensor_tensor(
                        out=mixinT[:, dc, tok], in0=m1[par][:], in1=m2[par][:], op=ALU.add)))(),
                        reads=[("m1", par), ("m2", par)], writes=[("mix", dc, 4 * sp + j) for j in range(4)])
            S.barrier()
        yaT_es.close()
        mx_es.close()
        if "mixinT" in debug:
            o = dbg_out("mixinT", [128, KC, S_TOK], BF16)
            S.dma("sp", lambda e, o=o: e.dma_start(out=o, in_=mixinT[:]), slot(),
                  reads=[("mix", dc, i) for dc in range(KC) for i in range(NT)], final=True)

        p5_es = contextlib.ExitStack()
        x1b = sbt(p5_es, "x1b", [128, NT, D], BF16)
        logit = sbt(p5_es, "logit", [128, NT, 36], F32)
        G4 = 4
        with contextlib.ExitStack() as es4:
            wo = sbt(es4, "wo", [128, KC, D], BF16)
            wpg = sbt(es4, "wpg", [128, KC, D], BF16)
            wple = sbt(es4, "wple", [128, 2, D], BF16)
            wr = sbt(es4, "wr", [128, KC, 36], F32)
            wrh = sbt(es4, "wrh", [128, KC, 36], BF16)
            wrhf = sbt(es4, "wrhf", [128, KC, 36], F32)
            wrl = sbt(es4, "wrl", [128, KC, 36], BF16)
            x1lo = [sbt(es4, "x1lo%d" % i, [128, D], BF16) for i in range(2)]
            brb = sbt(es4, "brb", [128, 36], F32)
            l1g = sbt(es4, "l1g", [128, D], F32)
            l1b = sbt(es4, "l1b", [128, D], F32)
            xres = [sbt(es4, "xres%d" % i, [128, D], F32) for i in range(2)]
            rg = sbt(es4, "rg", [128, G4, D], F32)
            st12 = sbt(es4, "st12", [128, G4, 12], F32)
            mv1 = sbt(es4, "mv1", [128, G4, 2], F32)
            xe1 = sbt(es4, "xe1", [128, G4], F32)
            x1f = [sbt(es4, "x1f%d" % i, [128, D], F32) for i in range(2)]
            x1Tf = [sbt(es4, "x1Tl%d" % i, [128, KC, 128], BF16) for i in range(2)]
            x1Tb = [sbt(es4, "x1Tb%d" % i, [128, KC, 128], BF16) for i in range(2)]
            pst = [sbt(es4, "pst%d" % i, [128, 256], BF16) for i in range(2)]
            pT = [sbt(es4, "pT%d" % i, [128, 2, 128], BF16) for i in range(2)]
            tp = [sbt(es4, "tp%d" % i, [128, D], BF16) for i in range(2)]
            ple2 = [sbt(es4, "ple2_%d" % i, [128, D], F32) for i in range(2)]
            S.dma("pool", lambda e: e.dma_start(out=wo[:], in_=wo_d.rearrange("(c p) d -> p c d", p=128)), slot("c"), writes=["wo"])
            S.dma("pool", lambda e: e.dma_start(out=wpg[:], in_=wpg_d.rearrange("(c p) d -> p c d", p=128)), slot("c"), writes=["wpg"])
            S.dma("pool", lambda e: e.dma_start(out=wple[:], in_=wple_d.rearrange("(c p) d -> p c d", p=128)), slot("c"), writes=["wple"])
            S.dma("sp", lambda e: e.dma_start(out=wr[:], in_=wr_d.rearrange("(c p) d -> p c d", p=128)), slot("c"), writes=["wr_"])
            S.dma("sp", lambda e: e.dma_start(out=brb[:], in_=br_d[0].partition_broadcast(128)), slot("c"), writes=["brb"])
            S.op("dve", lambda e: e.tensor_copy(wrh[:], wr[:]), reads=["wr_"], writes=["wrh"])
            S.op("dve", lambda e: e.tensor_copy(wrhf[:], wrh[:]), reads=["wrh"], writes=["wrhf"])
            S.op("dve", lambda e: e.tensor_tensor(out=wrl[:], in0=wr[:], in1=wrhf[:], op=ALU.subtract), reads=["wr_", "wrhf"], writes=["wrl"])
            S.dma("sp", lambda e: e.dma_start(out=l1g[:], in_=ln1g_d[0].partition_broadcast(128)), slot("c"), writes=["l1g"])
            S.dma("sp", lambda e: e.dma_start(out=l1b[:], in_=ln1b_d[0].partition_broadcast(128)), slot("c"), writes=["l1b"])
            rs_es = contextlib.ExitStack()
            rsq = {}
            for gi0 in range(0, NT, G4):
                tag = "L1_%d" % gi0
                for gi in range(G4):
                    i = gi0 + gi
                    xr = xres[i % 2]
                    S.dma("sp", (lambda i=i, xr=xr: (lambda e: e.dma_start(out=xr[:], in_=x_d[i * 128:(i + 1) * 128, :])))(),
                          "xres%d" % (i % 2), writes=[("xres", i % 2)])
                    S.op("act", (lambda xr=xr: (lambda e: e.activation(out=xr[:], in_=xr[:], func=AF.Identity, scale=ALPHA)))(),
                         reads=[], writes=[("xres", i % 2)])
                    for half in range(2):
                        bk = half
                        for dc in range(KC):
                            S.op("pe", (lambda bk=bk, dc=dc, i=i, half=half: (lambda e: e.matmul(
                                ps[:, bk, :], lhsT=mixinT[:, dc, i * 128:(i + 1) * 128], rhs=wo[:, dc, half * 512:(half + 1) * 512],
                                start=(dc == 0), stop=(dc == KC - 1))))(),
                                reads=[("mix", dc, i), "wo"], writes=[B(bk)])
                        S.op("dve", (lambda bk=bk, gi=gi, half=half, xr=xr: (lambda e: e.scalar_tensor_tensor(
                            out=rg[:, gi, half * 512:(half + 1) * 512], in0=ps[:, bk, :], scalar=0.5,
                            in1=xr[:, half * 512:(half + 1) * 512], op0=ALU.mult, op1=ALU.add)))(),
                            reads=[("xres", i % 2)], writes=[B(bk), ("rg", gi, half)])
                        S.op("dve", (lambda gi=gi, half=half: (lambda e: e.bn_stats(
                            out=st12[:, gi, half * 6:(half + 1) * 6], in_=rg[:, gi, half * 512:(half + 1) * 512])))(),
                            reads=[("rg", gi, half)], writes=[("st12", gi, half)])
                    S.op("dve", (lambda gi=gi: (lambda e: e.bn_aggr(out=mv1[:, gi, :], in_=st12[:, gi, :])))(),
                         reads=[("st12", gi, 0), ("st12", gi, 1)], writes=[("mv1", gi)])
                S.op("dve", lambda e: e.tensor_scalar(out=xe1[:], in0=mv1[:, :, 1], scalar1=LN_EPS, scalar2=None, op0=ALU.add),
                     reads=[("mv1", gi) for gi in range(G4)], writes=[("rsx", tag)])
                if "L1" not in rsq:
                    rsq["L1"] = (sbt(es4, "rs_tfL1", [128, G4], F32), sbt(es4, "rs_yiL1", [128, G4], I32), sbt(es4, "rs_aL1", [128, G4], F32))
                tf_, yi_, a__ = rsq["L1"]
                kx, ky, ka, kt = ("rsx", tag), ("rsy", "L1"), ("rsa", "L1"), ("rst", "L1")
                S.op("dve", lambda e: e.tensor_copy(tf_[:], xe1[:].bitcast(I32)), reads=[kx], writes=[kt])
                S.op("dve", lambda e: e.tensor_scalar(out=tf_[:], in0=tf_[:], scalar1=-0.5, scalar2=1597463007.0,
                                                      op0=ALU.mult, op1=ALU.add), reads=[kt], writes=[kt])
                S.op("dve", lambda e: e.tensor_copy(yi_[:], tf_[:]), reads=[kt], writes=[ky])
                y1 = yi_[:].bitcast(F32)
                for _ in range(2):
                    S.op("dve", lambda e: e.tensor_tensor(out=a__[:], in0=y1, in1=y1, op=ALU.mult), reads=[ky], writes=[ka])
                    S.op("dve", lambda e: e.tensor_tensor(out=a__[:], in0=a__[:], in1=xe1[:], op=ALU.mult), reads=[ka, kx], writes=[ka])
                    S.op("dve", lambda e: e.tensor_scalar(out=a__[:], in0=a__[:], scalar1=-0.5, scalar2=1.5,
                                                          op0=ALU.mult, op1=ALU.add), reads=[ka], writes=[ka])
                    S.op("dve", lambda e: e.tensor_tensor(out=y1, in0=y1, in1=a__[:], op=ALU.mult), reads=[ka, ky], writes=[ky])
                for gi in range(G4):
                    i = gi0 + gi
                    xf = x1f[i % 2]
                    kxf = ("x1f", i % 2)
                    S.op("dve", (lambda gi=gi, xf=xf: (lambda e: e.tensor_scalar(
                        out=xf[:], in0=rg[:, gi, :], scalar1=mv1[:, gi, 0:1], scalar2=y1[:, gi:gi + 1],
                        op0=ALU.subtract, op1=ALU.mult)))(),
                        reads=[("rg", gi, 0), ("rg", gi, 1), ("mv1", gi), ky], writes=[kxf])
                    S.op("pool", (lambda xf=xf: (lambda e: e.tensor_tensor(out=xf[:], in0=xf[:], in1=l1g[:], op=ALU.mult)))(),
                         reads=["l1g"], writes=[kxf])
                    S.op("pool", (lambda xf=xf: (lambda e: e.tensor_tensor(out=xf[:], in0=xf[:], in1=l1b[:], op=ALU.add)))(),
                         reads=["l1b"], writes=[kxf])
                    S.op("act", (lambda xf=xf, i=i: (lambda e: e.activation(out=x1b[:, i, :], in_=xf[:], func=AF.Copy)))(),
                         reads=[kxf], writes=[("x1b", i)])
                    xlo = x1lo[i % 2]
                    S.op("dve", (lambda xf=xf, xlo=xlo, i=i: (lambda e: e.tensor_tensor(out=xlo[:], in0=xf[:], in1=x1b[:, i, :], op=ALU.subtract)))(),
                         reads=[kxf, ("x1b", i)], writes=[("x1lo", i % 2)])
                    xtf, xtb = x1Tf[i % 2], x1Tb[i % 2]
                    psh_ = ps[:, 2, :].bitcast(BF16)
                    psl_ = ps[:, 3, :].bitcast(BF16)
                    for kc in range(KC):
                        S.op("pe", (lambda kc=kc, i=i, psh_=psh_: (lambda e: e.transpose(
                            out=psh_[:, kc * 128:(kc + 1) * 128], in_=x1b[:, i, kc * 128:(kc + 1) * 128], identity=identb[:])))(),
                            reads=[("x1b", i), "identb"], writes=[B(2)])
                    S.op("dve", (lambda xtb=xtb, psh_=psh_: (lambda e: e.tensor_copy(xtb[:], psh_.rearrange("p (j t) -> p j t", j=KC))))(),
                         reads=[], writes=[B(2), ("x1Tb", i % 2)])
                    for kc in range(KC):
                        S.op("pe", (lambda kc=kc, xlo=xlo, psl_=psl_: (lambda e: e.transpose(
                            out=psl_[:, kc * 128:(kc + 1) * 128], in_=xlo[:, kc * 128:(kc + 1) * 128], identity=identb[:])))(),
                            reads=[("x1lo", i % 2), "identb"], writes=[B(3)])
                    S.op("act", (lambda xtf=xtf, psl_=psl_: (lambda e: e.activation(out=xtf[:], in_=psl_.rearrange("p (j t) -> p j t", j=KC), func=AF.Copy)))(),
                         reads=[], writes=[B(3), ("x1Tl", i % 2)])
                    passes = [(xtb, wrh), (xtf, wrh), (xtb, wrl)]
                    for pi, (xa, wa_) in enumerate(passes):
                        for kc in range(KC):
                            S.op("pe", (lambda kc=kc, xa=xa, wa_=wa_, pi=pi: (lambda e: e.matmul(
                                ps[:, 4, 0:36], lhsT=xa[:, kc, :], rhs=wa_[:, kc, :], start=(pi == 0 and kc == 0), stop=(pi == 2 and kc == KC - 1))))(),
                                reads=[("x1Tb", i % 2), ("x1Tl", i % 2), "wrh", "wrl"], writes=[B(4)])
                    S.op("dve", (lambda i=i: (lambda e: e.tensor_tensor(out=logit[:, i, :], in0=ps[:, 4, 0:36], in1=brb[:], op=ALU.add)))(),
                         reads=["brb"], writes=[B(4), ("logit", i)])
                    pp = pst[i % 2]
                    S.dma("pool", (lambda i=i, pp=pp: (lambda e: e.dma_start(out=pp[:], in_=p_d[i * 128:(i + 1) * 128, :])))(),
                          "pst%d" % (i % 2), writes=[("pst", i % 2)])
                    psp_ = ps[:, 4, :].bitcast(BF16)
                    for c in range(2):
                        S.op("pe", (lambda c=c, pp=pp, psp_=psp_: (lambda e: e.transpose(
                            out=psp_[:, 512 + c * 128:512 + (c + 1) * 128], in_=pp[:, c * 128:(c + 1) * 128], identity=identb[:])))(),
                            reads=[("pst", i % 2), "identb"], writes=[B(4)])
                    S.op("act", (lambda i=i, psp_=psp_: (lambda e: e.activation(
                        out=pT[i % 2][:], in_=psp_[:, 512:768].rearrange("p (c t) -> p c t", c=2), func=AF.Copy)))(),
                        reads=[], writes=[B(4), ("pT", i % 2)])
                    for half in range(2):
                        bg = 5
                        bl = 6 + half
                        for kc in range(KC):
                            S.op("pe", (lambda kc=kc, half=half, xtb=xtb: (lambda e: e.matmul(
                                ps[:, 5, :], lhsT=xtb[:, kc, :], rhs=wpg[:, kc, half * 512:(half + 1) * 512],
                                start=(kc == 0), stop=(kc == KC - 1))))(),
                                reads=[("x1Tb", i % 2), "wpg"], writes=[B(5)])
                        S.op("act", (lambda half=half, i=i: (lambda e: e.activation(
                            out=tp[i % 2][:, half * 512:(half + 1) * 512], in_=ps[:, 5, :], func=AF.Tanh, scale=0.5)))(),
                            reads=[], writes=[B(5), ("tp", i % 2, half)])
                        for c in range(2):
                            S.op("pe", (lambda c=c, half=half, bl=bl, i=i: (lambda e: e.matmul(
                                ps[:, bl, :], lhsT=pT[i % 2][:, c, :], rhs=wple[:, c, half * 512:(half + 1) * 512],
                                start=(c == 0), stop=(c == 1))))(),
                                reads=[("pT", i % 2), "wple"], writes=[B(bl)])
                        S.op("dve", (lambda half=half, bl=bl, i=i: (lambda e: e.scalar_tensor_tensor(
                            out=ple2[i % 2][:, half * 512:(half + 1) * 512], in0=tp[i % 2][:, half * 512:(half + 1) * 512],
                            scalar=1.0, in1=ps[:, bl, :], op0=ALU.add, op1=ALU.mult)))(),
                            reads=[("tp", i % 2, half)], writes=[B(bl), ("ple2", i % 2, half)])
                    S.op("act", (lambda xf=xf: (lambda e: e.activation(out=xf[:], in_=xf[:], func=AF.Identity, scale=ALPHA)))(),
                         reads=[], writes=[kxf])
                    S.op("pool", (lambda i=i: (lambda e: e.tensor_scalar(
                        out=ple2[i % 2][:], in0=ple2[i % 2][:], scalar1=0.5, scalar2=0.0, op0=ALU.mult, op1=ALU.add)))(),
                        reads=[], writes=[("ple2", i % 2, 0), ("ple2", i % 2, 1)])
                    S.op("pool", (lambda xf=xf, i=i: (lambda e: e.tensor_tensor(
                        out=ple2[i % 2][:], in0=ple2[i % 2][:], in1=xf[:], op=ALU.add)))(),
                        reads=[kxf], writes=[("ple2", i % 2, 0), ("ple2", i % 2, 1)])
                    S.dma("sp", (lambda i=i: (lambda e: e.dma_start(out=base_d[i * 128:(i + 1) * 128, :], in_=ple2[i % 2][:])))(),
                          "base%d" % (i % 2), reads=[("ple2", i % 2, 0), ("ple2", i % 2, 1)], writes=[("base", i)])
            S.barrier()
        if "logit" in debug:
            o = dbg_out("logit", [128, NT, 36])
            S.dma("sp", lambda e, o=o: e.dma_start(out=o, in_=logit[:]), slot(), reads=[("logit", i) for i in range(NT)], final=True)
        if "x1b" in debug:
            o = dbg_out("x1b", [128, NT, D], BF16)
            S.dma("sp", lambda e, o=o: e.dma_start(out=o, in_=x1b[:]), slot(), reads=[("x1b", i) for i in range(NT)], final=True)

        with contextlib.ExitStack() as es5:
            LG = [("logit", i) for i in range(NT)]
            gmax = sbt(es5, "gmax", [128, NT], F32)
            gd = sbt(es5, "gd", [128, NT, 4], F32)
            gm = sbt(es5, "gm", [128, NT, 4], F32)
            gsum = sbt(es5, "gsum", [128, NT], F32)
            gp = sbt(es5, "gp", [128, NT], F32)
            selm = sbt(es5, "selm", [128, NT, 4, 8], F32)
            sel = sbt(es5, "sel", [128, NT, 8], F32)
            m8 = sbt(es5, "m8", [128, NT, 8], F32)
            e1 = sbt(es5, "e1", [128, NT, 8], F32)
            e2 = sbt(es5, "e2", [128, NT, 8], F32)
            dd = sbt(es5, "dd", [128, NT], F32)
            w1 = sbt(es5, "w1", [128, NT], F32)
            A1 = sbt(es5, "A1", [128, NT, 32], F32)
            A2 = sbt(es5, "A2", [128, NT, 32], F32)
            Ab = sbt(es5, "Ab", [128, NT, 32], BF16)
            cnt = sbt(es5, "cnt", [128, 32], F32)
            cnti = sbt(es5, "cnti", [128, 32], I32)
            ntf = sbt(es5, "ntf", [128, 32], F32)
            onesf = sbt(es5, "onesf", [128, 32], F32)
            cum = sbt(es5, "cum", [128, 32], F32)
            bas = sbt(es5, "bas", [128, 32], F32)
            rk = sbt(es5, "rk", [128, NT, 32], F32)
            tmpA = sbt(es5, "tmpA", [128, NT, 32], F32)
            slf = sbt(es5, "slf", [128, NT, 2], F32)
            cmp = sbt(es5, "cmp", [128, NTILE_E, 32], F32)
            tef = sbt(es5, "tef", [128, NTILE_E], F32)
            gl = logit[:, :, 0:4]
            el = logit[:, :, 4:36].rearrange("p n (g i) -> p n g i", g=4)
            V = "dve"
            S.op(V, lambda e: e.tensor_reduce(out=gmax[:], in_=gl, axis=AX.X, op=ALU.max), reads=LG, writes=["gmax"])
            S.op(V, lambda e: e.tensor_tensor(out=gd[:], in0=gl, in1=gmax[:].unsqueeze(2).to_broadcast([128, NT, 4]), op=ALU.subtract),
                 reads=LG + ["gmax"], writes=["gd"])
            S.op(V, lambda e: e.tensor_scalar(out=gm[:], in0=gd[:], scalar1=0.0, scalar2=None, op0=ALU.is_ge), reads=["gd"], writes=["gm"])
            S.op("act", lambda e: e.activation(out=gd[:], in_=gd[:], func=AF.Exp), reads=[], writes=["gd"])
            S.op(V, lambda e: e.tensor_reduce(out=gsum[:], in_=gd[:], axis=AX.X, op=ALU.add), reads=["gd"], writes=["gsum"])
            S.op(V, lambda e: e.reciprocal(out=gp[:], in_=gsum[:]), reads=["gsum"], writes=["gp"])
            S.op(V, lambda e: e.tensor_tensor(out=selm[:], in0=el, in1=gm[:].unsqueeze(3).to_broadcast([128, NT, 4, 8]), op=ALU.mult),
                 reads=LG + ["gm"], writes=["selm"])
            S.op(V, lambda e: e.tensor_reduce(out=sel[:], in_=selm[:].rearrange("p n g i -> p n i g"), axis=AX.X, op=ALU.add),
                 reads=["selm"], writes=["sel"])
            for i in range(NT):
                S.op(V, (lambda i=i: (lambda e: e.max(out=m8[:, i, :], in_=sel[:, i, :])))(), reads=["sel"], writes=[("m8", i)])
            M8 = [("m8", i) for i in range(NT)]
            S.op(V, lambda e: e.tensor_tensor(out=e1[:], in0=sel[:], in1=m8[:, :, 0:1].to_broadcast([128, NT, 8]), op=ALU.is_equal),
                 reads=["sel"] + M8, writes=["e1"])
            S.op(V, lambda e: e.tensor_tensor(out=e2[:], in0=sel[:], in1=m8[:, :, 1:2].to_broadcast([128, NT, 8]), op=ALU.is_equal),
                 reads=["sel"] + M8, writes=["e2"])
            S.op(V, lambda e: e.tensor_tensor(out=dd[:], in0=m8[:, :, 1], in1=m8[:, :, 0], op=ALU.subtract), reads=M8, writes=["dd"])
            S.op("act", lambda e: e.activation(out=dd[:], in_=dd[:], func=AF.Exp), reads=[], writes=["dd"])
            S.op(V, lambda e: e.tensor_scalar(out=w1[:], in0=dd[:], scalar1=1.0, scalar2=None, op0=ALU.add), reads=["dd"], writes=["w1"])
            S.op(V, lambda e: e.reciprocal(out=w1[:], in_=w1[:]), reads=[], writes=["w1"])
            S.op(V, lambda e: e.tensor_tensor(out=cw[:, :, 0], in0=w1[:], in1=gp[:], op=ALU.mult), reads=["w1", "gp"], writes=["cw0"])
            S.op(V, lambda e: e.tensor_tensor(out=dd[:], in0=dd[:], in1=w1[:], op=ALU.mult), reads=["w1"], writes=["dd"])
            S.op(V, lambda e: e.tensor_tensor(out=cw[:, :, 1], in0=dd[:], in1=gp[:], op=ALU.mult), reads=["dd", "gp"], writes=["cw1"])
            for (Ax, ex, nm) in ((A1, e1, "A1"), (A2, e2, "A2")):
                S.op(V, (lambda Ax=Ax, ex=ex: (lambda e: e.tensor_tensor(
                    out=Ax[:].rearrange("p n (g i) -> p n g i", g=4), in0=gm[:].unsqueeze(3).to_broadcast([128, NT, 4, 8]),
                    in1=ex[:].unsqueeze(2).to_broadcast([128, NT, 4, 8]), op=ALU.mult)))(),
                    reads=["gm", "e1", "e2"], writes=[nm])
            S.op(V, lambda e: e.tensor_tensor(out=Ab[:], in0=A1[:], in1=A2[:], op=ALU.add), reads=["A1", "A2"], writes=["Ab"])
            for i in range(NT):
                o_ap = ps[:, 0, i * 32:(i + 1) * 32]
                S.op("pe", (lambda i=i, o_ap=o_ap: (lambda e: e.matmul(o_ap, lhsT=lstr[:], rhs=Ab[:, i, :], start=True, stop=(i == 0))))(),
                     reads=["Ab", "lstr"], writes=[B(0)])
                for i2 in range(i):
                    S.op("pe", (lambda i2=i2, o_ap=o_ap, i=i: (lambda e: e.matmul(o_ap, lhsT=onesb[:], rhs=Ab[:, i2, :], start=False, stop=(i2 == i - 1))))(),
                         reads=["Ab", "onesb"], writes=[B(0)])
            for i in range(NT):
                S.op("pe", (lambda i=i: (lambda e: e.matmul(ps[:, 1, 0:32], lhsT=onesb[:], rhs=Ab[:, i, :], start=(i == 0), stop=(i == NT - 1))))(),
                     reads=["Ab", "onesb"], writes=[B(1)])
            S.op(V, lambda e: e.tensor_copy(rk[:], ps[:, 0, :].rearrange("p (n e) -> p n e", n=NT)), reads=[], writes=[B(0), "rk"])
            S.op(V, lambda e: e.tensor_scalar(out=cnt[:], in0=ps[:, 1, 0:32], scalar1=127.0, scalar2=None, op0=ALU.add), reads=[], writes=[B(1), "cnt"])
            S.op(V, lambda e: e.tensor_copy(cnti[:], cnt[:]), reads=["cnt"], writes=["cnti"])
            S.op(V, lambda e: e.tensor_scalar(out=cnti[:], in0=cnti[:], scalar1=7, scalar2=None, op0=ALU.arith_shift_right), reads=[], writes=["cnti"])
            S.op(V, lambda e: e.tensor_copy(ntf[:], cnti[:]), reads=["cnti"], writes=["ntf"])
            S.op(V, lambda e: e.memset(onesf[:], 1.0), writes=["onesf"])
            S.op(V, lambda e: e.tensor_tensor_scan(out=cum[:], data0=onesf[:], data1=ntf[:], initial=0.0, op0=ALU.mult, op1=ALU.add),
                 reads=["onesf", "ntf"], writes=["cum"])
            S.op(V, lambda e: e.tensor_tensor(out=bas[:], in0=cum[:], in1=ntf[:], op=ALU.subtract), reads=["cum", "ntf"], writes=["bas"])
            S.op(V, lambda e: e.tensor_scalar(out=bas[:], in0=bas[:], scalar1=128.0, scalar2=None, op0=ALU.mult), reads=[], writes=["bas"])
            S.op(V, lambda e: e.tensor_tensor(out=rk[:], in0=rk[:], in1=bas[:].unsqueeze(1).to_broadcast([128, NT, 32]), op=ALU.add),
                 reads=["bas"], writes=["rk"])
            for j, Ax in ((0, A1), (1, A2)):
                S.op(V, (lambda Ax=Ax: (lambda e: e.tensor_tensor(out=tmpA[:], in0=rk[:], in1=Ax[:], op=ALU.mult)))(),
                     reads=["rk", "A1", "A2"], writes=["tmpA"])
                S.op(V, (lambda j=j: (lambda e: e.tensor_reduce(out=slf[:, :, j], in_=tmpA[:], axis=AX.X, op=ALU.add)))(),
                     reads=["tmpA"], writes=[("slf", j)])
            S.op(V, lambda e: e.tensor_copy(slotu[:], slf[:]), reads=[("slf", 0), ("slf", 1)], writes=["slotu"])
            S.op(V, lambda e: e.tensor_tensor(out=cmp[:], in0=cum[:].unsqueeze(1).to_broadcast([128, NTILE_E, 32]),
                                              in1=kidx[:].unsqueeze(2).to_broadcast([128, NTILE_E, 32]), op=ALU.is_le),
                 reads=["cum", "kidx"], writes=["cmp"])
            S.op(V, lambda e: e.tensor_reduce(out=tef[:], in_=cmp[:], axis=AX.X, op=ALU.add), reads=["cmp"], writes=["tef"])
            S.op(V, lambda e: e.tensor_scalar(out=tef[:], in0=tef[:], scalar1=128.0, scalar2=None, op0=ALU.mult), reads=[], writes=["tef"])
            S.op(V, lambda e: e.tensor_copy(rowst[:], tef[:]), reads=["tef"], writes=["rowst"])
            S.op(V, lambda e: e.tensor_scalar(out=tef[:], in0=tef[:], scalar1=pidx[:, 0:1], scalar2=None, op0=ALU.add),
                 reads=["pidx", "rowst"], writes=["tef"])
            S.op(V, lambda e: e.tensor_copy(idxw[:], tef[:]), reads=["tef"], writes=["idxw"])
            if "route" in debug:
                o1 = dbg_out("slotu", [128, NT, 2], U32)
                o2 = dbg_out("cw", [128, NT, 2])
                o3 = dbg_out("idxw", [128, NTILE_E], U32)
                S.dma("sp", lambda e, o1=o1: e.dma_start(out=o1, in_=slotu[:]), slot(), reads=["slotu"], final=True)
                S.dma("sp", lambda e, o2=o2: e.dma_start(out=o2, in_=cw[:]), slot(), reads=["cw0", "cw1"], final=True)
                S.dma("sp", lambda e, o3=o3: e.dma_start(out=o3, in_=idxw[:]), slot(), reads=["idxw"], final=True)
            for i in range(NT if STOP_AFTER != "route" else 0):
                for j in range(2):
                    S.dma("pool", (lambda i=i, j=j: (lambda e: e.indirect_dma_start(
                        out=xs_d[:, :], out_offset=bass.IndirectOffsetOnAxis(ap=slotu[:, i, j:j + 1], axis=0),
                        in_=x1b[:, i, :], in_offset=None, bounds_check=preg(e, NTILE_E * 128 - 1), oob_is_err=False)))(),
                        "scat", reads=["slotu", ("x1b", i)] + [("xsz", q) for q in range(8)], writes=[("xs", i, j)])
            S.barrier()
        p5_es.close()
        mix_es.close()
        XS_ALL = [("xs", i, j) for i in range(NT) for j in range(2)]

        with contextlib.ExitStack() as es6:
            NST = 3
            wbuf = [sbt(es6, "wbuf%d" % i, [128, 6144], BF16) for i in range(3)]
            wst = [sbt(es6, "wst%d" % i, [128, 6144], F32) for i in range(NST)]
            xsb = [sbt(es6, "xsb%d" % i, [128, D], BF16) for i in range(2)]
            xsT = [sbt(es6, "xsT%d" % i, [128, KC, 128], BF16) for i in range(2)]
            tg = [sbt(es6, "tg%d" % i, [128, 256], F32) for i in range(2)]
            hb = [sbt(es6, "hb%d" % i, [128, 256], BF16) for i in range(2)]
            hT = [sbt(es6, "hT%d" % i, [128, 2, 128], BF16) for i in range(2)]
            ysb = [sbt(es6, "ysb%d" % i, [128, D], F32) for i in range(2)]
            for i in range(NST):
                S.op("pool", (lambda i=i: (lambda e: e.memset(wst[i][:], 0.0)))(), writes=[("wst", i)])

            def load_w(k):
                si = k % NST
                S.dma("pool", (lambda si=si, k=k: (lambda e: e.indirect_dma_start(
                    out=wst[si][:], out_offset=None, in_=wexp_d[:, :],
                    in_offset=bass.IndirectOffsetOnAxis(ap=idxw[:, k:k + 1], axis=0), bounds_check=preg(e, 4095), oob_is_err=False)))(),
                    "wst%d" % si, reads=["idxw"], writes=[("wst", si)])

            def cast_w(k):
                si = k % NST
                wi = k % 3
                S.op("act", (lambda si=si, wi=wi: (lambda e: e.activation(out=wbuf[wi][:, 0:2048], in_=wst[si][:, 0:2048], func=AF.Copy)))(),
                     reads=[("wst", si)], writes=[("wbuf", wi, 0)])
                S.op("dve", (lambda si=si, wi=wi: (lambda e: e.tensor_copy(wbuf[wi][:, 2048:4096], wst[si][:, 2048:4096])))(),
                     reads=[("wst", si)], writes=[("wbuf", wi, 1)])
                if k % 2:
                    S.op("act", (lambda si=si, wi=wi: (lambda e: e.activation(out=wbuf[wi][:, 4096:6144], in_=wst[si][:, 4096:6144], func=AF.Copy)))(),
                         reads=[("wst", si)], writes=[("wbuf", wi, 2)])
                else:
                    S.op("dve", (lambda si=si, wi=wi: (lambda e: e.tensor_copy(wbuf[wi][:, 4096:6144], wst[si][:, 4096:6144])))(),
                         reads=[("wst", si)], writes=[("wbuf", wi, 2)])

            NK = NTILE_E if STOP_AFTER != "route" else 0
            for k0 in range(min(NST, NK)):
                load_w(k0)

            def stage_a(k):
                cast_w(k)
                if k + NST < NK:
                    load_w(k + NST)
                wi = k % 3
                par = k % 2
                S.dma("sp", (lambda k=k, par=par: (lambda e: e.dma_start(out=xsb[par][:], in_=xs_d[k * 128:(k + 1) * 128, :])))(),
                      "xsb%d" % par, reads=XS_ALL, writes=[("xsb", par)])
                psb = ps[:, par, :].bitcast(BF16)
                for kc in range(KC):
                    S.op("pe", (lambda kc=kc, par=par, psb=psb: (lambda e: e.transpose(
                        out=psb[:, kc * 128:(kc + 1) * 128], in_=xsb[par][:, kc * 128:(kc + 1) * 128], identity=identb[:])))(),
                        reads=[("xsb", par), "identb"], writes=[B(par)])
                evac_copy(xsT[par][:], psb.rearrange("p (c t) -> p c t", c=KC), reads=[], writes=[B(par), ("xsT", par)])
                bgu = 2 + par
                for kc in range(KC):
                    S.op("pe", (lambda kc=kc, par=par, wi=wi, bgu=bgu: (lambda e: e.matmul(
                        ps[:, bgu, :], lhsT=xsT[par][:, kc, :], rhs=wbuf[wi][:, kc * 512:(kc + 1) * 512],
                        start=(kc == 0), stop=(kc == KC - 1))))(),
                        reads=[("xsT", par), ("wbuf", wi, kc // 4)], writes=[B(bgu)])
                S.op("act", (lambda par=par, bgu=bgu: (lambda e: e.activation(out=tg[par][:], in_=ps[:, bgu, 0:256], func=AF.Tanh, scale=0.5)))(),
                     reads=[], writes=[B(bgu), ("tg", par)])
                S.op("dve", (lambda par=par, bgu=bgu: (lambda e: e.scalar_tensor_tensor(
                    out=tg[par][:], in0=tg[par][:], scalar=1.0, in1=ps[:, bgu, 0:256], op0=ALU.add, op1=ALU.mult)))(),
                    reads=[], writes=[B(bgu), ("tg", par)])
                S.op("dve", (lambda par=par, bgu=bgu: (lambda e: e.scalar_tensor_tensor(
                    out=hb[par][:], in0=tg[par][:], scalar=0.5, in1=ps[:, bgu, 256:512], op0=ALU.mult, op1=ALU.mult)))(),
                    reads=[("tg", par)], writes=[B(bgu), ("hb", par)])

            def stage_b(k):
                wi = k % 3
                par = k % 2
                psh = ps[:, 4 + par, :].bitcast(BF16)
                for c in range(2):
                    S.op("pe", (lambda c=c, par=par, psh=psh: (lambda e: e.transpose(
                        out=psh[:, c * 128:(c + 1) * 128], in_=hb[par][:, c * 128:(c + 1) * 128], identity=identb[:])))(),
                        reads=[("hb", par), "identb"], writes=[B(4 + par)])
                S.op("act", (lambda par=par, psh=psh: (lambda e: e.activation(
                    out=hT[par][:], in_=psh[:, 0:256].rearrange("p (c t) -> p c t", c=2), func=AF.Copy)))(),
                    reads=[], writes=[B(4 + par), ("hT", par)])
                for half in range(2):
                    by = 6 + half
                    for c in range(2):
                        S.op("pe", (lambda c=c, half=half, par=par, wi=wi, by=by: (lambda e: e.matmul(
                            ps[:, by, :], lhsT=hT[par][:, c, :],
                            rhs=wbuf[wi][:, 4096 + c * 1024 + half * 512:4096 + c * 1024 + (half + 1) * 512],
                            start=(c == 0), stop=(c == 1))))(),
                            reads=[("hT", par), ("wbuf", wi, 2)], writes=[B(by)])
                    evac_copy(ysb[par][:, half * 512:(half + 1) * 512], ps[:, by, :], reads=[], writes=[B(by), ("ysb", par, half)])
                S.dma("sp", (lambda k=k, par=par: (lambda e: e.dma_start(out=ys_d[k * 128:(k + 1) * 128, :], in_=ysb[par][:])))(),
                      "ysb%d" % par, reads=[("ysb", par, 0), ("ysb", par, 1)], writes=[("ys", k)])

            if NK:
                stage_a(0)
            for k in range(NK):
                if k + 1 < NK:
                    stage_a(k + 1)
                stage_b(k)
            S.barrier()
        YS_ALL = [("ys", k) for k in range(NTILE_E)]

        G7 = 4
        with contextlib.ExitStack() as es7:
            l2g = sbt(es7, "l2g", [128, D], F32)
            l2b = sbt(es7, "l2b", [128, D], F32)
            g1 = [sbt(es7, "g1_%d" % i, [128, D], F32) for i in range(4)]
            g2 = [sbt(es7, "g2_%d" % i, [128, D], F32) for i in range(4)]
            bt = [sbt(es7, "bt%d" % i, [128, D], F32) for i in range(4)]
            rg2 = sbt(es7, "rg2", [128, G7, D], F32)
            st12b = sbt(es7, "st12b", [128, G7, 12], F32)
            mv2 = sbt(es7, "mv2", [128, G7, 2], F32)
            xe2 = sbt(es7, "xe2", [128, G7], F32)
            tf7_ = sbt(es7, "rs_tfL2", [128, G7], F32)
            yi7_ = sbt(es7, "rs_yiL2", [128, G7], I32)
            a7__ = sbt(es7, "rs_aL2", [128, G7], F32)
            ot = [sbt(es7, "ot%d" % i, [128, D], F32) for i in range(2)]
            S.dma("sp", lambda e: e.dma_start(out=l2g[:], in_=ln2g_d[0].partition_broadcast(128)), slot("c"), writes=["l2g"])
            S.dma("sp", lambda e: e.dma_start(out=l2b[:], in_=ln2b_d[0].partition_broadcast(128)), slot("c"), writes=["l2b"])
            for gi0 in range(0, NT if STOP_AFTER != "route" else 0, G7):
                tag = "L2_%d" % gi0
                for gi in range(G7):
                    i = gi0 + gi
                    par = i % 4
                    S.dma("pool", (lambda i=i, par=par: (lambda e: e.indirect_dma_start(
                        out=g1[par][:], out_offset=None, in_=ys_d[:, :],
                        in_offset=bass.IndirectOffsetOnAxis(ap=slotu[:, i, 0:1], axis=0),
                        bounds_check=preg(e, NTILE_E * 128 - 1), oob_is_err=False)))(),
                        "g1_%d" % par, reads=YS_ALL + ["slotu"], writes=[("g1", par)])
                    S.dma("pool", (lambda i=i, par=par: (lambda e: e.indirect_dma_start(
                        out=g2[par][:], out_offset=None, in_=ys_d[:, :],
                        in_offset=bass.IndirectOffsetOnAxis(ap=slotu[:, i, 1:2], axis=0),
                        bounds_check=preg(e, NTILE_E * 128 - 1), oob_is_err=False)))(),
                        "g2_%d" % par, reads=YS_ALL + ["slotu"], writes=[("g2", par)])
                    S.dma("sp", (lambda i=i, par=par: (lambda e: e.dma_start(out=bt[par][:], in_=base_d[i * 128:(i + 1) * 128, :])))(),
                          "bt%d" % par, reads=[("base", i)], writes=[("bt", par)])
                    S.op("dve", (lambda i=i, par=par: (lambda e: e.scalar_tensor_tensor(
                        out=bt[par][:], in0=g1[par][:], scalar=cw[:, i, 0:1], in1=bt[par][:], op0=ALU.mult, op1=ALU.add)))(),
                        reads=[("g1", par), "cw0"], writes=[("bt", par)])
                    S.op("dve", (lambda i=i, par=par, gi=gi: (lambda e: e.scalar_tensor_tensor(
                        out=rg2[:, gi, :], in0=g2[par][:], scalar=cw[:, i, 1:2], in1=bt[par][:], op0=ALU.mult, op1=ALU.add)))(),
                        reads=[("g2", par), "cw1", ("bt", par)], writes=[("rg2", gi)])
                    for half in range(2):
                        S.op("dve", (lambda gi=gi, half=half: (lambda e: e.bn_stats(
                            out=st12b[:, gi, half * 6:(half + 1) * 6], in_=rg2[:, gi, half * 512:(half + 1) * 512])))(),
                            reads=[("rg2", gi)], writes=[("st12b", gi, half)])
                    S.op("dve", (lambda gi=gi: (lambda e: e.bn_aggr(out=mv2[:, gi, :], in_=st12b[:, gi, :])))(),
                         reads=[("st12b", gi, 0), ("st12b", gi, 1)], writes=[("mv2", gi)])
                S.op("dve", lambda e: e.tensor_scalar(out=xe2[:], in0=mv2[:, :, 1], scalar1=LN_EPS, scalar2=None, op0=ALU.add),
                     reads=[("mv2", gi) for gi in range(G7)], writes=[("rsx", tag)])
                kx, ky, ka, kt = ("rsx", tag), ("rsy", "L2"), ("rsa", "L2"), ("rst", "L2")
                S.op("dve", lambda e: e.tensor_copy(tf7_[:], xe2[:].bitcast(I32)), reads=[kx], writes=[kt])
                S.op("dve", lambda e: e.tensor_scalar(out=tf7_[:], in0=tf7_[:], scalar1=-0.5, scalar2=1597463007.0,
                                                      op0=ALU.mult, op1=ALU.add), reads=[kt], writes=[kt])
                S.op("dve", lambda e: e.tensor_copy(yi7_[:], tf7_[:]), reads=[kt], writes=[ky])
                y2 = yi7_[:].bitcast(F32)
                for _ in range(2):
                    S.op("dve", lambda e: e.tensor_tensor(out=a7__[:], in0=y2, in1=y2, op=ALU.mult), reads=[ky], writes=[ka])
                    S.op("dve", lambda e: e.tensor_tensor(out=a7__[:], in0=a7__[:], in1=xe2[:], op=ALU.mult), reads=[ka, kx], writes=[ka])
                    S.op("dve", lambda e: e.tensor_scalar(out=a7__[:], in0=a7__[:], scalar1=-0.5, scalar2=1.5,
                                                          op0=ALU.mult, op1=ALU.add), reads=[ka], writes=[ka])
                    S.op("dve", lambda e: e.tensor_tensor(out=y2, in0=y2, in1=a7__[:], op=ALU.mult), reads=[ka, ky], writes=[ky])
                for gi in range(G7):
                    i = gi0 + gi
                    par = i % 2
                    S.op("dve", (lambda gi=gi, par=par: (lambda e: e.tensor_scalar(
                        out=ot[par][:], in0=rg2[:, gi, :], scalar1=mv2[:, gi, 0:1], scalar2=y2[:, gi:gi + 1],
                        op0=ALU.subtract, op1=ALU.mult)))(),
                        reads=[("rg2", gi), ("mv2", gi), ky], writes=[("ot", par)])
                    S.op("pool", (lambda par=par: (lambda e: e.tensor_tensor(out=ot[par][:], in0=ot[par][:], in1=l2g[:], op=ALU.mult)))(),
                         reads=["l2g"], writes=[("ot", par)])
                    S.op("pool", (lambda par=par: (lambda e: e.tensor_tensor(out=ot[par][:], in0=ot[par][:], in1=l2b[:], op=ALU.add)))(),
                         reads=["l2b"], writes=[("ot", par)])
                    S.dma("sp", (lambda i=i, par=par: (lambda e: e.dma_start(out=out_d[i * 128:(i + 1) * 128, :], in_=ot[par][:])))(),
                          "ot%d" % par, reads=[("ot", par)], writes=[("out", i)], final=True)
        S.emit()
    return nc, dbg


def _consts():
    k = np.arange(128)[:, None]
    q = np.arange(128)[None, :]
    own = (k <= q).astype(np.float32)
    prev = (k >= q).astype(np.float32)
    bf = ml_dtypes.bfloat16
    return {
        "c_identf": np.eye(128, dtype=np.float32),
        "c_identb": np.eye(128, dtype=np.float32).astype(bf),
        "c_mask4": np.concatenate([prev, own, prev, own], axis=1).astype(bf),
        "c_mown4": np.concatenate([own, own, own, own], axis=1).astype(bf),
        "c_lstrict": (k < q).astype(np.float32).astype(bf),
        "c_onesb": np.ones((128, 128), np.float32).astype(bf),
        "c_kidx": np.broadcast_to(np.arange(NTILE_E, dtype=np.float32)[None, :], (128, NTILE_E)).copy(),
        "c_pidx": np.arange(128, dtype=np.float32).reshape(128, 1),
    }


def _shared_inputs(inp):
    f = lambda a: np.ascontiguousarray(np.asarray(a, dtype=np.float32))
    wg = f(inp["w_gate"])[0].reshape(32, 8, 128, 256)
    wu = f(inp["w_up"])[0].reshape(32, 8, 128, 256)
    gu = np.concatenate([wg, wu], axis=3)
    gu = np.ascontiguousarray(gu.transpose(0, 2, 1, 3))
    wgu0 = np.ascontiguousarray(gu[:, :, 0:4, :]).reshape(4096, 2048)
    wgu1 = np.ascontiguousarray(gu[:, :, 4:8, :]).reshape(4096, 2048)
    wd = f(inp["w_down"])[0].reshape(32, 2, 128, 1024)
    wdn = np.ascontiguousarray(wd.transpose(0, 2, 1, 3)).reshape(4096, 2048)
    sh = {
        "w_in": np.ascontiguousarray(f(inp["w_in"])[0][:, WIN_PERM]),
        "a_ln_g": f(inp["a_ln_g"]).reshape(1, 512),
        "a_ln_b": f(inp["a_ln_b"]).reshape(1, 512),
        "a_wsT": np.ascontiguousarray(f(inp["a_ws"])[0].transpose(2, 0, 1)),
        "a_bs": f(inp["a_bs"])[0].reshape(1, 1024),
        "w_a": f(inp["w_a_proj"])[0],
        "w_b": f(inp["w_b_proj"])[0],
        "w_o": f(inp["w_o"])[0],
        "ln1_g": f(inp["ln1_g"]).reshape(1, D),
        "ln1_b": f(inp["ln1_b"]).reshape(1, D),
        "w_r": np.ascontiguousarray(np.concatenate([f(inp["w_group_router"])[0], f(inp["w_expert_router"])[0].reshape(D, 32)], axis=1)),
        "b_r": np.concatenate([f(inp["b_group_router"])[0], f(inp["b_expert_router"])[0].reshape(32)]).reshape(1, 36),
        "wexp": np.ascontiguousarray(np.concatenate([wgu0.reshape(4096, 2048), wgu1.reshape(4096, 2048), wdn], axis=1)),
        "w_ple": f(inp["w_ple"])[0],
        "w_pg": f(inp["w_ple_gate"])[0],
        "ln2_g": f(inp["ln2_g"]).reshape(1, D),
        "ln2_b": f(inp["ln2_b"]).reshape(1, D),
    }
    sh.update(_consts())
    return sh


_NC_CACHE = {}


def kernel(**inputs):
    x = np.asarray(inputs["x"], dtype=np.float32)
    p = np.asarray(inputs["p"], dtype=np.float32)
    if "nc" not in _NC_CACHE:
        _NC_CACHE["nc"] = build_nc()[0]
    nc = _NC_CACHE["nc"]
    sh = _shared_inputs(inputs)
    n = x.shape[0]
    in_maps = []
    for c in range(n):
        m = dict(sh)
        m["x"] = np.ascontiguousarray(x[c])
        m["p"] = np.ascontiguousarray(p[0, c])
        in_maps.append(m)
    res = run_bass_kernel_spmd(nc, in_maps, core_ids=list(range(n)))
    return np.stack([np.asarray(r["out"], dtype=np.float32) for r in res.results], axis=0)
```

```python
import contextlib
import numpy as np
import ml_dtypes
import concourse.bass as bass
import concourse.mybir as mybir
from concourse.bass_utils import run_bass_kernel_spmd
from concourse.alu_op_type import AluOpType as ALU

F32 = mybir.dt.float32
BF16 = mybir.dt.bfloat16
U32 = mybir.dt.uint32
I32 = mybir.dt.int32
AF = mybir.ActivationFunctionType
AX = mybir.AxisListType

S_TOK = 2048
D = 1024
NT = 16
KC = 8
ALPHA = 2.0 ** 0.25
LN_EPS = 1e-5
GELU_C = 0.7978845608028654
NTILE_E = 63
ENGS = ("pe", "act", "dve", "pool", "sp")
DEBUG = []
HEAD_BARRIER = False
STOP_AFTER = None


class Op:
    __slots__ = ("eng", "fn", "deps", "idx", "is_dma", "sem", "val", "signal")

    def __init__(self, eng, fn, is_dma):
        self.eng = eng
        self.fn = fn
        self.deps = []
        self.is_dma = is_dma
        self.sem = None
        self.val = None
        self.signal = False


class Sched:
    def __init__(self, nc):
        self.nc = nc
        self.ops = []
        self.last_w = {}
        self.readers = {}
        self.dma_slots = {}
        self.final_dma = []
        self.bar_deps = []
        self.bar_need = set()
        self.last_eng = {}
        self.last_slot = {}

    def barrier(self):
        self.bar_deps = list(self.last_eng.values()) + list(self.last_slot.values())
        self.bar_need = set(ENGS)

    def _add(self, op, reads, writes):
        op.idx = len(self.ops)
        deps = set()
        for r in reads:
            w = self.last_w.get(r)
            if w is not None:
                deps.add(w)
        for r in writes:
            w = self.last_w.get(r)
            if w is not None:
                deps.add(w)
            for rd in self.readers.get(r, ()):
                deps.add(rd)
        if op.eng in self.bar_need:
            self.bar_need.discard(op.eng)
            deps.update(self.bar_deps)
        deps.discard(op.idx)
        for d in sorted(deps):
            dop = self.ops[d]
            if dop.eng == op.eng and not dop.is_dma and op.eng in ("pe", "sp"):
                continue
            op.deps.append(d)
            dop.signal = True
        for r in reads:
            self.readers.setdefault(r, []).append(op.idx)
        for r in writes:
            self.last_w[r] = op.idx
            self.readers[r] = []
        self.ops.append(op)
        if not op.is_dma:
            self.last_eng[op.eng] = op.idx
        return op

    def op(self, eng, fn, reads=(), writes=()):
        return self._add(Op(eng, fn, False), tuple(reads), tuple(writes))

    def dma(self, eng, fn, slot, reads=(), writes=(), final=False):
        op = Op(eng, fn, True)
        self.dma_slots.setdefault(slot, []).append(op)
        op.signal = True
        self._add(op, tuple(reads), tuple(writes))
        self.last_slot[slot] = op.idx
        if final:
            self.final_dma.append(op)
        return op

    def emit(self):
        nc = self.nc
        with contextlib.ExitStack() as es:
            esem = {e: es.enter_context(nc.semaphore("s_" + e)) for e in ENGS}
            ssem = {s: es.enter_context(nc.semaphore("d%d" % i)) for i, s in enumerate(self.dma_slots)}
            cnt = {e: 0 for e in ENGS}
            for op in self.ops:
                if op.is_dma:
                    continue
                if op.signal:
                    cnt[op.eng] += 1
                    op.sem = esem[op.eng]
                    op.val = cnt[op.eng]
            for s, ops in self.dma_slots.items():
                c = 0
                for op in ops:
                    c += 16
                    op.sem = ssem[s]
                    op.val = c
            block = es.enter_context(nc.Block())
            per_eng = {e: [o for o in self.ops if o.eng == e] for e in ENGS}

            def run(engname, eng):
                waited = {}
                for op in per_eng[engname]:
                    need = {}
                    for d in op.deps:
                        dop = self.ops[d]
                        k = id(dop.sem)
                        if waited.get(k, 0) >= dop.val:
                            continue
                        if k not in need or need[k][1] < dop.val:
                            need[k] = (dop.sem, dop.val)
                    for k, (sem, val) in need.items():
                        eng.wait_ge(sem, val)
                        waited[k] = val
                    ins = op.fn(eng)
                    if op.is_dma:
                        ins.then_inc(op.sem, 16)
                    elif op.signal:
                        ins.then_inc(op.sem, 1)
                if engname == "sp":
                    fin = {}
                    for op in self.final_dma:
                        k = id(op.sem)
                        if k not in fin or fin[k][1] < op.val:
                            fin[k] = (op.sem, op.val)
                    for sem, val in fin.values():
                        eng.wait_ge(sem, val)

            @block.tensor
            def _(e):
                run("pe", e)

            @block.scalar
            def _(e):
                run("act", e)

            @block.vector
            def _(e):
                run("dve", e)

            @block.gpsimd
            def _(e):
                run("pool", e)

            @block.sync
            def _(e):
                run("sp", e)


def _win_perm():
    perm = []
    blocks = {}

    def add(name, cols):
        blocks[name] = (len(perm), len(cols))
        perm.extend(cols)

    add("u", list(range(0, 512)))
    add("v", list(range(512, 1024)))

    def zb(s, g, h):
        base = 1024 + ((s * 3 + g) * 8 + h) * 64
        return list(range(base, base + 64))

    for ps_ in range(2):
        for g in range(3):
            cols = []
            for h in range(4 * ps_, 4 * ps_ + 4):
                cols += zb(2, g, h)
            add(("vv", ps_, g), cols)
        for hp in range(2 * ps_, 2 * ps_ + 2):
            for s, nm in ((0, "q"), (1, "k")):
                cols = []
                for g in range(3):
                    cols += zb(s, g, 2 * hp) + zb(s, g, 2 * hp + 1)
                add((nm, hp), cols)
    ga = 5632
    gb = 5632 + 1024
    add(("g", 0), list(range(ga, ga + 512)))
    add(("g", 1), list(range(gb, gb + 512)))
    add(("g", 2), list(range(ga + 512, ga + 1024)))
    add(("g", 3), list(range(gb + 512, gb + 1024)))
    assert len(perm) == 7680 and sorted(perm) == list(range(7680))
    return np.array(perm), blocks


WIN_PERM, WIN_BLOCKS = _win_perm()


def build_nc(debug=()):
    nc = bass.Bass("TRN2", target_bir_lowering=False)

    def din(name, shape, dt=F32):
        return nc.dram_tensor(name, list(shape), dt, kind="ExternalInput").ap()

    x_d = din("x", [S_TOK, D])
    p_d = din("p", [S_TOK, 256])
    win_d = din("w_in", [D, 7680])
    alng_d = din("a_ln_g", [1, 512])
    alnb_d = din("a_ln_b", [1, 512])
    awsT_d = din("a_wsT", [128, 8, 128])
    abs_d = din("a_bs", [1, 1024])
    wa_d = din("w_a", [512, D])
    wb_d = din("w_b", [512, D])
    wo_d = din("w_o", [D, D])
    ln1g_d = din("ln1_g", [1, D])
    ln1b_d = din("ln1_b", [1, D])
    wr_d = din("w_r", [D, 36])
    br_d = din("b_r", [1, 36])
    wexp_d = din("wexp", [4096, 6144])
    wple_d = din("w_ple", [256, D])
    wpg_d = din("w_pg", [D, D])
    ln2g_d = din("ln2_g", [1, D])
    ln2b_d = din("ln2_b", [1, D])
    identf_d = din("c_identf", [128, 128])
    identb_d = din("c_identb", [128, 128], BF16)
    mask4_d = din("c_mask4", [128, 512], BF16)
    mown4_d = din("c_mown4", [128, 512], BF16)
    lstr_d = din("c_lstrict", [128, 128], BF16)
    onesb_d = din("c_onesb", [128, 128], BF16)
    kidx_d = din("c_kidx", [128, NTILE_E])
    pidx_d = din("c_pidx", [128, 1])

    out_d = nc.dram_tensor("out", [S_TOK, D], F32, kind="ExternalOutput").ap()
    xs_d = nc.dram_tensor("xs_scr", [NTILE_E * 128, D], BF16, kind="Internal").ap()
    ys_d = nc.dram_tensor("ys_scr", [NTILE_E * 128, D], F32, kind="Internal").ap()
    base_d = nc.dram_tensor("base_scr", [S_TOK, D], F32, kind="Internal").ap()
    dbg = {}

    def dbg_out(name, shape, dt=F32):
        dbg[name] = nc.dram_tensor("dbg_" + name, list(shape), dt, kind="ExternalOutput").ap()
        return dbg[name]

    S = Sched(nc)
    uid = [0]

    def slot(prefix="o"):
        uid[0] += 1
        return "%s%d" % (prefix, uid[0])

    with contextlib.ExitStack() as es0:
        def sbt(es, name, shape, dt):
            return es.enter_context(nc.sbuf_tensor(name, list(shape), dt))

        ps = es0.enter_context(nc.psum_tensor("ps", [128, 8, 512], F32))

        def B(b):
            return ("B", b)

        identf = sbt(es0, "identf", [128, 128], F32)
        identb = sbt(es0, "identb", [128, 128], BF16)
        mask4 = sbt(es0, "mask4", [128, 512], BF16)
        mown4 = sbt(es0, "mown4", [128, 512], BF16)
        lstr = sbt(es0, "lstr", [128, 128], BF16)
        onesb = sbt(es0, "onesb", [128, 128], BF16)
        kidx = sbt(es0, "kidx", [128, NTILE_E], F32)
        pidx = sbt(es0, "pidx", [128, 1], F32)
        for t, d_, nm in ((identf, identf_d, "identf"), (identb, identb_d, "identb"), (mask4, mask4_d, "mask4"),
                          (mown4, mown4_d, "mown4"), (lstr, lstr_d, "lstr"), (onesb, onesb_d, "onesb"),
                          (kidx, kidx_d, "kidx"), (pidx, pidx_d, "pidx")):
            S.dma("sp", (lambda t=t, d_=d_: (lambda e: e.dma_start(out=t[:], in_=d_)))(), slot("c"), writes=[nm])

        slotu = sbt(es0, "slotu", [128, NT, 2], U32)
        cw = sbt(es0, "cw", [128, NT, 2], F32)
        idxw = sbt(es0, "idxw", [128, NTILE_E], U32)
        rowst = sbt(es0, "rowst", [128, NTILE_E], I32)
        zt = sbt(es0, "zt", [128, D], BF16)
        mix_es = contextlib.ExitStack()
        mixinT = sbt(mix_es, "mixinT", [128, KC, S_TOK], BF16)

        S.op("pool", lambda e: e.memset(zt[:], 0.0), writes=["zt"])

        mx_es = contextlib.ExitStack()
        xT = sbt(mx_es, "xT", [128, KC, S_TOK], BF16)
        obT = sbt(mx_es, "obT", [128, 4, S_TOK], BF16)
        wring = []
        win_v = win_d.rearrange("(kc p) c -> p kc c", p=128)
        wr_state = {"n": 0}
        WB = {}

        def load_wblk(name):
            c0, ncol = WIN_BLOCKS[name]
            bi = wr_state["n"] % len(wring)
            wr_state["n"] += 1
            buf_ = wring[bi]
            S.dma("pool", lambda e: e.dma_start(out=buf_[:, :, 0:ncol], in_=win_v[:, :, c0:c0 + ncol]),
                  "wr%s%d" % (buf_.name, bi), writes=[("wr", bi)])
            WB[bi] = buf_
            return bi

        reg_cache = {}

        def preg(e, val):
            if val not in reg_cache:
                reg_cache[val] = e.to_reg(val)
            return reg_cache[val]

        bank_rr = {"n": 0}

        def nb_(pool):
            bank_rr["n"] += 1
            return pool[bank_rr["n"] % len(pool)]

        evac_rr = {"n": 0}

        def evac_copy(out_ap, in_ap, reads, writes):
            evac_rr["n"] += 1
            if evac_rr["n"] % 2:
                S.op("act", lambda e: e.activation(out=out_ap, in_=in_ap, func=AF.Copy), reads=reads, writes=writes)
            else:
                S.op("dve", lambda e: e.tensor_copy(out_ap, in_ap), reads=reads, writes=writes)

        def rsqrt_batch(es, tag, x_ap, n):
            tf = sbt(es, "rs_tf" + tag, [128, n], F32)
            yi = sbt(es, "rs_yi" + tag, [128, n], I32)
            a_ = sbt(es, "rs_a" + tag, [128, n], F32)
            kx, ky, ka, kt = ("rsx", tag), ("rsy", tag), ("rsa", tag), ("rst", tag)
            S.op("dve", lambda e: e.tensor_copy(tf[:], x_ap.bitcast(I32)), reads=[kx], writes=[kt])
            S.op("dve", lambda e: e.tensor_scalar(out=tf[:], in0=tf[:], scalar1=-0.5, scalar2=1597463007.0,
                                                  op0=ALU.mult, op1=ALU.add), reads=[kt], writes=[kt])
            S.op("dve", lambda e: e.tensor_copy(yi[:], tf[:]), reads=[kt], writes=[ky])
            y = yi[:].bitcast(F32)
            for _ in range(2):
                S.op("dve", lambda e: e.tensor_tensor(out=a_[:], in0=y, in1=y, op=ALU.mult), reads=[ky], writes=[ka])
                S.op("dve", lambda e: e.tensor_tensor(out=a_[:], in0=a_[:], in1=x_ap, op=ALU.mult), reads=[ka, kx], writes=[ka])
                S.op("dve", lambda e: e.tensor_scalar(out=a_[:], in0=a_[:], scalar1=-0.5, scalar2=1.5,
                                                      op0=ALU.mult, op1=ALU.add), reads=[ka], writes=[ka])
                S.op("dve", lambda e: e.tensor_tensor(out=y, in0=y, in1=a_[:], op=ALU.mult), reads=[ka, ky], writes=[ky])
            return y, ky, kx


        with contextlib.ExitStack() as es1:
            wring[:] = [sbt(es1, "wringA%d" % i, [128, KC, 512], BF16) for i in range(3)]
            order = []
            for ps_ in range(2):
                order += [("vv", ps_, 0), ("vv", ps_, 1), ("vv", ps_, 2)]
                for hp in range(2 * ps_, 2 * ps_ + 2):
                    order += [("q", hp), ("k", hp)]
            loaded = {}
            nxt = [0]

            def ensure(upto):
                while nxt[0] < len(order) and nxt[0] <= upto:
                    loaded[order[nxt[0]]] = load_wblk(order[nxt[0]])
                    nxt[0] += 1

            ensure(1)
            with contextlib.ExitStack() as esx:
                NXS = 4
                xsf = [sbt(esx, "xsf%d" % i, [128, D], F32) for i in range(NXS)]
                xst = [sbt(esx, "xst%d" % i, [128, D], BF16) for i in range(2)]

                def ldx(i):
                    xf_ = xsf[i % NXS]
                    S.dma("sp" if i % 2 == 0 else "pool",
                          (lambda i=i, xf_=xf_: (lambda e: e.dma_start(out=xf_[:], in_=x_d[i * 128:(i + 1) * 128, :])))(),
                          "xsf%d" % (i % NXS), writes=[("xsf", i % NXS)])

                for i in range(min(NXS, NT)):
                    ldx(i)
                for i in range(NT):
                    xs_ = xst[i % 2]
                    xf_ = xsf[i % NXS]
                    if i % 2 == 0:
                        S.op("act", (lambda xs_=xs_, xf_=xf_: (lambda e: e.activation(out=xs_[:], in_=xf_[:], func=AF.Copy)))(),
                             reads=[("xsf", i % NXS)], writes=[("xst", i % 2)])
                    else:
                        S.op("dve", (lambda xs_=xs_, xf_=xf_: (lambda e: e.tensor_copy(xs_[:], xf_[:])))(),
                             reads=[("xsf", i % NXS)], writes=[("xst", i % 2)])
                    if i + NXS < NT:
                        ldx(i + NXS)
                    bk = nb_([0, 1, 2, 3])
                    psb = ps[:, bk, :].bitcast(BF16)
                    for kc in range(KC):
                        S.op("pe", (lambda psb=psb, kc=kc, xs_=xs_: (lambda e: e.transpose(
                            out=psb[:, kc * 128:(kc + 1) * 128], in_=xs_[:, kc * 128:(kc + 1) * 128], identity=identb[:])))(),
                            reads=[("xst", i % 2), "identb"], writes=[B(bk)])
                    evac_copy(xT[:, :, i * 128:(i + 1) * 128], psb.rearrange("p (j t) -> p j t", j=KC),
                              reads=[], writes=[B(bk), ("xT", i)])
                S.barrier()
            for q in range(NTILE_E):
                S.dma("sp", (lambda q=q: (lambda e: e.dma_start(out=xs_d[q * 128:(q + 1) * 128, :], in_=zt[:])))(),
                      "xsz", reads=["zt"], writes=[("xsz", q)])
            if "xT" in debug:
                o = dbg_out("xT", [128, KC, S_TOK], BF16)
                S.dma("sp", lambda e, o=o: e.dma_start(out=o, in_=xT[:]), slot(), reads=[("xT", i) for i in range(NT)], final=True)

            XT_ALL = [("xT", i) for i in range(NT)]
            Vaug = [sbt(es1, "vaug%d" % g, [128, 16, 4, 128], BF16) for g in range(3)]
            qk = sbt(es1, "qk", [128, 6, S_TOK], BF16)
            PTb = [sbt(es1, "ptb%d" % i, [128, 512], BF16) for i in range(4)]
            PT2 = sbt(es1, "pt2", [128, 16, 128], BF16)
            rd = [sbt(es1, "rd%d" % i, [64, 512], F32) for i in range(2)]
            for g in range(3):
                S.op("pool", (lambda g=g: (lambda e: e.memset(Vaug[g][:, :, :, 64:128], 1.0)))(), writes=[("vones", g)])

            def v_tile_tokens(g, t):
                if g == 0:
                    return slice(t * 128, (t + 1) * 128), [("xT", t)]
                if g == 1:
                    r4, nb = t // 4, t % 4
                    return slice(512 * nb + r4, 512 * (nb + 1), 4), [("xT", 4 * nb + j) for j in range(4)]
                return slice(t, S_TOK, 16), XT_ALL

            PROJ = [6, 7]
            SC = [2, 3, 4, 5]
            NSC = len(SC)
            DEPTH = 2
            pend = []

            def pipe(front, back):
                front()
                pend.append(back)
                while len(pend) > DEPTH:
                    b_ = pend.pop(0)
                    if b_ is not None:
                        b_()

            def flush():
                while pend:
                    b_ = pend.pop(0)
                    if b_ is not None:
                        b_()

            blk_i = [0]
            pt_rr = [0]
            acc_rr = [0]
            st_rr = [0]

            for ps_ in range(2):
                for g in range(3):
                    ensure(blk_i[0] + 2)
                    wb_i = loaded[("vv", ps_, g)]
                    blk_i[0] += 1
                    for t0 in range(0, 16, 2):
                        bk = nb_(PROJ)
                        rk = []
                        for tt in range(2):
                            sl, keys = v_tile_tokens(g, t0 + tt)
                            rk += keys
                            for kc in range(KC):
                                S.op("pe", (lambda bk=bk, tt=tt, kc=kc, sl=sl, wt=WB[wb_i]: (lambda e: e.matmul(
                                    ps[:, bk, tt * 256:(tt + 1) * 256], lhsT=xT[:, kc, sl], rhs=wt[:, kc, 0:256],
                                    start=(kc == 0), stop=(kc == KC - 1))))(),
                                    reads=keys + [("wr", wb_i)], writes=[B(bk)])
                        evac_copy(Vaug[g][:, t0:t0 + 2, :, 0:64],
                                  ps[:, bk, :].rearrange("p (t h d) -> p t h d", t=2, h=4),
                                  reads=[], writes=[B(bk), ("V", g, t0), ("V", g, t0 + 1)])
                for hp in range(2 * ps_, 2 * ps_ + 2):
                    for si, nm in ((0, "q"), (1, "k")):
                        ensure(blk_i[0] + 2)
                        wb_i = loaded[(nm, hp)]
                        blk_i[0] += 1
                        for g in range(3):
                            sl_ = si * 3 + g
                            for sp in range(4):
                                bk = nb_(PROJ)
                                for kc in range(KC):
                                    S.op("pe", (lambda bk=bk, kc=kc, g=g, sp=sp, wt=WB[wb_i]: (lambda e: e.matmul(
                                        ps[:, bk, :], lhsT=wt[:, kc, g * 128:(g + 1) * 128],
                                        rhs=xT[:, kc, sp * 512:(sp + 1) * 512], start=(kc == 0), stop=(kc == KC - 1))))(),
                                        reads=[("xT", 4 * sp + j) for j in range(4)] + [("wr", wb_i)], writes=[B(bk)])
                                if g == 0:
                                    o_ap = qk[:, sl_, sp * 512:(sp + 1) * 512]
                                    i_ap = ps[:, bk, :]
                                elif g == 1:
                                    o_ap = qk[:, sl_, :].rearrange("p (r n i) -> p r n i", r=4, n=4)[:, :, sp, :]
                                    i_ap = ps[:, bk, :].rearrange("p (i r) -> p r i", r=4)
                                else:
                                    o_ap = qk[:, sl_, :].rearrange("p (r a) -> p r a", r=16)[:, :, sp * 32:(sp + 1) * 32]
                                    i_ap = ps[:, bk, :].rearrange("p (a r) -> p r a", r=16)
                                evac_copy(o_ap, i_ap, reads=[], writes=[B(bk), ("qk", sl_, sp)])
                    for h in (2 * hp, 2 * hp + 1):
                        b0 = (h % 2) * 64
                        hl = h % 4
                        QK_ALL = lambda s_: [("qk", s_, sp) for sp in range(4)]
                        for grp in range(4):
                            def front2(grp=grp, b0=b0):
                                st_rr[0] += 1
                                bk = SC[st_rr[0] % NSC]
                                for jj in range(4):
                                    r = grp * 4 + jj
                                    S.op("pe", (lambda bk=bk, jj=jj, r=r, b0=b0: (lambda e: e.matmul(
                                        ps[:, bk, jj * 128:(jj + 1) * 128], lhsT=qk[b0:b0 + 64, 5, r * 128:(r + 1) * 128],
                                        rhs=qk[b0:b0 + 64, 2, r * 128:(r + 1) * 128], start=True, stop=True)))(),
                                        reads=QK_ALL(5) + QK_ALL(2), writes=[B(bk)])
                                pview = PT2[:, grp * 4:(grp + 1) * 4, :]
                                S.op("act", (lambda bk=bk, pview=pview: (lambda e: e.activation(
                                    out=pview, in_=ps[:, bk, :].rearrange("p (a b) -> p a b", a=4), func=AF.Exp, scale=0.125)))(),
                                    reads=[], writes=[B(bk), ("pt2", grp)])
                                S.op("dve" if grp % 2 == 0 else "pool", (lambda pview=pview: (lambda e: e.tensor_tensor(
                                    out=pview, in0=pview, in1=mown4[:].rearrange("p (a b) -> p a b", a=4), op=ALU.mult)))(),
                                    reads=["mown4"], writes=[("pt2", grp)])
                            pipe(front2, None)
                        for s in range(4):
                            acc_rr[0] += 1
                            ab = acc_rr[0] % 2
                            first = [True]
                            blocks_ = []
                            for j in range(4 * s, 4 * s + 4):
                                q_ap = qk[b0:b0 + 64, 0, j * 128:(j + 1) * 128]
                                o_ap = ps[:, ab, (j - 4 * s) * 128:(j - 4 * s + 1) * 128]
                                prev = None
                                if j > 0:
                                    prev = (qk[b0:b0 + 64, 3, (j - 1) * 128:j * 128], Vaug[0][:, j - 1, hl, :],
                                            [("qk", 3, (j - 1) // 4), ("V", 0, j - 1), ("vones", 0)])
                                own = (qk[b0:b0 + 64, 3, j * 128:(j + 1) * 128], Vaug[0][:, j, hl, :],
                                       [("qk", 3, j // 4), ("V", 0, j), ("vones", 0)])
                                blocks_.append((q_ap, [("qk", 0, s)], o_ap, prev, own))
                            for r4 in range(4):
                                q_ap = qk[b0:b0 + 64, 1, r4 * 512 + s * 128:r4 * 512 + (s + 1) * 128]
                                o_ap = ps[:, ab, r4:512:4]
                                prev = None
                                if s > 0:
                                    prev = (qk[b0:b0 + 64, 4, r4 * 512 + (s - 1) * 128:r4 * 512 + s * 128],
                                            Vaug[1][:, r4 * 4 + s - 1, hl, :],
                                            [("qk", 4, s - 1), ("V", 1, r4 * 4 + s - 1), ("vones", 1)])
                                own = (qk[b0:b0 + 64, 4, r4 * 512 + s * 128:r4 * 512 + (s + 1) * 128],
                                       Vaug[1][:, r4 * 4 + s, hl, :],
                                       [("qk", 4, s), ("V", 1, r4 * 4 + s), ("vones", 1)])
                                blocks_.append((q_ap, [("qk", 1, s)], o_ap, prev, own))
                            for bp in range(0, 8, 2):
                                st = {}

                                def front(bp=bp, st=st, blocks_=blocks_):
                                    st_rr[0] += 1
                                    sb_ = SC[st_rr[0] % NSC]
                                    ptb = PTb[st_rr[0] % NSC]
                                    ptk = ("ptb", st_rr[0] % NSC)
                                    used = []
                                    pv = []
                                    for bi_, blk in enumerate(blocks_[bp:bp + 2]):
                                        q_ap, qkeys, o_ap, prev, own = blk
                                        for kind, it in ((0, prev), (1, own)):
                                            if it is None:
                                                continue
                                            sl_i = bi_ * 2 + kind
                                            used.append(sl_i)
                                            k_ap, v_ap, rkeys = it
                                            S.op("pe", (lambda sb_=sb_, sl_i=sl_i, k_ap=k_ap, q_ap=q_ap: (lambda e: e.matmul(
                                                ps[:, sb_, sl_i * 128:(sl_i + 1) * 128], lhsT=k_ap, rhs=q_ap, start=True, stop=True)))(),
                                                reads=qkeys + [rkeys[0]], writes=[B(sb_)])
                                            pv.append((sl_i, v_ap, o_ap, rkeys[1:]))
                                    if used == [0, 1, 2, 3]:
                                        sel = lambda ap: ap
                                    elif used == [1, 2, 3]:
                                        sel = lambda ap: ap[:, 128:512]
                                    else:
                                        assert used == [1, 3], used
                                        sel = lambda ap: ap.rearrange("p (a b) -> p a b", a=4)[:, 1:4:2, :]
                                    S.op("act", (lambda sb_=sb_, ptb=ptb, sel=sel: (lambda e: e.activation(
                                        out=sel(ptb[:]), in_=sel(ps[:, sb_, :]), func=AF.Exp, scale=0.125)))(),
                                        reads=[], writes=[B(sb_), ptk])
                                    S.op("dve" if bp < 4 else "pool", (lambda ptb=ptb, sel=sel: (lambda e: e.tensor_tensor(
                                        out=sel(ptb[:]), in0=sel(ptb[:]), in1=sel(mask4[:]), op=ALU.mult)))(),
                                        reads=["mask4"], writes=[ptk])
                                    st["pv"], st["ptb"], st["ptk"] = pv, ptb, ptk

                                def back(bp=bp, st=st, first=first, ab=ab, s=s, hl=hl, b0=b0, hp=hp, h=h):
                                    ptb, ptk = st["ptb"], st["ptk"]
                                    for sl_i, v_ap, o_ap, rkeys in st["pv"]:
                                        st_flag = first[0]
                                        first[0] = False
                                        S.op("pe", (lambda sl_i=sl_i, v_ap=v_ap, o_ap=o_ap, ptb=ptb, st_flag=st_flag: (lambda e: e.matmul(
                                            o_ap, lhsT=v_ap, rhs=ptb[:, sl_i * 128:(sl_i + 1) * 128], start=st_flag, stop=False)))(),
                                            reads=[ptk] + rkeys, writes=[B(ab)])
                                    if bp != 6:
                                        return
                                    for r in range(16):
                                        S.op("pe", (lambda r=r, s=s, ab=ab, hl=hl: (lambda e: e.matmul(
                                            ps[:, ab, r:512:16], lhsT=Vaug[2][:, r, hl, :], rhs=PT2[:, r, 32 * s:32 * (s + 1)],
                                            start=False, stop=(r == 15))))(),
                                            reads=[("pt2", r // 4), ("V", 2, r), ("vones", 2)], writes=[B(ab)])
                                    rdt = rd[ab]
                                    S.op("dve", (lambda ab=ab, rdt=rdt: (lambda e: e.reciprocal(out=rdt[:], in_=ps[64:128, ab, :])))(),
                                         reads=[], writes=[B(ab), ("rd", ab)])
                                    S.op("dve", (lambda ab=ab, rdt=rdt, b0=b0, hp=hp, s=s: (lambda e: e.tensor_tensor(
                                        out=obT[b0:b0 + 64, hp, s * 512:(s + 1) * 512], in0=ps[0:64, ab, :], in1=rdt[:], op=ALU.mult)))(),
                                        reads=[("rd", ab)], writes=[B(ab), ("obT", hp, s, h % 2)])
                                pipe(front, back)
                        flush()
                        if HEAD_BARRIER:
                            S.barrier()
            S.barrier()
        OBT_ALL = [("obT", hp, s, hh) for hp in range(4) for s in range(4) for hh in range(2)]
        if "obT" in debug:
            o = dbg_out("obT", [128, 4, S_TOK], BF16)
            S.dma("sp", lambda e, o=o: e.dma_start(out=o, in_=obT[:]), slot(), reads=OBT_ALL, final=True)

        XT_ALL = [("xT", i) for i in range(NT)]
        yaT_es = contextlib.ExitStack()
        yaT = sbt(yaT_es, "yaT", [128, 4, S_TOK], BF16)
        wring[:] = [sbt(yaT_es, "wringB%d" % i, [128, KC, 512], BF16) for i in range(4)]
        wr_state["n"] = 0
        with contextlib.ExitStack() as es2:
            vg = sbt(es2, "vg", [128, NT, 512], F32)
            lng = sbt(es2, "lng", [128, 512], F32)
            lnb = sbt(es2, "lnb", [128, 512], F32)
            wsf = sbt(es2, "wsf", [128, 8, 128], F32)
            WmT = sbt(es2, "wmT", [128, 8, 128], BF16)
            bsf = sbt(es2, "bsf", [2, 1024], F32)
            bsh = sbt(es2, "bsh", [2, 1024], BF16)
            bshf = sbt(es2, "bshf", [2, 1024], F32)
            bsl = sbt(es2, "bsl", [2, 1024], BF16)
            sqb = [sbt(es2, "sqb%d" % i, [128, 512], F32) for i in range(3)]
            tnb = [sbt(es2, "tnb%d" % i, [128, 512], F32) for i in range(3)]
            st6 = sbt(es2, "st6", [128, NT, 6], F32)
            mv = sbt(es2, "mv", [128, NT, 2], F32)
            xe = sbt(es2, "xeA", [128, NT], F32)
            lnt = [sbt(es2, "lnt%d" % i, [128, 512], F32) for i in range(2)]
            vln = [sbt(es2, "vln%d" % i, [128, 512], BF16) for i in range(2)]
            bu = load_wblk("u")
            bv = load_wblk("v")
            S.dma("sp", lambda e: e.dma_start(out=lng[:], in_=alng_d[0].partition_broadcast(128)), slot("c"), writes=["lng"])
            S.dma("sp", lambda e: e.dma_start(out=lnb[:], in_=alnb_d[0].partition_broadcast(128)), slot("c"), writes=["lnb"])
            S.dma("sp", lambda e: e.dma_start(out=wsf[:], in_=awsT_d), slot("c"), writes=["wsf"])
            S.dma("sp", lambda e: e.dma_start(out=bsf[0:1, :], in_=abs_d), slot("c"), writes=["bsf0"])
            S.dma("sp", lambda e: e.dma_start(out=bsf[1:2, :], in_=abs_d), slot("c"), writes=["bsf1"])
            S.op("dve", lambda e: e.tensor_tensor(out=WmT[:], in0=wsf[:],
                                                  in1=mown4[:].rearrange("p (a b) -> p a b", a=4)[:, 0:1, :].to_broadcast([128, 8, 128]),
                                                  op=ALU.mult), reads=["wsf", "mown4"], writes=["WmT"])
            S.op("dve", lambda e: e.tensor_copy(bsh[:], bsf[:]), reads=["bsf0", "bsf1"], writes=["bsh"])
            S.op("dve", lambda e: e.tensor_copy(bshf[:], bsh[:]), reads=["bsh"], writes=["bshf"])
            S.op("dve", lambda e: e.tensor_tensor(out=bshf[:], in0=bsf[:], in1=bshf[:], op=ALU.subtract), reads=["bshf"], writes=["bshf"])
            S.op("dve", lambda e: e.tensor_copy(bsl[:], bshf[:]), reads=["bshf"], writes=["bsl"])
            S.dma("sp", lambda e: e.dma_start(out=bsh[1:2, :], in_=bsl[1:2, :]), slot("c"), reads=["bsl", "bsh"], writes=["bsh"])

            grr = [0]

            def gelu_front(bk):
                grr[0] += 1
                idx = grr[0] % 3
                sq = sqb[idx]
                ks = ("sqb", idx)
                S.op("act", lambda e: e.activation(out=sq[:], in_=ps[:, bk, :], func=AF.Square), reads=[], writes=[B(bk), ks])
                S.op("pool", lambda e: e.tensor_scalar(out=sq[:], in0=sq[:], scalar1=0.044715, scalar2=1.0,
                                                       op0=ALU.mult, op1=ALU.add), reads=[], writes=[ks])
                S.op("dve", lambda e: e.tensor_tensor(out=sq[:], in0=sq[:], in1=ps[:, bk, :], op=ALU.mult),
                     reads=[], writes=[B(bk), ks])
                return bk, idx

            def gelu_back(st, out_ap, wkeys, after=None):
                bk, idx = st
                sq, tn = sqb[idx], tnb[idx]
                ks, kt = ("sqb", idx), ("tnb", idx)
                S.op("act", lambda e: e.activation(out=tn[:], in_=sq[:], func=AF.Tanh, scale=GELU_C), reads=[ks], writes=[kt])
                S.op("dve", lambda e: e.scalar_tensor_tensor(out=out_ap, in0=tn[:], scalar=1.0, in1=ps[:, bk, :],
                                                             op0=ALU.add, op1=ALU.mult), reads=[kt], writes=[B(bk)] + wkeys)
                if after is not None:
                    after()

            gpend = []

            def gelu_pipe(bk, out_ap, wkeys, after=None):
                st = gelu_front(bk)
                if gpend:
                    gelu_back(*gpend.pop(0))
                gpend.append((st, out_ap, wkeys, after))

            ALLB = [0, 1, 2, 3, 4, 5, 6, 7]
            for fc in range(4):
                for sp in range(4):
                    bk = nb_(ALLB)
                    for kc in range(KC):
                        S.op("pe", (lambda bk=bk, kc=kc, fc=fc, sp=sp, wt=WB[bu]: (lambda e: e.matmul(
                            ps[:, bk, :], lhsT=wt[:, kc, fc * 128:(fc + 1) * 128], rhs=xT[:, kc, sp * 512:(sp + 1) * 512],
                            start=(kc == 0), stop=(kc == KC - 1))))(),
                            reads=[("xT", 4 * sp + j) for j in range(4)] + [("wr", bu)], writes=[B(bk)])
                    gelu_pipe(bk, yaT[:, fc, sp * 512:(sp + 1) * 512], [("yaT", fc, 4 * sp + j) for j in range(4)])
            for i in range(NT):
                bk = nb_(ALLB)
                for kc in range(KC):
                    S.op("pe", (lambda bk=bk, kc=kc, i=i, wt=WB[bv]: (lambda e: e.matmul(
                        ps[:, bk, :], lhsT=xT[:, kc, i * 128:(i + 1) * 128], rhs=wt[:, kc, 0:512],
                        start=(kc == 0), stop=(kc == KC - 1))))(),
                        reads=[("xT", i), ("wr", bv)], writes=[B(bk)])

                def stats(i=i):
                    S.op("dve", (lambda i=i: (lambda e: e.bn_stats(out=st6[:, i, :], in_=vg[:, i, :])))(), reads=[("vg", i)], writes=[("st6", i)])
                    S.op("dve", (lambda i=i: (lambda e: e.bn_aggr(out=mv[:, i, :], in_=st6[:, i, :])))(), reads=[("st6", i)], writes=[("mvA", i)])
                gelu_pipe(bk, vg[:, i, :], [("vg", i)], stats)
            while gpend:
                gelu_back(*gpend.pop(0))
            S.op("dve", lambda e: e.tensor_scalar(out=xe[:], in0=mv[:, :, 1], scalar1=4.0 * LN_EPS, scalar2=None, op0=ALU.add),
                 reads=[("mvA", i) for i in range(NT)], writes=[("rsx", "A")])
            rstd, krs, _ = rsqrt_batch(es2, "A", xe[:], NT)

            def ln_tile(i):
                lt = lnt[i % 2]
                vl = vln[i % 2]
                S.op("dve", (lambda i=i, lt=lt: (lambda e: e.scalar_tensor_tensor(
                    out=lt[:], in0=vg[:, i, :], scalar=mv[:, i, 0:1], in1=lng[:], op0=ALU.subtract, op1=ALU.mult)))(),
                    reads=[("vg", i), ("mvA", i), "lng"], writes=[("lnt", i % 2)])
                S.op("dve", (lambda i=i, lt=lt, vl=vl: (lambda e: e.scalar_tensor_tensor(
                    out=vl[:], in0=lt[:], scalar=rstd[:, i:i + 1], in1=lnb[:], op0=ALU.mult, op1=ALU.add)))(),
                    reads=["lnb", ("lnt", i % 2), krs], writes=[("vln", i % 2)])

            ln_tile(0)
            for i in range(NT):
                vl = vln[i % 2]
                bk = nb_(ALLB)
                for cc in range(4):
                    for gg in range(2):
                        g = 2 * cc + gg
                        o_ap = ps[gg * 64:(gg + 1) * 64, bk, cc * 128:(cc + 1) * 128]
                        S.op("pe", (lambda o_ap=o_ap, g=g, vl=vl: (lambda e: e.matmul(
                            o_ap, lhsT=vl[:, g * 64:(g + 1) * 64], rhs=WmT[:, g, :], start=True, stop=False)))(),
                            reads=[("vln", i % 2), "WmT"], writes=[B(bk)])
                        S.op("pe", (lambda o_ap=o_ap, g=g: (lambda e: e.matmul(
                            o_ap, lhsT=onesb[0:2, 0:64], rhs=bsh[0:2, g * 128:(g + 1) * 128], start=False, stop=True)))(),
                            reads=["bsh", "onesb"], writes=[B(bk)])
                if i + 1 < NT:
                    ln_tile(i + 1)
                ya_v = yaT[:, :, i * 128:(i + 1) * 128]
                S.op("dve", (lambda bk=bk, ya_v=ya_v: (lambda e: e.scalar_tensor_tensor(
                    out=ya_v, in0=ps[:, bk, :].rearrange("p (c t) -> p c t", c=4), scalar=0.5, in1=ya_v, op0=ALU.mult, op1=ALU.mult)))(),
                    reads=[], writes=[B(bk)] + [("yaT", fc, i) for fc in range(4)])
            S.barrier()
        YAT_ALL = [("yaT", fc, i) for fc in range(4) for i in range(NT)]
        if "yaT" in debug:
            o = dbg_out("yaT", [128, 4, S_TOK], BF16)
            S.dma("sp", lambda e, o=o: e.dma_start(out=o, in_=yaT[:]), slot(), reads=YAT_ALL, final=True)

        with contextlib.ExitStack() as es3:
            wa = sbt(es3, "wa", [128, 4, D], BF16)
            wb = sbt(es3, "wb", [128, 4, D], BF16)
            ta = [sbt(es3, "ta%d" % i, [128, 512], BF16) for i in range(2)]
            tb = [sbt(es3, "tb%d" % i, [128, 512], BF16) for i in range(2)]
            m1 = [sbt(es3, "m1_%d" % i, [128, 512], F32) for i in range(2)]
            m2 = [sbt(es3, "m2_%d" % i, [128, 512], F32) for i in range(2)]
            S.dma("pool", lambda e: e.dma_start(out=wa[:], in_=wa_d.rearrange("(c p) d -> p c d", p=128)), slot("c"), writes=["wa"])
            S.dma("pool", lambda e: e.dma_start(out=wb[:], in_=wb_d.rearrange("(c p) d -> p c d", p=128)), slot("c"), writes=["wb"])
            gblk = {}
            gblk[0] = load_wblk(("g", 0))
            gblk[1] = load_wblk(("g", 1))
            gblk[2] = load_wblk(("g", 2))
            gblk[3] = load_wblk(("g", 3))
            it_ = 0
            for dc in range(KC):
                ga_i = gblk[2 * (dc // 4)]
                gb_i = gblk[2 * (dc // 4) + 1]
                for sp in range(4):
                    it_ += 1
                    par = it_ % 2
                    bA, bB, bC, bD = [4 * par + j for j in range(4)]
                    tok = slice(sp * 512, (sp + 1) * 512)
                    xk = [("xT", 4 * sp + j) for j in range(4)]
                    for cc in range(4):
                        S.op("pe", (lambda cc=cc, bA=bA, dc=dc, tok=tok: (lambda e: e.matmul(
                            ps[:, bA, :], lhsT=wa[:, cc, dc * 128:(dc + 1) * 128], rhs=yaT[:, cc, tok], start=(cc == 0), stop=(cc == 3))))(),
                            reads=["wa"] + [("yaT", cc, 4 * sp + j) for j in range(4)], writes=[B(bA)])
                    for cc in range(4):
                        S.op("pe", (lambda cc=cc, bB=bB, dc=dc, tok=tok: (lambda e: e.matmul(
                            ps[:, bB, :], lhsT=wb[:, cc, dc * 128:(dc + 1) * 128], rhs=obT[:, cc, tok], start=(cc == 0), stop=(cc == 3))))(),
                            reads=["wb"] + [("obT", cc, sp, 0), ("obT", cc, sp, 1)], writes=[B(bB)])
                    for (bX, gi) in ((bC, ga_i), (bD, gb_i)):
                        for kc in range(KC):
                            S.op("pe", (lambda kc=kc, bX=bX, wt=WB[gi], dc=dc, tok=tok: (lambda e: e.matmul(
                                ps[:, bX, :], lhsT=wt[:, kc, (dc % 4) * 128:(dc % 4 + 1) * 128], rhs=xT[:, kc, tok],
                                start=(kc == 0), stop=(kc == KC - 1))))(),
                                reads=xk + [("wr", gi)], writes=[B(bX)])
                    S.op("act", (lambda bC=bC, par=par: (lambda e: e.activation(out=ta[par][:], in_=ps[:, bC, :], func=AF.Tanh, scale=0.5)))(),
                         reads=[], writes=[B(bC), ("ta", par)])
                    S.op("act", (lambda bD=bD, par=par: (lambda e: e.activation(out=tb[par][:], in_=ps[:, bD, :], func=AF.Tanh, scale=0.5)))(),
                         reads=[], writes=[B(bD), ("tb", par)])
                    S.op("dve", (lambda bA=bA, par=par: (lambda e: e.scalar_tensor_tensor(
                        out=m1[par][:], in0=ta[par][:], scalar=1.0, in1=ps[:, bA, :], op0=ALU.add, op1=ALU.mult)))(),
                        reads=[("ta", par)], writes=[B(bA), ("m1", par)])
                    S.op("dve", (lambda bB=bB, par=par: (lambda e: e.scalar_tensor_tensor(
                        out=m2[par][:], in0=tb[par][:], scalar=1.0, in1=ps[:, bB, :], op0=ALU.add, op1=ALU.mult)))(),
                        reads=[("tb", par)], writes=[B(bB), ("m2", par)])
                    S.op("pool", (lambda par=par, dc=dc, tok=tok: (lambda e: e.tensor_tensor(
                        out=mixinT[:, dc, tok], in0=m1[par][:], in1=m2[par][:], op=ALU.add)))(),
                        reads=[("m1", par), ("m2", par)], writes=[("mix", dc, 4 * sp + j) for j in range(4)])
            S.barrier()
        yaT_es.close()
        mx_es.close()
        if "mixinT" in debug:
            o = dbg_out("mixinT", [128, KC, S_TOK], BF16)
            S.dma("sp", lambda e, o=o: e.dma_start(out=o, in_=mixinT[:]), slot(),
                  reads=[("mix", dc, i) for dc in range(KC) for i in range(NT)], final=True)

        p5_es = contextlib.ExitStack()
        x1b = sbt(p5_es, "x1b", [128, NT, D], BF16)
        logit = sbt(p5_es, "logit", [128, NT, 36], F32)
        G4 = 2
        with contextlib.ExitStack() as es4:
            wo = sbt(es4, "wo", [128, KC, D], BF16)
            wpg = sbt(es4, "wpg", [128, KC, D], BF16)
            wple = sbt(es4, "wple", [128, 2, D], BF16)
            wr = sbt(es4, "wr", [128, KC, 36], F32)
            wrh = sbt(es4, "wrh", [128, KC, 36], BF16)
            wrhf = sbt(es4, "wrhf", [128, KC, 36], F32)
            wrl = sbt(es4, "wrl", [128, KC, 36], BF16)
            x1lo = [sbt(es4, "x1lo%d" % i, [128, D], BF16) for i in range(3)]
            brb = sbt(es4, "brb", [128, 36], F32)
            l1g = sbt(es4, "l1g", [128, D], F32)
            l1b = sbt(es4, "l1b", [128, D], F32)
            xres = [sbt(es4, "xres%d" % i, [128, D], F32) for i in range(2)]
            rg = [sbt(es4, "rg%d" % q, [128, G4, D], F32) for q in range(2)]
            st12 = [sbt(es4, "st12_%d" % q, [128, G4, 12], F32) for q in range(2)]
            mv1 = [sbt(es4, "mv1_%d" % q, [128, G4, 2], F32) for q in range(2)]
            xe1 = [sbt(es4, "xe1_%d" % q, [128, G4], F32) for q in range(2)]
            rsq1 = [(sbt(es4, "rs_tfL1_%d" % q, [128, G4], F32), sbt(es4, "rs_yiL1_%d" % q, [128, G4], I32),
                     sbt(es4, "rs_aL1_%d" % q, [128, G4], F32)) for q in range(2)]
            x1f = [sbt(es4, "x1f%d" % i, [128, D], F32) for i in range(3)]
            x1Tf = [sbt(es4, "x1Tl%d" % i, [128, KC, 128], BF16) for i in range(2)]
            x1Tb = [sbt(es4, "x1Tb%d" % i, [128, KC, 128], BF16) for i in range(2)]
            pst = [sbt(es4, "pst%d" % i, [128, 256], BF16) for i in range(2)]
            pT = [sbt(es4, "pT%d" % i, [128, 2, 128], BF16) for i in range(2)]
            tp = [sbt(es4, "tp%d" % i, [128, D], BF16) for i in range(2)]
            ple2 = [sbt(es4, "ple2_%d" % i, [128, D], F32) for i in range(2)]
            S.dma("pool", lambda e: e.dma_start(out=wo[:], in_=wo_d.rearrange("(c p) d -> p c d", p=128)), slot("c"), writes=["wo"])
            S.dma("pool", lambda e: e.dma_start(out=wpg[:], in_=wpg_d.rearrange("(c p) d -> p c d", p=128)), slot("c"), writes=["wpg"])
            S.dma("pool", lambda e: e.dma_start(out=wple[:], in_=wple_d.rearrange("(c p) d -> p c d", p=128)), slot("c"), writes=["wple"])
            S.dma("sp", lambda e: e.dma_start(out=wr[:], in_=wr_d.rearrange("(c p) d -> p c d", p=128)), slot("c"), writes=["wr_"])
            S.dma("sp", lambda e: e.dma_start(out=brb[:], in_=br_d[0].partition_broadcast(128)), slot("c"), writes=["brb"])
            S.op("dve", lambda e: e.tensor_copy(wrh[:], wr[:]), reads=["wr_"], writes=["wrh"])
            S.op("dve", lambda e: e.tensor_copy(wrhf[:], wrh[:]), reads=["wrh"], writes=["wrhf"])
            S.op("dve", lambda e: e.tensor_tensor(out=wrl[:], in0=wr[:], in1=wrhf[:], op=ALU.subtract), reads=["wr_", "wrhf"], writes=["wrl"])
            S.dma("sp", lambda e: e.dma_start(out=l1g[:], in_=ln1g_d[0].partition_broadcast(128)), slot("c"), writes=["l1g"])
            S.dma("sp", lambda e: e.dma_start(out=l1b[:], in_=ln1b_d[0].partition_broadcast(128)), slot("c"), writes=["l1b"])
            NG4 = NT // G4

            def load_xres(i):
                xr = xres[i % 2]
                S.dma("sp", (lambda i=i, xr=xr: (lambda e: e.dma_start(out=xr[:], in_=x_d[i * 128:(i + 1) * 128, :])))(),
                      "xres%d" % (i % 2), writes=[("xres", i % 2)])

            def part1_tile(g, gi):
                gp = g % 2
                i = g * G4 + gi
                xr = xres[i % 2]
                if i == 0:
                    load_xres(0)
                if i + 1 < NT:
                    load_xres(i + 1)
                S.op("act", (lambda xr=xr: (lambda e: e.activation(out=xr[:], in_=xr[:], func=AF.Identity, scale=ALPHA)))(),
                     reads=[], writes=[("xres", i % 2)])
                for half in range(2):
                    bk = half
                    for dc in range(KC):
                        S.op("pe", (lambda bk=bk, dc=dc, i=i, half=half: (lambda e: e.matmul(
                            ps[:, bk, :], lhsT=mixinT[:, dc, i * 128:(i + 1) * 128], rhs=wo[:, dc, half * 512:(half + 1) * 512],
                            start=(dc == 0), stop=(dc == KC - 1))))(),
                            reads=[("mix", dc, i), "wo"], writes=[B(bk)])
                    S.op("dve", (lambda bk=bk, gi=gi, gp=gp, half=half, xr=xr: (lambda e: e.scalar_tensor_tensor(
                        out=rg[gp][:, gi, half * 512:(half + 1) * 512], in0=ps[:, bk, :], scalar=0.5,
                        in1=xr[:, half * 512:(half + 1) * 512], op0=ALU.mult, op1=ALU.add)))(),
                        reads=[("xres", i % 2)], writes=[B(bk), ("rg", gp, gi, half)])
                    S.op("dve", (lambda gi=gi, gp=gp, half=half: (lambda e: e.bn_stats(
                        out=st12[gp][:, gi, half * 6:(half + 1) * 6], in_=rg[gp][:, gi, half * 512:(half + 1) * 512])))(),
                        reads=[("rg", gp, gi, half)], writes=[("st12", gp, gi, half)])
                S.op("dve", (lambda gi=gi, gp=gp: (lambda e: e.bn_aggr(out=mv1[gp][:, gi, :], in_=st12[gp][:, gi, :])))(),
                     reads=[("st12", gp, gi, 0), ("st12", gp, gi, 1)], writes=[("mv1", gp, gi)])

            def rsqrt_grp(g):
                gp = g % 2
                tag = "L1_%d" % g
                xe_ = xe1[gp]
                tf_, yi_, a__ = rsq1[gp]
                kx, ky, ka, kt = ("rsx", "L1", gp), ("rsy", "L1", gp), ("rsa", "L1", gp), ("rst", "L1", gp)
                S.op("dve", lambda e: e.tensor_scalar(out=xe_[:], in0=mv1[gp][:, :, 1], scalar1=LN_EPS, scalar2=None, op0=ALU.add),
                     reads=[("mv1", gp, gi) for gi in range(G4)], writes=[kx])
                S.op("dve", lambda e: e.tensor_copy(tf_[:], xe_[:].bitcast(I32)), reads=[kx], writes=[kt])
                S.op("dve", lambda e: e.tensor_scalar(out=tf_[:], in0=tf_[:], scalar1=-0.5, scalar2=1597463007.0,
                                                      op0=ALU.mult, op1=ALU.add), reads=[kt], writes=[kt])
                S.op("dve", lambda e: e.tensor_copy(yi_[:], tf_[:]), reads=[kt], writes=[ky])
                y1 = yi_[:].bitcast(F32)
                for _ in range(2):
                    S.op("dve", lambda e: e.tensor_tensor(out=a__[:], in0=y1, in1=y1, op=ALU.mult), reads=[ky], writes=[ka])
                    S.op("dve", lambda e: e.tensor_tensor(out=a__[:], in0=a__[:], in1=xe_[:], op=ALU.mult), reads=[ka, kx], writes=[ka])
                    S.op("dve", lambda e: e.tensor_scalar(out=a__[:], in0=a__[:], scalar1=-0.5, scalar2=1.5,
                                                          op0=ALU.mult, op1=ALU.add), reads=[ka], writes=[ka])
                    S.op("dve", lambda e: e.tensor_tensor(out=y1, in0=y1, in1=a__[:], op=ALU.mult), reads=[ka, ky], writes=[ky])

            def ln_norm(g, gi):
                gp = g % 2
                i = g * G4 + gi
                y1 = rsq1[gp][1][:].bitcast(F32)
                ky = ("rsy", "L1", gp)
                xf = x1f[i % 3]
                kxf = ("x1f", i % 3)
                S.op("dve", (lambda gi=gi, gp=gp, xf=xf: (lambda e: e.scalar_tensor_tensor(
                    out=xf[:], in0=rg[gp][:, gi, :], scalar=mv1[gp][:, gi, 0:1], in1=l1g[:], op0=ALU.subtract, op1=ALU.mult)))(),
                    reads=[("rg", gp, gi, 0), ("rg", gp, gi, 1), ("mv1", gp, gi), "l1g"], writes=[kxf])
                S.op("dve", (lambda gi=gi, xf=xf, y1=y1: (lambda e: e.scalar_tensor_tensor(
                    out=xf[:], in0=xf[:], scalar=y1[:, gi:gi + 1], in1=l1b[:], op0=ALU.mult, op1=ALU.add)))(),
                    reads=[ky, "l1b"], writes=[kxf])
                S.op("act", (lambda xf=xf, i=i: (lambda e: e.activation(out=x1b[:, i, :], in_=xf[:], func=AF.Copy)))(),
                     reads=[kxf], writes=[("x1b", i)])

            def ln_lo(g, gi):
                i = g * G4 + gi
                xf = x1f[i % 3]
                xlo = x1lo[i % 3]
                S.op("dve", (lambda xf=xf, xlo=xlo, i=i: (lambda e: e.tensor_tensor(out=xlo[:], in0=xf[:], in1=x1b[:, i, :], op=ALU.subtract)))(),
                     reads=[("x1f", i % 3), ("x1b", i)], writes=[("x1lo", i % 3)])

            def load_p(i):
                pp = pst[i % 2]
                S.dma("pool", (lambda i=i, pp=pp: (lambda e: e.dma_start(out=pp[:], in_=p_d[i * 128:(i + 1) * 128, :])))(),
                      "pst%d" % (i % 2), writes=[("pst", i % 2)])

            def part2_tile(g, gi, mid=None):
                i = g * G4 + gi
                xf = x1f[i % 3]
                kxf = ("x1f", i % 3)
                xlo = x1lo[i % 3]
                if i == 0:
                    load_p(0)
                if i + 1 < NT:
                    load_p(i + 1)
                if True:
                    if True:
                        xtf, xtb = x1Tf[i % 2], x1Tb[i % 2]
                        psh_ = ps[:, 2, :].bitcast(BF16)
                        psl_ = ps[:, 3, :].bitcast(BF16)
                        for kc in range(KC):
                            S.op("pe", (lambda kc=kc, i=i, psh_=psh_: (lambda e: e.transpose(
                                out=psh_[:, kc * 128:(kc + 1) * 128], in_=x1b[:, i, kc * 128:(kc + 1) * 128], identity=identb[:])))(),
                                reads=[("x1b", i), "identb"], writes=[B(2)])
                        S.op("dve", (lambda xtb=xtb, psh_=psh_: (lambda e: e.tensor_copy(xtb[:], psh_.rearrange("p (j t) -> p j t", j=KC))))(),
                             reads=[], writes=[B(2), ("x1Tb", i % 2)])
                        for kc in range(KC):
                            S.op("pe", (lambda kc=kc, xlo=xlo, psl_=psl_: (lambda e: e.transpose(
                                out=psl_[:, kc * 128:(kc + 1) * 128], in_=xlo[:, kc * 128:(kc + 1) * 128], identity=identb[:])))(),
                                reads=[("x1lo", i % 3), "identb"], writes=[B(3)])
                        S.op("act", (lambda xtf=xtf, psl_=psl_: (lambda e: e.activation(out=xtf[:], in_=psl_.rearrange("p (j t) -> p j t", j=KC), func=AF.Copy)))(),
                             reads=[], writes=[B(3), ("x1Tl", i % 2)])
                        passes = [(xtb, wrh), (xtf, wrh), (xtb, wrl)]
                        for pi, (xa, wa_) in enumerate(passes):
                            for kc in range(KC):
                                S.op("pe", (lambda kc=kc, xa=xa, wa_=wa_, pi=pi: (lambda e: e.matmul(
                                    ps[:, 4, 0:36], lhsT=xa[:, kc, :], rhs=wa_[:, kc, :], start=(pi == 0 and kc == 0), stop=(pi == 2 and kc == KC - 1))))(),
                                    reads=[("x1Tb", i % 2), ("x1Tl", i % 2), "wrh", "wrl"], writes=[B(4)])
                        S.op("dve", (lambda i=i: (lambda e: e.tensor_tensor(out=logit[:, i, :], in0=ps[:, 4, 0:36], in1=brb[:], op=ALU.add)))(),
                             reads=["brb"], writes=[B(4), ("logit", i)])
                        pp = pst[i % 2]
                        psp_ = ps[:, 4, :].bitcast(BF16)
                        for c in range(2):
                            S.op("pe", (lambda c=c, pp=pp, psp_=psp_: (lambda e: e.transpose(
                                out=psp_[:, 512 + c * 128:512 + (c + 1) * 128], in_=pp[:, c * 128:(c + 1) * 128], identity=identb[:])))(),
                                reads=[("pst", i % 2), "identb"], writes=[B(4)])
                        S.op("act", (lambda i=i, psp_=psp_: (lambda e: e.activation(
                            out=pT[i % 2][:], in_=psp_[:, 512:768].rearrange("p (c t) -> p c t", c=2), func=AF.Copy)))(),
                            reads=[], writes=[B(4), ("pT", i % 2)])
                        if mid is not None:
                            mid()
                        for half in range(2):
                            bg = 5
                            bl = 6 + half
                            for kc in range(KC):
                                S.op("pe", (lambda kc=kc, half=half, xtb=xtb: (lambda e: e.matmul(
                                    ps[:, 5, :], lhsT=xtb[:, kc, :], rhs=wpg[:, kc, half * 512:(half + 1) * 512],
                                    start=(kc == 0), stop=(kc == KC - 1))))(),
                                    reads=[("x1Tb", i % 2), "wpg"], writes=[B(5)])
                            S.op("act", (lambda half=half, i=i: (lambda e: e.activation(
                                out=tp[i % 2][:, half * 512:(half + 1) * 512], in_=ps[:, 5, :], func=AF.Tanh, scale=0.5)))(),
                                reads=[], writes=[B(5), ("tp", i % 2, half)])
                            for c in range(2):
                                S.op("pe", (lambda c=c, half=half, bl=bl, i=i: (lambda e: e.matmul(
                                    ps[:, bl, :], lhsT=pT[i % 2][:, c, :], rhs=wple[:, c, half * 512:(half + 1) * 512],
                                    start=(c == 0), stop=(c == 1))))(),
                                    reads=[("pT", i % 2), "wple"], writes=[B(bl)])
                            S.op("dve", (lambda half=half, bl=bl, i=i: (lambda e: e.scalar_tensor_tensor(
                                out=ple2[i % 2][:, half * 512:(half + 1) * 512], in0=tp[i % 2][:, half * 512:(half + 1) * 512],
                                scalar=1.0, in1=ps[:, bl, :], op0=ALU.add, op1=ALU.mult)))(),
                                reads=[("tp", i % 2, half)], writes=[B(bl), ("ple2", i % 2, half)])
                        S.op("act", (lambda xf=xf: (lambda e: e.activation(out=xf[:], in_=xf[:], func=AF.Identity, scale=ALPHA)))(),
                             reads=[], writes=[kxf])
                        S.op("pool", (lambda i=i: (lambda e: e.tensor_scalar(
                            out=ple2[i % 2][:], in0=ple2[i % 2][:], scalar1=0.5, scalar2=0.0, op0=ALU.mult, op1=ALU.add)))(),
                            reads=[], writes=[("ple2", i % 2, 0), ("ple2", i % 2, 1)])
                        S.op("pool", (lambda xf=xf, i=i: (lambda e: e.tensor_tensor(
                            out=ple2[i % 2][:], in0=ple2[i % 2][:], in1=xf[:], op=ALU.add)))(),
                            reads=[kxf], writes=[("ple2", i % 2, 0), ("ple2", i % 2, 1)])
                        S.dma("sp", (lambda i=i: (lambda e: e.dma_start(out=base_d[i * 128:(i + 1) * 128, :], in_=ple2[i % 2][:])))(),
                              "base%d" % (i % 2), reads=[("ple2", i % 2, 0), ("ple2", i % 2, 1)], writes=[("base", i)])

            def ln_grp(g):
                rsqrt_grp(g)
                for gi in range(G4):
                    ln_norm(g, gi)
                for gi in range(G4):
                    ln_lo(g, gi)

            for gi in range(G4):
                part1_tile(0, gi)
            ln_grp(0)
            for g in range(NG4):
                for gi in range(G4):
                    mid = None
                    if g + 1 < NG4:
                        part1_tile(g + 1, gi)
                        if gi == G4 - 1:
                            mid = (lambda g=g: ln_grp(g + 1))
                    part2_tile(g, gi, mid)
            S.barrier()
        if "logit" in debug:
            o = dbg_out("logit", [128, NT, 36])
            S.dma("sp", lambda e, o=o: e.dma_start(out=o, in_=logit[:]), slot(), reads=[("logit", i) for i in range(NT)], final=True)
        if "x1b" in debug:
            o = dbg_out("x1b", [128, NT, D], BF16)
            S.dma("sp", lambda e, o=o: e.dma_start(out=o, in_=x1b[:]), slot(), reads=[("x1b", i) for i in range(NT)], final=True)

        with contextlib.ExitStack() as es5:
            LG = [("logit", i) for i in range(NT)]
            gmax = sbt(es5, "gmax", [128, NT], F32)
            gd = sbt(es5, "gd", [128, NT, 4], F32)
            gm = sbt(es5, "gm", [128, NT, 4], F32)
            gsum = sbt(es5, "gsum", [128, NT], F32)
            gp = sbt(es5, "gp", [128, NT], F32)
            selm = sbt(es5, "selm", [128, NT, 4, 8], F32)
            sel = sbt(es5, "sel", [128, NT, 8], F32)
            m8 = sbt(es5, "m8", [128, NT, 8], F32)
            e1 = sbt(es5, "e1", [128, NT, 8], F32)
            e2 = sbt(es5, "e2", [128, NT, 8], F32)
            dd = sbt(es5, "dd", [128, NT], F32)
            w1 = sbt(es5, "w1", [128, NT], F32)
            A1 = sbt(es5, "A1", [128, NT, 32], F32)
            A2 = sbt(es5, "A2", [128, NT, 32], F32)
            Ab = sbt(es5, "Ab", [128, NT, 32], BF16)
            cnt = sbt(es5, "cnt", [128, 32], F32)
            cnti = sbt(es5, "cnti", [128, 32], I32)
            ntf = sbt(es5, "ntf", [128, 32], F32)
            onesf = sbt(es5, "onesf", [128, 32], F32)
            cum = sbt(es5, "cum", [128, 32], F32)
            bas = sbt(es5, "bas", [128, 32], F32)
            rk = sbt(es5, "rk", [128, NT, 32], F32)
            tmpA = sbt(es5, "tmpA", [128, NT, 32], F32)
            slf = sbt(es5, "slf", [128, NT, 2], F32)
            cmp = sbt(es5, "cmp", [128, NTILE_E, 32], F32)
            tef = sbt(es5, "tef", [128, NTILE_E], F32)
            gl = logit[:, :, 0:4]
            el = logit[:, :, 4:36].rearrange("p n (g i) -> p n g i", g=4)
            V = "dve"
            S.op(V, lambda e: e.tensor_reduce(out=gmax[:], in_=gl, axis=AX.X, op=ALU.max), reads=LG, writes=["gmax"])
            S.op(V, lambda e: e.tensor_tensor(out=gd[:], in0=gl, in1=gmax[:].unsqueeze(2).to_broadcast([128, NT, 4]), op=ALU.subtract),
                 reads=LG + ["gmax"], writes=["gd"])
            S.op(V, lambda e: e.tensor_scalar(out=gm[:], in0=gd[:], scalar1=0.0, scalar2=None, op0=ALU.is_ge), reads=["gd"], writes=["gm"])
            S.op("act", lambda e: e.activation(out=gd[:], in_=gd[:], func=AF.Exp), reads=[], writes=["gd"])
            S.op(V, lambda e: e.tensor_reduce(out=gsum[:], in_=gd[:], axis=AX.X, op=ALU.add), reads=["gd"], writes=["gsum"])
            S.op(V, lambda e: e.reciprocal(out=gp[:], in_=gsum[:]), reads=["gsum"], writes=["gp"])
            S.op(V, lambda e: e.tensor_tensor(out=selm[:], in0=el, in1=gm[:].unsqueeze(3).to_broadcast([128, NT, 4, 8]), op=ALU.mult),
                 reads=LG + ["gm"], writes=["selm"])
            S.op(V, lambda e: e.tensor_reduce(out=sel[:], in_=selm[:].rearrange("p n g i -> p n i g"), axis=AX.X, op=ALU.add),
                 reads=["selm"], writes=["sel"])
            for i in range(NT):
                S.op(V, (lambda i=i: (lambda e: e.max(out=m8[:, i, :], in_=sel[:, i, :])))(), reads=["sel"], writes=[("m8", i)])
            M8 = [("m8", i) for i in range(NT)]
            S.op(V, lambda e: e.tensor_tensor(out=e1[:], in0=sel[:], in1=m8[:, :, 0:1].to_broadcast([128, NT, 8]), op=ALU.is_equal),
                 reads=["sel"] + M8, writes=["e1"])
            S.op(V, lambda e: e.tensor_tensor(out=e2[:], in0=sel[:], in1=m8[:, :, 1:2].to_broadcast([128, NT, 8]), op=ALU.is_equal),
                 reads=["sel"] + M8, writes=["e2"])
            S.op(V, lambda e: e.tensor_tensor(out=dd[:], in0=m8[:, :, 1], in1=m8[:, :, 0], op=ALU.subtract), reads=M8, writes=["dd"])
            S.op("act", lambda e: e.activation(out=dd[:], in_=dd[:], func=AF.Exp), reads=[], writes=["dd"])
            S.op(V, lambda e: e.tensor_scalar(out=w1[:], in0=dd[:], scalar1=1.0, scalar2=None, op0=ALU.add), reads=["dd"], writes=["w1"])
            S.op(V, lambda e: e.reciprocal(out=w1[:], in_=w1[:]), reads=[], writes=["w1"])
            S.op(V, lambda e: e.tensor_tensor(out=cw[:, :, 0], in0=w1[:], in1=gp[:], op=ALU.mult), reads=["w1", "gp"], writes=["cw0"])
            S.op(V, lambda e: e.tensor_tensor(out=dd[:], in0=dd[:], in1=w1[:], op=ALU.mult), reads=["w1"], writes=["dd"])
            S.op(V, lambda e: e.tensor_tensor(out=cw[:, :, 1], in0=dd[:], in1=gp[:], op=ALU.mult), reads=["dd", "gp"], writes=["cw1"])
            for (Ax, ex, nm) in ((A1, e1, "A1"), (A2, e2, "A2")):
                S.op(V, (lambda Ax=Ax, ex=ex: (lambda e: e.tensor_tensor(
                    out=Ax[:].rearrange("p n (g i) -> p n g i", g=4), in0=gm[:].unsqueeze(3).to_broadcast([128, NT, 4, 8]),
                    in1=ex[:].unsqueeze(2).to_broadcast([128, NT, 4, 8]), op=ALU.mult)))(),
                    reads=["gm", "e1", "e2"], writes=[nm])
            S.op(V, lambda e: e.tensor_tensor(out=Ab[:], in0=A1[:], in1=A2[:], op=ALU.add), reads=["A1", "A2"], writes=["Ab"])
            for i in range(NT):
                o_ap = ps[:, 0, i * 32:(i + 1) * 32]
                S.op("pe", (lambda i=i, o_ap=o_ap: (lambda e: e.matmul(o_ap, lhsT=lstr[:], rhs=Ab[:, i, :], start=True, stop=(i == 0))))(),
                     reads=["Ab", "lstr"], writes=[B(0)])
                for i2 in range(i):
                    S.op("pe", (lambda i2=i2, o_ap=o_ap, i=i: (lambda e: e.matmul(o_ap, lhsT=onesb[:], rhs=Ab[:, i2, :], start=False, stop=(i2 == i - 1))))(),
                         reads=["Ab", "onesb"], writes=[B(0)])
            for i in range(NT):
                S.op("pe", (lambda i=i: (lambda e: e.matmul(ps[:, 1, 0:32], lhsT=onesb[:], rhs=Ab[:, i, :], start=(i == 0), stop=(i == NT - 1))))(),
                     reads=["Ab", "onesb"], writes=[B(1)])
            S.op(V, lambda e: e.tensor_copy(rk[:], ps[:, 0, :].rearrange("p (n e) -> p n e", n=NT)), reads=[], writes=[B(0), "rk"])
            S.op(V, lambda e: e.tensor_scalar(out=cnt[:], in0=ps[:, 1, 0:32], scalar1=127.0, scalar2=None, op0=ALU.add), reads=[], writes=[B(1), "cnt"])
            S.op(V, lambda e: e.tensor_copy(cnti[:], cnt[:]), reads=["cnt"], writes=["cnti"])
            S.op(V, lambda e: e.tensor_scalar(out=cnti[:], in0=cnti[:], scalar1=7, scalar2=None, op0=ALU.arith_shift_right), reads=[], writes=["cnti"])
            S.op(V, lambda e: e.tensor_copy(ntf[:], cnti[:]), reads=["cnti"], writes=["ntf"])
            S.op(V, lambda e: e.memset(onesf[:], 1.0), writes=["onesf"])
            S.op(V, lambda e: e.tensor_tensor_scan(out=cum[:], data0=onesf[:], data1=ntf[:], initial=0.0, op0=ALU.mult, op1=ALU.add),
                 reads=["onesf", "ntf"], writes=["cum"])
            S.op(V, lambda e: e.tensor_tensor(out=bas[:], in0=cum[:], in1=ntf[:], op=ALU.subtract), reads=["cum", "ntf"], writes=["bas"])
            S.op(V, lambda e: e.tensor_scalar(out=bas[:], in0=bas[:], scalar1=128.0, scalar2=None, op0=ALU.mult), reads=[], writes=["bas"])
            S.op(V, lambda e: e.tensor_tensor(out=rk[:], in0=rk[:], in1=bas[:].unsqueeze(1).to_broadcast([128, NT, 32]), op=ALU.add),
                 reads=["bas"], writes=["rk"])
            for j, Ax in ((0, A1), (1, A2)):
                S.op(V, (lambda Ax=Ax: (lambda e: e.tensor_tensor(out=tmpA[:], in0=rk[:], in1=Ax[:], op=ALU.mult)))(),
                     reads=["rk", "A1", "A2"], writes=["tmpA"])
                S.op(V, (lambda j=j: (lambda e: e.tensor_reduce(out=slf[:, :, j], in_=tmpA[:], axis=AX.X, op=ALU.add)))(),
                     reads=["tmpA"], writes=[("slf", j)])
            S.op(V, lambda e: e.tensor_copy(slotu[:], slf[:]), reads=[("slf", 0), ("slf", 1)], writes=["slotu"])
            S.op(V, lambda e: e.tensor_tensor(out=cmp[:], in0=cum[:].unsqueeze(1).to_broadcast([128, NTILE_E, 32]),
                                              in1=kidx[:].unsqueeze(2).to_broadcast([128, NTILE_E, 32]), op=ALU.is_le),
                 reads=["cum", "kidx"], writes=["cmp"])
            S.op(V, lambda e: e.tensor_reduce(out=tef[:], in_=cmp[:], axis=AX.X, op=ALU.add), reads=["cmp"], writes=["tef"])
            S.op(V, lambda e: e.tensor_scalar(out=tef[:], in0=tef[:], scalar1=128.0, scalar2=None, op0=ALU.mult), reads=[], writes=["tef"])
            S.op(V, lambda e: e.tensor_copy(rowst[:], tef[:]), reads=["tef"], writes=["rowst"])
            S.op(V, lambda e: e.tensor_scalar(out=tef[:], in0=tef[:], scalar1=pidx[:, 0:1], scalar2=None, op0=ALU.add),
                 reads=["pidx", "rowst"], writes=["tef"])
            S.op(V, lambda e: e.tensor_copy(idxw[:], tef[:]), reads=["tef"], writes=["idxw"])
            if "route" in debug:
                o1 = dbg_out("slotu", [128, NT, 2], U32)
                o2 = dbg_out("cw", [128, NT, 2])
                o3 = dbg_out("idxw", [128, NTILE_E], U32)
                S.dma("sp", lambda e, o1=o1: e.dma_start(out=o1, in_=slotu[:]), slot(), reads=["slotu"], final=True)
                S.dma("sp", lambda e, o2=o2: e.dma_start(out=o2, in_=cw[:]), slot(), reads=["cw0", "cw1"], final=True)
                S.dma("sp", lambda e, o3=o3: e.dma_start(out=o3, in_=idxw[:]), slot(), reads=["idxw"], final=True)
            for i in range(NT if STOP_AFTER != "route" else 0):
                for j in range(2):
                    S.dma("pool", (lambda i=i, j=j: (lambda e: e.indirect_dma_start(
                        out=xs_d[:, :], out_offset=bass.IndirectOffsetOnAxis(ap=slotu[:, i, j:j + 1], axis=0),
                        in_=x1b[:, i, :], in_offset=None, bounds_check=preg(e, NTILE_E * 128 - 1), oob_is_err=False)))(),
                        "scat", reads=["slotu", ("x1b", i)] + [("xsz", q) for q in range(NTILE_E)], writes=[("xs", i, j)])
            S.barrier()
        p5_es.close()
        mix_es.close()
        XS_ALL = [("xs", i, j) for i in range(NT) for j in range(2)]

        with contextlib.ExitStack() as es6:
            NST = 4
            NWB = 4
            NXB = 4
            NXT = 3
            wbuf = [sbt(es6, "wbuf%d" % i, [128, 6144], BF16) for i in range(NWB)]
            wst = [sbt(es6, "wst%d" % i, [128, 6144], F32) for i in range(NST)]
            xsb = [sbt(es6, "xsb%d" % i, [128, D], BF16) for i in range(NXB)]
            xsT = [sbt(es6, "xsT%d" % i, [128, KC, 128], BF16) for i in range(NXT)]
            tg = [sbt(es6, "tg%d" % i, [128, 256], F32) for i in range(2)]
            hb = [sbt(es6, "hb%d" % i, [128, 256], BF16) for i in range(2)]
            hT = [sbt(es6, "hT%d" % i, [128, 2, 128], BF16) for i in range(2)]
            ysb = [sbt(es6, "ysb%d" % i, [128, D], F32) for i in range(2)]

            order_t = list(range(4))
            a_, b_ = list(range(4, 32)), list(range(32, NTILE_E))
            while a_ or b_:
                if a_:
                    order_t.append(a_.pop(0))
                if b_:
                    order_t.append(b_.pop(0))
            assert sorted(order_t) == list(range(NTILE_E))

            def T(k):
                return order_t[k]

            def load_w(k):
                si = k % NST
                S.dma("pool", (lambda si=si, k=k: (lambda e: e.indirect_dma_start(
                    out=wst[si][:], out_offset=None, in_=wexp_d[:, :],
                    in_offset=bass.IndirectOffsetOnAxis(ap=idxw[:, T(k):T(k) + 1], axis=0), bounds_check=preg(e, 4095), oob_is_err=False)))(),
                    "wst%d" % si, reads=["idxw"], writes=[("wst", si)])

            def cast_w(k):
                si = k % NST
                wi = k % NWB
                S.op("act", (lambda si=si, wi=wi: (lambda e: e.activation(out=wbuf[wi][:, 0:2048], in_=wst[si][:, 0:2048], func=AF.Copy)))(),
                     reads=[("wst", si)], writes=[("wbuf", wi, 0)])
                S.op("dve", (lambda si=si, wi=wi: (lambda e: e.tensor_copy(wbuf[wi][:, 2048:4096], wst[si][:, 2048:4096])))(),
                     reads=[("wst", si)], writes=[("wbuf", wi, 1)])
                S.op("dve", (lambda si=si, wi=wi: (lambda e: e.tensor_copy(wbuf[wi][:, 4096:6144], wst[si][:, 4096:6144])))(),
                     reads=[("wst", si)], writes=[("wbuf", wi, 2)])

            def load_x(k):
                xi = k % NXB
                S.dma("sp", (lambda k=k, xi=xi: (lambda e: e.dma_start(out=xsb[xi][:], in_=xs_d[T(k) * 128:(T(k) + 1) * 128, :])))(),
                      "xsb%d" % xi, reads=XS_ALL, writes=[("xsb", xi)])

            NK = NTILE_E if STOP_AFTER != "route" else 0

            def stage_t(k):
                par = k % 2
                xi = k % NXB
                ti = k % NXT
                psb = ps[:, par, :].bitcast(BF16)
                for kc in range(KC):
                    S.op("pe", (lambda kc=kc, xi=xi, psb=psb: (lambda e: e.transpose(
                        out=psb[:, kc * 128:(kc + 1) * 128], in_=xsb[xi][:, kc * 128:(kc + 1) * 128], identity=identb[:])))(),
                        reads=[("xsb", xi), "identb"], writes=[B(par)])
                evac_copy(xsT[ti][:], psb.rearrange("p (c t) -> p c t", c=KC), reads=[], writes=[B(par), ("xsT", ti)])

            def stage_g(k):
                wi = k % NWB
                par = k % 2
                ti = k % NXT
                bgu = 2 + par
                for kc in range(KC):
                    S.op("pe", (lambda kc=kc, ti=ti, wi=wi, bgu=bgu: (lambda e: e.matmul(
                        ps[:, bgu, :], lhsT=xsT[ti][:, kc, :], rhs=wbuf[wi][:, kc * 512:(kc + 1) * 512],
                        start=(kc == 0), stop=(kc == KC - 1))))(),
                        reads=[("xsT", ti), ("wbuf", wi, kc // 4)], writes=[B(bgu)])
                S.op("act", (lambda par=par, bgu=bgu: (lambda e: e.activation(out=tg[par][:], in_=ps[:, bgu, 0:256], func=AF.Tanh, scale=0.5)))(),
                     reads=[], writes=[B(bgu), ("tg", par)])
                S.op("dve", (lambda par=par, bgu=bgu: (lambda e: e.scalar_tensor_tensor(
                    out=tg[par][:], in0=tg[par][:], scalar=1.0, in1=ps[:, bgu, 0:256], op0=ALU.add, op1=ALU.mult)))(),
                    reads=[], writes=[B(bgu), ("tg", par)])
                S.op("dve", (lambda par=par, bgu=bgu: (lambda e: e.scalar_tensor_tensor(
                    out=hb[par][:], in0=tg[par][:], scalar=0.5, in1=ps[:, bgu, 256:512], op0=ALU.mult, op1=ALU.mult)))(),
                    reads=[("tg", par)], writes=[B(bgu), ("hb", par)])

            def stage_b1(k):
                par = k % 2
                psh = ps[:, 4 + par, :].bitcast(BF16)
                for c in range(2):
                    S.op("pe", (lambda c=c, par=par, psh=psh: (lambda e: e.transpose(
                        out=psh[:, c * 128:(c + 1) * 128], in_=hb[par][:, c * 128:(c + 1) * 128], identity=identb[:])))(),
                        reads=[("hb", par), "identb"], writes=[B(4 + par)])
                S.op("act", (lambda par=par, psh=psh: (lambda e: e.activation(
                    out=hT[par][:], in_=psh[:, 0:256].rearrange("p (c t) -> p c t", c=2), func=AF.Copy)))(),
                    reads=[], writes=[B(4 + par), ("hT", par)])

            def stage_b2(k):
                wi = k % NWB
                par = k % 2
                for half in range(2):
                    by = 6 + half
                    for c in range(2):
                        S.op("pe", (lambda c=c, half=half, par=par, wi=wi, by=by: (lambda e: e.matmul(
                            ps[:, by, :], lhsT=hT[par][:, c, :],
                            rhs=wbuf[wi][:, 4096 + c * 1024 + half * 512:4096 + c * 1024 + (half + 1) * 512],
                            start=(c == 0), stop=(c == 1))))(),
                            reads=[("hT", par), ("wbuf", wi, 2)], writes=[B(by)])
                    evac_copy(ysb[par][:, half * 512:(half + 1) * 512], ps[:, by, :], reads=[], writes=[B(by), ("ysb", par, half)])
                S.dma("sp", (lambda k=k, par=par: (lambda e: e.dma_start(out=ys_d[T(k) * 128:(T(k) + 1) * 128, :], in_=ysb[par][:])))(),
                      "ysb%d" % par, reads=[("ysb", par, 0), ("ysb", par, 1)], writes=[("ys", T(k))])

            for k0 in range(min(NST, NK)):
                load_w(k0)
            for k0 in range(min(3, NK)):
                load_x(k0)
            for k0 in range(min(3, NK)):
                cast_w(k0)
                if k0 + NST < NK:
                    load_w(k0 + NST)
            for k0 in range(min(2, NK)):
                stage_t(k0)
            if NK:
                stage_g(0)
            for k in range(NK):
                stage_b1(k)
                if k + 3 < NK:
                    load_x(k + 3)
                if k + 2 < NK:
                    stage_t(k + 2)
                if k + 1 < NK:
                    stage_g(k + 1)
                stage_b2(k)
                if k + 3 < NK:
                    cast_w(k + 3)
                    if k + 3 + NST < NK:
                        load_w(k + 3 + NST)
            S.barrier()
        YS_ALL = [("ys", k) for k in range(NTILE_E)]

        G7 = 4
        NG7 = NT // G7
        with contextlib.ExitStack() as es7:
            l2g = sbt(es7, "l2g", [128, D], F32)
            l2b = sbt(es7, "l2b", [128, D], F32)
            g1 = [sbt(es7, "g1_%d" % i, [128, D], F32) for i in range(4)]
            g2 = [sbt(es7, "g2_%d" % i, [128, D], F32) for i in range(4)]
            bt = [sbt(es7, "bt%d" % i, [128, D], F32) for i in range(4)]
            ot = [sbt(es7, "ot%d" % i, [128, D], F32) for i in range(4)]
            rg2 = [sbt(es7, "rg2_%d" % q, [128, G7, D], F32) for q in range(2)]
            st12b = [sbt(es7, "st12b_%d" % q, [128, G7, 12], F32) for q in range(2)]
            mv2 = [sbt(es7, "mv2_%d" % q, [128, G7, 2], F32) for q in range(2)]
            xe2 = [sbt(es7, "xe2_%d" % q, [128, G7], F32) for q in range(2)]
            sx2 = [sbt(es7, "sx2_%d" % q, [128, G7], F32) for q in range(2)]
            sq2 = [sbt(es7, "sq2_%d" % q, [128, G7], F32) for q in range(2)]
            mean2 = [sbt(es7, "mean2_%d" % q, [128, G7], F32) for q in range(2)]
            junk = sbt(es7, "junk7", [128, D], BF16)
            rstd2 = [sbt(es7, "rstd2_%d" % q, [128, G7], F32) for q in range(2)]
            nmr2 = [sbt(es7, "nmr2_%d" % q, [128, G7], F32) for q in range(2)]
            rsq2 = [(sbt(es7, "rs_tfL2_%d" % q, [128, G7], F32), sbt(es7, "rs_yiL2_%d" % q, [128, G7], I32),
                     sbt(es7, "rs_aL2_%d" % q, [128, G7], F32)) for q in range(2)]
            S.dma("sp", lambda e: e.dma_start(out=l2g[:], in_=ln2g_d[0].partition_broadcast(128)), slot("c"), writes=["l2g"])
            S.dma("sp", lambda e: e.dma_start(out=l2b[:], in_=ln2b_d[0].partition_broadcast(128)), slot("c"), writes=["l2b"])

            def p1_dma(g, gi):
                i = g * G7 + gi
                par = i % 4
                S.dma("pool", (lambda i=i, par=par: (lambda e: e.indirect_dma_start(
                    out=g1[par][:], out_offset=None, in_=ys_d[:, :],
                    in_offset=bass.IndirectOffsetOnAxis(ap=slotu[:, i, 0:1], axis=0),
                    bounds_check=preg(e, NTILE_E * 128 - 1), oob_is_err=False)))(),
                    "g1_%d" % par, reads=YS_ALL + ["slotu"], writes=[("g1", par)])
                S.dma("pool", (lambda i=i, par=par: (lambda e: e.indirect_dma_start(
                    out=g2[par][:], out_offset=None, in_=ys_d[:, :],
                    in_offset=bass.IndirectOffsetOnAxis(ap=slotu[:, i, 1:2], axis=0),
                    bounds_check=preg(e, NTILE_E * 128 - 1), oob_is_err=False)))(),
                    "g2_%d" % par, reads=YS_ALL + ["slotu"], writes=[("g2", par)])
                S.dma("sp", (lambda i=i, par=par: (lambda e: e.dma_start(out=bt[par][:], in_=base_d[i * 128:(i + 1) * 128, :])))(),
                      "bt%d" % par, reads=[("base", i)], writes=[("bt", par)])

            def p1_dve(g, gi):
                gp = g % 2
                i = g * G7 + gi
                par = i % 4
                S.op("dve", (lambda i=i, par=par: (lambda e: e.scalar_tensor_tensor(
                    out=bt[par][:], in0=g1[par][:], scalar=cw[:, i, 0:1], in1=bt[par][:], op0=ALU.mult, op1=ALU.add)))(),
                    reads=[("g1", par), "cw0"], writes=[("bt", par)])
                S.op("dve", (lambda i=i, par=par, gi=gi, gp=gp: (lambda e: e.scalar_tensor_tensor(
                    out=rg2[gp][:, gi, :], in0=g2[par][:], scalar=cw[:, i, 1:2], in1=bt[par][:], op0=ALU.mult, op1=ALU.add)))(),
                    reads=[("g2", par), "cw1", ("bt", par)], writes=[("rg2", gp, gi)])
                S.op("act", (lambda gi=gi, gp=gp: (lambda e: e.activation(
                    out=junk[:], in_=rg2[gp][:, gi, :], func=AF.Identity, accum_out=sx2[gp][:, gi:gi + 1])))(),
                    reads=[("rg2", gp, gi)], writes=["junk", ("sx2", gp, gi)])
                S.op("act", (lambda gi=gi, gp=gp: (lambda e: e.activation(
                    out=junk[:], in_=rg2[gp][:, gi, :], func=AF.Square, accum_out=sq2[gp][:, gi:gi + 1])))(),
                    reads=[("rg2", gp, gi)], writes=["junk", ("sq2", gp, gi)])

            def rs_grp(g):
                gp = g % 2
                xe_ = xe2[gp]
                tf_, yi_, a__ = rsq2[gp]
                kx, ky, ka, kt = ("rsx", "L2", gp), ("rsy", "L2", gp), ("rsa", "L2", gp), ("rst", "L2", gp)
                mean_ = mean2[gp]
                S.op("dve", lambda e: e.tensor_scalar(out=mean_[:], in0=sx2[gp][:], scalar1=1.0 / D, scalar2=None, op0=ALU.mult),
                     reads=[("sx2", gp, gi) for gi in range(G7)], writes=[("mean2", gp)])
                S.op("dve", lambda e: e.tensor_tensor(out=xe_[:], in0=mean_[:], in1=mean_[:], op=ALU.mult),
                     reads=[("mean2", gp)], writes=[kx])
                S.op("dve", lambda e: e.scalar_tensor_tensor(out=xe_[:], in0=sq2[gp][:], scalar=1.0 / D, in1=xe_[:],
                                                             op0=ALU.mult, op1=ALU.subtract),
                     reads=[("sq2", gp, gi) for gi in range(G7)], writes=[kx])
                S.op("dve", lambda e: e.tensor_scalar(out=xe_[:], in0=xe_[:], scalar1=LN_EPS, scalar2=None, op0=ALU.add),
                     reads=[], writes=[kx])
                S.op("dve", lambda e: e.tensor_copy(tf_[:], xe_[:].bitcast(I32)), reads=[kx], writes=[kt])
                S.op("dve", lambda e: e.tensor_scalar(out=tf_[:], in0=tf_[:], scalar1=-0.5, scalar2=1597463007.0,
                                                      op0=ALU.mult, op1=ALU.add), reads=[kt], writes=[kt])
                S.op("dve", lambda e: e.tensor_copy(yi_[:], tf_[:]), reads=[kt], writes=[ky])
                y2 = yi_[:].bitcast(F32)
                for it in range(2):
                    S.op("dve", lambda e: e.tensor_tensor(out=a__[:], in0=y2, in1=y2, op=ALU.mult), reads=[ky], writes=[ka])
                    S.op("dve", lambda e: e.tensor_tensor(out=a__[:], in0=a__[:], in1=xe_[:], op=ALU.mult), reads=[ka, kx], writes=[ka])
                    S.op("dve", lambda e: e.tensor_scalar(out=a__[:], in0=a__[:], scalar1=-0.5, scalar2=1.5,
                                                          op0=ALU.mult, op1=ALU.add), reads=[ka], writes=[ka])
                    if it == 0:
                        S.op("dve", lambda e: e.tensor_tensor(out=y2, in0=y2, in1=a__[:], op=ALU.mult), reads=[ka, ky], writes=[ky])
                    else:
                        S.op("dve", lambda e: e.tensor_tensor(out=rstd2[gp][:], in0=y2, in1=a__[:], op=ALU.mult),
                             reads=[ka, ky], writes=[("rstd2", gp)])
                S.op("dve", lambda e: e.scalar_tensor_tensor(out=nmr2[gp][:], in0=mean2[gp][:], scalar=-1.0, in1=rstd2[gp][:],
                                                             op0=ALU.mult, op1=ALU.mult),
                     reads=[("rstd2", gp), ("mean2", gp)], writes=[("nmr2", gp)])

            def p2_norm(g, gi):
                gp = g % 2
                i = g * G7 + gi
                par = i % 4
                S.op("act", (lambda gi=gi, gp=gp, par=par: (lambda e: e.activation(
                    out=ot[par][:], in_=rg2[gp][:, gi, :], func=AF.Identity,
                    scale=rstd2[gp][:, gi:gi + 1], bias=nmr2[gp][:, gi:gi + 1])))(),
                    reads=[("rg2", gp, gi), ("rstd2", gp), ("nmr2", gp)], writes=[("ot", par)])

            def p2_tile(g, gi):
                gp = g % 2
                i = g * G7 + gi
                par = i % 4
                S.op("dve", (lambda par=par: (lambda e: e.tensor_tensor(out=ot[par][:], in0=ot[par][:], in1=l2g[:], op=ALU.mult)))(),
                     reads=["l2g"], writes=[("ot", par)])
                S.op("pool", (lambda par=par: (lambda e: e.tensor_tensor(out=ot[par][:], in0=ot[par][:], in1=l2b[:], op=ALU.add)))(),
                     reads=["l2b"], writes=[("ot", par)])
                S.dma("sp", (lambda i=i, par=par: (lambda e: e.dma_start(out=out_d[i * 128:(i + 1) * 128, :], in_=ot[par][:])))(),
                      "ot%d" % par, reads=[("ot", par)], writes=[("out", i)], final=True)

            if STOP_AFTER != "route":
                for gi in range(G7):
                    p1_dma(0, gi)
                for gi in range(G7):
                    p1_dve(0, gi)
                rs_grp(0)
                for g in range(NG7):
                    if g + 1 < NG7:
                        for gi in range(G7):
                            p1_dma(g + 1, gi)
                    for gi in range(G7):
                        p2_norm(g, gi)
                    for gi in range(G7):
                        p2_tile(g, gi)
                        if g + 1 < NG7:
                            p1_dve(g + 1, gi)
                    if g + 1 < NG7:
                        rs_grp(g + 1)
        S.emit()
    return nc, dbg


def _consts():
    k = np.arange(128)[:, None]
    q = np.arange(128)[None, :]
    own = (k <= q).astype(np.float32)
    prev = (k >= q).astype(np.float32)
    bf = ml_dtypes.bfloat16
    return {
        "c_identf": np.eye(128, dtype=np.float32),
        "c_identb": np.eye(128, dtype=np.float32).astype(bf),
        "c_mask4": np.concatenate([prev, own, prev, own], axis=1).astype(bf),
        "c_mown4": np.concatenate([own, own, own, own], axis=1).astype(bf),
        "c_lstrict": (k < q).astype(np.float32).astype(bf),
        "c_onesb": np.ones((128, 128), np.float32).astype(bf),
        "c_kidx": np.broadcast_to(np.arange(NTILE_E, dtype=np.float32)[None, :], (128, NTILE_E)).copy(),
        "c_pidx": np.arange(128, dtype=np.float32).reshape(128, 1),
    }


def _shared_inputs(inp):
    f = lambda a: np.ascontiguousarray(np.asarray(a, dtype=np.float32))
    wg = f(inp["w_gate"])[0].reshape(32, 8, 128, 256)
    wu = f(inp["w_up"])[0].reshape(32, 8, 128, 256)
    gu = np.concatenate([wg, wu], axis=3)
    gu = np.ascontiguousarray(gu.transpose(0, 2, 1, 3))
    wgu0 = np.ascontiguousarray(gu[:, :, 0:4, :]).reshape(4096, 2048)
    wgu1 = np.ascontiguousarray(gu[:, :, 4:8, :]).reshape(4096, 2048)
    wd = f(inp["w_down"])[0].reshape(32, 2, 128, 1024)
    wdn = np.ascontiguousarray(wd.transpose(0, 2, 1, 3)).reshape(4096, 2048)
    sh = {
        "w_in": np.ascontiguousarray(f(inp["w_in"])[0][:, WIN_PERM]),
        "a_ln_g": f(inp["a_ln_g"]).reshape(1, 512),
        "a_ln_b": f(inp["a_ln_b"]).reshape(1, 512),
        "a_wsT": np.ascontiguousarray(f(inp["a_ws"])[0].transpose(2, 0, 1)),
        "a_bs": f(inp["a_bs"])[0].reshape(1, 1024),
        "w_a": f(inp["w_a_proj"])[0],
        "w_b": f(inp["w_b_proj"])[0],
        "w_o": f(inp["w_o"])[0],
        "ln1_g": f(inp["ln1_g"]).reshape(1, D),
        "ln1_b": f(inp["ln1_b"]).reshape(1, D),
        "w_r": np.ascontiguousarray(np.concatenate([f(inp["w_group_router"])[0], f(inp["w_expert_router"])[0].reshape(D, 32)], axis=1)),
        "b_r": np.concatenate([f(inp["b_group_router"])[0], f(inp["b_expert_router"])[0].reshape(32)]).reshape(1, 36),
        "wexp": np.ascontiguousarray(np.concatenate([wgu0.reshape(4096, 2048), wgu1.reshape(4096, 2048), wdn], axis=1)),
        "w_ple": f(inp["w_ple"])[0],
        "w_pg": f(inp["w_ple_gate"])[0],
        "ln2_g": f(inp["ln2_g"]).reshape(1, D),
        "ln2_b": f(inp["ln2_b"]).reshape(1, D),
    }
    sh.update(_consts())
    return sh


_NC_CACHE = {}


def kernel(**inputs):
    x = np.asarray(inputs["x"], dtype=np.float32)
    p = np.asarray(inputs["p"], dtype=np.float32)
    if "nc" not in _NC_CACHE:
        _NC_CACHE["nc"] = build_nc()[0]
    nc = _NC_CACHE["nc"]
    sh = _shared_inputs(inputs)
    n = x.shape[0]
    in_maps = []
    for c in range(n):
        m = dict(sh)
        m["x"] = np.ascontiguousarray(x[c])
        m["p"] = np.ascontiguousarray(p[0, c])
        in_maps.append(m)
    res = run_bass_kernel_spmd(nc, in_maps, core_ids=list(range(n)))
    return np.stack([np.asarray(r["out"], dtype=np.float32) for r in res.results], axis=0)
```

```python
import contextlib
import numpy as np
import ml_dtypes
import concourse.bass as bass
import concourse.mybir as mybir
from concourse.bass_utils import run_bass_kernel_spmd
from concourse.alu_op_type import AluOpType as ALU

F32 = mybir.dt.float32
BF16 = mybir.dt.bfloat16
U32 = mybir.dt.uint32
I32 = mybir.dt.int32
AF = mybir.ActivationFunctionType
AX = mybir.AxisListType

S_TOK = 2048
D = 1024
NT = 16
KC = 8
ALPHA = 2.0 ** 0.25
LN_EPS = 1e-5
GELU_C = 0.7978845608028654
NTILE_E = 63
ENGS = ("pe", "act", "dve", "pool", "sp")
DEBUG = []
HEAD_BARRIER = False
STOP_AFTER = None


class Op:
    __slots__ = ("eng", "fn", "deps", "idx", "is_dma", "sem", "val", "signal")

    def __init__(self, eng, fn, is_dma):
        self.eng = eng
        self.fn = fn
        self.deps = []
        self.is_dma = is_dma
        self.sem = None
        self.val = None
        self.signal = False


class Sched:
    def __init__(self, nc):
        self.nc = nc
        self.ops = []
        self.last_w = {}
        self.readers = {}
        self.dma_slots = {}
        self.final_dma = []
        self.bar_deps = []
        self.bar_need = set()
        self.last_eng = {}
        self.last_slot = {}

    def barrier(self):
        self.bar_deps = list(self.last_eng.values()) + list(self.last_slot.values())
        self.bar_need = set(ENGS)

    def _add(self, op, reads, writes):
        op.idx = len(self.ops)
        deps = set()
        for r in reads:
            w = self.last_w.get(r)
            if w is not None:
                deps.add(w)
        for r in writes:
            w = self.last_w.get(r)
            if w is not None:
                deps.add(w)
            for rd in self.readers.get(r, ()):
                deps.add(rd)
        if op.eng in self.bar_need:
            self.bar_need.discard(op.eng)
            deps.update(self.bar_deps)
        deps.discard(op.idx)
        for d in sorted(deps):
            dop = self.ops[d]
            if dop.eng == op.eng and not dop.is_dma and op.eng in ("pe", "sp"):
                continue
            op.deps.append(d)
            dop.signal = True
        for r in reads:
            self.readers.setdefault(r, []).append(op.idx)
        for r in writes:
            self.last_w[r] = op.idx
            self.readers[r] = []
        self.ops.append(op)
        if not op.is_dma:
            self.last_eng[op.eng] = op.idx
        return op

    def op(self, eng, fn, reads=(), writes=()):
        return self._add(Op(eng, fn, False), tuple(reads), tuple(writes))

    def dma(self, eng, fn, slot, reads=(), writes=(), final=False):
        op = Op(eng, fn, True)
        self.dma_slots.setdefault(slot, []).append(op)
        op.signal = True
        self._add(op, tuple(reads), tuple(writes))
        self.last_slot[slot] = op.idx
        if final:
            self.final_dma.append(op)
        return op

    def emit(self):
        nc = self.nc
        with contextlib.ExitStack() as es:
            esem = {e: es.enter_context(nc.semaphore("s_" + e)) for e in ENGS}
            ssem = {s: es.enter_context(nc.semaphore("d%d" % i)) for i, s in enumerate(self.dma_slots)}
            cnt = {e: 0 for e in ENGS}
            for op in self.ops:
                if op.is_dma:
                    continue
                if op.signal:
                    cnt[op.eng] += 1
                    op.sem = esem[op.eng]
                    op.val = cnt[op.eng]
            for s, ops in self.dma_slots.items():
                c = 0
                for op in ops:
                    c += 16
                    op.sem = ssem[s]
                    op.val = c
            block = es.enter_context(nc.Block())
            per_eng = {e: [o for o in self.ops if o.eng == e] for e in ENGS}

            def run(engname, eng):
                waited = {}
                for op in per_eng[engname]:
                    need = {}
                    for d in op.deps:
                        dop = self.ops[d]
                        k = id(dop.sem)
                        if waited.get(k, 0) >= dop.val:
                            continue
                        if k not in need or need[k][1] < dop.val:
                            need[k] = (dop.sem, dop.val)
                    for k, (sem, val) in need.items():
                        eng.wait_ge(sem, val)
                        waited[k] = val
                    ins = op.fn(eng)
                    if op.is_dma:
                        ins.then_inc(op.sem, 16)
                    elif op.signal:
                        ins.then_inc(op.sem, 1)
                if engname == "sp":
                    fin = {}
                    for op in self.final_dma:
                        k = id(op.sem)
                        if k not in fin or fin[k][1] < op.val:
                            fin[k] = (op.sem, op.val)
                    for sem, val in fin.values():
                        eng.wait_ge(sem, val)

            @block.tensor
            def _(e):
                run("pe", e)

            @block.scalar
            def _(e):
                run("act", e)

            @block.vector
            def _(e):
                run("dve", e)

            @block.gpsimd
            def _(e):
                run("pool", e)

            @block.sync
            def _(e):
                run("sp", e)


def _win_perm():
    perm = []
    blocks = {}

    def add(name, cols):
        blocks[name] = (len(perm), len(cols))
        perm.extend(cols)

    add("u", list(range(0, 512)))
    add("v", list(range(512, 1024)))

    def zb(s, g, h):
        base = 1024 + ((s * 3 + g) * 8 + h) * 64
        return list(range(base, base + 64))

    for ps_ in range(2):
        for g in range(3):
            cols = []
            for h in range(4 * ps_, 4 * ps_ + 4):
                cols += zb(2, g, h)
            add(("vv", ps_, g), cols)
        for hp in range(2 * ps_, 2 * ps_ + 2):
            for s, nm in ((0, "q"), (1, "k")):
                cols = []
                for g in range(3):
                    cols += zb(s, g, 2 * hp) + zb(s, g, 2 * hp + 1)
                add((nm, hp), cols)
    ga = 5632
    gb = 5632 + 1024
    add(("g", 0), list(range(ga, ga + 512)))
    add(("g", 1), list(range(gb, gb + 512)))
    add(("g", 2), list(range(ga + 512, ga + 1024)))
    add(("g", 3), list(range(gb + 512, gb + 1024)))
    assert len(perm) == 7680 and sorted(perm) == list(range(7680))
    return np.array(perm), blocks


WIN_PERM, WIN_BLOCKS = _win_perm()


def build_nc(debug=()):
    nc = bass.Bass("TRN2", target_bir_lowering=False)

    def din(name, shape, dt=F32):
        return nc.dram_tensor(name, list(shape), dt, kind="ExternalInput").ap()

    x_d = din("x", [S_TOK, D])
    p_d = din("p", [S_TOK, 256])
    win_d = din("w_in", [D, 7680])
    alng_d = din("a_ln_g", [1, 512])
    alnb_d = din("a_ln_b", [1, 512])
    awsT_d = din("a_wsT", [128, 8, 128])
    abs_d = din("a_bs", [1, 1024])
    wa_d = din("w_a", [512, D])
    wb_d = din("w_b", [512, D])
    wo_d = din("w_o", [D, D])
    ln1g_d = din("ln1_g", [1, D])
    ln1b_d = din("ln1_b", [1, D])
    wr_d = din("w_r", [D, 36])
    br_d = din("b_r", [1, 36])
    wexp_d = din("wexp", [4096, 6144])
    wple_d = din("w_ple", [256, D])
    wpg_d = din("w_pg", [D, D])
    ln2g_d = din("ln2_g", [1, D])
    ln2b_d = din("ln2_b", [1, D])
    identf_d = din("c_identf", [128, 128])
    identb_d = din("c_identb", [128, 128], BF16)
    mask4_d = din("c_mask4", [128, 512], BF16)
    mown4_d = din("c_mown4", [128, 512], BF16)
    lstr_d = din("c_lstrict", [128, 128], BF16)
    onesb_d = din("c_onesb", [128, 128], BF16)
    kidx_d = din("c_kidx", [128, NTILE_E])
    pidx_d = din("c_pidx", [128, 1])

    out_d = nc.dram_tensor("out", [S_TOK, D], F32, kind="ExternalOutput").ap()
    xs_d = nc.dram_tensor("xs_scr", [NTILE_E * 128, D], BF16, kind="Internal").ap()
    ys_d = nc.dram_tensor("ys_scr", [NTILE_E * 128, D], F32, kind="Internal").ap()
    base_d = nc.dram_tensor("base_scr", [S_TOK, D], F32, kind="Internal").ap()
    dbg = {}

    def dbg_out(name, shape, dt=F32):
        dbg[name] = nc.dram_tensor("dbg_" + name, list(shape), dt, kind="ExternalOutput").ap()
        return dbg[name]

    S = Sched(nc)
    uid = [0]

    def slot(prefix="o"):
        uid[0] += 1
        return "%s%d" % (prefix, uid[0])

    with contextlib.ExitStack() as es0:
        def sbt(es, name, shape, dt):
            return es.enter_context(nc.sbuf_tensor(name, list(shape), dt))

        ps = es0.enter_context(nc.psum_tensor("ps", [128, 8, 512], F32))

        def B(b):
            return ("B", b)

        identf = sbt(es0, "identf", [128, 128], F32)
        identb = sbt(es0, "identb", [128, 128], BF16)
        mask4 = sbt(es0, "mask4", [128, 512], BF16)
        mown4 = sbt(es0, "mown4", [128, 512], BF16)
        lstr = sbt(es0, "lstr", [128, 128], BF16)
        onesb = sbt(es0, "onesb", [128, 128], BF16)
        kidx = sbt(es0, "kidx", [128, NTILE_E], F32)
        pidx = sbt(es0, "pidx", [128, 1], F32)
        for t, d_, nm in ((identf, identf_d, "identf"), (identb, identb_d, "identb"), (mask4, mask4_d, "mask4"),
                          (mown4, mown4_d, "mown4"), (lstr, lstr_d, "lstr"), (onesb, onesb_d, "onesb"),
                          (kidx, kidx_d, "kidx"), (pidx, pidx_d, "pidx")):
            S.dma("sp", (lambda t=t, d_=d_: (lambda e: e.dma_start(out=t[:], in_=d_)))(), slot("c"), writes=[nm])

        slotu = sbt(es0, "slotu", [128, NT, 2], U32)
        cw = sbt(es0, "cw", [128, NT, 2], F32)
        idxw = sbt(es0, "idxw", [128, NTILE_E], U32)
        rowst = sbt(es0, "rowst", [128, NTILE_E], I32)
        zt = sbt(es0, "zt", [128, D], BF16)
        mix_es = contextlib.ExitStack()
        mixinT = sbt(mix_es, "mixinT", [128, KC, S_TOK], BF16)

        S.op("pool", lambda e: e.memset(zt[:], 0.0), writes=["zt"])

        mx_es = contextlib.ExitStack()
        xT = sbt(mx_es, "xT", [128, KC, S_TOK], BF16)
        obT = sbt(mx_es, "obT", [128, 4, S_TOK], BF16)
        wring = []
        win_v = win_d.rearrange("(kc p) c -> p kc c", p=128)
        wr_state = {"n": 0}
        WB = {}

        def load_wblk(name):
            c0, ncol = WIN_BLOCKS[name]
            bi = wr_state["n"] % len(wring)
            wr_state["n"] += 1
            buf_ = wring[bi]
            S.dma("pool", lambda e: e.dma_start(out=buf_[:, :, 0:ncol], in_=win_v[:, :, c0:c0 + ncol]),
                  "wr%s%d" % (buf_.name, bi), writes=[("wr", bi)])
            WB[bi] = buf_
            return bi

        reg_cache = {}

        def preg(e, val):
            if val not in reg_cache:
                reg_cache[val] = e.to_reg(val)
            return reg_cache[val]

        bank_rr = {"n": 0}

        def nb_(pool):
            bank_rr["n"] += 1
            return pool[bank_rr["n"] % len(pool)]

        evac_rr = {"n": 0}

        def evac_copy(out_ap, in_ap, reads, writes):
            evac_rr["n"] += 1
            if evac_rr["n"] % 2:
                S.op("act", lambda e: e.activation(out=out_ap, in_=in_ap, func=AF.Copy), reads=reads, writes=writes)
            else:
                S.op("dve", lambda e: e.tensor_copy(out_ap, in_ap), reads=reads, writes=writes)

        def rsqrt_batch(es, tag, x_ap, n):
            tf = sbt(es, "rs_tf" + tag, [128, n], F32)
            yi = sbt(es, "rs_yi" + tag, [128, n], I32)
            a_ = sbt(es, "rs_a" + tag, [128, n], F32)
            kx, ky, ka, kt = ("rsx", tag), ("rsy", tag), ("rsa", tag), ("rst", tag)
            S.op("dve", lambda e: e.tensor_copy(tf[:], x_ap.bitcast(I32)), reads=[kx], writes=[kt])
            S.op("dve", lambda e: e.tensor_scalar(out=tf[:], in0=tf[:], scalar1=-0.5, scalar2=1597463007.0,
                                                  op0=ALU.mult, op1=ALU.add), reads=[kt], writes=[kt])
            S.op("dve", lambda e: e.tensor_copy(yi[:], tf[:]), reads=[kt], writes=[ky])
            y = yi[:].bitcast(F32)
            for _ in range(2):
                S.op("dve", lambda e: e.tensor_tensor(out=a_[:], in0=y, in1=y, op=ALU.mult), reads=[ky], writes=[ka])
                S.op("dve", lambda e: e.tensor_tensor(out=a_[:], in0=a_[:], in1=x_ap, op=ALU.mult), reads=[ka, kx], writes=[ka])
                S.op("dve", lambda e: e.tensor_scalar(out=a_[:], in0=a_[:], scalar1=-0.5, scalar2=1.5,
                                                      op0=ALU.mult, op1=ALU.add), reads=[ka], writes=[ka])
                S.op("dve", lambda e: e.tensor_tensor(out=y, in0=y, in1=a_[:], op=ALU.mult), reads=[ka, ky], writes=[ky])
            return y, ky, kx


        with contextlib.ExitStack() as es1:
            wring[:] = [sbt(es1, "wringA%d" % i, [128, KC, 512], BF16) for i in range(3)]
            order = []
            for ps_ in range(2):
                order += [("vv", ps_, 0), ("vv", ps_, 1), ("vv", ps_, 2)]
                for hp in range(2 * ps_, 2 * ps_ + 2):
                    order += [("q", hp), ("k", hp)]
            loaded = {}
            nxt = [0]

            def ensure(upto):
                while nxt[0] < len(order) and nxt[0] <= upto:
                    loaded[order[nxt[0]]] = load_wblk(order[nxt[0]])
                    nxt[0] += 1

            ensure(1)
            with contextlib.ExitStack() as esx:
                NXS = 4
                xsf = [sbt(esx, "xsf%d" % i, [128, D], F32) for i in range(NXS)]
                xst = [sbt(esx, "xst%d" % i, [128, D], BF16) for i in range(2)]

                def ldx(i):
                    xf_ = xsf[i % NXS]
                    S.dma("sp" if i % 2 == 0 else "pool",
                          (lambda i=i, xf_=xf_: (lambda e: e.dma_start(out=xf_[:], in_=x_d[i * 128:(i + 1) * 128, :])))(),
                          "xsf%d" % (i % NXS), writes=[("xsf", i % NXS)])

                for i in range(min(NXS, NT)):
                    ldx(i)
                for i in range(NT):
                    xs_ = xst[i % 2]
                    xf_ = xsf[i % NXS]
                    if i % 2 == 0:
                        S.op("act", (lambda xs_=xs_, xf_=xf_: (lambda e: e.activation(out=xs_[:], in_=xf_[:], func=AF.Copy)))(),
                             reads=[("xsf", i % NXS)], writes=[("xst", i % 2)])
                    else:
                        S.op("dve", (lambda xs_=xs_, xf_=xf_: (lambda e: e.tensor_copy(xs_[:], xf_[:])))(),
                             reads=[("xsf", i % NXS)], writes=[("xst", i % 2)])
                    if i + NXS < NT:
                        ldx(i + NXS)
                    bk = nb_([0, 1, 2, 3])
                    psb = ps[:, bk, :].bitcast(BF16)
                    for kc in range(KC):
                        S.op("pe", (lambda psb=psb, kc=kc, xs_=xs_: (lambda e: e.transpose(
                            out=psb[:, kc * 128:(kc + 1) * 128], in_=xs_[:, kc * 128:(kc + 1) * 128], identity=identb[:])))(),
                            reads=[("xst", i % 2), "identb"], writes=[B(bk)])
                    evac_copy(xT[:, :, i * 128:(i + 1) * 128], psb.rearrange("p (j t) -> p j t", j=KC),
                              reads=[], writes=[B(bk), ("xT", i)])
                S.barrier()
            for q in range(NTILE_E):
                S.dma("sp", (lambda q=q: (lambda e: e.dma_start(out=xs_d[q * 128:(q + 1) * 128, :], in_=zt[:])))(),
                      "xsz", reads=["zt"], writes=[("xsz", q)])
            if "xT" in debug:
                o = dbg_out("xT", [128, KC, S_TOK], BF16)
                S.dma("sp", lambda e, o=o: e.dma_start(out=o, in_=xT[:]), slot(), reads=[("xT", i) for i in range(NT)], final=True)

            XT_ALL = [("xT", i) for i in range(NT)]
            Vaug = [sbt(es1, "vaug%d" % g, [128, 16, 4, 128], BF16) for g in range(3)]
            qk = sbt(es1, "qk", [128, 6, S_TOK], BF16)
            PTb = [sbt(es1, "ptb%d" % i, [128, 512], BF16) for i in range(4)]
            PT2 = sbt(es1, "pt2", [128, 16, 128], BF16)
            rd = [sbt(es1, "rd%d" % i, [64, 512], F32) for i in range(2)]
            for g in range(3):
                S.op("pool", (lambda g=g: (lambda e: e.memset(Vaug[g][:, :, :, 64:128], 1.0)))(), writes=[("vones", g)])

            def v_tile_tokens(g, t):
                if g == 0:
                    return slice(t * 128, (t + 1) * 128), [("xT", t)]
                if g == 1:
                    r4, nb = t // 4, t % 4
                    return slice(512 * nb + r4, 512 * (nb + 1), 4), [("xT", 4 * nb + j) for j in range(4)]
                return slice(t, S_TOK, 16), XT_ALL

            PROJ = [6, 7]
            SC = [2, 3, 4, 5]
            NSC = len(SC)
            DEPTH = 2
            pend = []

            def pipe(front, back):
                front()
                pend.append(back)
                while len(pend) > DEPTH:
                    b_ = pend.pop(0)
                    if b_ is not None:
                        b_()

            def flush():
                while pend:
                    b_ = pend.pop(0)
                    if b_ is not None:
                        b_()

            blk_i = [0]
            pt_rr = [0]
            acc_rr = [0]
            st_rr = [0]

            for ps_ in range(2):
                for g in range(3):
                    ensure(blk_i[0] + 2)
                    wb_i = loaded[("vv", ps_, g)]
                    blk_i[0] += 1
                    for t0 in range(0, 16, 2):
                        bk = nb_(PROJ)
                        rk = []
                        for tt in range(2):
                            sl, keys = v_tile_tokens(g, t0 + tt)
                            rk += keys
                            for kc in range(KC):
                                S.op("pe", (lambda bk=bk, tt=tt, kc=kc, sl=sl, wt=WB[wb_i]: (lambda e: e.matmul(
                                    ps[:, bk, tt * 256:(tt + 1) * 256], lhsT=xT[:, kc, sl], rhs=wt[:, kc, 0:256],
                                    start=(kc == 0), stop=(kc == KC - 1))))(),
                                    reads=keys + [("wr", wb_i)], writes=[B(bk)])
                        evac_copy(Vaug[g][:, t0:t0 + 2, :, 0:64],
                                  ps[:, bk, :].rearrange("p (t h d) -> p t h d", t=2, h=4),
                                  reads=[], writes=[B(bk), ("V", g, t0), ("V", g, t0 + 1)])
                for hp in range(2 * ps_, 2 * ps_ + 2):
                    for si, nm in ((0, "q"), (1, "k")):
                        ensure(blk_i[0] + 2)
                        wb_i = loaded[(nm, hp)]
                        blk_i[0] += 1
                        for g in range(3):
                            sl_ = si * 3 + g
                            for sp in range(4):
                                bk = nb_(PROJ)
                                for kc in range(KC):
                                    S.op("pe", (lambda bk=bk, kc=kc, g=g, sp=sp, wt=WB[wb_i]: (lambda e: e.matmul(
                                        ps[:, bk, :], lhsT=wt[:, kc, g * 128:(g + 1) * 128],
                                        rhs=xT[:, kc, sp * 512:(sp + 1) * 512], start=(kc == 0), stop=(kc == KC - 1))))(),
                                        reads=[("xT", 4 * sp + j) for j in range(4)] + [("wr", wb_i)], writes=[B(bk)])
                                if g == 0:
                                    o_ap = qk[:, sl_, sp * 512:(sp + 1) * 512]
                                    i_ap = ps[:, bk, :]
                                elif g == 1:
                                    o_ap = qk[:, sl_, :].rearrange("p (r n i) -> p r n i", r=4, n=4)[:, :, sp, :]
                                    i_ap = ps[:, bk, :].rearrange("p (i r) -> p r i", r=4)
                                else:
                                    o_ap = qk[:, sl_, :].rearrange("p (r a) -> p r a", r=16)[:, :, sp * 32:(sp + 1) * 32]
                                    i_ap = ps[:, bk, :].rearrange("p (a r) -> p r a", r=16)
                                evac_copy(o_ap, i_ap, reads=[], writes=[B(bk), ("qk", sl_, sp)])
                    for h in (2 * hp, 2 * hp + 1):
                        b0 = (h % 2) * 64
                        hl = h % 4
                        QK_ALL = lambda s_: [("qk", s_, sp) for sp in range(4)]
                        for grp in range(4):
                            def front2(grp=grp, b0=b0):
                                st_rr[0] += 1
                                bk = SC[st_rr[0] % NSC]
                                for jj in range(4):
                                    r = grp * 4 + jj
                                    S.op("pe", (lambda bk=bk, jj=jj, r=r, b0=b0: (lambda e: e.matmul(
                                        ps[:, bk, jj * 128:(jj + 1) * 128], lhsT=qk[b0:b0 + 64, 5, r * 128:(r + 1) * 128],
                                        rhs=qk[b0:b0 + 64, 2, r * 128:(r + 1) * 128], start=True, stop=True)))(),
                                        reads=QK_ALL(5) + QK_ALL(2), writes=[B(bk)])
                                pview = PT2[:, grp * 4:(grp + 1) * 4, :]
                                S.op("act", (lambda bk=bk, pview=pview: (lambda e: e.activation(
                                    out=pview, in_=ps[:, bk, :].rearrange("p (a b) -> p a b", a=4), func=AF.Exp, scale=0.125)))(),
                                    reads=[], writes=[B(bk), ("pt2", grp)])
                                S.op("dve" if grp % 2 == 0 else "pool", (lambda pview=pview: (lambda e: e.tensor_tensor(
                                    out=pview, in0=pview, in1=mown4[:].rearrange("p (a b) -> p a b", a=4), op=ALU.mult)))(),
                                    reads=["mown4"], writes=[("pt2", grp)])
                            pipe(front2, None)
                        for s in range(4):
                            acc_rr[0] += 1
                            ab = acc_rr[0] % 2
                            first = [True]
                            blocks_ = []
                            for j in range(4 * s, 4 * s + 4):
                                q_ap = qk[b0:b0 + 64, 0, j * 128:(j + 1) * 128]
                                o_ap = ps[:, ab, (j - 4 * s) * 128:(j - 4 * s + 1) * 128]
                                prev = None
                                if j > 0:
                                    prev = (qk[b0:b0 + 64, 3, (j - 1) * 128:j * 128], Vaug[0][:, j - 1, hl, :],
                                            [("qk", 3, (j - 1) // 4), ("V", 0, j - 1), ("vones", 0)])
                                own = (qk[b0:b0 + 64, 3, j * 128:(j + 1) * 128], Vaug[0][:, j, hl, :],
                                       [("qk", 3, j // 4), ("V", 0, j), ("vones", 0)])
                                blocks_.append((q_ap, [("qk", 0, s)], o_ap, prev, own))
                            for r4 in range(4):
                                q_ap = qk[b0:b0 + 64, 1, r4 * 512 + s * 128:r4 * 512 + (s + 1) * 128]
                                o_ap = ps[:, ab, r4:512:4]
                                prev = None
                                if s > 0:
                                    prev = (qk[b0:b0 + 64, 4, r4 * 512 + (s - 1) * 128:r4 * 512 + s * 128],
                                            Vaug[1][:, r4 * 4 + s - 1, hl, :],
                                            [("qk", 4, s - 1), ("V", 1, r4 * 4 + s - 1), ("vones", 1)])
                                own = (qk[b0:b0 + 64, 4, r4 * 512 + s * 128:r4 * 512 + (s + 1) * 128],
                                       Vaug[1][:, r4 * 4 + s, hl, :],
                                       [("qk", 4, s), ("V", 1, r4 * 4 + s), ("vones", 1)])
                                blocks_.append((q_ap, [("qk", 1, s)], o_ap, prev, own))
                            for bp in range(0, 8, 2):
                                st = {}

                                def front(bp=bp, st=st, blocks_=blocks_):
                                    st_rr[0] += 1
                                    sb_ = SC[st_rr[0] % NSC]
                                    ptb = PTb[st_rr[0] % NSC]
                                    ptk = ("ptb", st_rr[0] % NSC)
                                    used = []
                                    pv = []
                                    for bi_, blk in enumerate(blocks_[bp:bp + 2]):
                                        q_ap, qkeys, o_ap, prev, own = blk
                                        for kind, it in ((0, prev), (1, own)):
                                            if it is None:
                                                continue
                                            sl_i = bi_ * 2 + kind
                                            used.append(sl_i)
                                            k_ap, v_ap, rkeys = it
                                            S.op("pe", (lambda sb_=sb_, sl_i=sl_i, k_ap=k_ap, q_ap=q_ap: (lambda e: e.matmul(
                                                ps[:, sb_, sl_i * 128:(sl_i + 1) * 128], lhsT=k_ap, rhs=q_ap, start=True, stop=True)))(),
                                                reads=qkeys + [rkeys[0]], writes=[B(sb_)])
                                            pv.append((sl_i, v_ap, o_ap, rkeys[1:]))
                                    if used == [0, 1, 2, 3]:
                                        sel = lambda ap: ap
                                    elif used == [1, 2, 3]:
                                        sel = lambda ap: ap[:, 128:512]
                                    else:
                                        assert used == [1, 3], used
                                        sel = lambda ap: ap.rearrange("p (a b) -> p a b", a=4)[:, 1:4:2, :]
                                    S.op("act", (lambda sb_=sb_, ptb=ptb, sel=sel: (lambda e: e.activation(
                                        out=sel(ptb[:]), in_=sel(ps[:, sb_, :]), func=AF.Exp, scale=0.125)))(),
                                        reads=[], writes=[B(sb_), ptk])
                                    S.op("dve" if bp < 4 else "pool", (lambda ptb=ptb, sel=sel: (lambda e: e.tensor_tensor(
                                        out=sel(ptb[:]), in0=sel(ptb[:]), in1=sel(mask4[:]), op=ALU.mult)))(),
                                        reads=["mask4"], writes=[ptk])
                                    st["pv"], st["ptb"], st["ptk"] = pv, ptb, ptk

                                def back(bp=bp, st=st, first=first, ab=ab, s=s, hl=hl, b0=b0, hp=hp, h=h):
                                    ptb, ptk = st["ptb"], st["ptk"]
                                    for sl_i, v_ap, o_ap, rkeys in st["pv"]:
                                        st_flag = first[0]
                                        first[0] = False
                                        S.op("pe", (lambda sl_i=sl_i, v_ap=v_ap, o_ap=o_ap, ptb=ptb, st_flag=st_flag: (lambda e: e.matmul(
                                            o_ap, lhsT=v_ap, rhs=ptb[:, sl_i * 128:(sl_i + 1) * 128], start=st_flag, stop=False)))(),
                                            reads=[ptk] + rkeys, writes=[B(ab)])
                                    if bp != 6:
                                        return
                                    for r in range(16):
                                        S.op("pe", (lambda r=r, s=s, ab=ab, hl=hl: (lambda e: e.matmul(
                                            ps[:, ab, r:512:16], lhsT=Vaug[2][:, r, hl, :], rhs=PT2[:, r, 32 * s:32 * (s + 1)],
                                            start=False, stop=(r == 15))))(),
                                            reads=[("pt2", r // 4), ("V", 2, r), ("vones", 2)], writes=[B(ab)])
                                    rdt = rd[ab]
                                    S.op("dve", (lambda ab=ab, rdt=rdt: (lambda e: e.reciprocal(out=rdt[:], in_=ps[64:128, ab, :])))(),
                                         reads=[], writes=[B(ab), ("rd", ab)])
                                    S.op("dve", (lambda ab=ab, rdt=rdt, b0=b0, hp=hp, s=s: (lambda e: e.tensor_tensor(
                                        out=obT[b0:b0 + 64, hp, s * 512:(s + 1) * 512], in0=ps[0:64, ab, :], in1=rdt[:], op=ALU.mult)))(),
                                        reads=[("rd", ab)], writes=[B(ab), ("obT", hp, s, h % 2)])
                                pipe(front, back)
                        flush()
                        if HEAD_BARRIER:
                            S.barrier()
            S.barrier()
        OBT_ALL = [("obT", hp, s, hh) for hp in range(4) for s in range(4) for hh in range(2)]
        if "obT" in debug:
            o = dbg_out("obT", [128, 4, S_TOK], BF16)
            S.dma("sp", lambda e, o=o: e.dma_start(out=o, in_=obT[:]), slot(), reads=OBT_ALL, final=True)

        XT_ALL = [("xT", i) for i in range(NT)]
        yaT_es = contextlib.ExitStack()
        yaT = sbt(yaT_es, "yaT", [128, 4, S_TOK], BF16)
        wring[:] = [sbt(yaT_es, "wringB%d" % i, [128, KC, 512], BF16) for i in range(4)]
        wr_state["n"] = 0
        with contextlib.ExitStack() as es2:
            vg = sbt(es2, "vg", [128, NT, 512], F32)
            lng = sbt(es2, "lng", [128, 512], F32)
            lnb = sbt(es2, "lnb", [128, 512], F32)
            wsf = sbt(es2, "wsf", [128, 8, 128], F32)
            WmT = sbt(es2, "wmT", [128, 8, 128], BF16)
            bsf = sbt(es2, "bsf", [2, 1024], F32)
            bsh = sbt(es2, "bsh", [2, 1024], BF16)
            bshf = sbt(es2, "bshf", [2, 1024], F32)
            bsl = sbt(es2, "bsl", [2, 1024], BF16)
            sqb = [sbt(es2, "sqb%d" % i, [128, 512], F32) for i in range(3)]
            tnb = [sbt(es2, "tnb%d" % i, [128, 512], F32) for i in range(3)]
            st6 = sbt(es2, "st6", [128, NT, 6], F32)
            mv = sbt(es2, "mv", [128, NT, 2], F32)
            xe = sbt(es2, "xeA", [128, NT], F32)
            lnt = [sbt(es2, "lnt%d" % i, [128, 512], F32) for i in range(2)]
            vln = [sbt(es2, "vln%d" % i, [128, 512], BF16) for i in range(2)]
            bu = load_wblk("u")
            bv = load_wblk("v")
            S.dma("sp", lambda e: e.dma_start(out=lng[:], in_=alng_d[0].partition_broadcast(128)), slot("c"), writes=["lng"])
            S.dma("sp", lambda e: e.dma_start(out=lnb[:], in_=alnb_d[0].partition_broadcast(128)), slot("c"), writes=["lnb"])
            S.dma("sp", lambda e: e.dma_start(out=wsf[:], in_=awsT_d), slot("c"), writes=["wsf"])
            S.dma("sp", lambda e: e.dma_start(out=bsf[0:1, :], in_=abs_d), slot("c"), writes=["bsf0"])
            S.dma("sp", lambda e: e.dma_start(out=bsf[1:2, :], in_=abs_d), slot("c"), writes=["bsf1"])
            S.op("dve", lambda e: e.tensor_tensor(out=WmT[:], in0=wsf[:],
                                                  in1=mown4[:].rearrange("p (a b) -> p a b", a=4)[:, 0:1, :].to_broadcast([128, 8, 128]),
                                                  op=ALU.mult), reads=["wsf", "mown4"], writes=["WmT"])
            S.op("dve", lambda e: e.tensor_copy(bsh[:], bsf[:]), reads=["bsf0", "bsf1"], writes=["bsh"])
            S.op("dve", lambda e: e.tensor_copy(bshf[:], bsh[:]), reads=["bsh"], writes=["bshf"])
            S.op("dve", lambda e: e.tensor_tensor(out=bshf[:], in0=bsf[:], in1=bshf[:], op=ALU.subtract), reads=["bshf"], writes=["bshf"])
            S.op("dve", lambda e: e.tensor_copy(bsl[:], bshf[:]), reads=["bshf"], writes=["bsl"])
            S.dma("sp", lambda e: e.dma_start(out=bsh[1:2, :], in_=bsl[1:2, :]), slot("c"), reads=["bsl", "bsh"], writes=["bsh"])

            grr = [0]

            def gelu_front(bk):
                grr[0] += 1
                idx = grr[0] % 3
                sq = sqb[idx]
                ks = ("sqb", idx)
                S.op("act", lambda e: e.activation(out=sq[:], in_=ps[:, bk, :], func=AF.Square), reads=[], writes=[B(bk), ks])
                S.op("pool", lambda e: e.tensor_scalar(out=sq[:], in0=sq[:], scalar1=0.044715, scalar2=1.0,
                                                       op0=ALU.mult, op1=ALU.add), reads=[], writes=[ks])
                S.op("dve", lambda e: e.tensor_tensor(out=sq[:], in0=sq[:], in1=ps[:, bk, :], op=ALU.mult),
                     reads=[], writes=[B(bk), ks])
                return bk, idx

            def gelu_back(st, out_ap, wkeys, after=None):
                bk, idx = st
                sq, tn = sqb[idx], tnb[idx]
                ks, kt = ("sqb", idx), ("tnb", idx)
                S.op("act", lambda e: e.activation(out=tn[:], in_=sq[:], func=AF.Tanh, scale=GELU_C), reads=[ks], writes=[kt])
                S.op("dve", lambda e: e.scalar_tensor_tensor(out=out_ap, in0=tn[:], scalar=1.0, in1=ps[:, bk, :],
                                                             op0=ALU.add, op1=ALU.mult), reads=[kt], writes=[B(bk)] + wkeys)
                if after is not None:
                    after()

            gpend = []

            def gelu_pipe(bk, out_ap, wkeys, after=None):
                st = gelu_front(bk)
                if gpend:
                    gelu_back(*gpend.pop(0))
                gpend.append((st, out_ap, wkeys, after))

            ALLB = [0, 1, 2, 3, 4, 5, 6, 7]
            for fc in range(4):
                for sp in range(4):
                    bk = nb_(ALLB)
                    for kc in range(KC):
                        S.op("pe", (lambda bk=bk, kc=kc, fc=fc, sp=sp, wt=WB[bu]: (lambda e: e.matmul(
                            ps[:, bk, :], lhsT=wt[:, kc, fc * 128:(fc + 1) * 128], rhs=xT[:, kc, sp * 512:(sp + 1) * 512],
                            start=(kc == 0), stop=(kc == KC - 1))))(),
                            reads=[("xT", 4 * sp + j) for j in range(4)] + [("wr", bu)], writes=[B(bk)])
                    gelu_pipe(bk, yaT[:, fc, sp * 512:(sp + 1) * 512], [("yaT", fc, 4 * sp + j) for j in range(4)])
            for i in range(NT):
                bk = nb_(ALLB)
                for kc in range(KC):
                    S.op("pe", (lambda bk=bk, kc=kc, i=i, wt=WB[bv]: (lambda e: e.matmul(
                        ps[:, bk, :], lhsT=xT[:, kc, i * 128:(i + 1) * 128], rhs=wt[:, kc, 0:512],
                        start=(kc == 0), stop=(kc == KC - 1))))(),
                        reads=[("xT", i), ("wr", bv)], writes=[B(bk)])

                def stats(i=i):
                    S.op("dve", (lambda i=i: (lambda e: e.bn_stats(out=st6[:, i, :], in_=vg[:, i, :])))(), reads=[("vg", i)], writes=[("st6", i)])
                    S.op("dve", (lambda i=i: (lambda e: e.bn_aggr(out=mv[:, i, :], in_=st6[:, i, :])))(), reads=[("st6", i)], writes=[("mvA", i)])
                gelu_pipe(bk, vg[:, i, :], [("vg", i)], stats)
            while gpend:
                gelu_back(*gpend.pop(0))
            S.op("dve", lambda e: e.tensor_scalar(out=xe[:], in0=mv[:, :, 1], scalar1=4.0 * LN_EPS, scalar2=None, op0=ALU.add),
                 reads=[("mvA", i) for i in range(NT)], writes=[("rsx", "A")])
            rstd, krs, _ = rsqrt_batch(es2, "A", xe[:], NT)

            def ln_tile(i):
                lt = lnt[i % 2]
                vl = vln[i % 2]
                S.op("dve", (lambda i=i, lt=lt: (lambda e: e.scalar_tensor_tensor(
                    out=lt[:], in0=vg[:, i, :], scalar=mv[:, i, 0:1], in1=lng[:], op0=ALU.subtract, op1=ALU.mult)))(),
                    reads=[("vg", i), ("mvA", i), "lng"], writes=[("lnt", i % 2)])
                S.op("dve", (lambda i=i, lt=lt, vl=vl: (lambda e: e.scalar_tensor_tensor(
                    out=vl[:], in0=lt[:], scalar=rstd[:, i:i + 1], in1=lnb[:], op0=ALU.mult, op1=ALU.add)))(),
                    reads=["lnb", ("lnt", i % 2), krs], writes=[("vln", i % 2)])

            ln_tile(0)
            for i in range(NT):
                vl = vln[i % 2]
                bk = nb_(ALLB)
                for cc in range(4):
                    for gg in range(2):
                        g = 2 * cc + gg
                        o_ap = ps[gg * 64:(gg + 1) * 64, bk, cc * 128:(cc + 1) * 128]
                        S.op("pe", (lambda o_ap=o_ap, g=g, vl=vl: (lambda e: e.matmul(
                            o_ap, lhsT=vl[:, g * 64:(g + 1) * 64], rhs=WmT[:, g, :], start=True, stop=False)))(),
                            reads=[("vln", i % 2), "WmT"], writes=[B(bk)])
                        S.op("pe", (lambda o_ap=o_ap, g=g: (lambda e: e.matmul(
                            o_ap, lhsT=onesb[0:2, 0:64], rhs=bsh[0:2, g * 128:(g + 1) * 128], start=False, stop=True)))(),
                            reads=["bsh", "onesb"], writes=[B(bk)])
                if i + 1 < NT:
                    ln_tile(i + 1)
                ya_v = yaT[:, :, i * 128:(i + 1) * 128]
                S.op("dve", (lambda bk=bk, ya_v=ya_v: (lambda e: e.scalar_tensor_tensor(
                    out=ya_v, in0=ps[:, bk, :].rearrange("p (c t) -> p c t", c=4), scalar=0.5, in1=ya_v, op0=ALU.mult, op1=ALU.mult)))(),
                    reads=[], writes=[B(bk)] + [("yaT", fc, i) for fc in range(4)])
            S.barrier()
        YAT_ALL = [("yaT", fc, i) for fc in range(4) for i in range(NT)]
        if "yaT" in debug:
            o = dbg_out("yaT", [128, 4, S_TOK], BF16)
            S.dma("sp", lambda e, o=o: e.dma_start(out=o, in_=yaT[:]), slot(), reads=YAT_ALL, final=True)

        with contextlib.ExitStack() as es3:
            wa = sbt(es3, "wa", [128, 4, D], BF16)
            wb = sbt(es3, "wb", [128, 4, D], BF16)
            ta = [sbt(es3, "ta%d" % i, [128, 512], BF16) for i in range(2)]
            tb = [sbt(es3, "tb%d" % i, [128, 512], BF16) for i in range(2)]
            m1 = [sbt(es3, "m1_%d" % i, [128, 512], F32) for i in range(2)]
            m2 = [sbt(es3, "m2_%d" % i, [128, 512], F32) for i in range(2)]
            S.dma("pool", lambda e: e.dma_start(out=wa[:], in_=wa_d.rearrange("(c p) d -> p c d", p=128)), slot("c"), writes=["wa"])
            S.dma("pool", lambda e: e.dma_start(out=wb[:], in_=wb_d.rearrange("(c p) d -> p c d", p=128)), slot("c"), writes=["wb"])
            gblk = {}
            gblk[0] = load_wblk(("g", 0))
            gblk[1] = load_wblk(("g", 1))
            gblk[2] = load_wblk(("g", 2))
            gblk[3] = load_wblk(("g", 3))
            it_ = 0
            for dc in range(KC):
                ga_i = gblk[2 * (dc // 4)]
                gb_i = gblk[2 * (dc // 4) + 1]
                for sp in range(4):
                    it_ += 1
                    par = it_ % 2
                    bA, bB, bC, bD = [4 * par + j for j in range(4)]
                    tok = slice(sp * 512, (sp + 1) * 512)
                    xk = [("xT", 4 * sp + j) for j in range(4)]
                    for cc in range(4):
                        S.op("pe", (lambda cc=cc, bA=bA, dc=dc, tok=tok: (lambda e: e.matmul(
                            ps[:, bA, :], lhsT=wa[:, cc, dc * 128:(dc + 1) * 128], rhs=yaT[:, cc, tok], start=(cc == 0), stop=(cc == 3))))(),
                            reads=["wa"] + [("yaT", cc, 4 * sp + j) for j in range(4)], writes=[B(bA)])
                    for cc in range(4):
                        S.op("pe", (lambda cc=cc, bB=bB, dc=dc, tok=tok: (lambda e: e.matmul(
                            ps[:, bB, :], lhsT=wb[:, cc, dc * 128:(dc + 1) * 128], rhs=obT[:, cc, tok], start=(cc == 0), stop=(cc == 3))))(),
                            reads=["wb"] + [("obT", cc, sp, 0), ("obT", cc, sp, 1)], writes=[B(bB)])
                    for (bX, gi) in ((bC, ga_i), (bD, gb_i)):
                        for kc in range(KC):
                            S.op("pe", (lambda kc=kc, bX=bX, wt=WB[gi], dc=dc, tok=tok: (lambda e: e.matmul(
                                ps[:, bX, :], lhsT=wt[:, kc, (dc % 4) * 128:(dc % 4 + 1) * 128], rhs=xT[:, kc, tok],
                                start=(kc == 0), stop=(kc == KC - 1))))(),
                                reads=xk + [("wr", gi)], writes=[B(bX)])
                    S.op("act", (lambda bC=bC, par=par: (lambda e: e.activation(out=ta[par][:], in_=ps[:, bC, :], func=AF.Tanh, scale=0.5)))(),
                         reads=[], writes=[B(bC), ("ta", par)])
                    S.op("act", (lambda bD=bD, par=par: (lambda e: e.activation(out=tb[par][:], in_=ps[:, bD, :], func=AF.Tanh, scale=0.5)))(),
                         reads=[], writes=[B(bD), ("tb", par)])
                    S.op("dve", (lambda bA=bA, par=par: (lambda e: e.scalar_tensor_tensor(
                        out=m1[par][:], in0=ta[par][:], scalar=1.0, in1=ps[:, bA, :], op0=ALU.add, op1=ALU.mult)))(),
                        reads=[("ta", par)], writes=[B(bA), ("m1", par)])
                    S.op("dve", (lambda bB=bB, par=par: (lambda e: e.scalar_tensor_tensor(
                        out=m2[par][:], in0=tb[par][:], scalar=1.0, in1=ps[:, bB, :], op0=ALU.add, op1=ALU.mult)))(),
                        reads=[("tb", par)], writes=[B(bB), ("m2", par)])
                    S.op("pool", (lambda par=par, dc=dc, tok=tok: (lambda e: e.tensor_tensor(
                        out=mixinT[:, dc, tok], in0=m1[par][:], in1=m2[par][:], op=ALU.add)))(),
                        reads=[("m1", par), ("m2", par)], writes=[("mix", dc, 4 * sp + j) for j in range(4)])
            S.barrier()
        yaT_es.close()
        mx_es.close()
        if "mixinT" in debug:
            o = dbg_out("mixinT", [128, KC, S_TOK], BF16)
            S.dma("sp", lambda e, o=o: e.dma_start(out=o, in_=mixinT[:]), slot(),
                  reads=[("mix", dc, i) for dc in range(KC) for i in range(NT)], final=True)

        p5_es = contextlib.ExitStack()
        x1b = sbt(p5_es, "x1b", [128, NT, D], BF16)
        logit = sbt(p5_es, "logit", [128, NT, 36], F32)
        G4 = 2
        with contextlib.ExitStack() as es4:
            wo = sbt(es4, "wo", [128, KC, D], BF16)
            wpg = sbt(es4, "wpg", [128, KC, D], BF16)
            wple = sbt(es4, "wple", [128, 2, D], BF16)
            wr = sbt(es4, "wr", [128, KC, 36], F32)
            wrh = sbt(es4, "wrh", [128, KC, 36], BF16)
            wrhf = sbt(es4, "wrhf", [128, KC, 36], F32)
            wrl = sbt(es4, "wrl", [128, KC, 36], BF16)
            x1lo = [sbt(es4, "x1lo%d" % i, [128, D], BF16) for i in range(3)]
            brb = sbt(es4, "brb", [128, 36], F32)
            l1g = sbt(es4, "l1g", [128, D], F32)
            l1b = sbt(es4, "l1b", [128, D], F32)
            xres = [sbt(es4, "xres%d" % i, [128, D], F32) for i in range(2)]
            rg = [sbt(es4, "rg%d" % q, [128, G4, D], F32) for q in range(2)]
            st12 = [sbt(es4, "st12_%d" % q, [128, G4, 12], F32) for q in range(2)]
            mv1 = [sbt(es4, "mv1_%d" % q, [128, G4, 2], F32) for q in range(2)]
            xe1 = [sbt(es4, "xe1_%d" % q, [128, G4], F32) for q in range(2)]
            rsq1 = [(sbt(es4, "rs_tfL1_%d" % q, [128, G4], F32), sbt(es4, "rs_yiL1_%d" % q, [128, G4], I32),
                     sbt(es4, "rs_aL1_%d" % q, [128, G4], F32)) for q in range(2)]
            x1f = [sbt(es4, "x1f%d" % i, [128, D], F32) for i in range(3)]
            x1Tf = [sbt(es4, "x1Tl%d" % i, [128, KC, 128], BF16) for i in range(2)]
            x1Tb = [sbt(es4, "x1Tb%d" % i, [128, KC, 128], BF16) for i in range(2)]
            pst = [sbt(es4, "pst%d" % i, [128, 256], BF16) for i in range(2)]
            pT = [sbt(es4, "pT%d" % i, [128, 2, 128], BF16) for i in range(2)]
            tp = [sbt(es4, "tp%d" % i, [128, D], BF16) for i in range(2)]
            ple2 = [sbt(es4, "ple2_%d" % i, [128, D], F32) for i in range(2)]
            S.dma("pool", lambda e: e.dma_start(out=wo[:], in_=wo_d.rearrange("(c p) d -> p c d", p=128)), slot("c"), writes=["wo"])
            S.dma("pool", lambda e: e.dma_start(out=wpg[:], in_=wpg_d.rearrange("(c p) d -> p c d", p=128)), slot("c"), writes=["wpg"])
            S.dma("pool", lambda e: e.dma_start(out=wple[:], in_=wple_d.rearrange("(c p) d -> p c d", p=128)), slot("c"), writes=["wple"])
            S.dma("sp", lambda e: e.dma_start(out=wr[:], in_=wr_d.rearrange("(c p) d -> p c d", p=128)), slot("c"), writes=["wr_"])
            S.dma("sp", lambda e: e.dma_start(out=brb[:], in_=br_d[0].partition_broadcast(128)), slot("c"), writes=["brb"])
            S.op("dve", lambda e: e.tensor_copy(wrh[:], wr[:]), reads=["wr_"], writes=["wrh"])
            S.op("dve", lambda e: e.tensor_copy(wrhf[:], wrh[:]), reads=["wrh"], writes=["wrhf"])
            S.op("dve", lambda e: e.tensor_tensor(out=wrl[:], in0=wr[:], in1=wrhf[:], op=ALU.subtract), reads=["wr_", "wrhf"], writes=["wrl"])
            S.dma("sp", lambda e: e.dma_start(out=l1g[:], in_=ln1g_d[0].partition_broadcast(128)), slot("c"), writes=["l1g"])
            S.dma("sp", lambda e: e.dma_start(out=l1b[:], in_=ln1b_d[0].partition_broadcast(128)), slot("c"), writes=["l1b"])
            NG4 = NT // G4

            def load_xres(i):
                xr = xres[i % 2]
                S.dma("sp", (lambda i=i, xr=xr: (lambda e: e.dma_start(out=xr[:], in_=x_d[i * 128:(i + 1) * 128, :])))(),
                      "xres%d" % (i % 2), writes=[("xres", i % 2)])

            def part1_tile(g, gi):
                gp = g % 2
                i = g * G4 + gi
                xr = xres[i % 2]
                if i == 0:
                    load_xres(0)
                if i + 1 < NT:
                    load_xres(i + 1)
                S.op("act", (lambda xr=xr: (lambda e: e.activation(out=xr[:], in_=xr[:], func=AF.Identity, scale=ALPHA)))(),
                     reads=[], writes=[("xres", i % 2)])
                for half in range(2):
                    bk = half
                    for dc in range(KC):
                        S.op("pe", (lambda bk=bk, dc=dc, i=i, half=half: (lambda e: e.matmul(
                            ps[:, bk, :], lhsT=mixinT[:, dc, i * 128:(i + 1) * 128], rhs=wo[:, dc, half * 512:(half + 1) * 512],
                            start=(dc == 0), stop=(dc == KC - 1))))(),
                            reads=[("mix", dc, i), "wo"], writes=[B(bk)])
                    S.op("dve", (lambda bk=bk, gi=gi, gp=gp, half=half, xr=xr: (lambda e: e.scalar_tensor_tensor(
                        out=rg[gp][:, gi, half * 512:(half + 1) * 512], in0=ps[:, bk, :], scalar=0.5,
                        in1=xr[:, half * 512:(half + 1) * 512], op0=ALU.mult, op1=ALU.add)))(),
                        reads=[("xres", i % 2)], writes=[B(bk), ("rg", gp, gi, half)])
                    S.op("dve", (lambda gi=gi, gp=gp, half=half: (lambda e: e.bn_stats(
                        out=st12[gp][:, gi, half * 6:(half + 1) * 6], in_=rg[gp][:, gi, half * 512:(half + 1) * 512])))(),
                        reads=[("rg", gp, gi, half)], writes=[("st12", gp, gi, half)])
                S.op("dve", (lambda gi=gi, gp=gp: (lambda e: e.bn_aggr(out=mv1[gp][:, gi, :], in_=st12[gp][:, gi, :])))(),
                     reads=[("st12", gp, gi, 0), ("st12", gp, gi, 1)], writes=[("mv1", gp, gi)])

            def rsqrt_grp(g):
                gp = g % 2
                tag = "L1_%d" % g
                xe_ = xe1[gp]
                tf_, yi_, a__ = rsq1[gp]
                kx, ky, ka, kt = ("rsx", "L1", gp), ("rsy", "L1", gp), ("rsa", "L1", gp), ("rst", "L1", gp)
                S.op("dve", lambda e: e.tensor_scalar(out=xe_[:], in0=mv1[gp][:, :, 1], scalar1=LN_EPS, scalar2=None, op0=ALU.add),
                     reads=[("mv1", gp, gi) for gi in range(G4)], writes=[kx])
                S.op("dve", lambda e: e.tensor_copy(tf_[:], xe_[:].bitcast(I32)), reads=[kx], writes=[kt])
                S.op("dve", lambda e: e.tensor_scalar(out=tf_[:], in0=tf_[:], scalar1=-0.5, scalar2=1597463007.0,
                                                      op0=ALU.mult, op1=ALU.add), reads=[kt], writes=[kt])
                S.op("dve", lambda e: e.tensor_copy(yi_[:], tf_[:]), reads=[kt], writes=[ky])
                y1 = yi_[:].bitcast(F32)
                for _ in range(2):
                    S.op("dve", lambda e: e.tensor_tensor(out=a__[:], in0=y1, in1=y1, op=ALU.mult), reads=[ky], writes=[ka])
                    S.op("dve", lambda e: e.tensor_tensor(out=a__[:], in0=a__[:], in1=xe_[:], op=ALU.mult), reads=[ka, kx], writes=[ka])
                    S.op("dve", lambda e: e.tensor_scalar(out=a__[:], in0=a__[:], scalar1=-0.5, scalar2=1.5,
                                                          op0=ALU.mult, op1=ALU.add), reads=[ka], writes=[ka])
                    S.op("dve", lambda e: e.tensor_tensor(out=y1, in0=y1, in1=a__[:], op=ALU.mult), reads=[ka, ky], writes=[ky])

            def ln_norm(g, gi):
                gp = g % 2
                i = g * G4 + gi
                y1 = rsq1[gp][1][:].bitcast(F32)
                ky = ("rsy", "L1", gp)
                xf = x1f[i % 3]
                kxf = ("x1f", i % 3)
                S.op("dve", (lambda gi=gi, gp=gp, xf=xf: (lambda e: e.scalar_tensor_tensor(
                    out=xf[:], in0=rg[gp][:, gi, :], scalar=mv1[gp][:, gi, 0:1], in1=l1g[:], op0=ALU.subtract, op1=ALU.mult)))(),
                    reads=[("rg", gp, gi, 0), ("rg", gp, gi, 1), ("mv1", gp, gi), "l1g"], writes=[kxf])
                S.op("dve", (lambda gi=gi, xf=xf, y1=y1: (lambda e: e.scalar_tensor_tensor(
                    out=xf[:], in0=xf[:], scalar=y1[:, gi:gi + 1], in1=l1b[:], op0=ALU.mult, op1=ALU.add)))(),
                    reads=[ky, "l1b"], writes=[kxf])
                S.op("act", (lambda xf=xf, i=i: (lambda e: e.activation(out=x1b[:, i, :], in_=xf[:], func=AF.Copy)))(),
                     reads=[kxf], writes=[("x1b", i)])

            def ln_lo(g, gi):
                i = g * G4 + gi
                xf = x1f[i % 3]
                xlo = x1lo[i % 3]
                S.op("dve", (lambda xf=xf, xlo=xlo, i=i: (lambda e: e.tensor_tensor(out=xlo[:], in0=xf[:], in1=x1b[:, i, :], op=ALU.subtract)))(),
                     reads=[("x1f", i % 3), ("x1b", i)], writes=[("x1lo", i % 3)])

            def load_p(i):
                pp = pst[i % 2]
                S.dma("pool", (lambda i=i, pp=pp: (lambda e: e.dma_start(out=pp[:], in_=p_d[i * 128:(i + 1) * 128, :])))(),
                      "pst%d" % (i % 2), writes=[("pst", i % 2)])

            def part2_tile(g, gi, mid=None):
                i = g * G4 + gi
                xf = x1f[i % 3]
                kxf = ("x1f", i % 3)
                xlo = x1lo[i % 3]
                if i == 0:
                    load_p(0)
                if i + 1 < NT:
                    load_p(i + 1)
                if True:
                    if True:
                        xtf, xtb = x1Tf[i % 2], x1Tb[i % 2]
                        psh_ = ps[:, 2, :].bitcast(BF16)
                        psl_ = ps[:, 3, :].bitcast(BF16)
                        for kc in range(KC):
                            S.op("pe", (lambda kc=kc, i=i, psh_=psh_: (lambda e: e.transpose(
                                out=psh_[:, kc * 128:(kc + 1) * 128], in_=x1b[:, i, kc * 128:(kc + 1) * 128], identity=identb[:])))(),
                                reads=[("x1b", i), "identb"], writes=[B(2)])
                        S.op("dve", (lambda xtb=xtb, psh_=psh_: (lambda e: e.tensor_copy(xtb[:], psh_.rearrange("p (j t) -> p j t", j=KC))))(),
                             reads=[], writes=[B(2), ("x1Tb", i % 2)])
                        for kc in range(KC):
                            S.op("pe", (lambda kc=kc, xlo=xlo, psl_=psl_: (lambda e: e.transpose(
                                out=psl_[:, kc * 128:(kc + 1) * 128], in_=xlo[:, kc * 128:(kc + 1) * 128], identity=identb[:])))(),
                                reads=[("x1lo", i % 3), "identb"], writes=[B(3)])
                        S.op("act", (lambda xtf=xtf, psl_=psl_: (lambda e: e.activation(out=xtf[:], in_=psl_.rearrange("p (j t) -> p j t", j=KC), func=AF.Copy)))(),
                             reads=[], writes=[B(3), ("x1Tl", i % 2)])
                        passes = [(xtb, wrh), (xtf, wrh), (xtb, wrl)]
                        for pi, (xa, wa_) in enumerate(passes):
                            for kc in range(KC):
                                S.op("pe", (lambda kc=kc, xa=xa, wa_=wa_, pi=pi: (lambda e: e.matmul(
                                    ps[:, 4, 0:36], lhsT=xa[:, kc, :], rhs=wa_[:, kc, :], start=(pi == 0 and kc == 0), stop=(pi == 2 and kc == KC - 1))))(),
                                    reads=[("x1Tb", i % 2), ("x1Tl", i % 2), "wrh", "wrl"], writes=[B(4)])
                        S.op("dve", (lambda i=i: (lambda e: e.tensor_tensor(out=logit[:, i, :], in0=ps[:, 4, 0:36], in1=brb[:], op=ALU.add)))(),
                             reads=["brb"], writes=[B(4), ("logit", i)])
                        pp = pst[i % 2]
                        psp_ = ps[:, 4, :].bitcast(BF16)
                        for c in range(2):
                            S.op("pe", (lambda c=c, pp=pp, psp_=psp_: (lambda e: e.transpose(
                                out=psp_[:, 512 + c * 128:512 + (c + 1) * 128], in_=pp[:, c * 128:(c + 1) * 128], identity=identb[:])))(),
                                reads=[("pst", i % 2), "identb"], writes=[B(4)])
                        S.op("act", (lambda i=i, psp_=psp_: (lambda e: e.activation(
                            out=pT[i % 2][:], in_=psp_[:, 512:768].rearrange("p (c t) -> p c t", c=2), func=AF.Copy)))(),
                            reads=[], writes=[B(4), ("pT", i % 2)])
                        if mid is not None:
                            mid()
                        for half in range(2):
                            bg = 5
                            bl = 6 + half
                            for kc in range(KC):
                                S.op("pe", (lambda kc=kc, half=half, xtb=xtb: (lambda e: e.matmul(
                                    ps[:, 5, :], lhsT=xtb[:, kc, :], rhs=wpg[:, kc, half * 512:(half + 1) * 512],
                                    start=(kc == 0), stop=(kc == KC - 1))))(),
                                    reads=[("x1Tb", i % 2), "wpg"], writes=[B(5)])
                            S.op("act", (lambda half=half, i=i: (lambda e: e.activation(
                                out=tp[i % 2][:, half * 512:(half + 1) * 512], in_=ps[:, 5, :], func=AF.Tanh, scale=0.5)))(),
                                reads=[], writes=[B(5), ("tp", i % 2, half)])
                            for c in range(2):
                                S.op("pe", (lambda c=c, half=half, bl=bl, i=i: (lambda e: e.matmul(
                                    ps[:, bl, :], lhsT=pT[i % 2][:, c, :], rhs=wple[:, c, half * 512:(half + 1) * 512],
                                    start=(c == 0), stop=(c == 1))))(),
                                    reads=[("pT", i % 2), "wple"], writes=[B(bl)])
                            S.op("dve", (lambda half=half, bl=bl, i=i: (lambda e: e.scalar_tensor_tensor(
                                out=ple2[i % 2][:, half * 512:(half + 1) * 512], in0=tp[i % 2][:, half * 512:(half + 1) * 512],
                                scalar=1.0, in1=ps[:, bl, :], op0=ALU.add, op1=ALU.mult)))(),
                                reads=[("tp", i % 2, half)], writes=[B(bl), ("ple2", i % 2, half)])
                        S.op("act", (lambda xf=xf: (lambda e: e.activation(out=xf[:], in_=xf[:], func=AF.Identity, scale=ALPHA)))(),
                             reads=[], writes=[kxf])
                        S.op("pool", (lambda i=i: (lambda e: e.tensor_scalar(
                            out=ple2[i % 2][:], in0=ple2[i % 2][:], scalar1=0.5, scalar2=0.0, op0=ALU.mult, op1=ALU.add)))(),
                            reads=[], writes=[("ple2", i % 2, 0), ("ple2", i % 2, 1)])
                        S.op("pool", (lambda xf=xf, i=i: (lambda e: e.tensor_tensor(
                            out=ple2[i % 2][:], in0=ple2[i % 2][:], in1=xf[:], op=ALU.add)))(),
                            reads=[kxf], writes=[("ple2", i % 2, 0), ("ple2", i % 2, 1)])
                        S.dma("sp", (lambda i=i: (lambda e: e.dma_start(out=base_d[i * 128:(i + 1) * 128, :], in_=ple2[i % 2][:])))(),
                              "base%d" % (i % 2), reads=[("ple2", i % 2, 0), ("ple2", i % 2, 1)], writes=[("base", i)])

            def ln_grp(g):
                rsqrt_grp(g)
                for gi in range(G4):
                    ln_norm(g, gi)
                for gi in range(G4):
                    ln_lo(g, gi)

            for gi in range(G4):
                part1_tile(0, gi)
            ln_grp(0)
            for g in range(NG4):
                for gi in range(G4):
                    mid = None
                    if g + 1 < NG4:
                        part1_tile(g + 1, gi)
                        if gi == G4 - 1:
                            mid = (lambda g=g: ln_grp(g + 1))
                    part2_tile(g, gi, mid)
            S.barrier()
        if "logit" in debug:
            o = dbg_out("logit", [128, NT, 36])
            S.dma("sp", lambda e, o=o: e.dma_start(out=o, in_=logit[:]), slot(), reads=[("logit", i) for i in range(NT)], final=True)
        if "x1b" in debug:
            o = dbg_out("x1b", [128, NT, D], BF16)
            S.dma("sp", lambda e, o=o: e.dma_start(out=o, in_=x1b[:]), slot(), reads=[("x1b", i) for i in range(NT)], final=True)

        with contextlib.ExitStack() as es5:
            LG = [("logit", i) for i in range(NT)]
            gmax = sbt(es5, "gmax", [128, NT], F32)
            gd = sbt(es5, "gd", [128, NT, 4], F32)
            gm = sbt(es5, "gm", [128, NT, 4], F32)
            gsum = sbt(es5, "gsum", [128, NT], F32)
            gp = sbt(es5, "gp", [128, NT], F32)
            selm = sbt(es5, "selm", [128, NT, 4, 8], F32)
            sel = sbt(es5, "sel", [128, NT, 8], F32)
            m8 = sbt(es5, "m8", [128, NT, 8], F32)
            e1 = sbt(es5, "e1", [128, NT, 8], F32)
            e2 = sbt(es5, "e2", [128, NT, 8], F32)
            dd = sbt(es5, "dd", [128, NT], F32)
            w1 = sbt(es5, "w1", [128, NT], F32)
            A1 = sbt(es5, "A1", [128, NT, 32], F32)
            A2 = sbt(es5, "A2", [128, NT, 32], F32)
            Ab = sbt(es5, "Ab", [128, NT, 32], BF16)
            cnt = sbt(es5, "cnt", [128, 32], F32)
            cnti = sbt(es5, "cnti", [128, 32], I32)
            ntf = sbt(es5, "ntf", [128, 32], F32)
            onesf = sbt(es5, "onesf", [128, 32], F32)
            cum = sbt(es5, "cum", [128, 32], F32)
            bas = sbt(es5, "bas", [128, 32], F32)
            rk = sbt(es5, "rk", [128, NT, 32], F32)
            tmpA = sbt(es5, "tmpA", [128, NT, 32], F32)
            slf = sbt(es5, "slf", [128, NT, 2], F32)
            cmp = sbt(es5, "cmp", [128, NTILE_E, 32], F32)
            tef = sbt(es5, "tef", [128, NTILE_E], F32)
            gl = logit[:, :, 0:4]
            el = logit[:, :, 4:36].rearrange("p n (g i) -> p n g i", g=4)
            V = "dve"
            S.op(V, lambda e: e.tensor_reduce(out=gmax[:], in_=gl, axis=AX.X, op=ALU.max), reads=LG, writes=["gmax"])
            S.op(V, lambda e: e.tensor_tensor(out=gd[:], in0=gl, in1=gmax[:].unsqueeze(2).to_broadcast([128, NT, 4]), op=ALU.subtract),
                 reads=LG + ["gmax"], writes=["gd"])
            S.op(V, lambda e: e.tensor_scalar(out=gm[:], in0=gd[:], scalar1=0.0, scalar2=None, op0=ALU.is_ge), reads=["gd"], writes=["gm"])
            S.op("act", lambda e: e.activation(out=gd[:], in_=gd[:], func=AF.Exp), reads=[], writes=["gd"])
            S.op(V, lambda e: e.tensor_reduce(out=gsum[:], in_=gd[:], axis=AX.X, op=ALU.add), reads=["gd"], writes=["gsum"])
            S.op(V, lambda e: e.reciprocal(out=gp[:], in_=gsum[:]), reads=["gsum"], writes=["gp"])
            S.op(V, lambda e: e.tensor_tensor(out=selm[:], in0=el, in1=gm[:].unsqueeze(3).to_broadcast([128, NT, 4, 8]), op=ALU.mult),
                 reads=LG + ["gm"], writes=["selm"])
            S.op(V, lambda e: e.tensor_reduce(out=sel[:], in_=selm[:].rearrange("p n g i -> p n i g"), axis=AX.X, op=ALU.add),
                 reads=["selm"], writes=["sel"])
            for i in range(NT):
                S.op(V, (lambda i=i: (lambda e: e.max(out=m8[:, i, :], in_=sel[:, i, :])))(), reads=["sel"], writes=[("m8", i)])
            M8 = [("m8", i) for i in range(NT)]
            S.op(V, lambda e: e.tensor_tensor(out=e1[:], in0=sel[:], in1=m8[:, :, 0:1].to_broadcast([128, NT, 8]), op=ALU.is_equal),
                 reads=["sel"] + M8, writes=["e1"])
            S.op(V, lambda e: e.tensor_tensor(out=e2[:], in0=sel[:], in1=m8[:, :, 1:2].to_broadcast([128, NT, 8]), op=ALU.is_equal),
                 reads=["sel"] + M8, writes=["e2"])
            S.op(V, lambda e: e.tensor_tensor(out=dd[:], in0=m8[:, :, 1], in1=m8[:, :, 0], op=ALU.subtract), reads=M8, writes=["dd"])
            S.op("act", lambda e: e.activation(out=dd[:], in_=dd[:], func=AF.Exp), reads=[], writes=["dd"])
            S.op(V, lambda e: e.tensor_scalar(out=w1[:], in0=dd[:], scalar1=1.0, scalar2=None, op0=ALU.add), reads=["dd"], writes=["w1"])
            S.op(V, lambda e: e.reciprocal(out=w1[:], in_=w1[:]), reads=[], writes=["w1"])
            S.op(V, lambda e: e.tensor_tensor(out=cw[:, :, 0], in0=w1[:], in1=gp[:], op=ALU.mult), reads=["w1", "gp"], writes=["cw0"])
            S.op(V, lambda e: e.tensor_tensor(out=dd[:], in0=dd[:], in1=w1[:], op=ALU.mult), reads=["w1"], writes=["dd"])
            S.op(V, lambda e: e.tensor_tensor(out=cw[:, :, 1], in0=dd[:], in1=gp[:], op=ALU.mult), reads=["dd", "gp"], writes=["cw1"])
            for (Ax, ex, nm) in ((A1, e1, "A1"), (A2, e2, "A2")):
                S.op(V, (lambda Ax=Ax, ex=ex: (lambda e: e.tensor_tensor(
                    out=Ax[:].rearrange("p n (g i) -> p n g i", g=4), in0=gm[:].unsqueeze(3).to_broadcast([128, NT, 4, 8]),
                    in1=ex[:].unsqueeze(2).to_broadcast([128, NT, 4, 8]), op=ALU.mult)))(),
                    reads=["gm", "e1", "e2"], writes=[nm])
            S.op(V, lambda e: e.tensor_tensor(out=Ab[:], in0=A1[:], in1=A2[:], op=ALU.add), reads=["A1", "A2"], writes=["Ab"])
            for i in range(NT):
                o_ap = ps[:, 0, i * 32:(i + 1) * 32]
                S.op("pe", (lambda i=i, o_ap=o_ap: (lambda e: e.matmul(o_ap, lhsT=lstr[:], rhs=Ab[:, i, :], start=True, stop=(i == 0))))(),
                     reads=["Ab", "lstr"], writes=[B(0)])
                for i2 in range(i):
                    S.op("pe", (lambda i2=i2, o_ap=o_ap, i=i: (lambda e: e.matmul(o_ap, lhsT=onesb[:], rhs=Ab[:, i2, :], start=False, stop=(i2 == i - 1))))(),
                         reads=["Ab", "onesb"], writes=[B(0)])
            for i in range(NT):
                S.op("pe", (lambda i=i: (lambda e: e.matmul(ps[:, 1, 0:32], lhsT=onesb[:], rhs=Ab[:, i, :], start=(i == 0), stop=(i == NT - 1))))(),
                     reads=["Ab", "onesb"], writes=[B(1)])
            S.op(V, lambda e: e.tensor_copy(rk[:], ps[:, 0, :].rearrange("p (n e) -> p n e", n=NT)), reads=[], writes=[B(0), "rk"])
            S.op(V, lambda e: e.tensor_scalar(out=cnt[:], in0=ps[:, 1, 0:32], scalar1=127.0, scalar2=None, op0=ALU.add), reads=[], writes=[B(1), "cnt"])
            S.op(V, lambda e: e.tensor_copy(cnti[:], cnt[:]), reads=["cnt"], writes=["cnti"])
            S.op(V, lambda e: e.tensor_scalar(out=cnti[:], in0=cnti[:], scalar1=7, scalar2=None, op0=ALU.arith_shift_right), reads=[], writes=["cnti"])
            S.op(V, lambda e: e.tensor_copy(ntf[:], cnti[:]), reads=["cnti"], writes=["ntf"])
            S.op(V, lambda e: e.memset(onesf[:], 1.0), writes=["onesf"])
            S.op(V, lambda e: e.tensor_tensor_scan(out=cum[:], data0=onesf[:], data1=ntf[:], initial=0.0, op0=ALU.mult, op1=ALU.add),
                 reads=["onesf", "ntf"], writes=["cum"])
            S.op(V, lambda e: e.tensor_tensor(out=bas[:], in0=cum[:], in1=ntf[:], op=ALU.subtract), reads=["cum", "ntf"], writes=["bas"])
            S.op(V, lambda e: e.tensor_scalar(out=bas[:], in0=bas[:], scalar1=128.0, scalar2=None, op0=ALU.mult), reads=[], writes=["bas"])
            S.op(V, lambda e: e.tensor_tensor(out=rk[:], in0=rk[:], in1=bas[:].unsqueeze(1).to_broadcast([128, NT, 32]), op=ALU.add),
                 reads=["bas"], writes=["rk"])
            for j, Ax in ((0, A1), (1, A2)):
                S.op(V, (lambda Ax=Ax: (lambda e: e.tensor_tensor(out=tmpA[:], in0=rk[:], in1=Ax[:], op=ALU.mult)))(),
                     reads=["rk", "A1", "A2"], writes=["tmpA"])
                S.op(V, (lambda j=j: (lambda e: e.tensor_reduce(out=slf[:, :, j], in_=tmpA[:], axis=AX.X, op=ALU.add)))(),
                     reads=["tmpA"], writes=[("slf", j)])
            S.op(V, lambda e: e.tensor_copy(slotu[:], slf[:]), reads=[("slf", 0), ("slf", 1)], writes=["slotu"])
            S.op(V, lambda e: e.tensor_tensor(out=cmp[:], in0=cum[:].unsqueeze(1).to_broadcast([128, NTILE_E, 32]),
                                              in1=kidx[:].unsqueeze(2).to_broadcast([128, NTILE_E, 32]), op=ALU.is_le),
                 reads=["cum", "kidx"], writes=["cmp"])
            S.op(V, lambda e: e.tensor_reduce(out=tef[:], in_=cmp[:], axis=AX.X, op=ALU.add), reads=["cmp"], writes=["tef"])
            S.op(V, lambda e: e.tensor_scalar(out=tef[:], in0=tef[:], scalar1=128.0, scalar2=None, op0=ALU.mult), reads=[], writes=["tef"])
            S.op(V, lambda e: e.tensor_copy(rowst[:], tef[:]), reads=["tef"], writes=["rowst"])
            S.op(V, lambda e: e.tensor_scalar(out=tef[:], in0=tef[:], scalar1=pidx[:, 0:1], scalar2=None, op0=ALU.add),
                 reads=["pidx", "rowst"], writes=["tef"])
            S.op(V, lambda e: e.tensor_copy(idxw[:], tef[:]), reads=["tef"], writes=["idxw"])
            if "route" in debug:
                o1 = dbg_out("slotu", [128, NT, 2], U32)
                o2 = dbg_out("cw", [128, NT, 2])
                o3 = dbg_out("idxw", [128, NTILE_E], U32)
                S.dma("sp", lambda e, o1=o1: e.dma_start(out=o1, in_=slotu[:]), slot(), reads=["slotu"], final=True)
                S.dma("sp", lambda e, o2=o2: e.dma_start(out=o2, in_=cw[:]), slot(), reads=["cw0", "cw1"], final=True)
                S.dma("sp", lambda e, o3=o3: e.dma_start(out=o3, in_=idxw[:]), slot(), reads=["idxw"], final=True)
            for i in range(NT if STOP_AFTER != "route" else 0):
                for j in range(2):
                    S.dma("pool", (lambda i=i, j=j: (lambda e: e.indirect_dma_start(
                        out=xs_d[:, :], out_offset=bass.IndirectOffsetOnAxis(ap=slotu[:, i, j:j + 1], axis=0),
                        in_=x1b[:, i, :], in_offset=None, bounds_check=preg(e, NTILE_E * 128 - 1), oob_is_err=False)))(),
                        "scat", reads=["slotu", ("x1b", i)] + [("xsz", q) for q in range(NTILE_E)], writes=[("xs", i, j)])
            S.barrier()
        p5_es.close()
        mix_es.close()
        XS_ALL = [("xs", i, j) for i in range(NT) for j in range(2)]

        with contextlib.ExitStack() as es6:
            NST = 4
            NWB = 4
            NXB = 4
            NXT = 3
            wbuf = [sbt(es6, "wbuf%d" % i, [128, 6144], BF16) for i in range(NWB)]
            wst = [sbt(es6, "wst%d" % i, [128, 6144], F32) for i in range(NST)]
            xsb = [sbt(es6, "xsb%d" % i, [128, D], BF16) for i in range(NXB)]
            xsT = [sbt(es6, "xsT%d" % i, [128, KC, 128], BF16) for i in range(NXT)]
            tg = [sbt(es6, "tg%d" % i, [128, 256], F32) for i in range(2)]
            hb = [sbt(es6, "hb%d" % i, [128, 256], BF16) for i in range(2)]
            hT = [sbt(es6, "hT%d" % i, [128, 2, 128], BF16) for i in range(2)]
            ysb = [sbt(es6, "ysb%d" % i, [128, D], F32) for i in range(2)]

            order_t = list(range(4))
            a_ = list(range(4, 32))
            hi_ = list(range(NTILE_E - 1, NTILE_E - 1 - 14, -1))
            lo_ = [t for t in range(32, NTILE_E) if t not in hi_]
            while a_:
                order_t.append(a_.pop(0))
                if a_:
                    order_t.append(a_.pop(0))
                if hi_:
                    order_t.append(hi_.pop(0))
            order_t += hi_ + lo_
            assert sorted(order_t) == list(range(NTILE_E))

            def T(k):
                return order_t[k]

            def load_w(k):
                si = k % NST
                S.dma("pool", (lambda si=si, k=k: (lambda e: e.indirect_dma_start(
                    out=wst[si][:], out_offset=None, in_=wexp_d[:, :],
                    in_offset=bass.IndirectOffsetOnAxis(ap=idxw[:, T(k):T(k) + 1], axis=0), bounds_check=preg(e, 4095), oob_is_err=False)))(),
                    "wst%d" % si, reads=["idxw"], writes=[("wst", si)])

            def cast_w(k):
                si = k % NST
                wi = k % NWB
                S.op("act", (lambda si=si, wi=wi: (lambda e: e.activation(out=wbuf[wi][:, 0:2048], in_=wst[si][:, 0:2048], func=AF.Copy)))(),
                     reads=[("wst", si)], writes=[("wbuf", wi, 0)])
                S.op("dve", (lambda si=si, wi=wi: (lambda e: e.tensor_copy(wbuf[wi][:, 2048:4096], wst[si][:, 2048:4096])))(),
                     reads=[("wst", si)], writes=[("wbuf", wi, 1)])
                S.op("dve", (lambda si=si, wi=wi: (lambda e: e.tensor_copy(wbuf[wi][:, 4096:6144], wst[si][:, 4096:6144])))(),
                     reads=[("wst", si)], writes=[("wbuf", wi, 2)])

            def load_x(k):
                xi = k % NXB
                S.dma("sp", (lambda k=k, xi=xi: (lambda e: e.dma_start(out=xsb[xi][:], in_=xs_d[T(k) * 128:(T(k) + 1) * 128, :])))(),
                      "xsb%d" % xi, reads=XS_ALL, writes=[("xsb", xi)])

            NK = NTILE_E if STOP_AFTER != "route" else 0

            def stage_t(k):
                par = k % 2
                xi = k % NXB
                ti = k % NXT
                psb = ps[:, par, :].bitcast(BF16)
                for kc in range(KC):
                    S.op("pe", (lambda kc=kc, xi=xi, psb=psb: (lambda e: e.transpose(
                        out=psb[:, kc * 128:(kc + 1) * 128], in_=xsb[xi][:, kc * 128:(kc + 1) * 128], identity=identb[:])))(),
                        reads=[("xsb", xi), "identb"], writes=[B(par)])
                evac_copy(xsT[ti][:], psb.rearrange("p (c t) -> p c t", c=KC), reads=[], writes=[B(par), ("xsT", ti)])

            def stage_g(k):
                wi = k % NWB
                par = k % 2
                ti = k % NXT
                bgu = 2 + par
                for kc in range(KC):
                    S.op("pe", (lambda kc=kc, ti=ti, wi=wi, bgu=bgu: (lambda e: e.matmul(
                        ps[:, bgu, :], lhsT=xsT[ti][:, kc, :], rhs=wbuf[wi][:, kc * 512:(kc + 1) * 512],
                        start=(kc == 0), stop=(kc == KC - 1))))(),
                        reads=[("xsT", ti), ("wbuf", wi, kc // 4)], writes=[B(bgu)])
                S.op("act", (lambda par=par, bgu=bgu: (lambda e: e.activation(out=tg[par][:], in_=ps[:, bgu, 0:256], func=AF.Tanh, scale=0.5)))(),
                     reads=[], writes=[B(bgu), ("tg", par)])
                S.op("dve", (lambda par=par, bgu=bgu: (lambda e: e.scalar_tensor_tensor(
                    out=tg[par][:], in0=tg[par][:], scalar=1.0, in1=ps[:, bgu, 0:256], op0=ALU.add, op1=ALU.mult)))(),
                    reads=[], writes=[B(bgu), ("tg", par)])
                S.op("dve", (lambda par=par, bgu=bgu: (lambda e: e.scalar_tensor_tensor(
                    out=hb[par][:], in0=tg[par][:], scalar=0.5, in1=ps[:, bgu, 256:512], op0=ALU.mult, op1=ALU.mult)))(),
                    reads=[("tg", par)], writes=[B(bgu), ("hb", par)])

            def stage_b1(k):
                par = k % 2
                psh = ps[:, 4 + par, :].bitcast(BF16)
                for c in range(2):
                    S.op("pe", (lambda c=c, par=par, psh=psh: (lambda e: e.transpose(
                        out=psh[:, c * 128:(c + 1) * 128], in_=hb[par][:, c * 128:(c + 1) * 128], identity=identb[:])))(),
                        reads=[("hb", par), "identb"], writes=[B(4 + par)])
                S.op("act", (lambda par=par, psh=psh: (lambda e: e.activation(
                    out=hT[par][:], in_=psh[:, 0:256].rearrange("p (c t) -> p c t", c=2), func=AF.Copy)))(),
                    reads=[], writes=[B(4 + par), ("hT", par)])

            def stage_b2(k):
                wi = k % NWB
                par = k % 2
                for half in range(2):
                    by = 6 + half
                    for c in range(2):
                        S.op("pe", (lambda c=c, half=half, par=par, wi=wi, by=by: (lambda e: e.matmul(
                            ps[:, by, :], lhsT=hT[par][:, c, :],
                            rhs=wbuf[wi][:, 4096 + c * 1024 + half * 512:4096 + c * 1024 + (half + 1) * 512],
                            start=(c == 0), stop=(c == 1))))(),
                            reads=[("hT", par), ("wbuf", wi, 2)], writes=[B(by)])
                    evac_copy(ysb[par][:, half * 512:(half + 1) * 512], ps[:, by, :], reads=[], writes=[B(by), ("ysb", par, half)])
                S.dma("sp", (lambda k=k, par=par: (lambda e: e.dma_start(out=ys_d[T(k) * 128:(T(k) + 1) * 128, :], in_=ysb[par][:])))(),
                      "ysb%d" % par, reads=[("ysb", par, 0), ("ysb", par, 1)], writes=[("ys", T(k))])

            for k0 in range(min(NST, NK)):
                load_w(k0)
            for k0 in range(min(3, NK)):
                load_x(k0)
            for k0 in range(min(3, NK)):
                cast_w(k0)
                if k0 + NST < NK:
                    load_w(k0 + NST)
            for k0 in range(min(2, NK)):
                stage_t(k0)
            if NK:
                stage_g(0)
            for k in range(NK):
                stage_b1(k)
                if k + 3 < NK:
                    load_x(k + 3)
                if k + 2 < NK:
                    stage_t(k + 2)
                if k + 1 < NK:
                    stage_g(k + 1)
                stage_b2(k)
                if k + 3 < NK:
                    cast_w(k + 3)
                    if k + 3 + NST < NK:
                        load_w(k + 3 + NST)
            S.barrier()
        YS_ALL = [("ys", k) for k in range(NTILE_E)]

        G7 = 4
        NG7 = NT // G7
        with contextlib.ExitStack() as es7:
            l2g = sbt(es7, "l2g", [128, D], F32)
            l2b = sbt(es7, "l2b", [128, D], F32)
            g1 = [sbt(es7, "g1_%d" % i, [128, D], F32) for i in range(4)]
            g2 = [sbt(es7, "g2_%d" % i, [128, D], F32) for i in range(4)]
            bt = [sbt(es7, "bt%d" % i, [128, D], F32) for i in range(4)]
            ot = [sbt(es7, "ot%d" % i, [128, D], F32) for i in range(4)]
            rg2 = [sbt(es7, "rg2_%d" % q, [128, G7, D], F32) for q in range(2)]
            st12b = [sbt(es7, "st12b_%d" % q, [128, G7, 12], F32) for q in range(2)]
            mv2 = [sbt(es7, "mv2_%d" % q, [128, G7, 2], F32) for q in range(2)]
            xe2 = [sbt(es7, "xe2_%d" % q, [128, G7], F32) for q in range(2)]
            sx2 = [sbt(es7, "sx2_%d" % q, [128, G7], F32) for q in range(2)]
            sq2 = [sbt(es7, "sq2_%d" % q, [128, G7], F32) for q in range(2)]
            mean2 = [sbt(es7, "mean2_%d" % q, [128, G7], F32) for q in range(2)]
            junk = sbt(es7, "junk7", [128, D], BF16)
            rstd2 = [sbt(es7, "rstd2_%d" % q, [128, G7], F32) for q in range(2)]
            nmr2 = [sbt(es7, "nmr2_%d" % q, [128, G7], F32) for q in range(2)]
            rsq2 = [(sbt(es7, "rs_tfL2_%d" % q, [128, G7], F32), sbt(es7, "rs_yiL2_%d" % q, [128, G7], I32),
                     sbt(es7, "rs_aL2_%d" % q, [128, G7], F32)) for q in range(2)]
            S.dma("sp", lambda e: e.dma_start(out=l2g[:], in_=ln2g_d[0].partition_broadcast(128)), slot("c"), writes=["l2g"])
            S.dma("sp", lambda e: e.dma_start(out=l2b[:], in_=ln2b_d[0].partition_broadcast(128)), slot("c"), writes=["l2b"])

            def p1_dma(g, gi):
                i = g * G7 + gi
                par = i % 4
                S.dma("pool", (lambda i=i, par=par: (lambda e: e.indirect_dma_start(
                    out=g1[par][:], out_offset=None, in_=ys_d[:, :],
                    in_offset=bass.IndirectOffsetOnAxis(ap=slotu[:, i, 0:1], axis=0),
                    bounds_check=preg(e, NTILE_E * 128 - 1), oob_is_err=False)))(),
                    "g1_%d" % par, reads=YS_ALL + ["slotu"], writes=[("g1", par)])
                S.dma("pool", (lambda i=i, par=par: (lambda e: e.indirect_dma_start(
                    out=g2[par][:], out_offset=None, in_=ys_d[:, :],
                    in_offset=bass.IndirectOffsetOnAxis(ap=slotu[:, i, 1:2], axis=0),
                    bounds_check=preg(e, NTILE_E * 128 - 1), oob_is_err=False)))(),
                    "g2_%d" % par, reads=YS_ALL + ["slotu"], writes=[("g2", par)])
                S.dma("sp", (lambda i=i, par=par: (lambda e: e.dma_start(out=bt[par][:], in_=base_d[i * 128:(i + 1) * 128, :])))(),
                      "bt%d" % par, reads=[("base", i)], writes=[("bt", par)])

            def p1_dve(g, gi):
                gp = g % 2
                i = g * G7 + gi
                par = i % 4
                S.op("dve", (lambda i=i, par=par: (lambda e: e.scalar_tensor_tensor(
                    out=bt[par][:], in0=g1[par][:], scalar=cw[:, i, 0:1], in1=bt[par][:], op0=ALU.mult, op1=ALU.add)))(),
                    reads=[("g1", par), "cw0"], writes=[("bt", par)])
                S.op("dve", (lambda i=i, par=par, gi=gi, gp=gp: (lambda e: e.scalar_tensor_tensor(
                    out=rg2[gp][:, gi, :], in0=g2[par][:], scalar=cw[:, i, 1:2], in1=bt[par][:], op0=ALU.mult, op1=ALU.add)))(),
                    reads=[("g2", par), "cw1", ("bt", par)], writes=[("rg2", gp, gi)])
                S.op("act", (lambda gi=gi, gp=gp: (lambda e: e.activation(
                    out=junk[:], in_=rg2[gp][:, gi, :], func=AF.Identity, accum_out=sx2[gp][:, gi:gi + 1])))(),
                    reads=[("rg2", gp, gi)], writes=["junk", ("sx2", gp, gi)])
                S.op("act", (lambda gi=gi, gp=gp: (lambda e: e.activation(
                    out=junk[:], in_=rg2[gp][:, gi, :], func=AF.Square, accum_out=sq2[gp][:, gi:gi + 1])))(),
                    reads=[("rg2", gp, gi)], writes=["junk", ("sq2", gp, gi)])

            def rs_grp(g):
                gp = g % 2
                xe_ = xe2[gp]
                tf_, yi_, a__ = rsq2[gp]
                kx, ky, ka, kt = ("rsx", "L2", gp), ("rsy", "L2", gp), ("rsa", "L2", gp), ("rst", "L2", gp)
                mean_ = mean2[gp]
                S.op("dve", lambda e: e.tensor_scalar(out=mean_[:], in0=sx2[gp][:], scalar1=1.0 / D, scalar2=None, op0=ALU.mult),
                     reads=[("sx2", gp, gi) for gi in range(G7)], writes=[("mean2", gp)])
                S.op("dve", lambda e: e.tensor_tensor(out=xe_[:], in0=mean_[:], in1=mean_[:], op=ALU.mult),
                     reads=[("mean2", gp)], writes=[kx])
                S.op("dve", lambda e: e.scalar_tensor_tensor(out=xe_[:], in0=sq2[gp][:], scalar=1.0 / D, in1=xe_[:],
                                                             op0=ALU.mult, op1=ALU.subtract),
                     reads=[("sq2", gp, gi) for gi in range(G7)], writes=[kx])
                S.op("dve", lambda e: e.tensor_scalar(out=xe_[:], in0=xe_[:], scalar1=LN_EPS, scalar2=None, op0=ALU.add),
                     reads=[], writes=[kx])
                S.op("dve", lambda e: e.tensor_copy(tf_[:], xe_[:].bitcast(I32)), reads=[kx], writes=[kt])
                S.op("dve", lambda e: e.tensor_scalar(out=tf_[:], in0=tf_[:], scalar1=-0.5, scalar2=1597463007.0,
                                                      op0=ALU.mult, op1=ALU.add), reads=[kt], writes=[kt])
                S.op("dve", lambda e: e.tensor_copy(yi_[:], tf_[:]), reads=[kt], writes=[ky])
                y2 = yi_[:].bitcast(F32)
                for it in range(2):
                    S.op("dve", lambda e: e.tensor_tensor(out=a__[:], in0=y2, in1=y2, op=ALU.mult), reads=[ky], writes=[ka])
                    S.op("dve", lambda e: e.tensor_tensor(out=a__[:], in0=a__[:], in1=xe_[:], op=ALU.mult), reads=[ka, kx], writes=[ka])
                    S.op("dve", lambda e: e.tensor_scalar(out=a__[:], in0=a__[:], scalar1=-0.5, scalar2=1.5,
                                                          op0=ALU.mult, op1=ALU.add), reads=[ka], writes=[ka])
                    if it == 0:
                        S.op("dve", lambda e: e.tensor_tensor(out=y2, in0=y2, in1=a__[:], op=ALU.mult), reads=[ka, ky], writes=[ky])
                    else:
                        S.op("dve", lambda e: e.tensor_tensor(out=rstd2[gp][:], in0=y2, in1=a__[:], op=ALU.mult),
                             reads=[ka, ky], writes=[("rstd2", gp)])
                S.op("dve", lambda e: e.scalar_tensor_tensor(out=nmr2[gp][:], in0=mean2[gp][:], scalar=-1.0, in1=rstd2[gp][:],
                                                             op0=ALU.mult, op1=ALU.mult),
                     reads=[("rstd2", gp), ("mean2", gp)], writes=[("nmr2", gp)])

            def p2_norm(g, gi):
                gp = g % 2
                i = g * G7 + gi
                par = i % 4
                S.op("act", (lambda gi=gi, gp=gp, par=par: (lambda e: e.activation(
                    out=ot[par][:], in_=rg2[gp][:, gi, :], func=AF.Identity,
                    scale=rstd2[gp][:, gi:gi + 1], bias=nmr2[gp][:, gi:gi + 1])))(),
                    reads=[("rg2", gp, gi), ("rstd2", gp), ("nmr2", gp)], writes=[("ot", par)])

            def p2_tile(g, gi):
                gp = g % 2
                i = g * G7 + gi
                par = i % 4
                S.op("dve", (lambda par=par: (lambda e: e.tensor_tensor(out=ot[par][:], in0=ot[par][:], in1=l2g[:], op=ALU.mult)))(),
                     reads=["l2g"], writes=[("ot", par)])
                S.op("pool", (lambda par=par: (lambda e: e.tensor_tensor(out=ot[par][:], in0=ot[par][:], in1=l2b[:], op=ALU.add)))(),
                     reads=["l2b"], writes=[("ot", par)])
                S.dma("sp", (lambda i=i, par=par: (lambda e: e.dma_start(out=out_d[i * 128:(i + 1) * 128, :], in_=ot[par][:])))(),
                      "ot%d" % par, reads=[("ot", par)], writes=[("out", i)], final=True)

            if STOP_AFTER != "route":
                for gi in range(G7):
                    p1_dma(0, gi)
                for gi in range(G7):
                    p1_dve(0, gi)
                rs_grp(0)
                for g in range(NG7):
                    if g + 1 < NG7:
                        for gi in range(G7):
                            p1_dma(g + 1, gi)
                    for gi in range(G7):
                        p2_norm(g, gi)
                    for gi in range(G7):
                        p2_tile(g, gi)
                        if g + 1 < NG7:
                            p1_dve(g + 1, gi)
                    if g + 1 < NG7:
                        rs_grp(g + 1)
        S.emit()
    return nc, dbg


def _consts():
    k = np.arange(128)[:, None]
    q = np.arange(128)[None, :]
    own = (k <= q).astype(np.float32)
    prev = (k >= q).astype(np.float32)
    bf = ml_dtypes.bfloat16
    return {
        "c_identf": np.eye(128, dtype=np.float32),
        "c_identb": np.eye(128, dtype=np.float32).astype(bf),
        "c_mask4": np.concatenate([prev, own, prev, own], axis=1).astype(bf),
        "c_mown4": np.concatenate([own, own, own, own], axis=1).astype(bf),
        "c_lstrict": (k < q).astype(np.float32).astype(bf),
        "c_onesb": np.ones((128, 128), np.float32).astype(bf),
        "c_kidx": np.broadcast_to(np.arange(NTILE_E, dtype=np.float32)[None, :], (128, NTILE_E)).copy(),
        "c_pidx": np.arange(128, dtype=np.float32).reshape(128, 1),
    }


def _shared_inputs(inp):
    f = lambda a: np.ascontiguousarray(np.asarray(a, dtype=np.float32))
    wg = f(inp["w_gate"])[0].reshape(32, 8, 128, 256)
    wu = f(inp["w_up"])[0].reshape(32, 8, 128, 256)
    gu = np.concatenate([wg, wu], axis=3)
    gu = np.ascontiguousarray(gu.transpose(0, 2, 1, 3))
    wgu0 = np.ascontiguousarray(gu[:, :, 0:4, :]).reshape(4096, 2048)
    wgu1 = np.ascontiguousarray(gu[:, :, 4:8, :]).reshape(4096, 2048)
    wd = f(inp["w_down"])[0].reshape(32, 2, 128, 1024)
    wdn = np.ascontiguousarray(wd.transpose(0, 2, 1, 3)).reshape(4096, 2048)
    sh = {
        "w_in": np.ascontiguousarray(f(inp["w_in"])[0][:, WIN_PERM]),
        "a_ln_g": f(inp["a_ln_g"]).reshape(1, 512),
        "a_ln_b": f(inp["a_ln_b"]).reshape(1, 512),
        "a_wsT": np.ascontiguousarray(f(inp["a_ws"])[0].transpose(2, 0, 1)),
        "a_bs": f(inp["a_bs"])[0].reshape(1, 1024),
        "w_a": f(inp["w_a_proj"])[0],
        "w_b": f(inp["w_b_proj"])[0],
        "w_o": f(inp["w_o"])[0],
        "ln1_g": f(inp["ln1_g"]).reshape(1, D),
        "ln1_b": f(inp["ln1_b"]).reshape(1, D),
        "w_r": np.ascontiguousarray(np.concatenate([f(inp["w_group_router"])[0], f(inp["w_expert_router"])[0].reshape(D, 32)], axis=1)),
        "b_r": np.concatenate([f(inp["b_group_router"])[0], f(inp["b_expert_router"])[0].reshape(32)]).reshape(1, 36),
        "wexp": np.ascontiguousarray(np.concatenate([wgu0.reshape(4096, 2048), wgu1.reshape(4096, 2048), wdn], axis=1)),
        "w_ple": f(inp["w_ple"])[0],
        "w_pg": f(inp["w_ple_gate"])[0],
        "ln2_g": f(inp["ln2_g"]).reshape(1, D),
        "ln2_b": f(inp["ln2_b"]).reshape(1, D),
    }
    sh.update(_consts())
    return sh


_NC_CACHE = {}


def kernel(**inputs):
    x = np.asarray(inputs["x"], dtype=np.float32)
    p = np.asarray(inputs["p"], dtype=np.float32)
    if "nc" not in _NC_CACHE:
        _NC_CACHE["nc"] = build_nc()[0]
    nc = _NC_CACHE["nc"]
    sh = _shared_inputs(inputs)
    n = x.shape[0]
    in_maps = []
    for c in range(n):
        m = dict(sh)
        m["x"] = np.ascontiguousarray(x[c])
        m["p"] = np.ascontiguousarray(p[0, c])
        in_maps.append(m)
    res = run_bass_kernel_spmd(nc, in_maps, core_ids=list(range(n)))
    return np.stack([np.asarray(r["out"], dtype=np.float32) for r in res.results], axis=0)
```

```python
import contextlib
import numpy as np
import ml_dtypes
import concourse.bass as bass
import concourse.mybir as mybir
from concourse.bass_utils import run_bass_kernel_spmd
from concourse.alu_op_type import AluOpType as ALU

F32 = mybir.dt.float32
BF16 = mybir.dt.bfloat16
U32 = mybir.dt.uint32
I32 = mybir.dt.int32
AF = mybir.ActivationFunctionType
AX = mybir.AxisListType

S_TOK = 2048
D = 1024
NT = 16
KC = 8
ALPHA = 2.0 ** 0.25
LN_EPS = 1e-5
GELU_C = 0.7978845608028654
NTILE_E = 63
ENGS = ("pe", "act", "dve", "pool", "sp")
DEBUG = []
HEAD_BARRIER = False
STOP_AFTER = None


class Op:
    __slots__ = ("eng", "fn", "deps", "idx", "is_dma", "sem", "val", "signal")

    def __init__(self, eng, fn, is_dma):
        self.eng = eng
        self.fn = fn
        self.deps = []
        self.is_dma = is_dma
        self.sem = None
        self.val = None
        self.signal = False


class Sched:
    def __init__(self, nc):
        self.nc = nc
        self.ops = []
        self.last_w = {}
        self.readers = {}
        self.dma_slots = {}
        self.final_dma = []
        self.bar_deps = []
        self.bar_need = set()
        self.last_eng = {}
        self.last_slot = {}

    def barrier(self):
        self.bar_deps = list(self.last_eng.values()) + list(self.last_slot.values())
        self.bar_need = set(ENGS)

    def _add(self, op, reads, writes):
        op.idx = len(self.ops)
        deps = set()
        for r in reads:
            w = self.last_w.get(r)
            if w is not None:
                deps.add(w)
        for r in writes:
            w = self.last_w.get(r)
            if w is not None:
                deps.add(w)
            for rd in self.readers.get(r, ()):
                deps.add(rd)
        if op.eng in self.bar_need:
            self.bar_need.discard(op.eng)
            deps.update(self.bar_deps)
        deps.discard(op.idx)
        for d in sorted(deps):
            dop = self.ops[d]
            if dop.eng == op.eng and not dop.is_dma and op.eng in ("pe", "sp"):
                continue
            op.deps.append(d)
            dop.signal = True
        for r in reads:
            self.readers.setdefault(r, []).append(op.idx)
        for r in writes:
            self.last_w[r] = op.idx
            self.readers[r] = []
        self.ops.append(op)
        if not op.is_dma:
            self.last_eng[op.eng] = op.idx
        return op

    def op(self, eng, fn, reads=(), writes=()):
        return self._add(Op(eng, fn, False), tuple(reads), tuple(writes))

    def dma(self, eng, fn, slot, reads=(), writes=(), final=False):
        op = Op(eng, fn, True)
        self.dma_slots.setdefault(slot, []).append(op)
        op.signal = True
        self._add(op, tuple(reads), tuple(writes))
        self.last_slot[slot] = op.idx
        if final:
            self.final_dma.append(op)
        return op

    def emit(self):
        nc = self.nc
        with contextlib.ExitStack() as es:
            esem = {e: es.enter_context(nc.semaphore("s_" + e)) for e in ENGS}
            ssem = {s: es.enter_context(nc.semaphore("d%d" % i)) for i, s in enumerate(self.dma_slots)}
            cnt = {e: 0 for e in ENGS}
            for op in self.ops:
                if op.is_dma:
                    continue
                if op.signal:
                    cnt[op.eng] += 1
                    op.sem = esem[op.eng]
                    op.val = cnt[op.eng]
            for s, ops in self.dma_slots.items():
                c = 0
                for op in ops:
                    c += 16
                    op.sem = ssem[s]
                    op.val = c
            block = es.enter_context(nc.Block())
            per_eng = {e: [o for o in self.ops if o.eng == e] for e in ENGS}

            def run(engname, eng):
                waited = {}
                for op in per_eng[engname]:
                    need = {}
                    for d in op.deps:
                        dop = self.ops[d]
                        k = id(dop.sem)
                        if waited.get(k, 0) >= dop.val:
                            continue
                        if k not in need or need[k][1] < dop.val:
                            need[k] = (dop.sem, dop.val)
                    for k, (sem, val) in need.items():
                        eng.wait_ge(sem, val)
                        waited[k] = val
                    ins = op.fn(eng)
                    if op.is_dma:
                        ins.then_inc(op.sem, 16)
                    elif op.signal:
                        ins.then_inc(op.sem, 1)
                if engname == "sp":
                    fin = {}
                    for op in self.final_dma:
                        k = id(op.sem)
                        if k not in fin or fin[k][1] < op.val:
                            fin[k] = (op.sem, op.val)
                    for sem, val in fin.values():
                        eng.wait_ge(sem, val)

            @block.tensor
            def _(e):
                run("pe", e)

            @block.scalar
            def _(e):
                run("act", e)

            @block.vector
            def _(e):
                run("dve", e)

            @block.gpsimd
            def _(e):
                run("pool", e)

            @block.sync
            def _(e):
                run("sp", e)


def _win_perm():
    perm = []
    blocks = {}

    def add(name, cols):
        blocks[name] = (len(perm), len(cols))
        perm.extend(cols)

    add("u", list(range(0, 512)))
    add("v", list(range(512, 1024)))

    def zb(s, g, h):
        base = 1024 + ((s * 3 + g) * 8 + h) * 64
        return list(range(base, base + 64))

    for ps_ in range(2):
        for g in range(3):
            cols = []
            for h in range(4 * ps_, 4 * ps_ + 4):
                cols += zb(2, g, h)
            add(("vv", ps_, g), cols)
        for hp in range(2 * ps_, 2 * ps_ + 2):
            for s, nm in ((0, "q"), (1, "k")):
                cols = []
                for g in range(3):
                    cols += zb(s, g, 2 * hp) + zb(s, g, 2 * hp + 1)
                add((nm, hp), cols)
    ga = 5632
    gb = 5632 + 1024
    add(("g", 0), list(range(ga, ga + 512)))
    add(("g", 1), list(range(gb, gb + 512)))
    add(("g", 2), list(range(ga + 512, ga + 1024)))
    add(("g", 3), list(range(gb + 512, gb + 1024)))
    assert len(perm) == 7680 and sorted(perm) == list(range(7680))
    return np.array(perm), blocks


WIN_PERM, WIN_BLOCKS = _win_perm()


def build_nc(debug=()):
    nc = bass.Bass("TRN2", target_bir_lowering=False)

    def din(name, shape, dt=F32):
        return nc.dram_tensor(name, list(shape), dt, kind="ExternalInput").ap()

    x_d = din("x", [S_TOK, D])
    p_d = din("p", [S_TOK, 256])
    win_d = din("w_in", [D, 7680])
    alng_d = din("a_ln_g", [1, 512])
    alnb_d = din("a_ln_b", [1, 512])
    awsT_d = din("a_wsT", [128, 8, 128])
    abs_d = din("a_bs", [1, 1024])
    wa_d = din("w_a", [512, D])
    wb_d = din("w_b", [512, D])
    wo_d = din("w_o", [D, D])
    ln1g_d = din("ln1_g", [1, D])
    ln1b_d = din("ln1_b", [1, D])
    wr_d = din("w_r", [D, 36])
    br_d = din("b_r", [1, 36])
    wexp_d = [din("wexp%d" % c, [4096, 2048]) for c in range(3)]
    wple_d = din("w_ple", [256, D])
    wpg_d = din("w_pg", [D, D])
    ln2g_d = din("ln2_g", [1, D])
    ln2b_d = din("ln2_b", [1, D])
    identf_d = din("c_identf", [128, 128])
    identb_d = din("c_identb", [128, 128], BF16)
    mask4_d = din("c_mask4", [128, 512], BF16)
    mown4_d = din("c_mown4", [128, 512], BF16)
    lstr_d = din("c_lstrict", [128, 128], BF16)
    onesb_d = din("c_onesb", [128, 128], BF16)
    kidx_d = din("c_kidx", [128, NTILE_E])
    pidx_d = din("c_pidx", [128, 1])

    out_d = nc.dram_tensor("out", [S_TOK, D], F32, kind="ExternalOutput").ap()
    xs_d = nc.dram_tensor("xs_scr", [NTILE_E * 128, D], BF16, kind="Internal").ap()
    ys_d = nc.dram_tensor("ys_scr", [NTILE_E * 128, D], F32, kind="Internal").ap()
    base_d = nc.dram_tensor("base_scr", [S_TOK, D], F32, kind="Internal").ap()
    dbg = {}

    def dbg_out(name, shape, dt=F32):
        dbg[name] = nc.dram_tensor("dbg_" + name, list(shape), dt, kind="ExternalOutput").ap()
        return dbg[name]

    S = Sched(nc)
    uid = [0]

    def slot(prefix="o"):
        uid[0] += 1
        return "%s%d" % (prefix, uid[0])

    with contextlib.ExitStack() as es0:
        def sbt(es, name, shape, dt):
            return es.enter_context(nc.sbuf_tensor(name, list(shape), dt))

        ps = es0.enter_context(nc.psum_tensor("ps", [128, 8, 512], F32))

        def B(b):
            return ("B", b)

        identf = sbt(es0, "identf", [128, 128], F32)
        identb = sbt(es0, "identb", [128, 128], BF16)
        mask4 = sbt(es0, "mask4", [128, 512], BF16)
        mown4 = sbt(es0, "mown4", [128, 512], BF16)
        lstr = sbt(es0, "lstr", [128, 128], BF16)
        onesb = sbt(es0, "onesb", [128, 128], BF16)
        kidx = sbt(es0, "kidx", [128, NTILE_E], F32)
        pidx = sbt(es0, "pidx", [128, 1], F32)
        for t, d_, nm in ((identf, identf_d, "identf"), (identb, identb_d, "identb"), (mask4, mask4_d, "mask4"),
                          (mown4, mown4_d, "mown4"), (lstr, lstr_d, "lstr"), (onesb, onesb_d, "onesb"),
                          (kidx, kidx_d, "kidx"), (pidx, pidx_d, "pidx")):
            S.dma("sp", (lambda t=t, d_=d_: (lambda e: e.dma_start(out=t[:], in_=d_)))(), slot("c"), writes=[nm])

        slotu = sbt(es0, "slotu", [128, NT, 2], U32)
        cw = sbt(es0, "cw", [128, NT, 2], F32)
        idxw = sbt(es0, "idxw", [128, NTILE_E], U32)
        rowst = sbt(es0, "rowst", [128, NTILE_E], I32)
        zt = sbt(es0, "zt", [128, D], BF16)
        mix_es = contextlib.ExitStack()
        mixinT = sbt(mix_es, "mixinT", [128, KC, S_TOK], BF16)

        S.op("pool", lambda e: e.memset(zt[:], 0.0), writes=["zt"])

        mx_es = contextlib.ExitStack()
        xT = sbt(mx_es, "xT", [128, KC, S_TOK], BF16)
        obT = sbt(mx_es, "obT", [128, 4, S_TOK], BF16)
        wring = []
        win_v = win_d.rearrange("(kc p) c -> p kc c", p=128)
        wr_state = {"n": 0}
        WB = {}

        def load_wblk(name):
            c0, ncol = WIN_BLOCKS[name]
            bi = wr_state["n"] % len(wring)
            wr_state["n"] += 1
            buf_ = wring[bi]
            S.dma("pool", lambda e: e.dma_start(out=buf_[:, :, 0:ncol], in_=win_v[:, :, c0:c0 + ncol]),
                  "wr%s%d" % (buf_.name, bi), writes=[("wr", bi)])
            WB[bi] = buf_
            return bi

        reg_cache = {}

        def preg(e, val):
            if val not in reg_cache:
                reg_cache[val] = e.to_reg(val)
            return reg_cache[val]

        bank_rr = {"n": 0}

        def nb_(pool):
            bank_rr["n"] += 1
            return pool[bank_rr["n"] % len(pool)]

        evac_rr = {"n": 0}

        def evac_copy(out_ap, in_ap, reads, writes):
            evac_rr["n"] += 1
            if evac_rr["n"] % 2:
                S.op("act", lambda e: e.activation(out=out_ap, in_=in_ap, func=AF.Copy), reads=reads, writes=writes)
            else:
                S.op("dve", lambda e: e.tensor_copy(out_ap, in_ap), reads=reads, writes=writes)

        def rsqrt_batch(es, tag, x_ap, n):
            tf = sbt(es, "rs_tf" + tag, [128, n], F32)
            yi = sbt(es, "rs_yi" + tag, [128, n], I32)
            a_ = sbt(es, "rs_a" + tag, [128, n], F32)
            kx, ky, ka, kt = ("rsx", tag), ("rsy", tag), ("rsa", tag), ("rst", tag)
            S.op("dve", lambda e: e.tensor_copy(tf[:], x_ap.bitcast(I32)), reads=[kx], writes=[kt])
            S.op("dve", lambda e: e.tensor_scalar(out=tf[:], in0=tf[:], scalar1=-0.5, scalar2=1597463007.0,
                                                  op0=ALU.mult, op1=ALU.add), reads=[kt], writes=[kt])
            S.op("dve", lambda e: e.tensor_copy(yi[:], tf[:]), reads=[kt], writes=[ky])
            y = yi[:].bitcast(F32)
            for _ in range(2):
                S.op("dve", lambda e: e.tensor_tensor(out=a_[:], in0=y, in1=y, op=ALU.mult), reads=[ky], writes=[ka])
                S.op("dve", lambda e: e.tensor_tensor(out=a_[:], in0=a_[:], in1=x_ap, op=ALU.mult), reads=[ka, kx], writes=[ka])
                S.op("dve", lambda e: e.tensor_scalar(out=a_[:], in0=a_[:], scalar1=-0.5, scalar2=1.5,
                                                      op0=ALU.mult, op1=ALU.add), reads=[ka], writes=[ka])
                S.op("dve", lambda e: e.tensor_tensor(out=y, in0=y, in1=a_[:], op=ALU.mult), reads=[ka, ky], writes=[ky])
            return y, ky, kx


        with contextlib.ExitStack() as es1:
            wring[:] = [sbt(es1, "wringA%d" % i, [128, KC, 512], BF16) for i in range(3)]
            order = []
            for ps_ in range(2):
                order += [("vv", ps_, 0), ("vv", ps_, 1), ("vv", ps_, 2)]
                for hp in range(2 * ps_, 2 * ps_ + 2):
                    order += [("q", hp), ("k", hp)]
            loaded = {}
            nxt = [0]

            def ensure(upto):
                while nxt[0] < len(order) and nxt[0] <= upto:
                    loaded[order[nxt[0]]] = load_wblk(order[nxt[0]])
                    nxt[0] += 1

            ensure(1)
            with contextlib.ExitStack() as esx:
                NXS = 4
                xsf = [sbt(esx, "xsf%d" % i, [128, D], F32) for i in range(NXS)]
                xst = [sbt(esx, "xst%d" % i, [128, D], BF16) for i in range(2)]

                def ldx(i):
                    xf_ = xsf[i % NXS]
                    S.dma("sp" if i % 2 == 0 else "pool",
                          (lambda i=i, xf_=xf_: (lambda e: e.dma_start(out=xf_[:], in_=x_d[i * 128:(i + 1) * 128, :])))(),
                          "xsf%d" % (i % NXS), writes=[("xsf", i % NXS)])

                for i in range(min(NXS, NT)):
                    ldx(i)
                for i in range(NT):
                    xs_ = xst[i % 2]
                    xf_ = xsf[i % NXS]
                    if i % 2 == 0:
                        S.op("act", (lambda xs_=xs_, xf_=xf_: (lambda e: e.activation(out=xs_[:], in_=xf_[:], func=AF.Copy)))(),
                             reads=[("xsf", i % NXS)], writes=[("xst", i % 2)])
                    else:
                        S.op("dve", (lambda xs_=xs_, xf_=xf_: (lambda e: e.tensor_copy(xs_[:], xf_[:])))(),
                             reads=[("xsf", i % NXS)], writes=[("xst", i % 2)])
                    if i + NXS < NT:
                        ldx(i + NXS)
                    bk = nb_([0, 1, 2, 3])
                    psb = ps[:, bk, :].bitcast(BF16)
                    for kc in range(KC):
                        S.op("pe", (lambda psb=psb, kc=kc, xs_=xs_: (lambda e: e.transpose(
                            out=psb[:, kc * 128:(kc + 1) * 128], in_=xs_[:, kc * 128:(kc + 1) * 128], identity=identb[:])))(),
                            reads=[("xst", i % 2), "identb"], writes=[B(bk)])
                    evac_copy(xT[:, :, i * 128:(i + 1) * 128], psb.rearrange("p (j t) -> p j t", j=KC),
                              reads=[], writes=[B(bk), ("xT", i)])
                S.barrier()
            for q in range(NTILE_E):
                S.dma("sp", (lambda q=q: (lambda e: e.dma_start(out=xs_d[q * 128:(q + 1) * 128, :], in_=zt[:])))(),
                      "xsz", reads=["zt"], writes=[("xsz", q)])
            if "xT" in debug:
                o = dbg_out("xT", [128, KC, S_TOK], BF16)
                S.dma("sp", lambda e, o=o: e.dma_start(out=o, in_=xT[:]), slot(), reads=[("xT", i) for i in range(NT)], final=True)

            XT_ALL = [("xT", i) for i in range(NT)]
            Vaug = [sbt(es1, "vaug%d" % g, [128, 16, 4, 128], BF16) for g in range(3)]
            qk = sbt(es1, "qk", [128, 6, S_TOK], BF16)
            PTb = [sbt(es1, "ptb%d" % i, [128, 512], BF16) for i in range(4)]
            PT2 = sbt(es1, "pt2", [128, 16, 128], BF16)
            rd = [sbt(es1, "rd%d" % i, [64, 512], F32) for i in range(2)]
            for g in range(3):
                S.op("pool", (lambda g=g: (lambda e: e.memset(Vaug[g][:, :, :, 64:128], 1.0)))(), writes=[("vones", g)])

            def v_tile_tokens(g, t):
                if g == 0:
                    return slice(t * 128, (t + 1) * 128), [("xT", t)]
                if g == 1:
                    r4, nb = t // 4, t % 4
                    return slice(512 * nb + r4, 512 * (nb + 1), 4), [("xT", 4 * nb + j) for j in range(4)]
                return slice(t, S_TOK, 16), XT_ALL

            PROJ = [6, 7]
            SC = [2, 3, 4, 5]
            NSC = len(SC)
            DEPTH = 2
            pend = []

            def pipe(front, back):
                front()
                pend.append(back)
                while len(pend) > DEPTH:
                    b_ = pend.pop(0)
                    if b_ is not None:
                        b_()

            def flush():
                while pend:
                    b_ = pend.pop(0)
                    if b_ is not None:
                        b_()

            blk_i = [0]
            pt_rr = [0]
            acc_rr = [0]
            st_rr = [0]

            for ps_ in range(2):
                for g in range(3):
                    ensure(blk_i[0] + 2)
                    wb_i = loaded[("vv", ps_, g)]
                    blk_i[0] += 1
                    for t0 in range(0, 16, 2):
                        bk = nb_(PROJ)
                        rk = []
                        for tt in range(2):
                            sl, keys = v_tile_tokens(g, t0 + tt)
                            rk += keys
                            for kc in range(KC):
                                S.op("pe", (lambda bk=bk, tt=tt, kc=kc, sl=sl, wt=WB[wb_i]: (lambda e: e.matmul(
                                    ps[:, bk, tt * 256:(tt + 1) * 256], lhsT=xT[:, kc, sl], rhs=wt[:, kc, 0:256],
                                    start=(kc == 0), stop=(kc == KC - 1))))(),
                                    reads=keys + [("wr", wb_i)], writes=[B(bk)])
                        evac_copy(Vaug[g][:, t0:t0 + 2, :, 0:64],
                                  ps[:, bk, :].rearrange("p (t h d) -> p t h d", t=2, h=4),
                                  reads=[], writes=[B(bk), ("V", g, t0), ("V", g, t0 + 1)])
                for hp in range(2 * ps_, 2 * ps_ + 2):
                    for si, nm in ((0, "q"), (1, "k")):
                        ensure(blk_i[0] + 2)
                        wb_i = loaded[(nm, hp)]
                        blk_i[0] += 1
                        for g in range(3):
                            sl_ = si * 3 + g
                            for sp in range(4):
                                bk = nb_(PROJ)
                                for kc in range(KC):
                                    S.op("pe", (lambda bk=bk, kc=kc, g=g, sp=sp, wt=WB[wb_i]: (lambda e: e.matmul(
                                        ps[:, bk, :], lhsT=wt[:, kc, g * 128:(g + 1) * 128],
                                        rhs=xT[:, kc, sp * 512:(sp + 1) * 512], start=(kc == 0), stop=(kc == KC - 1))))(),
                                        reads=[("xT", 4 * sp + j) for j in range(4)] + [("wr", wb_i)], writes=[B(bk)])
                                if g == 0:
                                    o_ap = qk[:, sl_, sp * 512:(sp + 1) * 512]
                                    i_ap = ps[:, bk, :]
                                elif g == 1:
                                    o_ap = qk[:, sl_, :].rearrange("p (r n i) -> p r n i", r=4, n=4)[:, :, sp, :]
                                    i_ap = ps[:, bk, :].rearrange("p (i r) -> p r i", r=4)
                                else:
                                    o_ap = qk[:, sl_, :].rearrange("p (r a) -> p r a", r=16)[:, :, sp * 32:(sp + 1) * 32]
                                    i_ap = ps[:, bk, :].rearrange("p (a r) -> p r a", r=16)
                                evac_copy(o_ap, i_ap, reads=[], writes=[B(bk), ("qk", sl_, sp)])
                    for h in (2 * hp, 2 * hp + 1):
                        b0 = (h % 2) * 64
                        hl = h % 4
                        QK_ALL = lambda s_: [("qk", s_, sp) for sp in range(4)]
                        for grp in range(4):
                            def front2(grp=grp, b0=b0):
                                st_rr[0] += 1
                                bk = SC[st_rr[0] % NSC]
                                for jj in range(4):
                                    r = grp * 4 + jj
                                    S.op("pe", (lambda bk=bk, jj=jj, r=r, b0=b0: (lambda e: e.matmul(
                                        ps[:, bk, jj * 128:(jj + 1) * 128], lhsT=qk[b0:b0 + 64, 5, r * 128:(r + 1) * 128],
                                        rhs=qk[b0:b0 + 64, 2, r * 128:(r + 1) * 128], start=True, stop=True)))(),
                                        reads=QK_ALL(5) + QK_ALL(2), writes=[B(bk)])
                                pview = PT2[:, grp * 4:(grp + 1) * 4, :]
                                S.op("act", (lambda bk=bk, pview=pview: (lambda e: e.activation(
                                    out=pview, in_=ps[:, bk, :].rearrange("p (a b) -> p a b", a=4), func=AF.Exp, scale=0.125)))(),
                                    reads=[], writes=[B(bk), ("pt2", grp)])
                                S.op("dve" if grp % 2 == 0 else "pool", (lambda pview=pview: (lambda e: e.tensor_tensor(
                                    out=pview, in0=pview, in1=mown4[:].rearrange("p (a b) -> p a b", a=4), op=ALU.mult)))(),
                                    reads=["mown4"], writes=[("pt2", grp)])
                            pipe(front2, None)
                        for s in range(4):
                            acc_rr[0] += 1
                            ab = acc_rr[0] % 2
                            first = [True]
                            blocks_ = []
                            for j in range(4 * s, 4 * s + 4):
                                q_ap = qk[b0:b0 + 64, 0, j * 128:(j + 1) * 128]
                                o_ap = ps[:, ab, (j - 4 * s) * 128:(j - 4 * s + 1) * 128]
                                prev = None
                                if j > 0:
                                    prev = (qk[b0:b0 + 64, 3, (j - 1) * 128:j * 128], Vaug[0][:, j - 1, hl, :],
                                            [("qk", 3, (j - 1) // 4), ("V", 0, j - 1), ("vones", 0)])
                                own = (qk[b0:b0 + 64, 3, j * 128:(j + 1) * 128], Vaug[0][:, j, hl, :],
                                       [("qk", 3, j // 4), ("V", 0, j), ("vones", 0)])
                                blocks_.append((q_ap, [("qk", 0, s)], o_ap, prev, own))
                            for r4 in range(4):
                                q_ap = qk[b0:b0 + 64, 1, r4 * 512 + s * 128:r4 * 512 + (s + 1) * 128]
                                o_ap = ps[:, ab, r4:512:4]
                                prev = None
                                if s > 0:
                                    prev = (qk[b0:b0 + 64, 4, r4 * 512 + (s - 1) * 128:r4 * 512 + s * 128],
                                            Vaug[1][:, r4 * 4 + s - 1, hl, :],
                                            [("qk", 4, s - 1), ("V", 1, r4 * 4 + s - 1), ("vones", 1)])
                                own = (qk[b0:b0 + 64, 4, r4 * 512 + s * 128:r4 * 512 + (s + 1) * 128],
                                       Vaug[1][:, r4 * 4 + s, hl, :],
                                       [("qk", 4, s), ("V", 1, r4 * 4 + s), ("vones", 1)])
                                blocks_.append((q_ap, [("qk", 1, s)], o_ap, prev, own))
                            for bp in range(0, 8, 2):
                                st = {}

                                def front(bp=bp, st=st, blocks_=blocks_):
                                    st_rr[0] += 1
                                    sb_ = SC[st_rr[0] % NSC]
                                    ptb = PTb[st_rr[0] % NSC]
                                    ptk = ("ptb", st_rr[0] % NSC)
                                    used = []
                                    pv = []
                                    for bi_, blk in enumerate(blocks_[bp:bp + 2]):
                                        q_ap, qkeys, o_ap, prev, own = blk
                                        for kind, it in ((0, prev), (1, own)):
                                            if it is None:
                                                continue
                                            sl_i = bi_ * 2 + kind
                                            used.append(sl_i)
                                            k_ap, v_ap, rkeys = it
                                            S.op("pe", (lambda sb_=sb_, sl_i=sl_i, k_ap=k_ap, q_ap=q_ap: (lambda e: e.matmul(
                                                ps[:, sb_, sl_i * 128:(sl_i + 1) * 128], lhsT=k_ap, rhs=q_ap, start=True, stop=True)))(),
                                                reads=qkeys + [rkeys[0]], writes=[B(sb_)])
                                            pv.append((sl_i, v_ap, o_ap, rkeys[1:]))
                                    if used == [0, 1, 2, 3]:
                                        sel = lambda ap: ap
                                    elif used == [1, 2, 3]:
                                        sel = lambda ap: ap[:, 128:512]
                                    else:
                                        assert used == [1, 3], used
                                        sel = lambda ap: ap.rearrange("p (a b) -> p a b", a=4)[:, 1:4:2, :]
                                    S.op("act", (lambda sb_=sb_, ptb=ptb, sel=sel: (lambda e: e.activation(
                                        out=sel(ptb[:]), in_=sel(ps[:, sb_, :]), func=AF.Exp, scale=0.125)))(),
                                        reads=[], writes=[B(sb_), ptk])
                                    S.op("dve" if bp < 4 else "pool", (lambda ptb=ptb, sel=sel: (lambda e: e.tensor_tensor(
                                        out=sel(ptb[:]), in0=sel(ptb[:]), in1=sel(mask4[:]), op=ALU.mult)))(),
                                        reads=["mask4"], writes=[ptk])
                                    st["pv"], st["ptb"], st["ptk"] = pv, ptb, ptk

                                def back(bp=bp, st=st, first=first, ab=ab, s=s, hl=hl, b0=b0, hp=hp, h=h):
                                    ptb, ptk = st["ptb"], st["ptk"]
                                    for sl_i, v_ap, o_ap, rkeys in st["pv"]:
                                        st_flag = first[0]
                                        first[0] = False
                                        S.op("pe", (lambda sl_i=sl_i, v_ap=v_ap, o_ap=o_ap, ptb=ptb, st_flag=st_flag: (lambda e: e.matmul(
                                            o_ap, lhsT=v_ap, rhs=ptb[:, sl_i * 128:(sl_i + 1) * 128], start=st_flag, stop=False)))(),
                                            reads=[ptk] + rkeys, writes=[B(ab)])
                                    if bp != 6:
                                        return
                                    for r in range(16):
                                        S.op("pe", (lambda r=r, s=s, ab=ab, hl=hl: (lambda e: e.matmul(
                                            ps[:, ab, r:512:16], lhsT=Vaug[2][:, r, hl, :], rhs=PT2[:, r, 32 * s:32 * (s + 1)],
                                            start=False, stop=(r == 15))))(),
                                            reads=[("pt2", r // 4), ("V", 2, r), ("vones", 2)], writes=[B(ab)])
                                    rdt = rd[ab]
                                    S.op("dve", (lambda ab=ab, rdt=rdt: (lambda e: e.reciprocal(out=rdt[:], in_=ps[64:128, ab, :])))(),
                                         reads=[], writes=[B(ab), ("rd", ab)])
                                    S.op("dve", (lambda ab=ab, rdt=rdt, b0=b0, hp=hp, s=s: (lambda e: e.tensor_tensor(
                                        out=obT[b0:b0 + 64, hp, s * 512:(s + 1) * 512], in0=ps[0:64, ab, :], in1=rdt[:], op=ALU.mult)))(),
                                        reads=[("rd", ab)], writes=[B(ab), ("obT", hp, s, h % 2)])
                                pipe(front, back)
                        flush()
                        if HEAD_BARRIER:
                            S.barrier()
            S.barrier()
        OBT_ALL = [("obT", hp, s, hh) for hp in range(4) for s in range(4) for hh in range(2)]
        if "obT" in debug:
            o = dbg_out("obT", [128, 4, S_TOK], BF16)
            S.dma("sp", lambda e, o=o: e.dma_start(out=o, in_=obT[:]), slot(), reads=OBT_ALL, final=True)

        XT_ALL = [("xT", i) for i in range(NT)]
        yaT_es = contextlib.ExitStack()
        yaT = sbt(yaT_es, "yaT", [128, 4, S_TOK], BF16)
        wring[:] = [sbt(yaT_es, "wringB%d" % i, [128, KC, 512], BF16) for i in range(4)]
        wr_state["n"] = 0
        with contextlib.ExitStack() as es2:
            vg = sbt(es2, "vg", [128, NT, 512], F32)
            lng = sbt(es2, "lng", [128, 512], F32)
            lnb = sbt(es2, "lnb", [128, 512], F32)
            wsf = sbt(es2, "wsf", [128, 8, 128], F32)
            WmT = sbt(es2, "wmT", [128, 8, 128], BF16)
            bsf = sbt(es2, "bsf", [2, 1024], F32)
            bsh = sbt(es2, "bsh", [2, 1024], BF16)
            bshf = sbt(es2, "bshf", [2, 1024], F32)
            bsl = sbt(es2, "bsl", [2, 1024], BF16)
            sqb = [sbt(es2, "sqb%d" % i, [128, 512], F32) for i in range(3)]
            tnb = [sbt(es2, "tnb%d" % i, [128, 512], F32) for i in range(3)]
            st6 = sbt(es2, "st6", [128, NT, 6], F32)
            mv = sbt(es2, "mv", [128, NT, 2], F32)
            xe = sbt(es2, "xeA", [128, NT], F32)
            lnt = [sbt(es2, "lnt%d" % i, [128, 512], F32) for i in range(2)]
            vln = [sbt(es2, "vln%d" % i, [128, 512], BF16) for i in range(2)]
            bu = load_wblk("u")
            bv = load_wblk("v")
            S.dma("sp", lambda e: e.dma_start(out=lng[:], in_=alng_d[0].partition_broadcast(128)), slot("c"), writes=["lng"])
            S.dma("sp", lambda e: e.dma_start(out=lnb[:], in_=alnb_d[0].partition_broadcast(128)), slot("c"), writes=["lnb"])
            S.dma("sp", lambda e: e.dma_start(out=wsf[:], in_=awsT_d), slot("c"), writes=["wsf"])
            S.dma("sp", lambda e: e.dma_start(out=bsf[0:1, :], in_=abs_d), slot("c"), writes=["bsf0"])
            S.dma("sp", lambda e: e.dma_start(out=bsf[1:2, :], in_=abs_d), slot("c"), writes=["bsf1"])
            S.op("dve", lambda e: e.tensor_tensor(out=WmT[:], in0=wsf[:],
                                                  in1=mown4[:].rearrange("p (a b) -> p a b", a=4)[:, 0:1, :].to_broadcast([128, 8, 128]),
                                                  op=ALU.mult), reads=["wsf", "mown4"], writes=["WmT"])
            S.op("dve", lambda e: e.tensor_copy(bsh[:], bsf[:]), reads=["bsf0", "bsf1"], writes=["bsh"])
            S.op("dve", lambda e: e.tensor_copy(bshf[:], bsh[:]), reads=["bsh"], writes=["bshf"])
            S.op("dve", lambda e: e.tensor_tensor(out=bshf[:], in0=bsf[:], in1=bshf[:], op=ALU.subtract), reads=["bshf"], writes=["bshf"])
            S.op("dve", lambda e: e.tensor_copy(bsl[:], bshf[:]), reads=["bshf"], writes=["bsl"])
            S.dma("sp", lambda e: e.dma_start(out=bsh[1:2, :], in_=bsl[1:2, :]), slot("c"), reads=["bsl", "bsh"], writes=["bsh"])

            grr = [0]

            def gelu_front(bk):
                grr[0] += 1
                idx = grr[0] % 3
                sq = sqb[idx]
                ks = ("sqb", idx)
                S.op("act", lambda e: e.activation(out=sq[:], in_=ps[:, bk, :], func=AF.Square), reads=[], writes=[B(bk), ks])
                S.op("pool", lambda e: e.tensor_scalar(out=sq[:], in0=sq[:], scalar1=0.044715, scalar2=1.0,
                                                       op0=ALU.mult, op1=ALU.add), reads=[], writes=[ks])
                S.op("dve", lambda e: e.tensor_tensor(out=sq[:], in0=sq[:], in1=ps[:, bk, :], op=ALU.mult),
                     reads=[], writes=[B(bk), ks])
                return bk, idx

            def gelu_back(st, out_ap, wkeys, after=None):
                bk, idx = st
                sq, tn = sqb[idx], tnb[idx]
                ks, kt = ("sqb", idx), ("tnb", idx)
                S.op("act", lambda e: e.activation(out=tn[:], in_=sq[:], func=AF.Tanh, scale=GELU_C), reads=[ks], writes=[kt])
                S.op("dve", lambda e: e.scalar_tensor_tensor(out=out_ap, in0=tn[:], scalar=1.0, in1=ps[:, bk, :],
                                                             op0=ALU.add, op1=ALU.mult), reads=[kt], writes=[B(bk)] + wkeys)
                if after is not None:
                    after()

            gpend = []

            def gelu_pipe(bk, out_ap, wkeys, after=None):
                st = gelu_front(bk)
                if gpend:
                    gelu_back(*gpend.pop(0))
                gpend.append((st, out_ap, wkeys, after))

            ALLB = [0, 1, 2, 3, 4, 5, 6, 7]
            for fc in range(4):
                for sp in range(4):
                    bk = nb_(ALLB)
                    for kc in range(KC):
                        S.op("pe", (lambda bk=bk, kc=kc, fc=fc, sp=sp, wt=WB[bu]: (lambda e: e.matmul(
                            ps[:, bk, :], lhsT=wt[:, kc, fc * 128:(fc + 1) * 128], rhs=xT[:, kc, sp * 512:(sp + 1) * 512],
                            start=(kc == 0), stop=(kc == KC - 1))))(),
                            reads=[("xT", 4 * sp + j) for j in range(4)] + [("wr", bu)], writes=[B(bk)])
                    gelu_pipe(bk, yaT[:, fc, sp * 512:(sp + 1) * 512], [("yaT", fc, 4 * sp + j) for j in range(4)])
            for i in range(NT):
                bk = nb_(ALLB)
                for kc in range(KC):
                    S.op("pe", (lambda bk=bk, kc=kc, i=i, wt=WB[bv]: (lambda e: e.matmul(
                        ps[:, bk, :], lhsT=xT[:, kc, i * 128:(i + 1) * 128], rhs=wt[:, kc, 0:512],
                        start=(kc == 0), stop=(kc == KC - 1))))(),
                        reads=[("xT", i), ("wr", bv)], writes=[B(bk)])

                def stats(i=i):
                    S.op("dve", (lambda i=i: (lambda e: e.bn_stats(out=st6[:, i, :], in_=vg[:, i, :])))(), reads=[("vg", i)], writes=[("st6", i)])
                    S.op("dve", (lambda i=i: (lambda e: e.bn_aggr(out=mv[:, i, :], in_=st6[:, i, :])))(), reads=[("st6", i)], writes=[("mvA", i)])
                gelu_pipe(bk, vg[:, i, :], [("vg", i)], stats)
            while gpend:
                gelu_back(*gpend.pop(0))
            S.op("dve", lambda e: e.tensor_scalar(out=xe[:], in0=mv[:, :, 1], scalar1=4.0 * LN_EPS, scalar2=None, op0=ALU.add),
                 reads=[("mvA", i) for i in range(NT)], writes=[("rsx", "A")])
            rstd, krs, _ = rsqrt_batch(es2, "A", xe[:], NT)

            def ln_tile(i):
                lt = lnt[i % 2]
                vl = vln[i % 2]
                S.op("dve", (lambda i=i, lt=lt: (lambda e: e.scalar_tensor_tensor(
                    out=lt[:], in0=vg[:, i, :], scalar=mv[:, i, 0:1], in1=lng[:], op0=ALU.subtract, op1=ALU.mult)))(),
                    reads=[("vg", i), ("mvA", i), "lng"], writes=[("lnt", i % 2)])
                S.op("dve", (lambda i=i, lt=lt, vl=vl: (lambda e: e.scalar_tensor_tensor(
                    out=vl[:], in0=lt[:], scalar=rstd[:, i:i + 1], in1=lnb[:], op0=ALU.mult, op1=ALU.add)))(),
                    reads=["lnb", ("lnt", i % 2), krs], writes=[("vln", i % 2)])

            ln_tile(0)
            for i in range(NT):
                vl = vln[i % 2]
                bk = nb_(ALLB)
                for cc in range(4):
                    for gg in range(2):
                        g = 2 * cc + gg
                        o_ap = ps[gg * 64:(gg + 1) * 64, bk, cc * 128:(cc + 1) * 128]
                        S.op("pe", (lambda o_ap=o_ap, g=g, vl=vl: (lambda e: e.matmul(
                            o_ap, lhsT=vl[:, g * 64:(g + 1) * 64], rhs=WmT[:, g, :], start=True, stop=False)))(),
                            reads=[("vln", i % 2), "WmT"], writes=[B(bk)])
                        S.op("pe", (lambda o_ap=o_ap, g=g: (lambda e: e.matmul(
                            o_ap, lhsT=onesb[0:2, 0:64], rhs=bsh[0:2, g * 128:(g + 1) * 128], start=False, stop=True)))(),
                            reads=["bsh", "onesb"], writes=[B(bk)])
                if i + 1 < NT:
                    ln_tile(i + 1)
                ya_v = yaT[:, :, i * 128:(i + 1) * 128]
                S.op("dve", (lambda bk=bk, ya_v=ya_v: (lambda e: e.scalar_tensor_tensor(
                    out=ya_v, in0=ps[:, bk, :].rearrange("p (c t) -> p c t", c=4), scalar=0.5, in1=ya_v, op0=ALU.mult, op1=ALU.mult)))(),
                    reads=[], writes=[B(bk)] + [("yaT", fc, i) for fc in range(4)])
            S.barrier()
        YAT_ALL = [("yaT", fc, i) for fc in range(4) for i in range(NT)]
        if "yaT" in debug:
            o = dbg_out("yaT", [128, 4, S_TOK], BF16)
            S.dma("sp", lambda e, o=o: e.dma_start(out=o, in_=yaT[:]), slot(), reads=YAT_ALL, final=True)

        with contextlib.ExitStack() as es3:
            wa = sbt(es3, "wa", [128, 4, D], BF16)
            wb = sbt(es3, "wb", [128, 4, D], BF16)
            ta = [sbt(es3, "ta%d" % i, [128, 512], BF16) for i in range(2)]
            tb = [sbt(es3, "tb%d" % i, [128, 512], BF16) for i in range(2)]
            m1 = [sbt(es3, "m1_%d" % i, [128, 512], F32) for i in range(2)]
            m2 = [sbt(es3, "m2_%d" % i, [128, 512], F32) for i in range(2)]
            S.dma("pool", lambda e: e.dma_start(out=wa[:], in_=wa_d.rearrange("(c p) d -> p c d", p=128)), slot("c"), writes=["wa"])
            S.dma("pool", lambda e: e.dma_start(out=wb[:], in_=wb_d.rearrange("(c p) d -> p c d", p=128)), slot("c"), writes=["wb"])
            gblk = {}
            gblk[0] = load_wblk(("g", 0))
            gblk[1] = load_wblk(("g", 1))
            gblk[2] = load_wblk(("g", 2))
            gblk[3] = load_wblk(("g", 3))
            it_ = 0
            for dc in range(KC):
                ga_i = gblk[2 * (dc // 4)]
                gb_i = gblk[2 * (dc // 4) + 1]
                for sp in range(4):
                    it_ += 1
                    par = it_ % 2
                    bA, bB, bC, bD = [4 * par + j for j in range(4)]
                    tok = slice(sp * 512, (sp + 1) * 512)
                    xk = [("xT", 4 * sp + j) for j in range(4)]
                    for cc in range(4):
                        S.op("pe", (lambda cc=cc, bA=bA, dc=dc, tok=tok: (lambda e: e.matmul(
                            ps[:, bA, :], lhsT=wa[:, cc, dc * 128:(dc + 1) * 128], rhs=yaT[:, cc, tok], start=(cc == 0), stop=(cc == 3))))(),
                            reads=["wa"] + [("yaT", cc, 4 * sp + j) for j in range(4)], writes=[B(bA)])
                    for cc in range(4):
                        S.op("pe", (lambda cc=cc, bB=bB, dc=dc, tok=tok: (lambda e: e.matmul(
                            ps[:, bB, :], lhsT=wb[:, cc, dc * 128:(dc + 1) * 128], rhs=obT[:, cc, tok], start=(cc == 0), stop=(cc == 3))))(),
                            reads=["wb"] + [("obT", cc, sp, 0), ("obT", cc, sp, 1)], writes=[B(bB)])
                    for (bX, gi) in ((bC, ga_i), (bD, gb_i)):
                        for kc in range(KC):
                            S.op("pe", (lambda kc=kc, bX=bX, wt=WB[gi], dc=dc, tok=tok: (lambda e: e.matmul(
                                ps[:, bX, :], lhsT=wt[:, kc, (dc % 4) * 128:(dc % 4 + 1) * 128], rhs=xT[:, kc, tok],
                                start=(kc == 0), stop=(kc == KC - 1))))(),
                                reads=xk + [("wr", gi)], writes=[B(bX)])
                    S.op("act", (lambda bC=bC, par=par: (lambda e: e.activation(out=ta[par][:], in_=ps[:, bC, :], func=AF.Tanh, scale=0.5)))(),
                         reads=[], writes=[B(bC), ("ta", par)])
                    S.op("act", (lambda bD=bD, par=par: (lambda e: e.activation(out=tb[par][:], in_=ps[:, bD, :], func=AF.Tanh, scale=0.5)))(),
                         reads=[], writes=[B(bD), ("tb", par)])
                    S.op("dve", (lambda bA=bA, par=par: (lambda e: e.scalar_tensor_tensor(
                        out=m1[par][:], in0=ta[par][:], scalar=1.0, in1=ps[:, bA, :], op0=ALU.add, op1=ALU.mult)))(),
                        reads=[("ta", par)], writes=[B(bA), ("m1", par)])
                    S.op("dve", (lambda bB=bB, par=par: (lambda e: e.scalar_tensor_tensor(
                        out=m2[par][:], in0=tb[par][:], scalar=1.0, in1=ps[:, bB, :], op0=ALU.add, op1=ALU.mult)))(),
                        reads=[("tb", par)], writes=[B(bB), ("m2", par)])
                    S.op("pool", (lambda par=par, dc=dc, tok=tok: (lambda e: e.tensor_tensor(
                        out=mixinT[:, dc, tok], in0=m1[par][:], in1=m2[par][:], op=ALU.add)))(),
                        reads=[("m1", par), ("m2", par)], writes=[("mix", dc, 4 * sp + j) for j in range(4)])
            S.barrier()
        yaT_es.close()
        mx_es.close()
        if "mixinT" in debug:
            o = dbg_out("mixinT", [128, KC, S_TOK], BF16)
            S.dma("sp", lambda e, o=o: e.dma_start(out=o, in_=mixinT[:]), slot(),
                  reads=[("mix", dc, i) for dc in range(KC) for i in range(NT)], final=True)

        p5_es = contextlib.ExitStack()
        x1b = sbt(p5_es, "x1b", [128, NT, D], BF16)
        logit = sbt(p5_es, "logit", [128, NT, 36], F32)
        G4 = 2
        with contextlib.ExitStack() as es4:
            wo = sbt(es4, "wo", [128, KC, D], BF16)
            wpg = sbt(es4, "wpg", [128, KC, D], BF16)
            wple = sbt(es4, "wple", [128, 2, D], BF16)
            wr = sbt(es4, "wr", [128, KC, 36], F32)
            wrh = sbt(es4, "wrh", [128, KC, 36], BF16)
            wrhf = sbt(es4, "wrhf", [128, KC, 36], F32)
            wrl = sbt(es4, "wrl", [128, KC, 36], BF16)
            x1lo = [sbt(es4, "x1lo%d" % i, [128, D], BF16) for i in range(3)]
            brb = sbt(es4, "brb", [128, 36], F32)
            l1g = sbt(es4, "l1g", [128, D], F32)
            l1b = sbt(es4, "l1b", [128, D], F32)
            xres = [sbt(es4, "xres%d" % i, [128, D], F32) for i in range(2)]
            rg = [sbt(es4, "rg%d" % q, [128, G4, D], F32) for q in range(2)]
            st12 = [sbt(es4, "st12_%d" % q, [128, G4, 12], F32) for q in range(2)]
            mv1 = [sbt(es4, "mv1_%d" % q, [128, G4, 2], F32) for q in range(2)]
            xe1 = [sbt(es4, "xe1_%d" % q, [128, G4], F32) for q in range(2)]
            rsq1 = [(sbt(es4, "rs_tfL1_%d" % q, [128, G4], F32), sbt(es4, "rs_yiL1_%d" % q, [128, G4], I32),
                     sbt(es4, "rs_aL1_%d" % q, [128, G4], F32)) for q in range(2)]
            x1f = [sbt(es4, "x1f%d" % i, [128, D], F32) for i in range(3)]
            x1Tf = [sbt(es4, "x1Tl%d" % i, [128, KC, 128], BF16) for i in range(2)]
            x1Tb = [sbt(es4, "x1Tb%d" % i, [128, KC, 128], BF16) for i in range(2)]
            pst = [sbt(es4, "pst%d" % i, [128, 256], BF16) for i in range(2)]
            pT = [sbt(es4, "pT%d" % i, [128, 2, 128], BF16) for i in range(2)]
            tp = [sbt(es4, "tp%d" % i, [128, D], BF16) for i in range(2)]
            ple2 = [sbt(es4, "ple2_%d" % i, [128, D], F32) for i in range(2)]
            S.dma("pool", lambda e: e.dma_start(out=wo[:], in_=wo_d.rearrange("(c p) d -> p c d", p=128)), slot("c"), writes=["wo"])
            S.dma("pool", lambda e: e.dma_start(out=wpg[:], in_=wpg_d.rearrange("(c p) d -> p c d", p=128)), slot("c"), writes=["wpg"])
            S.dma("pool", lambda e: e.dma_start(out=wple[:], in_=wple_d.rearrange("(c p) d -> p c d", p=128)), slot("c"), writes=["wple"])
            S.dma("sp", lambda e: e.dma_start(out=wr[:], in_=wr_d.rearrange("(c p) d -> p c d", p=128)), slot("c"), writes=["wr_"])
            S.dma("sp", lambda e: e.dma_start(out=brb[:], in_=br_d[0].partition_broadcast(128)), slot("c"), writes=["brb"])
            S.op("dve", lambda e: e.tensor_copy(wrh[:], wr[:]), reads=["wr_"], writes=["wrh"])
            S.op("dve", lambda e: e.tensor_copy(wrhf[:], wrh[:]), reads=["wrh"], writes=["wrhf"])
            S.op("dve", lambda e: e.tensor_tensor(out=wrl[:], in0=wr[:], in1=wrhf[:], op=ALU.subtract), reads=["wr_", "wrhf"], writes=["wrl"])
            S.dma("sp", lambda e: e.dma_start(out=l1g[:], in_=ln1g_d[0].partition_broadcast(128)), slot("c"), writes=["l1g"])
            S.dma("sp", lambda e: e.dma_start(out=l1b[:], in_=ln1b_d[0].partition_broadcast(128)), slot("c"), writes=["l1b"])
            NG4 = NT // G4

            def load_xres(i):
                xr = xres[i % 2]
                S.dma("sp", (lambda i=i, xr=xr: (lambda e: e.dma_start(out=xr[:], in_=x_d[i * 128:(i + 1) * 128, :])))(),
                      "xres%d" % (i % 2), writes=[("xres", i % 2)])

            def part1_tile(g, gi):
                gp = g % 2
                i = g * G4 + gi
                xr = xres[i % 2]
                if i == 0:
                    load_xres(0)
                if i + 1 < NT:
                    load_xres(i + 1)
                S.op("act", (lambda xr=xr: (lambda e: e.activation(out=xr[:], in_=xr[:], func=AF.Identity, scale=ALPHA)))(),
                     reads=[], writes=[("xres", i % 2)])
                for half in range(2):
                    bk = half
                    for dc in range(KC):
                        S.op("pe", (lambda bk=bk, dc=dc, i=i, half=half: (lambda e: e.matmul(
                            ps[:, bk, :], lhsT=mixinT[:, dc, i * 128:(i + 1) * 128], rhs=wo[:, dc, half * 512:(half + 1) * 512],
                            start=(dc == 0), stop=(dc == KC - 1))))(),
                            reads=[("mix", dc, i), "wo"], writes=[B(bk)])
                    S.op("dve", (lambda bk=bk, gi=gi, gp=gp, half=half, xr=xr: (lambda e: e.scalar_tensor_tensor(
                        out=rg[gp][:, gi, half * 512:(half + 1) * 512], in0=ps[:, bk, :], scalar=0.5,
                        in1=xr[:, half * 512:(half + 1) * 512], op0=ALU.mult, op1=ALU.add)))(),
                        reads=[("xres", i % 2)], writes=[B(bk), ("rg", gp, gi, half)])
                    S.op("dve", (lambda gi=gi, gp=gp, half=half: (lambda e: e.bn_stats(
                        out=st12[gp][:, gi, half * 6:(half + 1) * 6], in_=rg[gp][:, gi, half * 512:(half + 1) * 512])))(),
                        reads=[("rg", gp, gi, half)], writes=[("st12", gp, gi, half)])
                S.op("dve", (lambda gi=gi, gp=gp: (lambda e: e.bn_aggr(out=mv1[gp][:, gi, :], in_=st12[gp][:, gi, :])))(),
                     reads=[("st12", gp, gi, 0), ("st12", gp, gi, 1)], writes=[("mv1", gp, gi)])

            def rsqrt_grp(g):
                gp = g % 2
                tag = "L1_%d" % g
                xe_ = xe1[gp]
                tf_, yi_, a__ = rsq1[gp]
                kx, ky, ka, kt = ("rsx", "L1", gp), ("rsy", "L1", gp), ("rsa", "L1", gp), ("rst", "L1", gp)
                S.op("dve", lambda e: e.tensor_scalar(out=xe_[:], in0=mv1[gp][:, :, 1], scalar1=LN_EPS, scalar2=None, op0=ALU.add),
                     reads=[("mv1", gp, gi) for gi in range(G4)], writes=[kx])
                S.op("dve", lambda e: e.tensor_copy(tf_[:], xe_[:].bitcast(I32)), reads=[kx], writes=[kt])
                S.op("dve", lambda e: e.tensor_scalar(out=tf_[:], in0=tf_[:], scalar1=-0.5, scalar2=1597463007.0,
                                                      op0=ALU.mult, op1=ALU.add), reads=[kt], writes=[kt])
                S.op("dve", lambda e: e.tensor_copy(yi_[:], tf_[:]), reads=[kt], writes=[ky])
                y1 = yi_[:].bitcast(F32)
                for _ in range(2):
                    S.op("dve", lambda e: e.tensor_tensor(out=a__[:], in0=y1, in1=y1, op=ALU.mult), reads=[ky], writes=[ka])
                    S.op("dve", lambda e: e.tensor_tensor(out=a__[:], in0=a__[:], in1=xe_[:], op=ALU.mult), reads=[ka, kx], writes=[ka])
                    S.op("dve", lambda e: e.tensor_scalar(out=a__[:], in0=a__[:], scalar1=-0.5, scalar2=1.5,
                                                          op0=ALU.mult, op1=ALU.add), reads=[ka], writes=[ka])
                    S.op("dve", lambda e: e.tensor_tensor(out=y1, in0=y1, in1=a__[:], op=ALU.mult), reads=[ka, ky], writes=[ky])

            def ln_norm(g, gi):
                gp = g % 2
                i = g * G4 + gi
                y1 = rsq1[gp][1][:].bitcast(F32)
                ky = ("rsy", "L1", gp)
                xf = x1f[i % 3]
                kxf = ("x1f", i % 3)
                S.op("dve", (lambda gi=gi, gp=gp, xf=xf: (lambda e: e.scalar_tensor_tensor(
                    out=xf[:], in0=rg[gp][:, gi, :], scalar=mv1[gp][:, gi, 0:1], in1=l1g[:], op0=ALU.subtract, op1=ALU.mult)))(),
                    reads=[("rg", gp, gi, 0), ("rg", gp, gi, 1), ("mv1", gp, gi), "l1g"], writes=[kxf])
                S.op("dve", (lambda gi=gi, xf=xf, y1=y1: (lambda e: e.scalar_tensor_tensor(
                    out=xf[:], in0=xf[:], scalar=y1[:, gi:gi + 1], in1=l1b[:], op0=ALU.mult, op1=ALU.add)))(),
                    reads=[ky, "l1b"], writes=[kxf])
                S.op("act", (lambda xf=xf, i=i: (lambda e: e.activation(out=x1b[:, i, :], in_=xf[:], func=AF.Copy)))(),
                     reads=[kxf], writes=[("x1b", i)])

            def ln_lo(g, gi):
                i = g * G4 + gi
                xf = x1f[i % 3]
                xlo = x1lo[i % 3]
                S.op("dve", (lambda xf=xf, xlo=xlo, i=i: (lambda e: e.tensor_tensor(out=xlo[:], in0=xf[:], in1=x1b[:, i, :], op=ALU.subtract)))(),
                     reads=[("x1f", i % 3), ("x1b", i)], writes=[("x1lo", i % 3)])

            def load_p(i):
                pp = pst[i % 2]
                S.dma("pool", (lambda i=i, pp=pp: (lambda e: e.dma_start(out=pp[:], in_=p_d[i * 128:(i + 1) * 128, :])))(),
                      "pst%d" % (i % 2), writes=[("pst", i % 2)])

            def part2_tile(g, gi, mid=None):
                i = g * G4 + gi
                xf = x1f[i % 3]
                kxf = ("x1f", i % 3)
                xlo = x1lo[i % 3]
                if i == 0:
                    load_p(0)
                if i + 1 < NT:
                    load_p(i + 1)
                if True:
                    if True:
                        xtf, xtb = x1Tf[i % 2], x1Tb[i % 2]
                        psh_ = ps[:, 2, :].bitcast(BF16)
                        psl_ = ps[:, 3, :].bitcast(BF16)
                        for kc in range(KC):
                            S.op("pe", (lambda kc=kc, i=i, psh_=psh_: (lambda e: e.transpose(
                                out=psh_[:, kc * 128:(kc + 1) * 128], in_=x1b[:, i, kc * 128:(kc + 1) * 128], identity=identb[:])))(),
                                reads=[("x1b", i), "identb"], writes=[B(2)])
                        S.op("dve", (lambda xtb=xtb, psh_=psh_: (lambda e: e.tensor_copy(xtb[:], psh_.rearrange("p (j t) -> p j t", j=KC))))(),
                             reads=[], writes=[B(2), ("x1Tb", i % 2)])
                        for kc in range(KC):
                            S.op("pe", (lambda kc=kc, xlo=xlo, psl_=psl_: (lambda e: e.transpose(
                                out=psl_[:, kc * 128:(kc + 1) * 128], in_=xlo[:, kc * 128:(kc + 1) * 128], identity=identb[:])))(),
                                reads=[("x1lo", i % 3), "identb"], writes=[B(3)])
                        S.op("act", (lambda xtf=xtf, psl_=psl_: (lambda e: e.activation(out=xtf[:], in_=psl_.rearrange("p (j t) -> p j t", j=KC), func=AF.Copy)))(),
                             reads=[], writes=[B(3), ("x1Tl", i % 2)])
                        passes = [(xtb, wrh), (xtf, wrh), (xtb, wrl)]
                        for pi, (xa, wa_) in enumerate(passes):
                            for kc in range(KC):
                                S.op("pe", (lambda kc=kc, xa=xa, wa_=wa_, pi=pi: (lambda e: e.matmul(
                                    ps[:, 4, 0:36], lhsT=xa[:, kc, :], rhs=wa_[:, kc, :], start=(pi == 0 and kc == 0), stop=(pi == 2 and kc == KC - 1))))(),
                                    reads=[("x1Tb", i % 2), ("x1Tl", i % 2), "wrh", "wrl"], writes=[B(4)])
                        S.op("dve", (lambda i=i: (lambda e: e.tensor_tensor(out=logit[:, i, :], in0=ps[:, 4, 0:36], in1=brb[:], op=ALU.add)))(),
                             reads=["brb"], writes=[B(4), ("logit", i)])
                        pp = pst[i % 2]
                        psp_ = ps[:, 4, :].bitcast(BF16)
                        for c in range(2):
                            S.op("pe", (lambda c=c, pp=pp, psp_=psp_: (lambda e: e.transpose(
                                out=psp_[:, 512 + c * 128:512 + (c + 1) * 128], in_=pp[:, c * 128:(c + 1) * 128], identity=identb[:])))(),
                                reads=[("pst", i % 2), "identb"], writes=[B(4)])
                        S.op("act", (lambda i=i, psp_=psp_: (lambda e: e.activation(
                            out=pT[i % 2][:], in_=psp_[:, 512:768].rearrange("p (c t) -> p c t", c=2), func=AF.Copy)))(),
                            reads=[], writes=[B(4), ("pT", i % 2)])
                        if mid is not None:
                            mid()
                        for half in range(2):
                            bg = 5
                            bl = 6 + half
                            for kc in range(KC):
                                S.op("pe", (lambda kc=kc, half=half, xtb=xtb: (lambda e: e.matmul(
                                    ps[:, 5, :], lhsT=xtb[:, kc, :], rhs=wpg[:, kc, half * 512:(half + 1) * 512],
                                    start=(kc == 0), stop=(kc == KC - 1))))(),
                                    reads=[("x1Tb", i % 2), "wpg"], writes=[B(5)])
                            S.op("act", (lambda half=half, i=i: (lambda e: e.activation(
                                out=tp[i % 2][:, half * 512:(half + 1) * 512], in_=ps[:, 5, :], func=AF.Tanh, scale=0.5)))(),
                                reads=[], writes=[B(5), ("tp", i % 2, half)])
                            for c in range(2):
                                S.op("pe", (lambda c=c, half=half, bl=bl, i=i: (lambda e: e.matmul(
                                    ps[:, bl, :], lhsT=pT[i % 2][:, c, :], rhs=wple[:, c, half * 512:(half + 1) * 512],
                                    start=(c == 0), stop=(c == 1))))(),
                                    reads=[("pT", i % 2), "wple"], writes=[B(bl)])
                            S.op("dve", (lambda half=half, bl=bl, i=i: (lambda e: e.scalar_tensor_tensor(
                                out=ple2[i % 2][:, half * 512:(half + 1) * 512], in0=tp[i % 2][:, half * 512:(half + 1) * 512],
                                scalar=1.0, in1=ps[:, bl, :], op0=ALU.add, op1=ALU.mult)))(),
                                reads=[("tp", i % 2, half)], writes=[B(bl), ("ple2", i % 2, half)])
                        S.op("act", (lambda xf=xf: (lambda e: e.activation(out=xf[:], in_=xf[:], func=AF.Identity, scale=ALPHA)))(),
                             reads=[], writes=[kxf])
                        S.op("pool", (lambda i=i: (lambda e: e.tensor_scalar(
                            out=ple2[i % 2][:], in0=ple2[i % 2][:], scalar1=0.5, scalar2=0.0, op0=ALU.mult, op1=ALU.add)))(),
                            reads=[], writes=[("ple2", i % 2, 0), ("ple2", i % 2, 1)])
                        S.op("pool", (lambda xf=xf, i=i: (lambda e: e.tensor_tensor(
                            out=ple2[i % 2][:], in0=ple2[i % 2][:], in1=xf[:], op=ALU.add)))(),
                            reads=[kxf], writes=[("ple2", i % 2, 0), ("ple2", i % 2, 1)])
                        S.dma("sp", (lambda i=i: (lambda e: e.dma_start(out=base_d[i * 128:(i + 1) * 128, :], in_=ple2[i % 2][:])))(),
                              "base%d" % (i % 2), reads=[("ple2", i % 2, 0), ("ple2", i % 2, 1)], writes=[("base", i)])

            def ln_grp(g):
                rsqrt_grp(g)
                for gi in range(G4):
                    ln_norm(g, gi)
                for gi in range(G4):
                    ln_lo(g, gi)

            for gi in range(G4):
                part1_tile(0, gi)
            ln_grp(0)
            for g in range(NG4):
                for gi in range(G4):
                    mid = None
                    if g + 1 < NG4:
                        part1_tile(g + 1, gi)
                        if gi == G4 - 1:
                            mid = (lambda g=g: ln_grp(g + 1))
                    part2_tile(g, gi, mid)
            S.barrier()
        if "logit" in debug:
            o = dbg_out("logit", [128, NT, 36])
            S.dma("sp", lambda e, o=o: e.dma_start(out=o, in_=logit[:]), slot(), reads=[("logit", i) for i in range(NT)], final=True)
        if "x1b" in debug:
            o = dbg_out("x1b", [128, NT, D], BF16)
            S.dma("sp", lambda e, o=o: e.dma_start(out=o, in_=x1b[:]), slot(), reads=[("x1b", i) for i in range(NT)], final=True)

        with contextlib.ExitStack() as es5:
            LG = [("logit", i) for i in range(NT)]
            gmax = sbt(es5, "gmax", [128, NT], F32)
            gd = sbt(es5, "gd", [128, NT, 4], F32)
            gm = sbt(es5, "gm", [128, NT, 4], F32)
            gsum = sbt(es5, "gsum", [128, NT], F32)
            gp = sbt(es5, "gp", [128, NT], F32)
            selm = sbt(es5, "selm", [128, NT, 4, 8], F32)
            sel = sbt(es5, "sel", [128, NT, 8], F32)
            m8 = sbt(es5, "m8", [128, NT, 8], F32)
            e1 = sbt(es5, "e1", [128, NT, 8], F32)
            e2 = sbt(es5, "e2", [128, NT, 8], F32)
            dd = sbt(es5, "dd", [128, NT], F32)
            w1 = sbt(es5, "w1", [128, NT], F32)
            A1 = sbt(es5, "A1", [128, NT, 32], F32)
            A2 = sbt(es5, "A2", [128, NT, 32], F32)
            Ab = sbt(es5, "Ab", [128, NT, 32], BF16)
            cnt = sbt(es5, "cnt", [128, 32], F32)
            cnti = sbt(es5, "cnti", [128, 32], I32)
            ntf = sbt(es5, "ntf", [128, 32], F32)
            onesf = sbt(es5, "onesf", [128, 32], F32)
            cum = sbt(es5, "cum", [128, 32], F32)
            bas = sbt(es5, "bas", [128, 32], F32)
            rk = sbt(es5, "rk", [128, NT, 32], F32)
            tmpA = sbt(es5, "tmpA", [128, NT, 32], F32)
            slf = sbt(es5, "slf", [128, NT, 2], F32)
            cmp = sbt(es5, "cmp", [128, NTILE_E, 32], F32)
            tef = sbt(es5, "tef", [128, NTILE_E], F32)
            gl = logit[:, :, 0:4]
            el = logit[:, :, 4:36].rearrange("p n (g i) -> p n g i", g=4)
            V = "dve"
            S.op(V, lambda e: e.tensor_reduce(out=gmax[:], in_=gl, axis=AX.X, op=ALU.max), reads=LG, writes=["gmax"])
            S.op(V, lambda e: e.tensor_tensor(out=gd[:], in0=gl, in1=gmax[:].unsqueeze(2).to_broadcast([128, NT, 4]), op=ALU.subtract),
                 reads=LG + ["gmax"], writes=["gd"])
            S.op(V, lambda e: e.tensor_scalar(out=gm[:], in0=gd[:], scalar1=0.0, scalar2=None, op0=ALU.is_ge), reads=["gd"], writes=["gm"])
            S.op("act", lambda e: e.activation(out=gd[:], in_=gd[:], func=AF.Exp), reads=[], writes=["gd"])
            S.op(V, lambda e: e.tensor_reduce(out=gsum[:], in_=gd[:], axis=AX.X, op=ALU.add), reads=["gd"], writes=["gsum"])
            S.op(V, lambda e: e.reciprocal(out=gp[:], in_=gsum[:]), reads=["gsum"], writes=["gp"])
            S.op(V, lambda e: e.tensor_tensor(out=selm[:], in0=el, in1=gm[:].unsqueeze(3).to_broadcast([128, NT, 4, 8]), op=ALU.mult),
                 reads=LG + ["gm"], writes=["selm"])
            S.op(V, lambda e: e.tensor_reduce(out=sel[:], in_=selm[:].rearrange("p n g i -> p n i g"), axis=AX.X, op=ALU.add),
                 reads=["selm"], writes=["sel"])
            for i in range(NT):
                S.op(V, (lambda i=i: (lambda e: e.max(out=m8[:, i, :], in_=sel[:, i, :])))(), reads=["sel"], writes=[("m8", i)])
            M8 = [("m8", i) for i in range(NT)]
            S.op(V, lambda e: e.tensor_tensor(out=e1[:], in0=sel[:], in1=m8[:, :, 0:1].to_broadcast([128, NT, 8]), op=ALU.is_equal),
                 reads=["sel"] + M8, writes=["e1"])
            S.op(V, lambda e: e.tensor_tensor(out=e2[:], in0=sel[:], in1=m8[:, :, 1:2].to_broadcast([128, NT, 8]), op=ALU.is_equal),
                 reads=["sel"] + M8, writes=["e2"])
            S.op(V, lambda e: e.tensor_tensor(out=dd[:], in0=m8[:, :, 1], in1=m8[:, :, 0], op=ALU.subtract), reads=M8, writes=["dd"])
            S.op("act", lambda e: e.activation(out=dd[:], in_=dd[:], func=AF.Exp), reads=[], writes=["dd"])
            S.op(V, lambda e: e.tensor_scalar(out=w1[:], in0=dd[:], scalar1=1.0, scalar2=None, op0=ALU.add), reads=["dd"], writes=["w1"])
            S.op(V, lambda e: e.reciprocal(out=w1[:], in_=w1[:]), reads=[], writes=["w1"])
            S.op(V, lambda e: e.tensor_tensor(out=cw[:, :, 0], in0=w1[:], in1=gp[:], op=ALU.mult), reads=["w1", "gp"], writes=["cw0"])
            S.op(V, lambda e: e.tensor_tensor(out=dd[:], in0=dd[:], in1=w1[:], op=ALU.mult), reads=["w1"], writes=["dd"])
            S.op(V, lambda e: e.tensor_tensor(out=cw[:, :, 1], in0=dd[:], in1=gp[:], op=ALU.mult), reads=["dd", "gp"], writes=["cw1"])
            for (Ax, ex, nm) in ((A1, e1, "A1"), (A2, e2, "A2")):
                S.op(V, (lambda Ax=Ax, ex=ex: (lambda e: e.tensor_tensor(
                    out=Ax[:].rearrange("p n (g i) -> p n g i", g=4), in0=gm[:].unsqueeze(3).to_broadcast([128, NT, 4, 8]),
                    in1=ex[:].unsqueeze(2).to_broadcast([128, NT, 4, 8]), op=ALU.mult)))(),
                    reads=["gm", "e1", "e2"], writes=[nm])
            S.op(V, lambda e: e.tensor_tensor(out=Ab[:], in0=A1[:], in1=A2[:], op=ALU.add), reads=["A1", "A2"], writes=["Ab"])
            for i in range(NT):
                o_ap = ps[:, 0, i * 32:(i + 1) * 32]
                S.op("pe", (lambda i=i, o_ap=o_ap: (lambda e: e.matmul(o_ap, lhsT=lstr[:], rhs=Ab[:, i, :], start=True, stop=(i == 0))))(),
                     reads=["Ab", "lstr"], writes=[B(0)])
                for i2 in range(i):
                    S.op("pe", (lambda i2=i2, o_ap=o_ap, i=i: (lambda e: e.matmul(o_ap, lhsT=onesb[:], rhs=Ab[:, i2, :], start=False, stop=(i2 == i - 1))))(),
                         reads=["Ab", "onesb"], writes=[B(0)])
            for i in range(NT):
                S.op("pe", (lambda i=i: (lambda e: e.matmul(ps[:, 1, 0:32], lhsT=onesb[:], rhs=Ab[:, i, :], start=(i == 0), stop=(i == NT - 1))))(),
                     reads=["Ab", "onesb"], writes=[B(1)])
            S.op(V, lambda e: e.tensor_copy(rk[:], ps[:, 0, :].rearrange("p (n e) -> p n e", n=NT)), reads=[], writes=[B(0), "rk"])
            S.op(V, lambda e: e.tensor_scalar(out=cnt[:], in0=ps[:, 1, 0:32], scalar1=127.0, scalar2=None, op0=ALU.add), reads=[], writes=[B(1), "cnt"])
            S.op(V, lambda e: e.tensor_copy(cnti[:], cnt[:]), reads=["cnt"], writes=["cnti"])
            S.op(V, lambda e: e.tensor_scalar(out=cnti[:], in0=cnti[:], scalar1=7, scalar2=None, op0=ALU.arith_shift_right), reads=[], writes=["cnti"])
            S.op(V, lambda e: e.tensor_copy(ntf[:], cnti[:]), reads=["cnti"], writes=["ntf"])
            S.op(V, lambda e: e.memset(onesf[:], 1.0), writes=["onesf"])
            S.op(V, lambda e: e.tensor_tensor_scan(out=cum[:], data0=onesf[:], data1=ntf[:], initial=0.0, op0=ALU.mult, op1=ALU.add),
                 reads=["onesf", "ntf"], writes=["cum"])
            S.op(V, lambda e: e.tensor_tensor(out=bas[:], in0=cum[:], in1=ntf[:], op=ALU.subtract), reads=["cum", "ntf"], writes=["bas"])
            S.op(V, lambda e: e.tensor_scalar(out=bas[:], in0=bas[:], scalar1=128.0, scalar2=None, op0=ALU.mult), reads=[], writes=["bas"])
            S.op(V, lambda e: e.tensor_tensor(out=rk[:], in0=rk[:], in1=bas[:].unsqueeze(1).to_broadcast([128, NT, 32]), op=ALU.add),
                 reads=["bas"], writes=["rk"])
            for j, Ax in ((0, A1), (1, A2)):
                S.op(V, (lambda Ax=Ax: (lambda e: e.tensor_tensor(out=tmpA[:], in0=rk[:], in1=Ax[:], op=ALU.mult)))(),
                     reads=["rk", "A1", "A2"], writes=["tmpA"])
                S.op(V, (lambda j=j: (lambda e: e.tensor_reduce(out=slf[:, :, j], in_=tmpA[:], axis=AX.X, op=ALU.add)))(),
                     reads=["tmpA"], writes=[("slf", j)])
            S.op(V, lambda e: e.tensor_copy(slotu[:], slf[:]), reads=[("slf", 0), ("slf", 1)], writes=["slotu"])
            S.op(V, lambda e: e.tensor_tensor(out=cmp[:], in0=cum[:].unsqueeze(1).to_broadcast([128, NTILE_E, 32]),
                                              in1=kidx[:].unsqueeze(2).to_broadcast([128, NTILE_E, 32]), op=ALU.is_le),
                 reads=["cum", "kidx"], writes=["cmp"])
            S.op(V, lambda e: e.tensor_reduce(out=tef[:], in_=cmp[:], axis=AX.X, op=ALU.add), reads=["cmp"], writes=["tef"])
            S.op(V, lambda e: e.tensor_scalar(out=tef[:], in0=tef[:], scalar1=128.0, scalar2=None, op0=ALU.mult), reads=[], writes=["tef"])
            S.op(V, lambda e: e.tensor_copy(rowst[:], tef[:]), reads=["tef"], writes=["rowst"])
            kidm = sbt(es5, "kidm", [128, NTILE_E], F32)
            tefp = sbt(es5, "tefp", [128, NTILE_E], F32)
            same = sbt(es5, "same", [128, NTILE_E], F32)
            S.op(V, lambda e: e.tensor_scalar(out=kidm[:], in0=kidx[:], scalar1=-1.0, scalar2=None, op0=ALU.add), reads=["kidx"], writes=["kidm"])
            S.op(V, lambda e: e.tensor_tensor(out=cmp[:], in0=cum[:].unsqueeze(1).to_broadcast([128, NTILE_E, 32]),
                                              in1=kidm[:].unsqueeze(2).to_broadcast([128, NTILE_E, 32]), op=ALU.is_le),
                 reads=["cum", "kidm"], writes=["cmp"])
            S.op(V, lambda e: e.tensor_reduce(out=tefp[:], in_=cmp[:], axis=AX.X, op=ALU.add), reads=["cmp"], writes=["tefp"])
            S.op(V, lambda e: e.tensor_scalar(out=tefp[:], in0=tefp[:], scalar1=128.0, scalar2=None, op0=ALU.mult), reads=[], writes=["tefp"])
            S.op(V, lambda e: e.memset(tefp[:, 0:1], -128.0), reads=[], writes=["tefp"])
            S.op(V, lambda e: e.tensor_tensor(out=same[:], in0=tef[:], in1=tefp[:], op=ALU.is_equal), reads=["tef", "tefp", "rowst"], writes=["same"])
            S.op(V, lambda e: e.tensor_scalar(out=tef[:], in0=tef[:], scalar1=pidx[:, 0:1], scalar2=None, op0=ALU.add),
                 reads=["pidx", "rowst", "same"], writes=["tef"])
            S.op(V, lambda e: e.scalar_tensor_tensor(out=tef[:], in0=same[:], scalar=8192.0, in1=tef[:], op0=ALU.mult, op1=ALU.add),
                 reads=["same"], writes=["tef"])
            S.op(V, lambda e: e.tensor_copy(idxw[:], tef[:]), reads=["tef"], writes=["idxw"])
            if "route" in debug:
                o1 = dbg_out("slotu", [128, NT, 2], U32)
                o2 = dbg_out("cw", [128, NT, 2])
                o3 = dbg_out("idxw", [128, NTILE_E], U32)
                S.dma("sp", lambda e, o1=o1: e.dma_start(out=o1, in_=slotu[:]), slot(), reads=["slotu"], final=True)
                S.dma("sp", lambda e, o2=o2: e.dma_start(out=o2, in_=cw[:]), slot(), reads=["cw0", "cw1"], final=True)
                S.dma("sp", lambda e, o3=o3: e.dma_start(out=o3, in_=idxw[:]), slot(), reads=["idxw"], final=True)
            for i in range(NT if STOP_AFTER != "route" else 0):
                for j in range(2):
                    S.dma("pool", (lambda i=i, j=j: (lambda e: e.indirect_dma_start(
                        out=xs_d[:, :], out_offset=bass.IndirectOffsetOnAxis(ap=slotu[:, i, j:j + 1], axis=0),
                        in_=x1b[:, i, :], in_offset=None, bounds_check=preg(e, NTILE_E * 128 - 1), oob_is_err=False)))(),
                        "scat", reads=["slotu", ("x1b", i)] + [("xsz", q) for q in range(NTILE_E)], writes=[("xs", i, j)])
            S.barrier()
        p5_es.close()
        mix_es.close()
        XS_ALL = [("xs", i, j) for i in range(NT) for j in range(2)]

        with contextlib.ExitStack() as es6:
            NST = 4
            NWB = 4
            NXB = 4
            NXT = 3
            wbuf = [sbt(es6, "wbuf%d" % i, [128, 6144], BF16) for i in range(NWB)]
            NCH = 3
            wst = [sbt(es6, "wst%d" % c, [128, 2048], F32) for c in range(NCH)]
            xsb = [sbt(es6, "xsb%d" % i, [128, D], BF16) for i in range(NXB)]
            xsT = [sbt(es6, "xsT%d" % i, [128, KC, 128], BF16) for i in range(NXT)]
            tg = [sbt(es6, "tg%d" % i, [128, 256], F32) for i in range(2)]
            hb = [sbt(es6, "hb%d" % i, [128, 256], BF16) for i in range(2)]
            hT = [sbt(es6, "hT%d" % i, [128, 2, 128], BF16) for i in range(2)]
            ysb = [sbt(es6, "ysb%d" % i, [128, D], F32) for i in range(2)]

            def load_w(k):
                for c in range(NCH):
                    S.dma("pool", (lambda c=c, k=k: (lambda e: e.indirect_dma_start(
                        out=wst[c][:], out_offset=None, in_=wexp_d[c][:, :],
                        in_offset=bass.IndirectOffsetOnAxis(ap=idxw[:, k:k + 1], axis=0), bounds_check=preg(e, 4095), oob_is_err=False)))(),
                        "wst%d" % c, reads=["idxw"], writes=[("wst", c)])

            def cast_w(k):
                wi = k % NWB
                S.op("act", (lambda wi=wi: (lambda e: e.activation(out=wbuf[wi][:, 0:2048], in_=wst[0][:], func=AF.Copy)))(),
                     reads=[("wst", 0)], writes=[("wbuf", wi, 0)])
                S.op("dve", (lambda wi=wi: (lambda e: e.tensor_copy(wbuf[wi][:, 2048:4096], wst[1][:])))(),
                     reads=[("wst", 1)], writes=[("wbuf", wi, 1)])
                S.op("dve", (lambda wi=wi: (lambda e: e.tensor_copy(wbuf[wi][:, 4096:6144], wst[2][:])))(),
                     reads=[("wst", 2)], writes=[("wbuf", wi, 2)])

            def load_x(k):
                xi = k % NXB
                S.dma("sp", (lambda k=k, xi=xi: (lambda e: e.dma_start(out=xsb[xi][:], in_=xs_d[k * 128:(k + 1) * 128, :])))(),
                      "xsb%d" % xi, reads=XS_ALL, writes=[("xsb", xi)])

            NK = NTILE_E if STOP_AFTER != "route" else 0

            def stage_t(k):
                par = k % 2
                xi = k % NXB
                ti = k % NXT
                psb = ps[:, par, :].bitcast(BF16)
                for kc in range(KC):
                    S.op("pe", (lambda kc=kc, xi=xi, psb=psb: (lambda e: e.transpose(
                        out=psb[:, kc * 128:(kc + 1) * 128], in_=xsb[xi][:, kc * 128:(kc + 1) * 128], identity=identb[:])))(),
                        reads=[("xsb", xi), "identb"], writes=[B(par)])
                evac_copy(xsT[ti][:], psb.rearrange("p (c t) -> p c t", c=KC), reads=[], writes=[B(par), ("xsT", ti)])

            def stage_g(k):
                wi = k % NWB
                par = k % 2
                ti = k % NXT
                bgu = 2 + par
                for kc in range(KC):
                    S.op("pe", (lambda kc=kc, ti=ti, wi=wi, bgu=bgu: (lambda e: e.matmul(
                        ps[:, bgu, :], lhsT=xsT[ti][:, kc, :], rhs=wbuf[wi][:, kc * 512:(kc + 1) * 512],
                        start=(kc == 0), stop=(kc == KC - 1))))(),
                        reads=[("xsT", ti), ("wbuf", wi, kc // 4)], writes=[B(bgu)])
                S.op("act", (lambda par=par, bgu=bgu: (lambda e: e.activation(out=tg[par][:], in_=ps[:, bgu, 0:256], func=AF.Tanh, scale=0.5)))(),
                     reads=[], writes=[B(bgu), ("tg", par)])
                S.op("dve", (lambda par=par, bgu=bgu: (lambda e: e.scalar_tensor_tensor(
                    out=tg[par][:], in0=tg[par][:], scalar=1.0, in1=ps[:, bgu, 0:256], op0=ALU.add, op1=ALU.mult)))(),
                    reads=[], writes=[B(bgu), ("tg", par)])
                S.op("dve", (lambda par=par, bgu=bgu: (lambda e: e.scalar_tensor_tensor(
                    out=hb[par][:], in0=tg[par][:], scalar=0.5, in1=ps[:, bgu, 256:512], op0=ALU.mult, op1=ALU.mult)))(),
                    reads=[("tg", par)], writes=[B(bgu), ("hb", par)])

            def stage_b1(k):
                par = k % 2
                psh = ps[:, 4 + par, :].bitcast(BF16)
                for c in range(2):
                    S.op("pe", (lambda c=c, par=par, psh=psh: (lambda e: e.transpose(
                        out=psh[:, c * 128:(c + 1) * 128], in_=hb[par][:, c * 128:(c + 1) * 128], identity=identb[:])))(),
                        reads=[("hb", par), "identb"], writes=[B(4 + par)])
                S.op("act", (lambda par=par, psh=psh: (lambda e: e.activation(
                    out=hT[par][:], in_=psh[:, 0:256].rearrange("p (c t) -> p c t", c=2), func=AF.Copy)))(),
                    reads=[], writes=[B(4 + par), ("hT", par)])

            def stage_b2(k):
                wi = k % NWB
                par = k % 2
                for half in range(2):
                    by = 6 + half
                    for c in range(2):
                        S.op("pe", (lambda c=c, half=half, par=par, wi=wi, by=by: (lambda e: e.matmul(
                            ps[:, by, :], lhsT=hT[par][:, c, :],
                            rhs=wbuf[wi][:, 4096 + c * 1024 + half * 512:4096 + c * 1024 + (half + 1) * 512],
                            start=(c == 0), stop=(c == 1))))(),
                            reads=[("hT", par), ("wbuf", wi, 2)], writes=[B(by)])
                    evac_copy(ysb[par][:, half * 512:(half + 1) * 512], ps[:, by, :], reads=[], writes=[B(by), ("ysb", par, half)])
                S.dma("sp", (lambda k=k, par=par: (lambda e: e.dma_start(out=ys_d[k * 128:(k + 1) * 128, :], in_=ysb[par][:])))(),
                      "ysb%d" % par, reads=[("ysb", par, 0), ("ysb", par, 1)], writes=[("ys", k)])

            for k0 in range(min(3, NK)):
                load_x(k0)
            if NK:
                load_w(0)
            for k0 in range(min(3, NK)):
                cast_w(k0)
                if k0 + 1 < NK:
                    load_w(k0 + 1)
            for k0 in range(min(2, NK)):
                stage_t(k0)
            if NK:
                stage_g(0)
            for k in range(NK):
                stage_b1(k)
                if k + 3 < NK:
                    load_x(k + 3)
                if k + 2 < NK:
                    stage_t(k + 2)
                if k + 1 < NK:
                    stage_g(k + 1)
                stage_b2(k)
                if k + 3 < NK:
                    cast_w(k + 3)
                    if k + 4 < NK:
                        load_w(k + 4)
            S.barrier()
        YS_ALL = [("ys", k) for k in range(NTILE_E)]

        G7 = 4
        NG7 = NT // G7
        with contextlib.ExitStack() as es7:
            l2g = sbt(es7, "l2g", [128, D], F32)
            l2b = sbt(es7, "l2b", [128, D], F32)
            g1 = [sbt(es7, "g1_%d" % i, [128, D], F32) for i in range(4)]
            g2 = [sbt(es7, "g2_%d" % i, [128, D], F32) for i in range(4)]
            bt = [sbt(es7, "bt%d" % i, [128, D], F32) for i in range(4)]
            ot = [sbt(es7, "ot%d" % i, [128, D], F32) for i in range(4)]
            rg2 = [sbt(es7, "rg2_%d" % q, [128, G7, D], F32) for q in range(2)]
            st12b = [sbt(es7, "st12b_%d" % q, [128, G7, 12], F32) for q in range(2)]
            mv2 = [sbt(es7, "mv2_%d" % q, [128, G7, 2], F32) for q in range(2)]
            xe2 = [sbt(es7, "xe2_%d" % q, [128, G7], F32) for q in range(2)]
            sx2 = [sbt(es7, "sx2_%d" % q, [128, G7], F32) for q in range(2)]
            sq2 = [sbt(es7, "sq2_%d" % q, [128, G7], F32) for q in range(2)]
            mean2 = [sbt(es7, "mean2_%d" % q, [128, G7], F32) for q in range(2)]
            junk = sbt(es7, "junk7", [128, D], BF16)
            rstd2 = [sbt(es7, "rstd2_%d" % q, [128, G7], F32) for q in range(2)]
            nmr2 = [sbt(es7, "nmr2_%d" % q, [128, G7], F32) for q in range(2)]
            rsq2 = [(sbt(es7, "rs_tfL2_%d" % q, [128, G7], F32), sbt(es7, "rs_yiL2_%d" % q, [128, G7], I32),
                     sbt(es7, "rs_aL2_%d" % q, [128, G7], F32)) for q in range(2)]
            S.dma("sp", lambda e: e.dma_start(out=l2g[:], in_=ln2g_d[0].partition_broadcast(128)), slot("c"), writes=["l2g"])
            S.dma("sp", lambda e: e.dma_start(out=l2b[:], in_=ln2b_d[0].partition_broadcast(128)), slot("c"), writes=["l2b"])

            def p1_dma(g, gi):
                i = g * G7 + gi
                par = i % 4
                S.dma("pool", (lambda i=i, par=par: (lambda e: e.indirect_dma_start(
                    out=g1[par][:], out_offset=None, in_=ys_d[:, :],
                    in_offset=bass.IndirectOffsetOnAxis(ap=slotu[:, i, 0:1], axis=0),
                    bounds_check=preg(e, NTILE_E * 128 - 1), oob_is_err=False)))(),
                    "g1_%d" % par, reads=YS_ALL + ["slotu"], writes=[("g1", par)])
                S.dma("pool", (lambda i=i, par=par: (lambda e: e.indirect_dma_start(
                    out=g2[par][:], out_offset=None, in_=ys_d[:, :],
                    in_offset=bass.IndirectOffsetOnAxis(ap=slotu[:, i, 1:2], axis=0),
                    bounds_check=preg(e, NTILE_E * 128 - 1), oob_is_err=False)))(),
                    "g2_%d" % par, reads=YS_ALL + ["slotu"], writes=[("g2", par)])
                S.dma("sp", (lambda i=i, par=par: (lambda e: e.dma_start(out=bt[par][:], in_=base_d[i * 128:(i + 1) * 128, :])))(),
                      "bt%d" % par, reads=[("base", i)], writes=[("bt", par)])

            def p1_dve(g, gi):
                gp = g % 2
                i = g * G7 + gi
                par = i % 4
                S.op("dve", (lambda i=i, par=par: (lambda e: e.scalar_tensor_tensor(
                    out=bt[par][:], in0=g1[par][:], scalar=cw[:, i, 0:1], in1=bt[par][:], op0=ALU.mult, op1=ALU.add)))(),
                    reads=[("g1", par), "cw0"], writes=[("bt", par)])
                S.op("dve", (lambda i=i, par=par, gi=gi, gp=gp: (lambda e: e.scalar_tensor_tensor(
                    out=rg2[gp][:, gi, :], in0=g2[par][:], scalar=cw[:, i, 1:2], in1=bt[par][:], op0=ALU.mult, op1=ALU.add)))(),
                    reads=[("g2", par), "cw1", ("bt", par)], writes=[("rg2", gp, gi)])
                S.op("act", (lambda gi=gi, gp=gp: (lambda e: e.activation(
                    out=junk[:], in_=rg2[gp][:, gi, :], func=AF.Identity, accum_out=sx2[gp][:, gi:gi + 1])))(),
                    reads=[("rg2", gp, gi)], writes=["junk", ("sx2", gp, gi)])
                S.op("act", (lambda gi=gi, gp=gp: (lambda e: e.activation(
                    out=junk[:], in_=rg2[gp][:, gi, :], func=AF.Square, accum_out=sq2[gp][:, gi:gi + 1])))(),
                    reads=[("rg2", gp, gi)], writes=["junk", ("sq2", gp, gi)])

            def rs_grp(g):
                gp = g % 2
                xe_ = xe2[gp]
                tf_, yi_, a__ = rsq2[gp]
                kx, ky, ka, kt = ("rsx", "L2", gp), ("rsy", "L2", gp), ("rsa", "L2", gp), ("rst", "L2", gp)
                mean_ = mean2[gp]
                S.op("dve", lambda e: e.tensor_scalar(out=mean_[:], in0=sx2[gp][:], scalar1=1.0 / D, scalar2=None, op0=ALU.mult),
                     reads=[("sx2", gp, gi) for gi in range(G7)], writes=[("mean2", gp)])
                S.op("dve", lambda e: e.tensor_tensor(out=xe_[:], in0=mean_[:], in1=mean_[:], op=ALU.mult),
                     reads=[("mean2", gp)], writes=[kx])
                S.op("dve", lambda e: e.scalar_tensor_tensor(out=xe_[:], in0=sq2[gp][:], scalar=1.0 / D, in1=xe_[:],
                                                             op0=ALU.mult, op1=ALU.subtract),
                     reads=[("sq2", gp, gi) for gi in range(G7)], writes=[kx])
                S.op("dve", lambda e: e.tensor_scalar(out=xe_[:], in0=xe_[:], scalar1=LN_EPS, scalar2=None, op0=ALU.add),
                     reads=[], writes=[kx])
                S.op("dve", lambda e: e.tensor_copy(tf_[:], xe_[:].bitcast(I32)), reads=[kx], writes=[kt])
                S.op("dve", lambda e: e.tensor_scalar(out=tf_[:], in0=tf_[:], scalar1=-0.5, scalar2=1597463007.0,
                                                      op0=ALU.mult, op1=ALU.add), reads=[kt], writes=[kt])
                S.op("dve", lambda e: e.tensor_copy(yi_[:], tf_[:]), reads=[kt], writes=[ky])
                y2 = yi_[:].bitcast(F32)
                for it in range(2):
                    S.op("dve", lambda e: e.tensor_tensor(out=a__[:], in0=y2, in1=y2, op=ALU.mult), reads=[ky], writes=[ka])
                    S.op("dve", lambda e: e.tensor_tensor(out=a__[:], in0=a__[:], in1=xe_[:], op=ALU.mult), reads=[ka, kx], writes=[ka])
                    S.op("dve", lambda e: e.tensor_scalar(out=a__[:], in0=a__[:], scalar1=-0.5, scalar2=1.5,
                                                          op0=ALU.mult, op1=ALU.add), reads=[ka], writes=[ka])
                    if it == 0:
                        S.op("dve", lambda e: e.tensor_tensor(out=y2, in0=y2, in1=a__[:], op=ALU.mult), reads=[ka, ky], writes=[ky])
                    else:
                        S.op("dve", lambda e: e.tensor_tensor(out=rstd2[gp][:], in0=y2, in1=a__[:], op=ALU.mult),
                             reads=[ka, ky], writes=[("rstd2", gp)])
                S.op("dve", lambda e: e.scalar_tensor_tensor(out=nmr2[gp][:], in0=mean2[gp][:], scalar=-1.0, in1=rstd2[gp][:],
                                                             op0=ALU.mult, op1=ALU.mult),
                     reads=[("rstd2", gp), ("mean2", gp)], writes=[("nmr2", gp)])

            def p2_norm(g, gi):
                gp = g % 2
                i = g * G7 + gi
                par = i % 4
                S.op("act", (lambda gi=gi, gp=gp, par=par: (lambda e: e.activation(
                    out=ot[par][:], in_=rg2[gp][:, gi, :], func=AF.Identity,
                    scale=rstd2[gp][:, gi:gi + 1], bias=nmr2[gp][:, gi:gi + 1])))(),
                    reads=[("rg2", gp, gi), ("rstd2", gp), ("nmr2", gp)], writes=[("ot", par)])

            def p2_tile(g, gi):
                gp = g % 2
                i = g * G7 + gi
                par = i % 4
                S.op("dve", (lambda par=par: (lambda e: e.tensor_tensor(out=ot[par][:], in0=ot[par][:], in1=l2g[:], op=ALU.mult)))(),
                     reads=["l2g"], writes=[("ot", par)])
                S.op("pool", (lambda par=par: (lambda e: e.tensor_tensor(out=ot[par][:], in0=ot[par][:], in1=l2b[:], op=ALU.add)))(),
                     reads=["l2b"], writes=[("ot", par)])
                S.dma("sp", (lambda i=i, par=par: (lambda e: e.dma_start(out=out_d[i * 128:(i + 1) * 128, :], in_=ot[par][:])))(),
                      "ot%d" % par, reads=[("ot", par)], writes=[("out", i)], final=True)

            if STOP_AFTER != "route":
                for gi in range(G7):
                    p1_dma(0, gi)
                for gi in range(G7):
                    p1_dve(0, gi)
                rs_grp(0)
                for g in range(NG7):
                    if g + 1 < NG7:
                        for gi in range(G7):
                            p1_dma(g + 1, gi)
                    for gi in range(G7):
                        p2_norm(g, gi)
                    for gi in range(G7):
                        p2_tile(g, gi)
                        if g + 1 < NG7:
                            p1_dve(g + 1, gi)
                    if g + 1 < NG7:
                        rs_grp(g + 1)
        S.emit()
    return nc, dbg


def _consts():
    k = np.arange(128)[:, None]
    q = np.arange(128)[None, :]
    own = (k <= q).astype(np.float32)
    prev = (k >= q).astype(np.float32)
    bf = ml_dtypes.bfloat16
    return {
        "c_identf": np.eye(128, dtype=np.float32),
        "c_identb": np.eye(128, dtype=np.float32).astype(bf),
        "c_mask4": np.concatenate([prev, own, prev, own], axis=1).astype(bf),
        "c_mown4": np.concatenate([own, own, own, own], axis=1).astype(bf),
        "c_lstrict": (k < q).astype(np.float32).astype(bf),
        "c_onesb": np.ones((128, 128), np.float32).astype(bf),
        "c_kidx": np.broadcast_to(np.arange(NTILE_E, dtype=np.float32)[None, :], (128, NTILE_E)).copy(),
        "c_pidx": np.arange(128, dtype=np.float32).reshape(128, 1),
    }


def _shared_inputs(inp):
    f = lambda a: np.ascontiguousarray(np.asarray(a, dtype=np.float32))
    wg = f(inp["w_gate"])[0].reshape(32, 8, 128, 256)
    wu = f(inp["w_up"])[0].reshape(32, 8, 128, 256)
    gu = np.concatenate([wg, wu], axis=3)
    gu = np.ascontiguousarray(gu.transpose(0, 2, 1, 3))
    wgu0 = np.ascontiguousarray(gu[:, :, 0:4, :]).reshape(4096, 2048)
    wgu1 = np.ascontiguousarray(gu[:, :, 4:8, :]).reshape(4096, 2048)
    wd = f(inp["w_down"])[0].reshape(32, 2, 128, 1024)
    wdn = np.ascontiguousarray(wd.transpose(0, 2, 1, 3)).reshape(4096, 2048)
    sh = {
        "w_in": np.ascontiguousarray(f(inp["w_in"])[0][:, WIN_PERM]),
        "a_ln_g": f(inp["a_ln_g"]).reshape(1, 512),
        "a_ln_b": f(inp["a_ln_b"]).reshape(1, 512),
        "a_wsT": np.ascontiguousarray(f(inp["a_ws"])[0].transpose(2, 0, 1)),
        "a_bs": f(inp["a_bs"])[0].reshape(1, 1024),
        "w_a": f(inp["w_a_proj"])[0],
        "w_b": f(inp["w_b_proj"])[0],
        "w_o": f(inp["w_o"])[0],
        "ln1_g": f(inp["ln1_g"]).reshape(1, D),
        "ln1_b": f(inp["ln1_b"]).reshape(1, D),
        "w_r": np.ascontiguousarray(np.concatenate([f(inp["w_group_router"])[0], f(inp["w_expert_router"])[0].reshape(D, 32)], axis=1)),
        "b_r": np.concatenate([f(inp["b_group_router"])[0], f(inp["b_expert_router"])[0].reshape(32)]).reshape(1, 36),
        "wexp0": np.ascontiguousarray(wgu0.reshape(4096, 2048)),
        "wexp1": np.ascontiguousarray(wgu1.reshape(4096, 2048)),
        "wexp2": np.ascontiguousarray(wdn.reshape(4096, 2048)),
        "w_ple": f(inp["w_ple"])[0],
        "w_pg": f(inp["w_ple_gate"])[0],
        "ln2_g": f(inp["ln2_g"]).reshape(1, D),
        "ln2_b": f(inp["ln2_b"]).reshape(1, D),
    }
    sh.update(_consts())
    return sh


_NC_CACHE = {}


def kernel(**inputs):
    x = np.asarray(inputs["x"], dtype=np.float32)
    p = np.asarray(inputs["p"], dtype=np.float32)
    if "nc" not in _NC_CACHE:
        _NC_CACHE["nc"] = build_nc()[0]
    nc = _NC_CACHE["nc"]
    sh = _shared_inputs(inputs)
    n = x.shape[0]
    in_maps = []
    for c in range(n):
        m = dict(sh)
        m["x"] = np.ascontiguousarray(x[c])
        m["p"] = np.ascontiguousarray(p[0, c])
        in_maps.append(m)
    res = run_bass_kernel_spmd(nc, in_maps, core_ids=list(range(n)))
    return np.stack([np.asarray(r["out"], dtype=np.float32) for r in res.results], axis=0)
```

```python
import contextlib
import numpy as np
import ml_dtypes
import concourse.bass as bass
import concourse.mybir as mybir
from concourse.bass_utils import run_bass_kernel_spmd
from concourse.alu_op_type import AluOpType as ALU

F32 = mybir.dt.float32
BF16 = mybir.dt.bfloat16
U32 = mybir.dt.uint32
I32 = mybir.dt.int32
AF = mybir.ActivationFunctionType
AX = mybir.AxisListType

S_TOK = 2048
D = 1024
NT = 16
KC = 8
ALPHA = 2.0 ** 0.25
LN_EPS = 1e-5
GELU_C = 0.7978845608028654
NTILE_E = 63
ENGS = ("pe", "act", "dve", "pool", "sp")
DEBUG = []
HEAD_BARRIER = False
STOP_AFTER = None


class Op:
    __slots__ = ("eng", "fn", "deps", "idx", "is_dma", "sem", "val", "signal")

    def __init__(self, eng, fn, is_dma):
        self.eng = eng
        self.fn = fn
        self.deps = []
        self.is_dma = is_dma
        self.sem = None
        self.val = None
        self.signal = False


class Sched:
    def __init__(self, nc):
        self.nc = nc
        self.ops = []
        self.last_w = {}
        self.readers = {}
        self.dma_slots = {}
        self.final_dma = []
        self.bar_deps = []
        self.bar_need = set()
        self.last_eng = {}
        self.last_slot = {}

    def barrier(self):
        self.bar_deps = list(self.last_eng.values()) + list(self.last_slot.values())
        self.bar_need = set(ENGS)

    def _add(self, op, reads, writes):
        op.idx = len(self.ops)
        deps = set()
        for r in reads:
            w = self.last_w.get(r)
            if w is not None:
                deps.add(w)
        for r in writes:
            w = self.last_w.get(r)
            if w is not None:
                deps.add(w)
            for rd in self.readers.get(r, ()):
                deps.add(rd)
        if op.eng in self.bar_need:
            self.bar_need.discard(op.eng)
            deps.update(self.bar_deps)
        deps.discard(op.idx)
        for d in sorted(deps):
            dop = self.ops[d]
            if dop.eng == op.eng and not dop.is_dma and op.eng in ("pe", "sp"):
                continue
            op.deps.append(d)
            dop.signal = True
        for r in reads:
            self.readers.setdefault(r, []).append(op.idx)
        for r in writes:
            self.last_w[r] = op.idx
            self.readers[r] = []
        self.ops.append(op)
        if not op.is_dma:
            self.last_eng[op.eng] = op.idx
        return op

    def op(self, eng, fn, reads=(), writes=()):
        return self._add(Op(eng, fn, False), tuple(reads), tuple(writes))

    def dma(self, eng, fn, slot, reads=(), writes=(), final=False):
        op = Op(eng, fn, True)
        self.dma_slots.setdefault(slot, []).append(op)
        op.signal = True
        self._add(op, tuple(reads), tuple(writes))
        self.last_slot[slot] = op.idx
        if final:
            self.final_dma.append(op)
        return op

    def emit(self):
        nc = self.nc
        with contextlib.ExitStack() as es:
            esem = {e: es.enter_context(nc.semaphore("s_" + e)) for e in ENGS}
            ssem = {s: es.enter_context(nc.semaphore("d%d" % i)) for i, s in enumerate(self.dma_slots)}
            cnt = {e: 0 for e in ENGS}
            for op in self.ops:
                if op.is_dma:
                    continue
                if op.signal:
                    cnt[op.eng] += 1
                    op.sem = esem[op.eng]
                    op.val = cnt[op.eng]
            for s, ops in self.dma_slots.items():
                c = 0
                for op in ops:
                    c += 16
                    op.sem = ssem[s]
                    op.val = c
            block = es.enter_context(nc.Block())
            per_eng = {e: [o for o in self.ops if o.eng == e] for e in ENGS}

            def run(engname, eng):
                waited = {}
                for op in per_eng[engname]:
                    need = {}
                    for d in op.deps:
                        dop = self.ops[d]
                        k = id(dop.sem)
                        if waited.get(k, 0) >= dop.val:
                            continue
                        if k not in need or need[k][1] < dop.val:
                            need[k] = (dop.sem, dop.val)
                    for k, (sem, val) in need.items():
                        eng.wait_ge(sem, val)
                        waited[k] = val
                    ins = op.fn(eng)
                    if op.is_dma:
                        ins.then_inc(op.sem, 16)
                    elif op.signal:
                        ins.then_inc(op.sem, 1)
                if engname == "sp":
                    fin = {}
                    for op in self.final_dma:
                        k = id(op.sem)
                        if k not in fin or fin[k][1] < op.val:
                            fin[k] = (op.sem, op.val)
                    for sem, val in fin.values():
                        eng.wait_ge(sem, val)

            @block.tensor
            def _(e):
                run("pe", e)

            @block.scalar
            def _(e):
                run("act", e)

            @block.vector
            def _(e):
                run("dve", e)

            @block.gpsimd
            def _(e):
                run("pool", e)

            @block.sync
            def _(e):
                run("sp", e)


def _win_perm():
    perm = []
    blocks = {}

    def add(name, cols):
        blocks[name] = (len(perm), len(cols))
        perm.extend(cols)

    add("u", list(range(0, 512)))
    add("v", list(range(512, 1024)))

    def zb(s, g, h):
        base = 1024 + ((s * 3 + g) * 8 + h) * 64
        return list(range(base, base + 64))

    for ps_ in range(2):
        for g in range(3):
            cols = []
            for h in range(4 * ps_, 4 * ps_ + 4):
                cols += zb(2, g, h)
            add(("vv", ps_, g), cols)
        for hp in range(2 * ps_, 2 * ps_ + 2):
            for s, nm in ((0, "q"), (1, "k")):
                cols = []
                for g in range(3):
                    cols += zb(s, g, 2 * hp) + zb(s, g, 2 * hp + 1)
                add((nm, hp), cols)
    ga = 5632
    gb = 5632 + 1024
    add(("g", 0), list(range(ga, ga + 512)))
    add(("g", 1), list(range(gb, gb + 512)))
    add(("g", 2), list(range(ga + 512, ga + 1024)))
    add(("g", 3), list(range(gb + 512, gb + 1024)))
    assert len(perm) == 7680 and sorted(perm) == list(range(7680))
    return np.array(perm), blocks


WIN_PERM, WIN_BLOCKS = _win_perm()


def build_nc(debug=()):
    nc = bass.Bass("TRN2", target_bir_lowering=False)

    def din(name, shape, dt=F32):
        return nc.dram_tensor(name, list(shape), dt, kind="ExternalInput").ap()

    x_d = din("x", [S_TOK, D])
    p_d = din("p", [S_TOK, 256])
    win_d = din("w_in", [D, 7680])
    alng_d = din("a_ln_g", [1, 512])
    alnb_d = din("a_ln_b", [1, 512])
    awsT_d = din("a_wsT", [128, 8, 128])
    abs_d = din("a_bs", [1, 1024])
    wa_d = din("w_a", [512, D])
    wb_d = din("w_b", [512, D])
    wo_d = din("w_o", [D, D])
    ln1g_d = din("ln1_g", [1, D])
    ln1b_d = din("ln1_b", [1, D])
    wr_d = din("w_r", [D, 36])
    br_d = din("b_r", [1, 36])
    wexp_d = [din("wexp%d" % c, [4096, 2048]) for c in range(3)]
    wple_d = din("w_ple", [256, D])
    wpg_d = din("w_pg", [D, D])
    ln2g_d = din("ln2_g", [1, D])
    ln2b_d = din("ln2_b", [1, D])
    identf_d = din("c_identf", [128, 128])
    identb_d = din("c_identb", [128, 128], BF16)
    mask4_d = din("c_mask4", [128, 512], BF16)
    mown4_d = din("c_mown4", [128, 512], BF16)
    lstr_d = din("c_lstrict", [128, 128], BF16)
    onesb_d = din("c_onesb", [128, 128], BF16)
    kidx_d = din("c_kidx", [128, NTILE_E])
    pidx_d = din("c_pidx", [128, 1])

    out_d = nc.dram_tensor("out", [S_TOK, D], F32, kind="ExternalOutput").ap()
    xs_d = nc.dram_tensor("xs_scr", [NTILE_E * 128, D], BF16, kind="Internal").ap()
    ys_d = nc.dram_tensor("ys_scr", [NTILE_E * 128, D], F32, kind="Internal").ap()
    base_d = nc.dram_tensor("base_scr", [S_TOK, D], F32, kind="Internal").ap()
    dbg = {}

    def dbg_out(name, shape, dt=F32):
        dbg[name] = nc.dram_tensor("dbg_" + name, list(shape), dt, kind="ExternalOutput").ap()
        return dbg[name]

    S = Sched(nc)
    uid = [0]

    def slot(prefix="o"):
        uid[0] += 1
        return "%s%d" % (prefix, uid[0])

    with contextlib.ExitStack() as es0:
        def sbt(es, name, shape, dt):
            return es.enter_context(nc.sbuf_tensor(name, list(shape), dt))

        ps = es0.enter_context(nc.psum_tensor("ps", [128, 8, 512], F32))

        def B(b):
            return ("B", b)

        identf = sbt(es0, "identf", [128, 128], F32)
        identb = sbt(es0, "identb", [128, 128], BF16)
        mask4 = sbt(es0, "mask4", [128, 512], BF16)
        mown4 = sbt(es0, "mown4", [128, 512], BF16)
        lstr = sbt(es0, "lstr", [128, 128], BF16)
        onesb = sbt(es0, "onesb", [128, 128], BF16)
        kidx = sbt(es0, "kidx", [128, NTILE_E], F32)
        pidx = sbt(es0, "pidx", [128, 1], F32)
        for t, d_, nm in ((identf, identf_d, "identf"), (identb, identb_d, "identb"), (mask4, mask4_d, "mask4"),
                          (mown4, mown4_d, "mown4"), (lstr, lstr_d, "lstr"), (onesb, onesb_d, "onesb"),
                          (kidx, kidx_d, "kidx"), (pidx, pidx_d, "pidx")):
            S.dma("sp", (lambda t=t, d_=d_: (lambda e: e.dma_start(out=t[:], in_=d_)))(), slot("c"), writes=[nm])

        slotu = sbt(es0, "slotu", [128, NT, 2], U32)
        cw = sbt(es0, "cw", [128, NT, 2], F32)
        idxw = sbt(es0, "idxw", [128, NTILE_E], U32)
        rowst = sbt(es0, "rowst", [128, NTILE_E], I32)
        zt = sbt(es0, "zt", [128, D], BF16)
        mix_es = contextlib.ExitStack()
        mixinT = sbt(mix_es, "mixinT", [128, KC, S_TOK], BF16)

        S.op("pool", lambda e: e.memset(zt[:], 0.0), writes=["zt"])

        mx_es = contextlib.ExitStack()
        xT = sbt(mx_es, "xT", [128, KC, S_TOK], BF16)
        obT = sbt(mx_es, "obT", [128, 4, S_TOK], BF16)
        wring = []
        win_v = win_d.rearrange("(kc p) c -> p kc c", p=128)
        wr_state = {"n": 0}
        WB = {}

        def load_wblk(name):
            c0, ncol = WIN_BLOCKS[name]
            bi = wr_state["n"] % len(wring)
            wr_state["n"] += 1
            buf_ = wring[bi]
            S.dma("pool", lambda e: e.dma_start(out=buf_[:, :, 0:ncol], in_=win_v[:, :, c0:c0 + ncol]),
                  "wr%s%d" % (buf_.name, bi), writes=[("wr", bi)])
            WB[bi] = buf_
            return bi

        reg_cache = {}

        def preg(e, val):
            if val not in reg_cache:
                reg_cache[val] = e.to_reg(val)
            return reg_cache[val]

        bank_rr = {"n": 0}

        def nb_(pool):
            bank_rr["n"] += 1
            return pool[bank_rr["n"] % len(pool)]

        evac_rr = {"n": 0}

        def evac_copy(out_ap, in_ap, reads, writes):
            evac_rr["n"] += 1
            if evac_rr["n"] % 2:
                S.op("act", lambda e: e.activation(out=out_ap, in_=in_ap, func=AF.Copy), reads=reads, writes=writes)
            else:
                S.op("dve", lambda e: e.tensor_copy(out_ap, in_ap), reads=reads, writes=writes)

        def rsqrt_batch(es, tag, x_ap, n):
            tf = sbt(es, "rs_tf" + tag, [128, n], F32)
            yi = sbt(es, "rs_yi" + tag, [128, n], I32)
            a_ = sbt(es, "rs_a" + tag, [128, n], F32)
            kx, ky, ka, kt = ("rsx", tag), ("rsy", tag), ("rsa", tag), ("rst", tag)
            S.op("dve", lambda e: e.tensor_copy(tf[:], x_ap.bitcast(I32)), reads=[kx], writes=[kt])
            S.op("dve", lambda e: e.tensor_scalar(out=tf[:], in0=tf[:], scalar1=-0.5, scalar2=1597463007.0,
                                                  op0=ALU.mult, op1=ALU.add), reads=[kt], writes=[kt])
            S.op("dve", lambda e: e.tensor_copy(yi[:], tf[:]), reads=[kt], writes=[ky])
            y = yi[:].bitcast(F32)
            for _ in range(2):
                S.op("dve", lambda e: e.tensor_tensor(out=a_[:], in0=y, in1=y, op=ALU.mult), reads=[ky], writes=[ka])
                S.op("dve", lambda e: e.tensor_tensor(out=a_[:], in0=a_[:], in1=x_ap, op=ALU.mult), reads=[ka, kx], writes=[ka])
                S.op("dve", lambda e: e.tensor_scalar(out=a_[:], in0=a_[:], scalar1=-0.5, scalar2=1.5,
                                                      op0=ALU.mult, op1=ALU.add), reads=[ka], writes=[ka])
                S.op("dve", lambda e: e.tensor_tensor(out=y, in0=y, in1=a_[:], op=ALU.mult), reads=[ka, ky], writes=[ky])
            return y, ky, kx


        with contextlib.ExitStack() as es1:
            wring[:] = [sbt(es1, "wringA%d" % i, [128, KC, 512], BF16) for i in range(3)]
            order = []
            for ps_ in range(2):
                order += [("vv", ps_, 0), ("vv", ps_, 1), ("vv", ps_, 2)]
                for hp in range(2 * ps_, 2 * ps_ + 2):
                    order += [("q", hp), ("k", hp)]
            loaded = {}
            nxt = [0]

            def ensure(upto):
                while nxt[0] < len(order) and nxt[0] <= upto:
                    loaded[order[nxt[0]]] = load_wblk(order[nxt[0]])
                    nxt[0] += 1

            ensure(1)
            with contextlib.ExitStack() as esx:
                NXS = 4
                xsf = [sbt(esx, "xsf%d" % i, [128, D], F32) for i in range(NXS)]
                xst = [sbt(esx, "xst%d" % i, [128, D], BF16) for i in range(2)]

                def ldx(i):
                    xf_ = xsf[i % NXS]
                    S.dma("sp" if i % 2 == 0 else "pool",
                          (lambda i=i, xf_=xf_: (lambda e: e.dma_start(out=xf_[:], in_=x_d[i * 128:(i + 1) * 128, :])))(),
                          "xsf%d" % (i % NXS), writes=[("xsf", i % NXS)])

                for i in range(min(NXS, NT)):
                    ldx(i)
                for i in range(NT):
                    xs_ = xst[i % 2]
                    xf_ = xsf[i % NXS]
                    if i % 2 == 0:
                        S.op("act", (lambda xs_=xs_, xf_=xf_: (lambda e: e.activation(out=xs_[:], in_=xf_[:], func=AF.Copy)))(),
                             reads=[("xsf", i % NXS)], writes=[("xst", i % 2)])
                    else:
                        S.op("dve", (lambda xs_=xs_, xf_=xf_: (lambda e: e.tensor_copy(xs_[:], xf_[:])))(),
                             reads=[("xsf", i % NXS)], writes=[("xst", i % 2)])
                    if i + NXS < NT:
                        ldx(i + NXS)
                    bk = nb_([0, 1, 2, 3])
                    psb = ps[:, bk, :].bitcast(BF16)
                    for kc in range(KC):
                        S.op("pe", (lambda psb=psb, kc=kc, xs_=xs_: (lambda e: e.transpose(
                            out=psb[:, kc * 128:(kc + 1) * 128], in_=xs_[:, kc * 128:(kc + 1) * 128], identity=identb[:])))(),
                            reads=[("xst", i % 2), "identb"], writes=[B(bk)])
                    evac_copy(xT[:, :, i * 128:(i + 1) * 128], psb.rearrange("p (j t) -> p j t", j=KC),
                              reads=[], writes=[B(bk), ("xT", i)])
                S.barrier()
            for q in range(NTILE_E):
                S.dma("sp", (lambda q=q: (lambda e: e.dma_start(out=xs_d[q * 128:(q + 1) * 128, :], in_=zt[:])))(),
                      "xsz", reads=["zt"], writes=[("xsz", q)])
            if "xT" in debug:
                o = dbg_out("xT", [128, KC, S_TOK], BF16)
                S.dma("sp", lambda e, o=o: e.dma_start(out=o, in_=xT[:]), slot(), reads=[("xT", i) for i in range(NT)], final=True)

            XT_ALL = [("xT", i) for i in range(NT)]
            Vaug = [sbt(es1, "vaug%d" % g, [128, 16, 4, 128], BF16) for g in range(3)]
            qk = sbt(es1, "qk", [128, 6, S_TOK], BF16)
            PTb = [sbt(es1, "ptb%d" % i, [128, 512], BF16) for i in range(4)]
            PT2 = sbt(es1, "pt2", [128, 16, 128], BF16)
            rd = [sbt(es1, "rd%d" % i, [64, 512], F32) for i in range(2)]
            for g in range(3):
                S.op("pool", (lambda g=g: (lambda e: e.memset(Vaug[g][:, :, :, 64:128], 1.0)))(), writes=[("vones", g)])

            def v_tile_tokens(g, t):
                if g == 0:
                    return slice(t * 128, (t + 1) * 128), [("xT", t)]
                if g == 1:
                    r4, nb = t // 4, t % 4
                    return slice(512 * nb + r4, 512 * (nb + 1), 4), [("xT", 4 * nb + j) for j in range(4)]
                return slice(t, S_TOK, 16), XT_ALL

            PROJ = [6, 7]
            SC = [2, 3, 4, 5]
            NSC = len(SC)
            DEPTH = 2
            pend = []

            def pipe(front, back):
                front()
                pend.append(back)
                while len(pend) > DEPTH:
                    b_ = pend.pop(0)
                    if b_ is not None:
                        b_()

            def flush():
                while pend:
                    b_ = pend.pop(0)
                    if b_ is not None:
                        b_()

            blk_i = [0]
            pt_rr = [0]
            acc_rr = [0]
            st_rr = [0]

            for ps_ in range(2):
                for g in range(3):
                    ensure(blk_i[0] + 2)
                    wb_i = loaded[("vv", ps_, g)]
                    blk_i[0] += 1
                    for t0 in range(0, 16, 2):
                        bk = nb_(PROJ)
                        rk = []
                        for tt in range(2):
                            sl, keys = v_tile_tokens(g, t0 + tt)
                            rk += keys
                            for kc in range(KC):
                                S.op("pe", (lambda bk=bk, tt=tt, kc=kc, sl=sl, wt=WB[wb_i]: (lambda e: e.matmul(
                                    ps[:, bk, tt * 256:(tt + 1) * 256], lhsT=xT[:, kc, sl], rhs=wt[:, kc, 0:256],
                                    start=(kc == 0), stop=(kc == KC - 1))))(),
                                    reads=keys + [("wr", wb_i)], writes=[B(bk)])
                        evac_copy(Vaug[g][:, t0:t0 + 2, :, 0:64],
                                  ps[:, bk, :].rearrange("p (t h d) -> p t h d", t=2, h=4),
                                  reads=[], writes=[B(bk), ("V", g, t0), ("V", g, t0 + 1)])
                for hp in range(2 * ps_, 2 * ps_ + 2):
                    for si, nm in ((0, "q"), (1, "k")):
                        ensure(blk_i[0] + 2)
                        wb_i = loaded[(nm, hp)]
                        blk_i[0] += 1
                        for g in range(3):
                            sl_ = si * 3 + g
                            for sp in range(4):
                                bk = nb_(PROJ)
                                for kc in range(KC):
                                    S.op("pe", (lambda bk=bk, kc=kc, g=g, sp=sp, wt=WB[wb_i]: (lambda e: e.matmul(
                                        ps[:, bk, :], lhsT=wt[:, kc, g * 128:(g + 1) * 128],
                                        rhs=xT[:, kc, sp * 512:(sp + 1) * 512], start=(kc == 0), stop=(kc == KC - 1))))(),
                                        reads=[("xT", 4 * sp + j) for j in range(4)] + [("wr", wb_i)], writes=[B(bk)])
                                if g == 0:
                                    o_ap = qk[:, sl_, sp * 512:(sp + 1) * 512]
                                    i_ap = ps[:, bk, :]
                                elif g == 1:
                                    o_ap = qk[:, sl_, :].rearrange("p (r n i) -> p r n i", r=4, n=4)[:, :, sp, :]
                                    i_ap = ps[:, bk, :].rearrange("p (i r) -> p r i", r=4)
                                else:
                                    o_ap = qk[:, sl_, :].rearrange("p (r a) -> p r a", r=16)[:, :, sp * 32:(sp + 1) * 32]
                                    i_ap = ps[:, bk, :].rearrange("p (a r) -> p r a", r=16)
                                evac_copy(o_ap, i_ap, reads=[], writes=[B(bk), ("qk", sl_, sp)])
                    for h in (2 * hp, 2 * hp + 1):
                        b0 = (h % 2) * 64
                        hl = h % 4
                        QK_ALL = lambda s_: [("qk", s_, sp) for sp in range(4)]
                        for grp in range(4):
                            def front2(grp=grp, b0=b0):
                                st_rr[0] += 1
                                bk = SC[st_rr[0] % NSC]
                                for jj in range(4):
                                    r = grp * 4 + jj
                                    S.op("pe", (lambda bk=bk, jj=jj, r=r, b0=b0: (lambda e: e.matmul(
                                        ps[:, bk, jj * 128:(jj + 1) * 128], lhsT=qk[b0:b0 + 64, 5, r * 128:(r + 1) * 128],
                                        rhs=qk[b0:b0 + 64, 2, r * 128:(r + 1) * 128], start=True, stop=True)))(),
                                        reads=QK_ALL(5) + QK_ALL(2), writes=[B(bk)])
                                pview = PT2[:, grp * 4:(grp + 1) * 4, :]
                                S.op("act", (lambda bk=bk, pview=pview: (lambda e: e.activation(
                                    out=pview, in_=ps[:, bk, :].rearrange("p (a b) -> p a b", a=4), func=AF.Exp, scale=0.125)))(),
                                    reads=[], writes=[B(bk), ("pt2", grp)])
                                S.op("dve" if grp % 2 == 0 else "pool", (lambda pview=pview: (lambda e: e.tensor_tensor(
                                    out=pview, in0=pview, in1=mown4[:].rearrange("p (a b) -> p a b", a=4), op=ALU.mult)))(),
                                    reads=["mown4"], writes=[("pt2", grp)])
                            pipe(front2, None)
                        for s in range(4):
                            acc_rr[0] += 1
                            ab = acc_rr[0] % 2
                            first = [True]
                            blocks_ = []
                            for j in range(4 * s, 4 * s + 4):
                                q_ap = qk[b0:b0 + 64, 0, j * 128:(j + 1) * 128]
                                o_ap = ps[:, ab, (j - 4 * s) * 128:(j - 4 * s + 1) * 128]
                                prev = None
                                if j > 0:
                                    prev = (qk[b0:b0 + 64, 3, (j - 1) * 128:j * 128], Vaug[0][:, j - 1, hl, :],
                                            [("qk", 3, (j - 1) // 4), ("V", 0, j - 1), ("vones", 0)])
                                own = (qk[b0:b0 + 64, 3, j * 128:(j + 1) * 128], Vaug[0][:, j, hl, :],
                                       [("qk", 3, j // 4), ("V", 0, j), ("vones", 0)])
                                blocks_.append((q_ap, [("qk", 0, s)], o_ap, prev, own))
                            for r4 in range(4):
                                q_ap = qk[b0:b0 + 64, 1, r4 * 512 + s * 128:r4 * 512 + (s + 1) * 128]
                                o_ap = ps[:, ab, r4:512:4]
                                prev = None
                                if s > 0:
                                    prev = (qk[b0:b0 + 64, 4, r4 * 512 + (s - 1) * 128:r4 * 512 + s * 128],
                                            Vaug[1][:, r4 * 4 + s - 1, hl, :],
                                            [("qk", 4, s - 1), ("V", 1, r4 * 4 + s - 1), ("vones", 1)])
                                own = (qk[b0:b0 + 64, 4, r4 * 512 + s * 128:r4 * 512 + (s + 1) * 128],
                                       Vaug[1][:, r4 * 4 + s, hl, :],
                                       [("qk", 4, s), ("V", 1, r4 * 4 + s), ("vones", 1)])
                                blocks_.append((q_ap, [("qk", 1, s)], o_ap, prev, own))
                            for bp in range(0, 8, 2):
                                st = {}

                                def front(bp=bp, st=st, blocks_=blocks_):
                                    st_rr[0] += 1
                                    sb_ = SC[st_rr[0] % NSC]
                                    ptb = PTb[st_rr[0] % NSC]
                                    ptk = ("ptb", st_rr[0] % NSC)
                                    used = []
                                    pv = []
                                    for bi_, blk in enumerate(blocks_[bp:bp + 2]):
                                        q_ap, qkeys, o_ap, prev, own = blk
                                        for kind, it in ((0, prev), (1, own)):
                                            if it is None:
                                                continue
                                            sl_i = bi_ * 2 + kind
                                            used.append(sl_i)
                                            k_ap, v_ap, rkeys = it
                                            S.op("pe", (lambda sb_=sb_, sl_i=sl_i, k_ap=k_ap, q_ap=q_ap: (lambda e: e.matmul(
                                                ps[:, sb_, sl_i * 128:(sl_i + 1) * 128], lhsT=k_ap, rhs=q_ap, start=True, stop=True)))(),
                                                reads=qkeys + [rkeys[0]], writes=[B(sb_)])
                                            pv.append((sl_i, v_ap, o_ap, rkeys[1:]))
                                    if used == [0, 1, 2, 3]:
                                        sel = lambda ap: ap
                                    elif used == [1, 2, 3]:
                                        sel = lambda ap: ap[:, 128:512]
                                    else:
                                        assert used == [1, 3], used
                                        sel = lambda ap: ap.rearrange("p (a b) -> p a b", a=4)[:, 1:4:2, :]
                                    S.op("act", (lambda sb_=sb_, ptb=ptb, sel=sel: (lambda e: e.activation(
                                        out=sel(ptb[:]), in_=sel(ps[:, sb_, :]), func=AF.Exp, scale=0.125)))(),
                                        reads=[], writes=[B(sb_), ptk])
                                    S.op("dve" if bp < 4 else "pool", (lambda ptb=ptb, sel=sel: (lambda e: e.tensor_tensor(
                                        out=sel(ptb[:]), in0=sel(ptb[:]), in1=sel(mask4[:]), op=ALU.mult)))(),
                                        reads=["mask4"], writes=[ptk])
                                    st["pv"], st["ptb"], st["ptk"] = pv, ptb, ptk

                                def back(bp=bp, st=st, first=first, ab=ab, s=s, hl=hl, b0=b0, hp=hp, h=h):
                                    ptb, ptk = st["ptb"], st["ptk"]
                                    for sl_i, v_ap, o_ap, rkeys in st["pv"]:
                                        st_flag = first[0]
                                        first[0] = False
                                        S.op("pe", (lambda sl_i=sl_i, v_ap=v_ap, o_ap=o_ap, ptb=ptb, st_flag=st_flag: (lambda e: e.matmul(
                                            o_ap, lhsT=v_ap, rhs=ptb[:, sl_i * 128:(sl_i + 1) * 128], start=st_flag, stop=False)))(),
                                            reads=[ptk] + rkeys, writes=[B(ab)])
                                    if bp != 6:
                                        return
                                    for r in range(16):
                                        S.op("pe", (lambda r=r, s=s, ab=ab, hl=hl: (lambda e: e.matmul(
                                            ps[:, ab, r:512:16], lhsT=Vaug[2][:, r, hl, :], rhs=PT2[:, r, 32 * s:32 * (s + 1)],
                                            start=False, stop=(r == 15))))(),
                                            reads=[("pt2", r // 4), ("V", 2, r), ("vones", 2)], writes=[B(ab)])
                                    rdt = rd[ab]
                                    S.op("dve", (lambda ab=ab, rdt=rdt: (lambda e: e.reciprocal(out=rdt[:], in_=ps[64:128, ab, :])))(),
                                         reads=[], writes=[B(ab), ("rd", ab)])
                                    S.op("dve", (lambda ab=ab, rdt=rdt, b0=b0, hp=hp, s=s: (lambda e: e.tensor_tensor(
                                        out=obT[b0:b0 + 64, hp, s * 512:(s + 1) * 512], in0=ps[0:64, ab, :], in1=rdt[:], op=ALU.mult)))(),
                                        reads=[("rd", ab)], writes=[B(ab), ("obT", hp, s, h % 2)])
                                pipe(front, back)
                        flush()
                        if HEAD_BARRIER:
                            S.barrier()
            S.barrier()
        OBT_ALL = [("obT", hp, s, hh) for hp in range(4) for s in range(4) for hh in range(2)]
        if "obT" in debug:
            o = dbg_out("obT", [128, 4, S_TOK], BF16)
            S.dma("sp", lambda e, o=o: e.dma_start(out=o, in_=obT[:]), slot(), reads=OBT_ALL, final=True)

        XT_ALL = [("xT", i) for i in range(NT)]
        yaT_es = contextlib.ExitStack()
        yaT = sbt(yaT_es, "yaT", [128, 4, S_TOK], BF16)
        wring[:] = [sbt(yaT_es, "wringB%d" % i, [128, KC, 512], BF16) for i in range(4)]
        wr_state["n"] = 0
        with contextlib.ExitStack() as es2:
            vg = sbt(es2, "vg", [128, NT, 512], F32)
            lng = sbt(es2, "lng", [128, 512], F32)
            lnb = sbt(es2, "lnb", [128, 512], F32)
            wsf = sbt(es2, "wsf", [128, 8, 128], F32)
            WmT = sbt(es2, "wmT", [128, 8, 128], BF16)
            bsf = sbt(es2, "bsf", [2, 1024], F32)
            bsh = sbt(es2, "bsh", [2, 1024], BF16)
            bshf = sbt(es2, "bshf", [2, 1024], F32)
            bsl = sbt(es2, "bsl", [2, 1024], BF16)
            sqb = [sbt(es2, "sqb%d" % i, [128, 512], F32) for i in range(3)]
            tnb = [sbt(es2, "tnb%d" % i, [128, 512], F32) for i in range(3)]
            st6 = sbt(es2, "st6", [128, NT, 6], F32)
            mv = sbt(es2, "mv", [128, NT, 2], F32)
            xe = sbt(es2, "xeA", [128, NT], F32)
            lnt = [sbt(es2, "lnt%d" % i, [128, 512], F32) for i in range(2)]
            vln = [sbt(es2, "vln%d" % i, [128, 512], BF16) for i in range(2)]
            bu = load_wblk("u")
            bv = load_wblk("v")
            S.dma("sp", lambda e: e.dma_start(out=lng[:], in_=alng_d[0].partition_broadcast(128)), slot("c"), writes=["lng"])
            S.dma("sp", lambda e: e.dma_start(out=lnb[:], in_=alnb_d[0].partition_broadcast(128)), slot("c"), writes=["lnb"])
            S.dma("sp", lambda e: e.dma_start(out=wsf[:], in_=awsT_d), slot("c"), writes=["wsf"])
            S.dma("sp", lambda e: e.dma_start(out=bsf[0:1, :], in_=abs_d), slot("c"), writes=["bsf0"])
            S.dma("sp", lambda e: e.dma_start(out=bsf[1:2, :], in_=abs_d), slot("c"), writes=["bsf1"])
            S.op("dve", lambda e: e.tensor_tensor(out=WmT[:], in0=wsf[:],
                                                  in1=mown4[:].rearrange("p (a b) -> p a b", a=4)[:, 0:1, :].to_broadcast([128, 8, 128]),
                                                  op=ALU.mult), reads=["wsf", "mown4"], writes=["WmT"])
            S.op("dve", lambda e: e.tensor_copy(bsh[:], bsf[:]), reads=["bsf0", "bsf1"], writes=["bsh"])
            S.op("dve", lambda e: e.tensor_copy(bshf[:], bsh[:]), reads=["bsh"], writes=["bshf"])
            S.op("dve", lambda e: e.tensor_tensor(out=bshf[:], in0=bsf[:], in1=bshf[:], op=ALU.subtract), reads=["bshf"], writes=["bshf"])
            S.op("dve", lambda e: e.tensor_copy(bsl[:], bshf[:]), reads=["bshf"], writes=["bsl"])
            S.dma("sp", lambda e: e.dma_start(out=bsh[1:2, :], in_=bsl[1:2, :]), slot("c"), reads=["bsl", "bsh"], writes=["bsh"])

            grr = [0]

            def gelu_front(bk):
                grr[0] += 1
                idx = grr[0] % 3
                sq = sqb[idx]
                ks = ("sqb", idx)
                S.op("act", lambda e: e.activation(out=sq[:], in_=ps[:, bk, :], func=AF.Square), reads=[], writes=[B(bk), ks])
                S.op("pool", lambda e: e.tensor_scalar(out=sq[:], in0=sq[:], scalar1=0.044715, scalar2=1.0,
                                                       op0=ALU.mult, op1=ALU.add), reads=[], writes=[ks])
                S.op("dve", lambda e: e.tensor_tensor(out=sq[:], in0=sq[:], in1=ps[:, bk, :], op=ALU.mult),
                     reads=[], writes=[B(bk), ks])
                return bk, idx

            def gelu_back(st, out_ap, wkeys, after=None):
                bk, idx = st
                sq, tn = sqb[idx], tnb[idx]
                ks, kt = ("sqb", idx), ("tnb", idx)
                S.op("act", lambda e: e.activation(out=tn[:], in_=sq[:], func=AF.Tanh, scale=GELU_C), reads=[ks], writes=[kt])
                S.op("dve", lambda e: e.scalar_tensor_tensor(out=out_ap, in0=tn[:], scalar=1.0, in1=ps[:, bk, :],
                                                             op0=ALU.add, op1=ALU.mult), reads=[kt], writes=[B(bk)] + wkeys)
                if after is not None:
                    after()

            gpend = []

            def gelu_pipe(bk, out_ap, wkeys, after=None):
                st = gelu_front(bk)
                if gpend:
                    gelu_back(*gpend.pop(0))
                gpend.append((st, out_ap, wkeys, after))

            ALLB = [0, 1, 2, 3, 4, 5, 6, 7]
            for fc in range(4):
                for sp in range(4):
                    bk = nb_(ALLB)
                    for kc in range(KC):
                        S.op("pe", (lambda bk=bk, kc=kc, fc=fc, sp=sp, wt=WB[bu]: (lambda e: e.matmul(
                            ps[:, bk, :], lhsT=wt[:, kc, fc * 128:(fc + 1) * 128], rhs=xT[:, kc, sp * 512:(sp + 1) * 512],
                            start=(kc == 0), stop=(kc == KC - 1))))(),
                            reads=[("xT", 4 * sp + j) for j in range(4)] + [("wr", bu)], writes=[B(bk)])
                    gelu_pipe(bk, yaT[:, fc, sp * 512:(sp + 1) * 512], [("yaT", fc, 4 * sp + j) for j in range(4)])
            for i in range(NT):
                bk = nb_(ALLB)
                for kc in range(KC):
                    S.op("pe", (lambda bk=bk, kc=kc, i=i, wt=WB[bv]: (lambda e: e.matmul(
                        ps[:, bk, :], lhsT=xT[:, kc, i * 128:(i + 1) * 128], rhs=wt[:, kc, 0:512],
                        start=(kc == 0), stop=(kc == KC - 1))))(),
                        reads=[("xT", i), ("wr", bv)], writes=[B(bk)])

                def stats(i=i):
                    S.op("dve", (lambda i=i: (lambda e: e.bn_stats(out=st6[:, i, :], in_=vg[:, i, :])))(), reads=[("vg", i)], writes=[("st6", i)])
                    S.op("dve", (lambda i=i: (lambda e: e.bn_aggr(out=mv[:, i, :], in_=st6[:, i, :])))(), reads=[("st6", i)], writes=[("mvA", i)])
                gelu_pipe(bk, vg[:, i, :], [("vg", i)], stats)
            while gpend:
                gelu_back(*gpend.pop(0))
            S.op("dve", lambda e: e.tensor_scalar(out=xe[:], in0=mv[:, :, 1], scalar1=4.0 * LN_EPS, scalar2=None, op0=ALU.add),
                 reads=[("mvA", i) for i in range(NT)], writes=[("rsx", "A")])
            rstd, krs, _ = rsqrt_batch(es2, "A", xe[:], NT)

            def ln_tile(i):
                lt = lnt[i % 2]
                vl = vln[i % 2]
                S.op("dve", (lambda i=i, lt=lt: (lambda e: e.scalar_tensor_tensor(
                    out=lt[:], in0=vg[:, i, :], scalar=mv[:, i, 0:1], in1=lng[:], op0=ALU.subtract, op1=ALU.mult)))(),
                    reads=[("vg", i), ("mvA", i), "lng"], writes=[("lnt", i % 2)])
                S.op("dve", (lambda i=i, lt=lt, vl=vl: (lambda e: e.scalar_tensor_tensor(
                    out=vl[:], in0=lt[:], scalar=rstd[:, i:i + 1], in1=lnb[:], op0=ALU.mult, op1=ALU.add)))(),
                    reads=["lnb", ("lnt", i % 2), krs], writes=[("vln", i % 2)])

            ln_tile(0)
            for i in range(NT):
                vl = vln[i % 2]
                bk = nb_(ALLB)
                for cc in range(4):
                    for gg in range(2):
                        g = 2 * cc + gg
                        o_ap = ps[gg * 64:(gg + 1) * 64, bk, cc * 128:(cc + 1) * 128]
                        S.op("pe", (lambda o_ap=o_ap, g=g, vl=vl: (lambda e: e.matmul(
                            o_ap, lhsT=vl[:, g * 64:(g + 1) * 64], rhs=WmT[:, g, :], start=True, stop=False)))(),
                            reads=[("vln", i % 2), "WmT"], writes=[B(bk)])
                        S.op("pe", (lambda o_ap=o_ap, g=g: (lambda e: e.matmul(
                            o_ap, lhsT=onesb[0:2, 0:64], rhs=bsh[0:2, g * 128:(g + 1) * 128], start=False, stop=True)))(),
                            reads=["bsh", "onesb"], writes=[B(bk)])
                if i + 1 < NT:
                    ln_tile(i + 1)
                ya_v = yaT[:, :, i * 128:(i + 1) * 128]
                S.op("dve", (lambda bk=bk, ya_v=ya_v: (lambda e: e.scalar_tensor_tensor(
                    out=ya_v, in0=ps[:, bk, :].rearrange("p (c t) -> p c t", c=4), scalar=0.5, in1=ya_v, op0=ALU.mult, op1=ALU.mult)))(),
                    reads=[], writes=[B(bk)] + [("yaT", fc, i) for fc in range(4)])
            S.barrier()
        YAT_ALL = [("yaT", fc, i) for fc in range(4) for i in range(NT)]
        if "yaT" in debug:
            o = dbg_out("yaT", [128, 4, S_TOK], BF16)
            S.dma("sp", lambda e, o=o: e.dma_start(out=o, in_=yaT[:]), slot(), reads=YAT_ALL, final=True)

        with contextlib.ExitStack() as es3:
            wa = sbt(es3, "wa", [128, 4, D], BF16)
            wb = sbt(es3, "wb", [128, 4, D], BF16)
            ta = [sbt(es3, "ta%d" % i, [128, 512], BF16) for i in range(2)]
            tb = [sbt(es3, "tb%d" % i, [128, 512], BF16) for i in range(2)]
            m1 = [sbt(es3, "m1_%d" % i, [128, 512], F32) for i in range(2)]
            m2 = [sbt(es3, "m2_%d" % i, [128, 512], F32) for i in range(2)]
            S.dma("pool", lambda e: e.dma_start(out=wa[:], in_=wa_d.rearrange("(c p) d -> p c d", p=128)), slot("c"), writes=["wa"])
            S.dma("pool", lambda e: e.dma_start(out=wb[:], in_=wb_d.rearrange("(c p) d -> p c d", p=128)), slot("c"), writes=["wb"])
            gblk = {}
            gblk[0] = load_wblk(("g", 0))
            gblk[1] = load_wblk(("g", 1))
            gblk[2] = load_wblk(("g", 2))
            gblk[3] = load_wblk(("g", 3))
            it_ = 0
            for dc in range(KC):
                ga_i = gblk[2 * (dc // 4)]
                gb_i = gblk[2 * (dc // 4) + 1]
                for sp in range(4):
                    it_ += 1
                    par = it_ % 2
                    bA, bB, bC, bD = [4 * par + j for j in range(4)]
                    tok = slice(sp * 512, (sp + 1) * 512)
                    xk = [("xT", 4 * sp + j) for j in range(4)]
                    for cc in range(4):
                        S.op("pe", (lambda cc=cc, bA=bA, dc=dc, tok=tok: (lambda e: e.matmul(
                            ps[:, bA, :], lhsT=wa[:, cc, dc * 128:(dc + 1) * 128], rhs=yaT[:, cc, tok], start=(cc == 0), stop=(cc == 3))))(),
                            reads=["wa"] + [("yaT", cc, 4 * sp + j) for j in range(4)], writes=[B(bA)])
                    for cc in range(4):
                        S.op("pe", (lambda cc=cc, bB=bB, dc=dc, tok=tok: (lambda e: e.matmul(
                            ps[:, bB, :], lhsT=wb[:, cc, dc * 128:(dc + 1) * 128], rhs=obT[:, cc, tok], start=(cc == 0), stop=(cc == 3))))(),
                            reads=["wb"] + [("obT", cc, sp, 0), ("obT", cc, sp, 1)], writes=[B(bB)])
                    for (bX, gi) in ((bC, ga_i), (bD, gb_i)):
                        for kc in range(KC):
                            S.op("pe", (lambda kc=kc, bX=bX, wt=WB[gi], dc=dc, tok=tok: (lambda e: e.matmul(
                                ps[:, bX, :], lhsT=wt[:, kc, (dc % 4) * 128:(dc % 4 + 1) * 128], rhs=xT[:, kc, tok],
                                start=(kc == 0), stop=(kc == KC - 1))))(),
                                reads=xk + [("wr", gi)], writes=[B(bX)])
                    S.op("act", (lambda bC=bC, par=par: (lambda e: e.activation(out=ta[par][:], in_=ps[:, bC, :], func=AF.Tanh, scale=0.5)))(),
                         reads=[], writes=[B(bC), ("ta", par)])
                    S.op("act", (lambda bD=bD, par=par: (lambda e: e.activation(out=tb[par][:], in_=ps[:, bD, :], func=AF.Tanh, scale=0.5)))(),
                         reads=[], writes=[B(bD), ("tb", par)])
                    S.op("dve", (lambda bA=bA, par=par: (lambda e: e.scalar_tensor_tensor(
                        out=m1[par][:], in0=ta[par][:], scalar=1.0, in1=ps[:, bA, :], op0=ALU.add, op1=ALU.mult)))(),
                        reads=[("ta", par)], writes=[B(bA), ("m1", par)])
                    S.op("dve", (lambda bB=bB, par=par: (lambda e: e.scalar_tensor_tensor(
                        out=m2[par][:], in0=tb[par][:], scalar=1.0, in1=ps[:, bB, :], op0=ALU.add, op1=ALU.mult)))(),
                        reads=[("tb", par)], writes=[B(bB), ("m2", par)])
                    S.op("pool", (lambda par=par, dc=dc, tok=tok: (lambda e: e.tensor_tensor(
                        out=mixinT[:, dc, tok], in0=m1[par][:], in1=m2[par][:], op=ALU.add)))(),
                        reads=[("m1", par), ("m2", par)], writes=[("mix", dc, 4 * sp + j) for j in range(4)])
            S.barrier()
        yaT_es.close()
        mx_es.close()
        if "mixinT" in debug:
            o = dbg_out("mixinT", [128, KC, S_TOK], BF16)
            S.dma("sp", lambda e, o=o: e.dma_start(out=o, in_=mixinT[:]), slot(),
                  reads=[("mix", dc, i) for dc in range(KC) for i in range(NT)], final=True)

        p5_es = contextlib.ExitStack()
        x1b = sbt(p5_es, "x1b", [128, NT, D], BF16)
        logit = sbt(p5_es, "logit", [128, NT, 36], F32)
        G4 = 2
        with contextlib.ExitStack() as es4:
            wo = sbt(es4, "wo", [128, KC, D], BF16)
            wpg = sbt(es4, "wpg", [128, KC, D], BF16)
            wple = sbt(es4, "wple", [128, 2, D], BF16)
            wr = sbt(es4, "wr", [128, KC, 36], F32)
            wrh = sbt(es4, "wrh", [128, KC, 36], BF16)
            wrhf = sbt(es4, "wrhf", [128, KC, 36], F32)
            wrl = sbt(es4, "wrl", [128, KC, 36], BF16)
            x1lo = [sbt(es4, "x1lo%d" % i, [128, D], BF16) for i in range(3)]
            brb = sbt(es4, "brb", [128, 36], F32)
            l1g = sbt(es4, "l1g", [128, D], F32)
            l1b = sbt(es4, "l1b", [128, D], F32)
            xres = [sbt(es4, "xres%d" % i, [128, D], F32) for i in range(2)]
            rg = [sbt(es4, "rg%d" % q, [128, G4, D], F32) for q in range(2)]
            st12 = [sbt(es4, "st12_%d" % q, [128, G4, 12], F32) for q in range(2)]
            mv1 = [sbt(es4, "mv1_%d" % q, [128, G4, 2], F32) for q in range(2)]
            xe1 = [sbt(es4, "xe1_%d" % q, [128, G4], F32) for q in range(2)]
            rsq1 = [(sbt(es4, "rs_tfL1_%d" % q, [128, G4], F32), sbt(es4, "rs_yiL1_%d" % q, [128, G4], I32),
                     sbt(es4, "rs_aL1_%d" % q, [128, G4], F32)) for q in range(2)]
            x1f = [sbt(es4, "x1f%d" % i, [128, D], F32) for i in range(3)]
            x1Tf = [sbt(es4, "x1Tl%d" % i, [128, KC, 128], BF16) for i in range(2)]
            x1Tb = [sbt(es4, "x1Tb%d" % i, [128, KC, 128], BF16) for i in range(2)]
            pst = [sbt(es4, "pst%d" % i, [128, 256], BF16) for i in range(2)]
            pT = [sbt(es4, "pT%d" % i, [128, 2, 128], BF16) for i in range(2)]
            tp = [sbt(es4, "tp%d" % i, [128, D], BF16) for i in range(2)]
            ple2 = [sbt(es4, "ple2_%d" % i, [128, D], F32) for i in range(2)]
            S.dma("pool", lambda e: e.dma_start(out=wo[:], in_=wo_d.rearrange("(c p) d -> p c d", p=128)), slot("c"), writes=["wo"])
            S.dma("pool", lambda e: e.dma_start(out=wpg[:], in_=wpg_d.rearrange("(c p) d -> p c d", p=128)), slot("c"), writes=["wpg"])
            S.dma("pool", lambda e: e.dma_start(out=wple[:], in_=wple_d.rearrange("(c p) d -> p c d", p=128)), slot("c"), writes=["wple"])
            S.dma("sp", lambda e: e.dma_start(out=wr[:], in_=wr_d.rearrange("(c p) d -> p c d", p=128)), slot("c"), writes=["wr_"])
            S.dma("sp", lambda e: e.dma_start(out=brb[:], in_=br_d[0].partition_broadcast(128)), slot("c"), writes=["brb"])
            S.op("dve", lambda e: e.tensor_copy(wrh[:], wr[:]), reads=["wr_"], writes=["wrh"])
            S.op("dve", lambda e: e.tensor_copy(wrhf[:], wrh[:]), reads=["wrh"], writes=["wrhf"])
            S.op("dve", lambda e: e.tensor_tensor(out=wrl[:], in0=wr[:], in1=wrhf[:], op=ALU.subtract), reads=["wr_", "wrhf"], writes=["wrl"])
            S.dma("sp", lambda e: e.dma_start(out=l1g[:], in_=ln1g_d[0].partition_broadcast(128)), slot("c"), writes=["l1g"])
            S.dma("sp", lambda e: e.dma_start(out=l1b[:], in_=ln1b_d[0].partition_broadcast(128)), slot("c"), writes=["l1b"])
            NG4 = NT // G4

            def load_xres(i):
                xr = xres[i % 2]
                S.dma("sp", (lambda i=i, xr=xr: (lambda e: e.dma_start(out=xr[:], in_=x_d[i * 128:(i + 1) * 128, :])))(),
                      "xres%d" % (i % 2), writes=[("xres", i % 2)])

            def part1_tile(g, gi):
                gp = g % 2
                i = g * G4 + gi
                xr = xres[i % 2]
                if i == 0:
                    load_xres(0)
                if i + 1 < NT:
                    load_xres(i + 1)
                S.op("act", (lambda xr=xr: (lambda e: e.activation(out=xr[:], in_=xr[:], func=AF.Identity, scale=ALPHA)))(),
                     reads=[], writes=[("xres", i % 2)])
                for half in range(2):
                    bk = half
                    for dc in range(KC):
                        S.op("pe", (lambda bk=bk, dc=dc, i=i, half=half: (lambda e: e.matmul(
                            ps[:, bk, :], lhsT=mixinT[:, dc, i * 128:(i + 1) * 128], rhs=wo[:, dc, half * 512:(half + 1) * 512],
                            start=(dc == 0), stop=(dc == KC - 1))))(),
                            reads=[("mix", dc, i), "wo"], writes=[B(bk)])
                    S.op("dve", (lambda bk=bk, gi=gi, gp=gp, half=half, xr=xr: (lambda e: e.scalar_tensor_tensor(
                        out=rg[gp][:, gi, half * 512:(half + 1) * 512], in0=ps[:, bk, :], scalar=0.5,
                        in1=xr[:, half * 512:(half + 1) * 512], op0=ALU.mult, op1=ALU.add)))(),
                        reads=[("xres", i % 2)], writes=[B(bk), ("rg", gp, gi, half)])
                    S.op("dve", (lambda gi=gi, gp=gp, half=half: (lambda e: e.bn_stats(
                        out=st12[gp][:, gi, half * 6:(half + 1) * 6], in_=rg[gp][:, gi, half * 512:(half + 1) * 512])))(),
                        reads=[("rg", gp, gi, half)], writes=[("st12", gp, gi, half)])
                S.op("dve", (lambda gi=gi, gp=gp: (lambda e: e.bn_aggr(out=mv1[gp][:, gi, :], in_=st12[gp][:, gi, :])))(),
                     reads=[("st12", gp, gi, 0), ("st12", gp, gi, 1)], writes=[("mv1", gp, gi)])

            def rsqrt_grp(g):
                gp = g % 2
                tag = "L1_%d" % g
                xe_ = xe1[gp]
                tf_, yi_, a__ = rsq1[gp]
                kx, ky, ka, kt = ("rsx", "L1", gp), ("rsy", "L1", gp), ("rsa", "L1", gp), ("rst", "L1", gp)
                S.op("dve", lambda e: e.tensor_scalar(out=xe_[:], in0=mv1[gp][:, :, 1], scalar1=LN_EPS, scalar2=None, op0=ALU.add),
                     reads=[("mv1", gp, gi) for gi in range(G4)], writes=[kx])
                S.op("dve", lambda e: e.tensor_copy(tf_[:], xe_[:].bitcast(I32)), reads=[kx], writes=[kt])
                S.op("dve", lambda e: e.tensor_scalar(out=tf_[:], in0=tf_[:], scalar1=-0.5, scalar2=1597463007.0,
                                                      op0=ALU.mult, op1=ALU.add), reads=[kt], writes=[kt])
                S.op("dve", lambda e: e.tensor_copy(yi_[:], tf_[:]), reads=[kt], writes=[ky])
                y1 = yi_[:].bitcast(F32)
                for _ in range(2):
                    S.op("dve", lambda e: e.tensor_tensor(out=a__[:], in0=y1, in1=y1, op=ALU.mult), reads=[ky], writes=[ka])
                    S.op("dve", lambda e: e.tensor_tensor(out=a__[:], in0=a__[:], in1=xe_[:], op=ALU.mult), reads=[ka, kx], writes=[ka])
                    S.op("dve", lambda e: e.tensor_scalar(out=a__[:], in0=a__[:], scalar1=-0.5, scalar2=1.5,
                                                          op0=ALU.mult, op1=ALU.add), reads=[ka], writes=[ka])
                    S.op("dve", lambda e: e.tensor_tensor(out=y1, in0=y1, in1=a__[:], op=ALU.mult), reads=[ka, ky], writes=[ky])

            def ln_norm(g, gi):
                gp = g % 2
                i = g * G4 + gi
                y1 = rsq1[gp][1][:].bitcast(F32)
                ky = ("rsy", "L1", gp)
                xf = x1f[i % 3]
                kxf = ("x1f", i % 3)
                S.op("dve", (lambda gi=gi, gp=gp, xf=xf: (lambda e: e.scalar_tensor_tensor(
                    out=xf[:], in0=rg[gp][:, gi, :], scalar=mv1[gp][:, gi, 0:1], in1=l1g[:], op0=ALU.subtract, op1=ALU.mult)))(),
                    reads=[("rg", gp, gi, 0), ("rg", gp, gi, 1), ("mv1", gp, gi), "l1g"], writes=[kxf])
                S.op("dve", (lambda gi=gi, xf=xf, y1=y1: (lambda e: e.scalar_tensor_tensor(
                    out=xf[:], in0=xf[:], scalar=y1[:, gi:gi + 1], in1=l1b[:], op0=ALU.mult, op1=ALU.add)))(),
                    reads=[ky, "l1b"], writes=[kxf])
                S.op("act", (lambda xf=xf, i=i: (lambda e: e.activation(out=x1b[:, i, :], in_=xf[:], func=AF.Copy)))(),
                     reads=[kxf], writes=[("x1b", i)])

            def ln_lo(g, gi):
                i = g * G4 + gi
                xf = x1f[i % 3]
                xlo = x1lo[i % 3]
                S.op("dve", (lambda xf=xf, xlo=xlo, i=i: (lambda e: e.tensor_tensor(out=xlo[:], in0=xf[:], in1=x1b[:, i, :], op=ALU.subtract)))(),
                     reads=[("x1f", i % 3), ("x1b", i)], writes=[("x1lo", i % 3)])

            def load_p(i):
                pp = pst[i % 2]
                S.dma("pool", (lambda i=i, pp=pp: (lambda e: e.dma_start(out=pp[:], in_=p_d[i * 128:(i + 1) * 128, :])))(),
                      "pst%d" % (i % 2), writes=[("pst", i % 2)])

            def part2_tile(g, gi, mid=None):
                i = g * G4 + gi
                xf = x1f[i % 3]
                kxf = ("x1f", i % 3)
                xlo = x1lo[i % 3]
                if i == 0:
                    load_p(0)
                if i + 1 < NT:
                    load_p(i + 1)
                if True:
                    if True:
                        xtf, xtb = x1Tf[i % 2], x1Tb[i % 2]
                        psh_ = ps[:, 2, :].bitcast(BF16)
                        psl_ = ps[:, 3, :].bitcast(BF16)
                        for kc in range(KC):
                            S.op("pe", (lambda kc=kc, i=i, psh_=psh_: (lambda e: e.transpose(
                                out=psh_[:, kc * 128:(kc + 1) * 128], in_=x1b[:, i, kc * 128:(kc + 1) * 128], identity=identb[:])))(),
                                reads=[("x1b", i), "identb"], writes=[B(2)])
                        S.op("dve", (lambda xtb=xtb, psh_=psh_: (lambda e: e.tensor_copy(xtb[:], psh_.rearrange("p (j t) -> p j t", j=KC))))(),
                             reads=[], writes=[B(2), ("x1Tb", i % 2)])
                        for kc in range(KC):
                            S.op("pe", (lambda kc=kc, xlo=xlo, psl_=psl_: (lambda e: e.transpose(
                                out=psl_[:, kc * 128:(kc + 1) * 128], in_=xlo[:, kc * 128:(kc + 1) * 128], identity=identb[:])))(),
                                reads=[("x1lo", i % 3), "identb"], writes=[B(3)])
                        S.op("act", (lambda xtf=xtf, psl_=psl_: (lambda e: e.activation(out=xtf[:], in_=psl_.rearrange("p (j t) -> p j t", j=KC), func=AF.Copy)))(),
                             reads=[], writes=[B(3), ("x1Tl", i % 2)])
                        pp = pst[i % 2]
                        psp_ = ps[:, 4, :].bitcast(BF16)
                        for c in range(2):
                            S.op("pe", (lambda c=c, pp=pp, psp_=psp_: (lambda e: e.transpose(
                                out=psp_[:, 512 + c * 128:512 + (c + 1) * 128], in_=pp[:, c * 128:(c + 1) * 128], identity=identb[:])))(),
                                reads=[("pst", i % 2), "identb"], writes=[B(4)])
                        S.op("act", (lambda i=i, psp_=psp_: (lambda e: e.activation(
                            out=pT[i % 2][:], in_=psp_[:, 512:768].rearrange("p (c t) -> p c t", c=2), func=AF.Copy)))(),
                            reads=[], writes=[B(4), ("pT", i % 2)])
                        if mid is not None:
                            mid()
                        def ple_half(half):
                            bg = 5
                            bl = 6 + half
                            for kc in range(KC):
                                S.op("pe", (lambda kc=kc, half=half, xtb=xtb: (lambda e: e.matmul(
                                    ps[:, 5, :], lhsT=xtb[:, kc, :], rhs=wpg[:, kc, half * 512:(half + 1) * 512],
                                    start=(kc == 0), stop=(kc == KC - 1))))(),
                                    reads=[("x1Tb", i % 2), "wpg"], writes=[B(5)])
                            S.op("act", (lambda half=half, i=i: (lambda e: e.activation(
                                out=tp[i % 2][:, half * 512:(half + 1) * 512], in_=ps[:, 5, :], func=AF.Tanh, scale=0.5)))(),
                                reads=[], writes=[B(5), ("tp", i % 2, half)])
                            for c in range(2):
                                S.op("pe", (lambda c=c, half=half, bl=bl, i=i: (lambda e: e.matmul(
                                    ps[:, bl, :], lhsT=pT[i % 2][:, c, :], rhs=wple[:, c, half * 512:(half + 1) * 512],
                                    start=(c == 0), stop=(c == 1))))(),
                                    reads=[("pT", i % 2), "wple"], writes=[B(bl)])
                            S.op("dve", (lambda half=half, bl=bl, i=i: (lambda e: e.scalar_tensor_tensor(
                                out=ple2[i % 2][:, half * 512:(half + 1) * 512], in0=tp[i % 2][:, half * 512:(half + 1) * 512],
                                scalar=1.0, in1=ps[:, bl, :], op0=ALU.add, op1=ALU.mult)))(),
                                reads=[("tp", i % 2, half)], writes=[B(bl), ("ple2", i % 2, half)])
                        ple_half(0)
                        passes = [(xtb, wrh), (xtf, wrh), (xtb, wrl)]
                        for pi, (xa, wa_) in enumerate(passes):
                            for kc in range(KC):
                                S.op("pe", (lambda kc=kc, xa=xa, wa_=wa_, pi=pi: (lambda e: e.matmul(
                                    ps[:, 4, 0:36], lhsT=xa[:, kc, :], rhs=wa_[:, kc, :], start=(pi == 0 and kc == 0), stop=(pi == 2 and kc == KC - 1))))(),
                                    reads=[("x1Tb", i % 2), ("x1Tl", i % 2), "wrh", "wrl"], writes=[B(4)])
                        S.op("dve", (lambda i=i: (lambda e: e.tensor_tensor(out=logit[:, i, :], in0=ps[:, 4, 0:36], in1=brb[:], op=ALU.add)))(),
                             reads=["brb"], writes=[B(4), ("logit", i)])
                        ple_half(1)
                        S.op("act", (lambda xf=xf: (lambda e: e.activation(out=xf[:], in_=xf[:], func=AF.Identity, scale=ALPHA)))(),
                             reads=[], writes=[kxf])
                        S.op("pool", (lambda i=i: (lambda e: e.tensor_scalar(
                            out=ple2[i % 2][:], in0=ple2[i % 2][:], scalar1=0.5, scalar2=0.0, op0=ALU.mult, op1=ALU.add)))(),
                            reads=[], writes=[("ple2", i % 2, 0), ("ple2", i % 2, 1)])
                        S.op("pool", (lambda xf=xf, i=i: (lambda e: e.tensor_tensor(
                            out=ple2[i % 2][:], in0=ple2[i % 2][:], in1=xf[:], op=ALU.add)))(),
                            reads=[kxf], writes=[("ple2", i % 2, 0), ("ple2", i % 2, 1)])
                        S.dma("sp", (lambda i=i: (lambda e: e.dma_start(out=base_d[i * 128:(i + 1) * 128, :], in_=ple2[i % 2][:])))(),
                              "base%d" % (i % 2), reads=[("ple2", i % 2, 0), ("ple2", i % 2, 1)], writes=[("base", i)])

            def ln_grp(g):
                rsqrt_grp(g)
                for gi in range(G4):
                    ln_norm(g, gi)
                for gi in range(G4):
                    ln_lo(g, gi)

            for gi in range(G4):
                part1_tile(0, gi)
            ln_grp(0)
            for g in range(NG4):
                for gi in range(G4):
                    mid = None
                    if g + 1 < NG4:
                        part1_tile(g + 1, gi)
                        if gi == G4 - 1:
                            mid = (lambda g=g: ln_grp(g + 1))
                    part2_tile(g, gi, mid)
            S.barrier()
        if "logit" in debug:
            o = dbg_out("logit", [128, NT, 36])
            S.dma("sp", lambda e, o=o: e.dma_start(out=o, in_=logit[:]), slot(), reads=[("logit", i) for i in range(NT)], final=True)
        if "x1b" in debug:
            o = dbg_out("x1b", [128, NT, D], BF16)
            S.dma("sp", lambda e, o=o: e.dma_start(out=o, in_=x1b[:]), slot(), reads=[("x1b", i) for i in range(NT)], final=True)

        with contextlib.ExitStack() as es5:
            LG = [("logit", i) for i in range(NT)]
            gmax = sbt(es5, "gmax", [128, NT], F32)
            gd = sbt(es5, "gd", [128, NT, 4], F32)
            gm = sbt(es5, "gm", [128, NT, 4], F32)
            gsum = sbt(es5, "gsum", [128, NT], F32)
            gp = sbt(es5, "gp", [128, NT], F32)
            selm = sbt(es5, "selm", [128, NT, 4, 8], F32)
            sel = sbt(es5, "sel", [128, NT, 8], F32)
            m8 = sbt(es5, "m8", [128, NT, 8], F32)
            e1 = sbt(es5, "e1", [128, NT, 8], F32)
            e2 = sbt(es5, "e2", [128, NT, 8], F32)
            dd = sbt(es5, "dd", [128, NT], F32)
            w1 = sbt(es5, "w1", [128, NT], F32)
            A1 = sbt(es5, "A1", [128, NT, 32], F32)
            A2 = sbt(es5, "A2", [128, NT, 32], F32)
            Ab = sbt(es5, "Ab", [128, NT, 32], BF16)
            cnt = sbt(es5, "cnt", [128, 32], F32)
            cnti = sbt(es5, "cnti", [128, 32], I32)
            ntf = sbt(es5, "ntf", [128, 32], F32)
            onesf = sbt(es5, "onesf", [128, 32], F32)
            cum = sbt(es5, "cum", [128, 32], F32)
            bas = sbt(es5, "bas", [128, 32], F32)
            rk = sbt(es5, "rk", [128, NT, 32], F32)
            tmpA = sbt(es5, "tmpA", [128, NT, 32], F32)
            slf = sbt(es5, "slf", [128, NT, 2], F32)
            cmp = sbt(es5, "cmp", [128, NTILE_E, 32], F32)
            tef = sbt(es5, "tef", [128, NTILE_E], F32)
            gl = logit[:, :, 0:4]
            el = logit[:, :, 4:36].rearrange("p n (g i) -> p n g i", g=4)
            V = "dve"
            S.op(V, lambda e: e.tensor_reduce(out=gmax[:], in_=gl, axis=AX.X, op=ALU.max), reads=LG, writes=["gmax"])
            S.op(V, lambda e: e.tensor_tensor(out=gd[:], in0=gl, in1=gmax[:].unsqueeze(2).to_broadcast([128, NT, 4]), op=ALU.subtract),
                 reads=LG + ["gmax"], writes=["gd"])
            S.op(V, lambda e: e.tensor_scalar(out=gm[:], in0=gd[:], scalar1=0.0, scalar2=None, op0=ALU.is_ge), reads=["gd"], writes=["gm"])
            S.op("act", lambda e: e.activation(out=gd[:], in_=gd[:], func=AF.Exp), reads=[], writes=["gd"])
            S.op(V, lambda e: e.tensor_reduce(out=gsum[:], in_=gd[:], axis=AX.X, op=ALU.add), reads=["gd"], writes=["gsum"])
            S.op(V, lambda e: e.reciprocal(out=gp[:], in_=gsum[:]), reads=["gsum"], writes=["gp"])
            S.op(V, lambda e: e.tensor_tensor(out=selm[:], in0=el, in1=gm[:].unsqueeze(3).to_broadcast([128, NT, 4, 8]), op=ALU.mult),
                 reads=LG + ["gm"], writes=["selm"])
            S.op(V, lambda e: e.tensor_reduce(out=sel[:], in_=selm[:].rearrange("p n g i -> p n i g"), axis=AX.X, op=ALU.add),
                 reads=["selm"], writes=["sel"])
            for i in range(NT):
                S.op(V, (lambda i=i: (lambda e: e.max(out=m8[:, i, :], in_=sel[:, i, :])))(), reads=["sel"], writes=[("m8", i)])
            M8 = [("m8", i) for i in range(NT)]
            S.op(V, lambda e: e.tensor_tensor(out=e1[:], in0=sel[:], in1=m8[:, :, 0:1].to_broadcast([128, NT, 8]), op=ALU.is_equal),
                 reads=["sel"] + M8, writes=["e1"])
            S.op(V, lambda e: e.tensor_tensor(out=e2[:], in0=sel[:], in1=m8[:, :, 1:2].to_broadcast([128, NT, 8]), op=ALU.is_equal),
                 reads=["sel"] + M8, writes=["e2"])
            S.op(V, lambda e: e.tensor_tensor(out=dd[:], in0=m8[:, :, 1], in1=m8[:, :, 0], op=ALU.subtract), reads=M8, writes=["dd"])
            S.op("act", lambda e: e.activation(out=dd[:], in_=dd[:], func=AF.Exp), reads=[], writes=["dd"])
            S.op(V, lambda e: e.tensor_scalar(out=w1[:], in0=dd[:], scalar1=1.0, scalar2=None, op0=ALU.add), reads=["dd"], writes=["w1"])
            S.op(V, lambda e: e.reciprocal(out=w1[:], in_=w1[:]), reads=[], writes=["w1"])
            S.op(V, lambda e: e.tensor_tensor(out=cw[:, :, 0], in0=w1[:], in1=gp[:], op=ALU.mult), reads=["w1", "gp"], writes=["cw0"])
            S.op(V, lambda e: e.tensor_tensor(out=dd[:], in0=dd[:], in1=w1[:], op=ALU.mult), reads=["w1"], writes=["dd"])
            S.op(V, lambda e: e.tensor_tensor(out=cw[:, :, 1], in0=dd[:], in1=gp[:], op=ALU.mult), reads=["dd", "gp"], writes=["cw1"])
            for (Ax, ex, nm) in ((A1, e1, "A1"), (A2, e2, "A2")):
                S.op(V, (lambda Ax=Ax, ex=ex: (lambda e: e.tensor_tensor(
                    out=Ax[:].rearrange("p n (g i) -> p n g i", g=4), in0=gm[:].unsqueeze(3).to_broadcast([128, NT, 4, 8]),
                    in1=ex[:].unsqueeze(2).to_broadcast([128, NT, 4, 8]), op=ALU.mult)))(),
                    reads=["gm", "e1", "e2"], writes=[nm])
            S.op(V, lambda e: e.tensor_tensor(out=Ab[:], in0=A1[:], in1=A2[:], op=ALU.add), reads=["A1", "A2"], writes=["Ab"])
            for i in range(NT):
                o_ap = ps[:, 0, i * 32:(i + 1) * 32]
                S.op("pe", (lambda i=i, o_ap=o_ap: (lambda e: e.matmul(o_ap, lhsT=lstr[:], rhs=Ab[:, i, :], start=True, stop=(i == 0))))(),
                     reads=["Ab", "lstr"], writes=[B(0)])
                for i2 in range(i):
                    S.op("pe", (lambda i2=i2, o_ap=o_ap, i=i: (lambda e: e.matmul(o_ap, lhsT=onesb[:], rhs=Ab[:, i2, :], start=False, stop=(i2 == i - 1))))(),
                         reads=["Ab", "onesb"], writes=[B(0)])
            for i in range(NT):
                S.op("pe", (lambda i=i: (lambda e: e.matmul(ps[:, 1, 0:32], lhsT=onesb[:], rhs=Ab[:, i, :], start=(i == 0), stop=(i == NT - 1))))(),
                     reads=["Ab", "onesb"], writes=[B(1)])
            S.op(V, lambda e: e.tensor_copy(rk[:], ps[:, 0, :].rearrange("p (n e) -> p n e", n=NT)), reads=[], writes=[B(0), "rk"])
            S.op(V, lambda e: e.tensor_scalar(out=cnt[:], in0=ps[:, 1, 0:32], scalar1=127.0, scalar2=None, op0=ALU.add), reads=[], writes=[B(1), "cnt"])
            S.op(V, lambda e: e.tensor_copy(cnti[:], cnt[:]), reads=["cnt"], writes=["cnti"])
            S.op(V, lambda e: e.tensor_scalar(out=cnti[:], in0=cnti[:], scalar1=7, scalar2=None, op0=ALU.arith_shift_right), reads=[], writes=["cnti"])
            S.op(V, lambda e: e.tensor_copy(ntf[:], cnti[:]), reads=["cnti"], writes=["ntf"])
            S.op(V, lambda e: e.memset(onesf[:], 1.0), writes=["onesf"])
            S.op(V, lambda e: e.tensor_tensor_scan(out=cum[:], data0=onesf[:], data1=ntf[:], initial=0.0, op0=ALU.mult, op1=ALU.add),
                 reads=["onesf", "ntf"], writes=["cum"])
            S.op(V, lambda e: e.tensor_tensor(out=bas[:], in0=cum[:], in1=ntf[:], op=ALU.subtract), reads=["cum", "ntf"], writes=["bas"])
            S.op(V, lambda e: e.tensor_scalar(out=bas[:], in0=bas[:], scalar1=128.0, scalar2=None, op0=ALU.mult), reads=[], writes=["bas"])
            S.op(V, lambda e: e.tensor_tensor(out=rk[:], in0=rk[:], in1=bas[:].unsqueeze(1).to_broadcast([128, NT, 32]), op=ALU.add),
                 reads=["bas"], writes=["rk"])
            for j, Ax in ((0, A1), (1, A2)):
                S.op(V, (lambda Ax=Ax: (lambda e: e.tensor_tensor(out=tmpA[:], in0=rk[:], in1=Ax[:], op=ALU.mult)))(),
                     reads=["rk", "A1", "A2"], writes=["tmpA"])
                S.op(V, (lambda j=j: (lambda e: e.tensor_reduce(out=slf[:, :, j], in_=tmpA[:], axis=AX.X, op=ALU.add)))(),
                     reads=["tmpA"], writes=[("slf", j)])
            S.op(V, lambda e: e.tensor_copy(slotu[:], slf[:]), reads=[("slf", 0), ("slf", 1)], writes=["slotu"])
            S.op(V, lambda e: e.tensor_tensor(out=cmp[:], in0=cum[:].unsqueeze(1).to_broadcast([128, NTILE_E, 32]),
                                              in1=kidx[:].unsqueeze(2).to_broadcast([128, NTILE_E, 32]), op=ALU.is_le),
                 reads=["cum", "kidx"], writes=["cmp"])
            S.op(V, lambda e: e.tensor_reduce(out=tef[:], in_=cmp[:], axis=AX.X, op=ALU.add), reads=["cmp"], writes=["tef"])
            S.op(V, lambda e: e.tensor_scalar(out=tef[:], in0=tef[:], scalar1=128.0, scalar2=None, op0=ALU.mult), reads=[], writes=["tef"])
            S.op(V, lambda e: e.tensor_copy(rowst[:], tef[:]), reads=["tef"], writes=["rowst"])
            kidm = sbt(es5, "kidm", [128, NTILE_E], F32)
            tefp = sbt(es5, "tefp", [128, NTILE_E], F32)
            same = sbt(es5, "same", [128, NTILE_E], F32)
            S.op(V, lambda e: e.tensor_scalar(out=kidm[:], in0=kidx[:], scalar1=-1.0, scalar2=None, op0=ALU.add), reads=["kidx"], writes=["kidm"])
            S.op(V, lambda e: e.tensor_tensor(out=cmp[:], in0=cum[:].unsqueeze(1).to_broadcast([128, NTILE_E, 32]),
                                              in1=kidm[:].unsqueeze(2).to_broadcast([128, NTILE_E, 32]), op=ALU.is_le),
                 reads=["cum", "kidm"], writes=["cmp"])
            S.op(V, lambda e: e.tensor_reduce(out=tefp[:], in_=cmp[:], axis=AX.X, op=ALU.add), reads=["cmp"], writes=["tefp"])
            S.op(V, lambda e: e.tensor_scalar(out=tefp[:], in0=tefp[:], scalar1=128.0, scalar2=None, op0=ALU.mult), reads=[], writes=["tefp"])
            S.op(V, lambda e: e.memset(tefp[:, 0:1], -128.0), reads=[], writes=["tefp"])
            S.op(V, lambda e: e.tensor_tensor(out=same[:], in0=tef[:], in1=tefp[:], op=ALU.is_equal), reads=["tef", "tefp", "rowst"], writes=["same"])
            S.op(V, lambda e: e.tensor_scalar(out=tef[:], in0=tef[:], scalar1=pidx[:, 0:1], scalar2=None, op0=ALU.add),
                 reads=["pidx", "rowst", "same"], writes=["tef"])
            S.op(V, lambda e: e.scalar_tensor_tensor(out=tef[:], in0=same[:], scalar=8192.0, in1=tef[:], op0=ALU.mult, op1=ALU.add),
                 reads=["same"], writes=["tef"])
            S.op(V, lambda e: e.tensor_copy(idxw[:], tef[:]), reads=["tef"], writes=["idxw"])
            if "route" in debug:
                o1 = dbg_out("slotu", [128, NT, 2], U32)
                o2 = dbg_out("cw", [128, NT, 2])
                o3 = dbg_out("idxw", [128, NTILE_E], U32)
                S.dma("sp", lambda e, o1=o1: e.dma_start(out=o1, in_=slotu[:]), slot(), reads=["slotu"], final=True)
                S.dma("sp", lambda e, o2=o2: e.dma_start(out=o2, in_=cw[:]), slot(), reads=["cw0", "cw1"], final=True)
                S.dma("sp", lambda e, o3=o3: e.dma_start(out=o3, in_=idxw[:]), slot(), reads=["idxw"], final=True)
            for i in range(NT if STOP_AFTER != "route" else 0):
                for j in range(2):
                    S.dma("pool", (lambda i=i, j=j: (lambda e: e.indirect_dma_start(
                        out=xs_d[:, :], out_offset=bass.IndirectOffsetOnAxis(ap=slotu[:, i, j:j + 1], axis=0),
                        in_=x1b[:, i, :], in_offset=None, bounds_check=preg(e, NTILE_E * 128 - 1), oob_is_err=False)))(),
                        "scat", reads=["slotu", ("x1b", i)] + [("xsz", q) for q in range(NTILE_E)], writes=[("xs", i, j)])
            S.barrier()
        p5_es.close()
        mix_es.close()
        XS_ALL = [("xs", i, j) for i in range(NT) for j in range(2)]

        with contextlib.ExitStack() as es6:
            NST = 4
            NWB = 4
            NXB = 4
            NXT = 3
            wbuf = [sbt(es6, "wbuf%d" % i, [128, 6144], BF16) for i in range(NWB)]
            NCH = 3
            wst = [sbt(es6, "wst%d" % c, [128, 2048], F32) for c in range(NCH)]
            xsb = [sbt(es6, "xsb%d" % i, [128, D], BF16) for i in range(NXB)]
            xsT = [sbt(es6, "xsT%d" % i, [128, KC, 128], BF16) for i in range(NXT)]
            tg = [sbt(es6, "tg%d" % i, [128, 256], F32) for i in range(2)]
            hb = [sbt(es6, "hb%d" % i, [128, 256], BF16) for i in range(2)]
            hT = [sbt(es6, "hT%d" % i, [128, 2, 128], BF16) for i in range(2)]
            ysb = [sbt(es6, "ysb%d" % i, [128, D], F32) for i in range(2)]

            def load_w(k):
                for c in range(NCH):
                    S.dma("pool", (lambda c=c, k=k: (lambda e: e.indirect_dma_start(
                        out=wst[c][:], out_offset=None, in_=wexp_d[c][:, :],
                        in_offset=bass.IndirectOffsetOnAxis(ap=idxw[:, k:k + 1], axis=0), bounds_check=preg(e, 4095), oob_is_err=False)))(),
                        "wst%d" % c, reads=["idxw"], writes=[("wst", c)])

            def cast_w(k):
                wi = k % NWB
                S.op("act", (lambda wi=wi: (lambda e: e.activation(out=wbuf[wi][:, 0:2048], in_=wst[0][:], func=AF.Copy)))(),
                     reads=[("wst", 0)], writes=[("wbuf", wi, 0)])
                S.op("dve", (lambda wi=wi: (lambda e: e.tensor_copy(wbuf[wi][:, 2048:4096], wst[1][:])))(),
                     reads=[("wst", 1)], writes=[("wbuf", wi, 1)])
                S.op("dve", (lambda wi=wi: (lambda e: e.tensor_copy(wbuf[wi][:, 4096:6144], wst[2][:])))(),
                     reads=[("wst", 2)], writes=[("wbuf", wi, 2)])

            def load_x(k):
                xi = k % NXB
                S.dma("sp", (lambda k=k, xi=xi: (lambda e: e.dma_start(out=xsb[xi][:], in_=xs_d[k * 128:(k + 1) * 128, :])))(),
                      "xsb%d" % xi, reads=XS_ALL, writes=[("xsb", xi)])

            NK = NTILE_E if STOP_AFTER != "route" else 0

            def stage_t(k):
                par = k % 2
                xi = k % NXB
                ti = k % NXT
                psb = ps[:, par, :].bitcast(BF16)
                for kc in range(KC):
                    S.op("pe", (lambda kc=kc, xi=xi, psb=psb: (lambda e: e.transpose(
                        out=psb[:, kc * 128:(kc + 1) * 128], in_=xsb[xi][:, kc * 128:(kc + 1) * 128], identity=identb[:])))(),
                        reads=[("xsb", xi), "identb"], writes=[B(par)])
                evac_copy(xsT[ti][:], psb.rearrange("p (c t) -> p c t", c=KC), reads=[], writes=[B(par), ("xsT", ti)])

            def stage_g(k):
                wi = k % NWB
                par = k % 2
                ti = k % NXT
                bgu = 2 + par
                for kc in range(KC):
                    S.op("pe", (lambda kc=kc, ti=ti, wi=wi, bgu=bgu: (lambda e: e.matmul(
                        ps[:, bgu, :], lhsT=xsT[ti][:, kc, :], rhs=wbuf[wi][:, kc * 512:(kc + 1) * 512],
                        start=(kc == 0), stop=(kc == KC - 1))))(),
                        reads=[("xsT", ti), ("wbuf", wi, kc // 4)], writes=[B(bgu)])
                S.op("act", (lambda par=par, bgu=bgu: (lambda e: e.activation(out=tg[par][:], in_=ps[:, bgu, 0:256], func=AF.Tanh, scale=0.5)))(),
                     reads=[], writes=[B(bgu), ("tg", par)])
                S.op("dve", (lambda par=par, bgu=bgu: (lambda e: e.scalar_tensor_tensor(
                    out=tg[par][:], in0=tg[par][:], scalar=1.0, in1=ps[:, bgu, 0:256], op0=ALU.add, op1=ALU.mult)))(),
                    reads=[], writes=[B(bgu), ("tg", par)])
                S.op("dve", (lambda par=par, bgu=bgu: (lambda e: e.scalar_tensor_tensor(
                    out=hb[par][:], in0=tg[par][:], scalar=0.5, in1=ps[:, bgu, 256:512], op0=ALU.mult, op1=ALU.mult)))(),
                    reads=[("tg", par)], writes=[B(bgu), ("hb", par)])

            def stage_b1(k):
                par = k % 2
                psh = ps[:, 4 + par, :].bitcast(BF16)
                for c in range(2):
                    S.op("pe", (lambda c=c, par=par, psh=psh: (lambda e: e.transpose(
                        out=psh[:, c * 128:(c + 1) * 128], in_=hb[par][:, c * 128:(c + 1) * 128], identity=identb[:])))(),
                        reads=[("hb", par), "identb"], writes=[B(4 + par)])
                S.op("act", (lambda par=par, psh=psh: (lambda e: e.activation(
                    out=hT[par][:], in_=psh[:, 0:256].rearrange("p (c t) -> p c t", c=2), func=AF.Copy)))(),
                    reads=[], writes=[B(4 + par), ("hT", par)])

            def stage_b2(k):
                wi = k % NWB
                par = k % 2
                for half in range(2):
                    by = 6 + half
                    for c in range(2):
                        S.op("pe", (lambda c=c, half=half, par=par, wi=wi, by=by: (lambda e: e.matmul(
                            ps[:, by, :], lhsT=hT[par][:, c, :],
                            rhs=wbuf[wi][:, 4096 + c * 1024 + half * 512:4096 + c * 1024 + (half + 1) * 512],
                            start=(c == 0), stop=(c == 1))))(),
                            reads=[("hT", par), ("wbuf", wi, 2)], writes=[B(by)])
                    evac_copy(ysb[par][:, half * 512:(half + 1) * 512], ps[:, by, :], reads=[], writes=[B(by), ("ysb", par, half)])
                S.dma("sp", (lambda k=k, par=par: (lambda e: e.dma_start(out=ys_d[k * 128:(k + 1) * 128, :], in_=ysb[par][:])))(),
                      "ysb%d" % par, reads=[("ysb", par, 0), ("ysb", par, 1)], writes=[("ys", k)])

            for k0 in range(min(3, NK)):
                load_x(k0)
            if NK:
                load_w(0)
            for k0 in range(min(3, NK)):
                cast_w(k0)
                if k0 + 1 < NK:
                    load_w(k0 + 1)
            for k0 in range(min(2, NK)):
                stage_t(k0)
            if NK:
                stage_g(0)
            for k in range(NK):
                stage_b1(k)
                if k + 3 < NK:
                    load_x(k + 3)
                if k + 2 < NK:
                    stage_t(k + 2)
                if k + 1 < NK:
                    stage_g(k + 1)
                stage_b2(k)
                if k + 3 < NK:
                    cast_w(k + 3)
                    if k + 4 < NK:
                        load_w(k + 4)
            S.barrier()
        YS_ALL = [("ys", k) for k in range(NTILE_E)]

        G7 = 4
        NG7 = NT // G7
        with contextlib.ExitStack() as es7:
            l2g = sbt(es7, "l2g", [128, D], F32)
            l2b = sbt(es7, "l2b", [128, D], F32)
            g1 = [sbt(es7, "g1_%d" % i, [128, D], F32) for i in range(4)]
            g2 = [sbt(es7, "g2_%d" % i, [128, D], F32) for i in range(4)]
            bt = [sbt(es7, "bt%d" % i, [128, D], F32) for i in range(4)]
            ot = [sbt(es7, "ot%d" % i, [128, D], F32) for i in range(4)]
            rg2 = [sbt(es7, "rg2_%d" % q, [128, G7, D], F32) for q in range(2)]
            st12b = [sbt(es7, "st12b_%d" % q, [128, G7, 12], F32) for q in range(2)]
            mv2 = [sbt(es7, "mv2_%d" % q, [128, G7, 2], F32) for q in range(2)]
            xe2 = [sbt(es7, "xe2_%d" % q, [128, G7], F32) for q in range(2)]
            sx2 = [sbt(es7, "sx2_%d" % q, [128, G7], F32) for q in range(2)]
            sq2 = [sbt(es7, "sq2_%d" % q, [128, G7], F32) for q in range(2)]
            mean2 = [sbt(es7, "mean2_%d" % q, [128, G7], F32) for q in range(2)]
            junk = sbt(es7, "junk7", [128, D], BF16)
            rstd2 = [sbt(es7, "rstd2_%d" % q, [128, G7], F32) for q in range(2)]
            nmr2 = [sbt(es7, "nmr2_%d" % q, [128, G7], F32) for q in range(2)]
            rsq2 = [(sbt(es7, "rs_tfL2_%d" % q, [128, G7], F32), sbt(es7, "rs_yiL2_%d" % q, [128, G7], I32),
                     sbt(es7, "rs_aL2_%d" % q, [128, G7], F32)) for q in range(2)]
            S.dma("sp", lambda e: e.dma_start(out=l2g[:], in_=ln2g_d[0].partition_broadcast(128)), slot("c"), writes=["l2g"])
            S.dma("sp", lambda e: e.dma_start(out=l2b[:], in_=ln2b_d[0].partition_broadcast(128)), slot("c"), writes=["l2b"])

            def p1_dma(g, gi):
                i = g * G7 + gi
                par = i % 4
                S.dma("pool", (lambda i=i, par=par: (lambda e: e.indirect_dma_start(
                    out=g1[par][:], out_offset=None, in_=ys_d[:, :],
                    in_offset=bass.IndirectOffsetOnAxis(ap=slotu[:, i, 0:1], axis=0),
                    bounds_check=preg(e, NTILE_E * 128 - 1), oob_is_err=False)))(),
                    "g1_%d" % par, reads=YS_ALL + ["slotu"], writes=[("g1", par)])
                S.dma("pool", (lambda i=i, par=par: (lambda e: e.indirect_dma_start(
                    out=g2[par][:], out_offset=None, in_=ys_d[:, :],
                    in_offset=bass.IndirectOffsetOnAxis(ap=slotu[:, i, 1:2], axis=0),
                    bounds_check=preg(e, NTILE_E * 128 - 1), oob_is_err=False)))(),
                    "g2_%d" % par, reads=YS_ALL + ["slotu"], writes=[("g2", par)])
                S.dma("sp", (lambda i=i, par=par: (lambda e: e.dma_start(out=bt[par][:], in_=base_d[i * 128:(i + 1) * 128, :])))(),
                      "bt%d" % par, reads=[("base", i)], writes=[("bt", par)])

            def p1_dve(g, gi):
                gp = g % 2
                i = g * G7 + gi
                par = i % 4
                S.op("dve", (lambda i=i, par=par: (lambda e: e.scalar_tensor_tensor(
                    out=bt[par][:], in0=g1[par][:], scalar=cw[:, i, 0:1], in1=bt[par][:], op0=ALU.mult, op1=ALU.add)))(),
                    reads=[("g1", par), "cw0"], writes=[("bt", par)])
                S.op("dve", (lambda i=i, par=par, gi=gi, gp=gp: (lambda e: e.scalar_tensor_tensor(
                    out=rg2[gp][:, gi, :], in0=g2[par][:], scalar=cw[:, i, 1:2], in1=bt[par][:], op0=ALU.mult, op1=ALU.add)))(),
                    reads=[("g2", par), "cw1", ("bt", par)], writes=[("rg2", gp, gi)])
                S.op("act", (lambda gi=gi, gp=gp: (lambda e: e.activation(
                    out=junk[:], in_=rg2[gp][:, gi, :], func=AF.Identity, accum_out=sx2[gp][:, gi:gi + 1])))(),
                    reads=[("rg2", gp, gi)], writes=["junk", ("sx2", gp, gi)])
                S.op("act", (lambda gi=gi, gp=gp: (lambda e: e.activation(
                    out=junk[:], in_=rg2[gp][:, gi, :], func=AF.Square, accum_out=sq2[gp][:, gi:gi + 1])))(),
                    reads=[("rg2", gp, gi)], writes=["junk", ("sq2", gp, gi)])

            def rs_grp(g):
                gp = g % 2
                xe_ = xe2[gp]
                tf_, yi_, a__ = rsq2[gp]
                kx, ky, ka, kt = ("rsx", "L2", gp), ("rsy", "L2", gp), ("rsa", "L2", gp), ("rst", "L2", gp)
                mean_ = mean2[gp]
                S.op("dve", lambda e: e.tensor_scalar(out=mean_[:], in0=sx2[gp][:], scalar1=1.0 / D, scalar2=None, op0=ALU.mult),
                     reads=[("sx2", gp, gi) for gi in range(G7)], writes=[("mean2", gp)])
                S.op("dve", lambda e: e.tensor_tensor(out=xe_[:], in0=mean_[:], in1=mean_[:], op=ALU.mult),
                     reads=[("mean2", gp)], writes=[kx])
                S.op("dve", lambda e: e.scalar_tensor_tensor(out=xe_[:], in0=sq2[gp][:], scalar=1.0 / D, in1=xe_[:],
                                                             op0=ALU.mult, op1=ALU.subtract),
                     reads=[("sq2", gp, gi) for gi in range(G7)], writes=[kx])
                S.op("dve", lambda e: e.tensor_scalar(out=xe_[:], in0=xe_[:], scalar1=LN_EPS, scalar2=None, op0=ALU.add),
                     reads=[], writes=[kx])
                S.op("dve", lambda e: e.tensor_copy(tf_[:], xe_[:].bitcast(I32)), reads=[kx], writes=[kt])
                S.op("dve", lambda e: e.tensor_scalar(out=tf_[:], in0=tf_[:], scalar1=-0.5, scalar2=1597463007.0,
                                                      op0=ALU.mult, op1=ALU.add), reads=[kt], writes=[kt])
                S.op("dve", lambda e: e.tensor_copy(yi_[:], tf_[:]), reads=[kt], writes=[ky])
                y2 = yi_[:].bitcast(F32)
                for it in range(2):
                    S.op("dve", lambda e: e.tensor_tensor(out=a__[:], in0=y2, in1=y2, op=ALU.mult), reads=[ky], writes=[ka])
                    S.op("dve", lambda e: e.tensor_tensor(out=a__[:], in0=a__[:], in1=xe_[:], op=ALU.mult), reads=[ka, kx], writes=[ka])
                    S.op("dve", lambda e: e.tensor_scalar(out=a__[:], in0=a__[:], scalar1=-0.5, scalar2=1.5,
                                                          op0=ALU.mult, op1=ALU.add), reads=[ka], writes=[ka])
                    if it == 0:
                        S.op("dve", lambda e: e.tensor_tensor(out=y2, in0=y2, in1=a__[:], op=ALU.mult), reads=[ka, ky], writes=[ky])
                    else:
                        S.op("dve", lambda e: e.tensor_tensor(out=rstd2[gp][:], in0=y2, in1=a__[:], op=ALU.mult),
                             reads=[ka, ky], writes=[("rstd2", gp)])
                S.op("dve", lambda e: e.scalar_tensor_tensor(out=nmr2[gp][:], in0=mean2[gp][:], scalar=-1.0, in1=rstd2[gp][:],
                                                             op0=ALU.mult, op1=ALU.mult),
                     reads=[("rstd2", gp), ("mean2", gp)], writes=[("nmr2", gp)])

            def p2_norm(g, gi):
                gp = g % 2
                i = g * G7 + gi
                par = i % 4
                S.op("act", (lambda gi=gi, gp=gp, par=par: (lambda e: e.activation(
                    out=ot[par][:], in_=rg2[gp][:, gi, :], func=AF.Identity,
                    scale=rstd2[gp][:, gi:gi + 1], bias=nmr2[gp][:, gi:gi + 1])))(),
                    reads=[("rg2", gp, gi), ("rstd2", gp), ("nmr2", gp)], writes=[("ot", par)])

            def p2_tile(g, gi):
                gp = g % 2
                i = g * G7 + gi
                par = i % 4
                S.op("dve", (lambda par=par: (lambda e: e.tensor_tensor(out=ot[par][:], in0=ot[par][:], in1=l2g[:], op=ALU.mult)))(),
                     reads=["l2g"], writes=[("ot", par)])
                S.op("pool", (lambda par=par: (lambda e: e.tensor_tensor(out=ot[par][:], in0=ot[par][:], in1=l2b[:], op=ALU.add)))(),
                     reads=["l2b"], writes=[("ot", par)])
                S.dma("sp", (lambda i=i, par=par: (lambda e: e.dma_start(out=out_d[i * 128:(i + 1) * 128, :], in_=ot[par][:])))(),
                      "ot%d" % par, reads=[("ot", par)], writes=[("out", i)], final=True)

            if STOP_AFTER != "route":
                for gi in range(G7):
                    p1_dma(0, gi)
                for gi in range(G7):
                    p1_dve(0, gi)
                rs_grp(0)
                for g in range(NG7):
                    if g + 1 < NG7:
                        for gi in range(G7):
                            p1_dma(g + 1, gi)
                    for gi in range(G7):
                        p2_norm(g, gi)
                    for gi in range(G7):
                        p2_tile(g, gi)
                        if g + 1 < NG7:
                            p1_dve(g + 1, gi)
                    if g + 1 < NG7:
                        rs_grp(g + 1)
        S.emit()
    return nc, dbg


def _consts():
    k = np.arange(128)[:, None]
    q = np.arange(128)[None, :]
    own = (k <= q).astype(np.float32)
    prev = (k >= q).astype(np.float32)
    bf = ml_dtypes.bfloat16
    return {
        "c_identf": np.eye(128, dtype=np.float32),
        "c_identb": np.eye(128, dtype=np.float32).astype(bf),
        "c_mask4": np.concatenate([prev, own, prev, own], axis=1).astype(bf),
        "c_mown4": np.concatenate([own, own, own, own], axis=1).astype(bf),
        "c_lstrict": (k < q).astype(np.float32).astype(bf),
        "c_onesb": np.ones((128, 128), np.float32).astype(bf),
        "c_kidx": np.broadcast_to(np.arange(NTILE_E, dtype=np.float32)[None, :], (128, NTILE_E)).copy(),
        "c_pidx": np.arange(128, dtype=np.float32).reshape(128, 1),
    }


def _shared_inputs(inp):
    f = lambda a: np.ascontiguousarray(np.asarray(a, dtype=np.float32))
    wg = f(inp["w_gate"])[0].reshape(32, 8, 128, 256)
    wu = f(inp["w_up"])[0].reshape(32, 8, 128, 256)
    gu = np.concatenate([wg, wu], axis=3)
    gu = np.ascontiguousarray(gu.transpose(0, 2, 1, 3))
    wgu0 = np.ascontiguousarray(gu[:, :, 0:4, :]).reshape(4096, 2048)
    wgu1 = np.ascontiguousarray(gu[:, :, 4:8, :]).reshape(4096, 2048)
    wd = f(inp["w_down"])[0].reshape(32, 2, 128, 1024)
    wdn = np.ascontiguousarray(wd.transpose(0, 2, 1, 3)).reshape(4096, 2048)
    sh = {
        "w_in": np.ascontiguousarray(f(inp["w_in"])[0][:, WIN_PERM]),
        "a_ln_g": f(inp["a_ln_g"]).reshape(1, 512),
        "a_ln_b": f(inp["a_ln_b"]).reshape(1, 512),
        "a_wsT": np.ascontiguousarray(f(inp["a_ws"])[0].transpose(2, 0, 1)),
        "a_bs": f(inp["a_bs"])[0].reshape(1, 1024),
        "w_a": f(inp["w_a_proj"])[0],
        "w_b": f(inp["w_b_proj"])[0],
        "w_o": f(inp["w_o"])[0],
        "ln1_g": f(inp["ln1_g"]).reshape(1, D),
        "ln1_b": f(inp["ln1_b"]).reshape(1, D),
        "w_r": np.ascontiguousarray(np.concatenate([f(inp["w_group_router"])[0], f(inp["w_expert_router"])[0].reshape(D, 32)], axis=1)),
        "b_r": np.concatenate([f(inp["b_group_router"])[0], f(inp["b_expert_router"])[0].reshape(32)]).reshape(1, 36),
        "wexp0": np.ascontiguousarray(wgu0.reshape(4096, 2048)),
        "wexp1": np.ascontiguousarray(wgu1.reshape(4096, 2048)),
        "wexp2": np.ascontiguousarray(wdn.reshape(4096, 2048)),
        "w_ple": f(inp["w_ple"])[0],
        "w_pg": f(inp["w_ple_gate"])[0],
        "ln2_g": f(inp["ln2_g"]).reshape(1, D),
        "ln2_b": f(inp["ln2_b"]).reshape(1, D),
    }
    sh.update(_consts())
    return sh


_NC_CACHE = {}


def kernel(**inputs):
    x = np.asarray(inputs["x"], dtype=np.float32)
    p = np.asarray(inputs["p"], dtype=np.float32)
    if "nc" not in _NC_CACHE:
        _NC_CACHE["nc"] = build_nc()[0]
    nc = _NC_CACHE["nc"]
    sh = _shared_inputs(inputs)
    n = x.shape[0]
    in_maps = []
    for c in range(n):
        m = dict(sh)
        m["x"] = np.ascontiguousarray(x[c])
        m["p"] = np.ascontiguousarray(p[0, c])
        in_maps.append(m)
    res = run_bass_kernel_spmd(nc, in_maps, core_ids=list(range(n)))
    return np.stack([np.asarray(r["out"], dtype=np.float32) for r in res.results], axis=0)
```
